# Optimizing a Trainium2 kernel written in Bass

```python
import jax, jax.numpy as jnp
from jax import lax
import numpy as np

D_MODEL = 1024
BATCH = 8
SEQ = 4096
DEPTH = 1

N_MEM = 256
EPS = 1e-6
CONV_WIDTH = D_MODEL
CONV_K = 31
ATT_HEADS = 8
ATT_HEAD_DIM = D_MODEL // ATT_HEADS
ATT_WIDTH = ATT_HEADS * ATT_HEAD_DIM
IDX_HEADS = 8
IDX_HEAD_DIM = 64
TOPK_MAX = 256
Q_BLOCK = 32
MEM_HEADS = 4
MEM_HEAD_DIM = D_MODEL // MEM_HEADS
MEM_WIDTH = MEM_HEADS * MEM_HEAD_DIM
N_BRANCH = 3
FFN_DIM = 2816
FFN_CONV_K = 3
IN_SPLITS = (2 * CONV_WIDTH, ATT_WIDTH, ATT_WIDTH, ATT_WIDTH,
             IDX_HEADS * IDX_HEAD_DIM, IDX_HEADS, IDX_HEAD_DIM, MEM_WIDTH)
IN_WIDTH = sum(IN_SPLITS)

kernel_name = 'hybrid_conv_dsa_memory_block'


def rms_norm(x, g):
    xf = x.astype(jnp.float32)
    y = xf * lax.rsqrt(jnp.mean(xf * xf, axis=-1, keepdims=True) + EPS)
    return (y * g.astype(jnp.float32)).astype(x.dtype)


def layer_norm(x, g, b):
    xf = x.astype(jnp.float32)
    mu = jnp.mean(xf, axis=-1, keepdims=True)
    xc = xf - mu
    var = jnp.mean(xc * xc, axis=-1, keepdims=True)
    y = xc * lax.rsqrt(var + EPS) * g.astype(jnp.float32) + b.astype(jnp.float32)
    return y.astype(x.dtype)


def causal_depthwise_conv(x, w, b):
    width, c = w.shape
    y = lax.conv_general_dilated(
        x, w[:, None, :].astype(x.dtype), window_strides=(1,),
        padding=[(width - 1, 0)], dimension_numbers=('NWC', 'WIO', 'NWC'),
        feature_group_count=c)
    return y + b.astype(x.dtype)


def conformer_conv(u, dw_w, dw_b, ln_g, ln_b, pw2):
    a, gate = jnp.split(u, 2, axis=-1)
    y = a * jax.nn.sigmoid(gate)
    y = causal_depthwise_conv(y, dw_w, dw_b)
    y = jax.nn.silu(layer_norm(y, ln_g, ln_b))
    return y @ pw2


def dsa_sparse_attention(q, k, v, q_idx, w_idx, k_idx):
    b, s, h, dh = q.shape
    topk = min(TOPK_MAX, s // 4)
    nb = s // Q_BLOCK
    kv = jnp.concatenate([k, v], axis=-1)
    k_idx_f = k_idx.astype(jnp.float32)
    key_pos = jnp.arange(s, dtype=jnp.int32)
    q_pos = key_pos.reshape(nb, Q_BLOCK)
    idx_scale = (IDX_HEADS ** -0.5) * (IDX_HEAD_DIM ** -0.5)
    att_scale = dh ** -0.5

    def to_blocks(a):
        return a.reshape((b, nb, Q_BLOCK) + a.shape[2:]).swapaxes(0, 1)

    def block(args):
        qb, qib, wb, tb = args
        dots = jnp.einsum('bthd,bsd->bths', qib.astype(jnp.float32), k_idx_f)
        score = jnp.einsum('bths,bth->bts', jax.nn.relu(dots),
                           wb.astype(jnp.float32)) * idx_scale
        causal = key_pos[None, :] <= tb[:, None]
        score = jnp.where(causal[None], score, -jnp.inf)
        _, sel = lax.top_k(score, topk)
        valid = sel <= tb[None, :, None]
        kv_sel = jax.vmap(lambda kvb, ib: kvb[ib])(kv, sel)
        k_sel, v_sel = jnp.split(kv_sel, 2, axis=-1)
        logits = jnp.einsum('bthd,btkhd->bthk', qb, k_sel).astype(jnp.float32) * att_scale
        logits = jnp.where(valid[:, :, None, :], logits, -jnp.inf)
        p = jax.nn.softmax(logits, axis=-1).astype(v_sel.dtype)
        return jnp.einsum('bthk,btkhd->bthd', p, v_sel)

    out = lax.map(block, (to_blocks(q), to_blocks(q_idx), to_blocks(w_idx), q_pos))
    return out.swapaxes(0, 1).reshape(b, s, h * dh)


def memory_attention(q, mk, mv):
    b, s, hm, dm = q.shape
    logits = jnp.einsum('bshd,bmhd->bhsm', q, mk).astype(jnp.float32) * (dm ** -0.5)
    p = jax.nn.softmax(logits, axis=-1).astype(mv.dtype)
    return jnp.einsum('bhsm,bmhd->bshd', p, mv).reshape(b, s, hm * dm)


def setup_inputs(seed: int = 0) -> dict:
    key = jax.random.key(seed)
    ks = iter(jax.random.split(key, 32))

    def nrm(shape, scale):
        return jax.random.normal(next(ks), shape, jnp.float32) * scale

    def gain(shape):
        return 1.0 + nrm(shape, 0.05)

    L, D = DEPTH, D_MODEL
    return {
        'x': nrm((BATCH, SEQ, D), 1.0),
        'mem': nrm((BATCH, N_MEM, D), 1.0),
        'norm1_pre_g': gain((L, D)),
        'w_in': nrm((L, D, IN_WIDTH), D ** -0.5),
        'conv_dw_w': nrm((L, CONV_K, CONV_WIDTH), CONV_K ** -0.5),
        'conv_dw_b': nrm((L, CONV_WIDTH), 0.02),
        'conv_ln_g': gain((L, CONV_WIDTH)),
        'conv_ln_b': nrm((L, CONV_WIDTH), 0.02),
        'conv_pw2': nrm((L, CONV_WIDTH, D), CONV_WIDTH ** -0.5),
        'mem_norm_g': gain((L, D)),
        'w_mem_kv': nrm((L, D, 2 * MEM_WIDTH), D ** -0.5),
        'w_gate': nrm((L, D, N_BRANCH * D), D ** -0.5),
        'b_gate': nrm((L, N_BRANCH * D), 0.02),
        'w_out': nrm((L, D, D), D ** -0.5),
        'norm1_post_g': gain((L, D)),
        'norm2_pre_g': gain((L, D)),
        'w_up': nrm((L, D, 2 * FFN_DIM), D ** -0.5),
        'ffn_dw_w': nrm((L, FFN_CONV_K, 2 * FFN_DIM), FFN_CONV_K ** -0.5),
        'ffn_dw_b': nrm((L, 2 * FFN_DIM), 0.02),
        'w_down': nrm((L, FFN_DIM, D), FFN_DIM ** -0.5),
        'norm2_post_g': gain((L, D)),
    }


def reference(x, mem, norm1_pre_g, w_in, conv_dw_w, conv_dw_b, conv_ln_g, conv_ln_b,
              conv_pw2, mem_norm_g, w_mem_kv, w_gate, b_gate, w_out, norm1_post_g,
              norm2_pre_g, w_up, ffn_dw_w, ffn_dw_b, w_down, norm2_post_g):
    b, s, d = x.shape
    split_at = [int(i) for i in np.cumsum(IN_SPLITS)[:-1]]
    for l in range(DEPTH):
        h = rms_norm(x, norm1_pre_g[l])
        proj = h @ w_in[l]
        conv_in, q, k, v, qi, wi, ki, qm = jnp.split(proj, split_at, axis=-1)
        y_conv = conformer_conv(conv_in, conv_dw_w[l], conv_dw_b[l],
                                conv_ln_g[l], conv_ln_b[l], conv_pw2[l])
        y_att = dsa_sparse_attention(
            q.reshape(b, s, ATT_HEADS, ATT_HEAD_DIM),
            k.reshape(b, s, ATT_HEADS, ATT_HEAD_DIM),
            v.reshape(b, s, ATT_HEADS, ATT_HEAD_DIM),
            qi.reshape(b, s, IDX_HEADS, IDX_HEAD_DIM), wi, ki)
        mkv = rms_norm(mem, mem_norm_g[l]) @ w_mem_kv[l]
        mk, mv = jnp.split(mkv, 2, axis=-1)
        m = mem.shape[1]
        y_mem = memory_attention(
            qm.reshape(b, s, MEM_HEADS, MEM_HEAD_DIM),
            mk.reshape(b, m, MEM_HEADS, MEM_HEAD_DIM),
            mv.reshape(b, m, MEM_HEADS, MEM_HEAD_DIM))
        g = jax.nn.sigmoid(h @ w_gate[l] + b_gate[l]).reshape(b, s, N_BRANCH, d)
        merged = g[:, :, 0] * y_conv + g[:, :, 1] * y_att + g[:, :, 2] * y_mem
        x = x + rms_norm(merged @ w_out[l], norm1_post_g[l])
        h2 = rms_norm(x, norm2_pre_g[l])
        u = causal_depthwise_conv(h2 @ w_up[l], ffn_dw_w[l], ffn_dw_b[l])
        u_gate, u_val = jnp.split(u, 2, axis=-1)
        x = x + rms_norm((jax.nn.silu(u_gate) * u_val) @ w_down[l], norm2_post_g[l])
    return x
```

```python
import numpy as np
from contextlib import ExitStack
import concourse.bass as bass
import concourse.mybir as mybir
from concourse.bass_utils import run_bass_kernel_spmd

F32 = mybir.dt.float32
BF16 = mybir.dt.bfloat16
I32 = mybir.dt.int32
AF = mybir.ActivationFunctionType
ALU = mybir.AluOpType
AX = mybir.AxisListType

S = 4096
D = 1024
NB = S // 128
NT = S // 512
EPS = 1e-6
FFN = 2816
NFC = FFN // 128
TOPK = 256
NIT = 18
NEG = -1.0e30

ENGS = ['pe', 'act', 'dve', 'pool', 'sp']
BLK = {'pe': 'tensor', 'act': 'scalar', 'dve': 'vector', 'pool': 'gpsimd', 'sp': 'sync'}
EPOCH = 20000
NDS = 24


class Prog:
    def __init__(self, nc, stack):
        self.nc = nc
        self.stack = stack
        self.lists = {e: [] for e in ENGS}
        self.count = {e: 0 for e in ENGS}
        self.sems = {e: [] for e in ENGS}
        self.waited = {e: {} for e in ENGS}
        self.lastw = {}
        self.reads = {}
        self.dsem = [stack.enter_context(nc.semaphore(f"dma{j}")) for j in range(NDS)]
        self.dval = [0] * NDS
        self.dnext = 0
        self.nsem = 0

    def _esem(self, e, idx):
        while len(self.sems[e]) <= idx:
            self.sems[e].append(self.stack.enter_context(
                self.nc.semaphore(f"s_{e}{len(self.sems[e])}")))
        return self.sems[e][idx]

    def _collect(self, e, r, w, extra):
        toks = list(extra)
        for k in r:
            t = self.lastw.get(k)
            if t is not None:
                toks.append(t)
        for k in w:
            t = self.lastw.get(k)
            if t is not None:
                toks.append(t)
            toks.extend(self.reads.get(k, ()))
        need = []
        for (sid, sem, v, te) in toks:
            if te == e and e == 'pe':
                continue
            if self.waited[e].get(sid, 0) >= v:
                continue
            self.waited[e][sid] = v
            need.append((sem, v))
        return need

    def _update(self, tok, r, w):
        for k in w:
            self.lastw[k] = tok
            self.reads[k] = []
        for k in r:
            self.reads.setdefault(k, []).append(tok)

    def op(self, e, fn, r=(), w=(), extra=()):
        need = self._collect(e, r, w, extra)
        n = self.count[e]
        ep, v = divmod(n, EPOCH)
        sem = self._esem(e, ep)
        self.count[e] = n + 1
        tok = ((e, ep), sem, v + 1, e)
        self.lists[e].append((need, fn, sem, 1))
        self._update(tok, r, w)
        return tok

    def pe(self, fn, r=(), w=(), extra=()):
        return self.op('pe', fn, r, w, extra)

    def act(self, fn, r=(), w=(), extra=()):
        return self.op('act', fn, r, w, extra)

    def dve(self, fn, r=(), w=(), extra=()):
        return self.op('dve', fn, r, w, extra)

    def pool(self, fn, r=(), w=(), extra=()):
        return self.op('pool', fn, r, w, extra)

    def dma(self, e, out, in_, r=(), w=(), extra=(), **kw):
        j = self.dnext
        self.dnext = (j + 1) % NDS
        sem = self.dsem[j]
        extra = list(extra)
        if self.dval[j] > 0:
            extra.append((('d', j), sem, self.dval[j], 'dma'))
        need = self._collect(e, r, w, extra)
        self.dval[j] += 16
        tok = (('d', j), sem, self.dval[j], 'dma')
        self.lists[e].append((need, lambda eng: eng.dma_start(out=out, in_=in_, **kw), sem, 16))
        self._update(tok, r, w)
        return tok

    def barrier(self):
        toks = []
        for e in ENGS:
            n = self.count[e]
            if n == 0:
                continue
            ep, v = divmod(n - 1, EPOCH)
            toks.append(((e, ep), self.sems[e][ep], v + 1, e))
        for j in range(NDS):
            if self.dval[j] > 0:
                toks.append((('d', j), self.dsem[j], self.dval[j], 'dma'))
        for e in ENGS:
            need = []
            for (sid, sem, v, te) in toks:
                if te == e:
                    continue
                if self.waited[e].get(sid, 0) >= v:
                    continue
                self.waited[e][sid] = v
                need.append((sem, v))
            if need:
                self.lists[e].append((need, None, None, 0))
        self.lastw = {}
        self.reads = {}

    def emit(self):
        with self.nc.Block() as block:
            for e in ENGS:
                lst = self.lists[e]
                if not lst:
                    continue

                def body(eng, lst=lst):
                    for (need, fn, sem, inc) in lst:
                        for (s, v) in need:
                            eng.wait_ge(s, v)
                        if fn is not None:
                            fn(eng).then_inc(sem, inc)

                getattr(block, BLK[e])(body)
        self.lists = {e: [] for e in ENGS}


PCOL = {}
_off = 0
for _name, _n in [('g1', D), ('gmem', D), ('gpost1', D), ('g2', D), ('gpost2', D),
                  ('convw', 8 * 31), ('convb', 8), ('lng', 8), ('lnb', 8),
                  ('bgate', 24), ('ffnw', 44 * 3), ('ffnb', 44)]:
    PCOL[_name] = (_off, _n)
    _off += _n
NPCOL = _off

WCM_COLS = 45 * 128
WTM_COLS = 1032


def pack_inputs(inp):
    f = np.float32
    w_in = np.asarray(inp['w_in'][0], f)
    c0 = 0
    conv = w_in[:, 0:2048]
    q = w_in[:, 2048:3072]
    k = w_in[:, 3072:4096]
    v = w_in[:, 4096:5120]
    qi = w_in[:, 5120:5632]
    wi = w_in[:, 5632:5640]
    ki = w_in[:, 5640:5704]
    qm = w_in[:, 5704:6728]
    a, g = conv[:, :1024], conv[:, 1024:]
    inter = np.stack([a.reshape(D, 8, 128), g.reshape(D, 8, 128)], axis=2).reshape(D, 2048)
    wcm = np.ascontiguousarray(np.concatenate([inter, q, k, qm, qi, ki, ki], axis=1))
    wtm = np.ascontiguousarray(np.concatenate([v, wi], axis=1))
    w_up = np.asarray(inp['w_up'][0], f)
    ug, uv = w_up[:, :FFN], w_up[:, FFN:]
    wup = np.ascontiguousarray(
        np.stack([ug.reshape(D, NFC, 128), uv.reshape(D, NFC, 128)], axis=2).reshape(D, 2 * FFN))
    P = np.zeros((128, NPCOL), f)

    def put(name, arr):
        o, n = PCOL[name]
        P[:, o:o + n] = arr

    put('g1', np.broadcast_to(inp['norm1_pre_g'][0], (128, D)))
    put('gmem', np.broadcast_to(inp['mem_norm_g'][0], (128, D)))
    put('gpost1', np.broadcast_to(inp['norm1_post_g'][0], (128, D)))
    put('g2', np.broadcast_to(inp['norm2_pre_g'][0], (128, D)))
    put('gpost2', np.broadcast_to(inp['norm2_post_g'][0], (128, D)))
    cw = np.asarray(inp['conv_dw_w'][0], f)
    put('convw', cw.T.reshape(8, 128, 31).transpose(1, 0, 2).reshape(128, 8 * 31))
    put('convb', np.asarray(inp['conv_dw_b'][0], f).reshape(8, 128).T)
    put('lng', np.asarray(inp['conv_ln_g'][0], f).reshape(8, 128).T)
    put('lnb', np.asarray(inp['conv_ln_b'][0], f).reshape(8, 128).T)
    put('bgate', np.asarray(inp['b_gate'][0], f).reshape(24, 128).T)
    fw = np.asarray(inp['ffn_dw_w'][0], f)
    fwg, fwv = fw[:, :FFN], fw[:, FFN:]
    fwi = np.stack([fwg.reshape(3, NFC, 128), fwv.reshape(3, NFC, 128)], axis=2).reshape(3, 44, 128)
    put('ffnw', fwi.transpose(2, 1, 0).reshape(128, 44 * 3))
    fb = np.asarray(inp['ffn_dw_b'][0], f)
    fbi = np.stack([fb[:FFN].reshape(NFC, 128), fb[FFN:].reshape(NFC, 128)], axis=1).reshape(44, 128)
    put('ffnb', fbi.T)
    shared = {
        'wcm': wcm, 'wtm': wtm, 'wup': wup, 'params': P,
        'pw2': np.ascontiguousarray(inp['conv_pw2'][0], f),
        'wmkv': np.ascontiguousarray(inp['w_mem_kv'][0], f),
        'wgate': np.ascontiguousarray(inp['w_gate'][0], f),
        'wout': np.ascontiguousarray(inp['w_out'][0], f),
        'wdown': np.ascontiguousarray(inp['w_down'][0], f),
    }
    return shared


def build(debug=(), upto=99):
    nc = bass.Bass("TRN2", target_bir_lowering=False)
    dbg = set(debug)

    def din(name, shape):
        return nc.dram_tensor(name, list(shape), F32, kind="ExternalInput").ap()

    def scratch(name, shape, dt):
        kind = "ExternalOutput" if name in dbg else "Internal"
        return nc.dram_tensor(name, list(shape), dt, kind=kind).ap()

    x = din("x", (S, D))
    mem = din("mem", (256, D))
    wcm = din("wcm", (D, WCM_COLS))
    wtm = din("wtm", (D, WTM_COLS))
    wup = din("wup", (D, 2 * FFN))
    params = din("params", (128, NPCOL))
    pw2 = din("pw2", (D, D))
    wmkv = din("wmkv", (D, 2 * D))
    wgate = din("wgate", (D, 3 * D))
    wout = din("wout", (D, D))
    wdown = din("wdown", (FFN, D))
    y = nc.dram_tensor("y", [S, D], F32, kind="ExternalOutput").ap()

    ycT = scratch("ycT", (D, S), F32)
    qT = scratch("qT", (D, S), BF16)
    kT = scratch("kT", (D, S), BF16)
    qmT = scratch("qmT", (D, S), BF16)
    qiT = scratch("qiT", (512, S), BF16)
    kiT = scratch("kiT", (128, S), BF16)
    vtm = scratch("vtm", (S, D), BF16)
    witm = scratch("witm", (S, 8), F32)
    gT = scratch("gT", (3 * D, S), BF16)
    mconvT = scratch("mconvT", (D, S), BF16)
    mattT = scratch("mattT", (D, S), BF16)
    mmemT = scratch("mmemT", (D, S), BF16)
    maskT = scratch("maskT", (S, S), BF16)
    x1d = scratch("x1d", (S, D), F32)
    aT = scratch("aT", (FFN, S), BF16)

    def pcols(t, name):
        o, n = PCOL[name]
        return t[:, o:o + n]

    with ExitStack() as gs:
        P = Prog(nc, gs)

        uniq = [0]

        def sb(st, name, shape, dt):
            uniq[0] += 1
            return st.enter_context(nc.sbuf_tensor(f"{name}_{uniq[0]}", list(shape), dt))

        def ps(st, name, shape, dt=F32):
            uniq[0] += 1
            return st.enter_context(nc.psum_tensor(f"{name}_{uniq[0]}", list(shape), dt))

        ident = sb(gs, "ident", (128, 128), BF16)
        ones_bf = sb(gs, "ones_bf", (128, 128), BF16)
        ones_f = sb(gs, "ones_f", (128, 128), F32)
        caus = sb(gs, "caus", (128, 128), F32)
        pv = sb(gs, "pv", (128, 472), F32)
        pvo = PCOL['convw'][0]

        def pvc(name, j0=0, n=None):
            o, nn = PCOL[name]
            o -= pvo
            if n is None:
                n = nn
            return pv[:, o + j0:o + j0 + n]

        with ExitStack() as st:
            iot = sb(st, "iot", (128, 128), I32)
            P.pool(lambda e: e.iota(out=iot[:], pattern=[[1, 128]], base=0, channel_multiplier=-1),
                   w=['iot'])
            P.dve(lambda e: e.tensor_scalar(out=ident[:], in0=iot[:], scalar1=0.0, scalar2=None,
                                            op0=ALU.is_equal), r=['iot'], w=['ident'])
            P.dve(lambda e: e.tensor_scalar(out=caus[:], in0=iot[:], scalar1=0.5, scalar2=NEG,
                                            op0=ALU.is_gt, op1=ALU.mult), r=['iot'], w=['caus'])
            P.dve(lambda e: e.memset(ones_bf[:], 1.0), w=['ones_bf'])
            P.dve(lambda e: e.memset(ones_f[:], 1.0), w=['ones_f'])
            P.dma('sp', pv[:], params[:, pvo:pvo + 472], w=['pv'])
            P.barrier()
            P.emit()

        hst = ExitStack()
        hT = sb(hst, "hT", (128, 8, S), BF16)

        def norm_transpose_phase(st, src, nblk, gname, dstT, tag):
            gbc = sb(st, tag + "gbc", (128, D), F32)
            xt = [sb(st, f"{tag}xt{i}", (128, D), F32) for i in range(2)]
            junk = sb(st, tag + "junk", (128, D), BF16)
            hb = [sb(st, f"{tag}hb{i}", (128, D), BF16) for i in range(2)]
            ss = [sb(st, f"{tag}ss{i}", (128, 1), F32) for i in range(2)]
            rt = [sb(st, f"{tag}rt{i}", (128, 1), F32) for i in range(2)]
            rs = [sb(st, f"{tag}rs{i}", (128, 1), F32) for i in range(2)]
            pt = [ps(st, f"{tag}pt{i}", (128, D), BF16) for i in range(2)]
            o, n = PCOL[gname]
            P.dma('sp', gbc[:], params[:, o:o + n], w=['gbc'])
            for i in range(nblk):
                s = i % 2
                P.dma('sp', xt[s][:], src[i * 128:(i + 1) * 128, :], w=[f'xt{s}'])
                P.dve(lambda e, s=s: e.scalar_tensor_tensor(
                    out=junk[:], in0=xt[s][:], scalar=1.0, in1=xt[s][:], op0=ALU.mult, op1=ALU.mult,
                    accum_out=ss[s][:]), r=[f'xt{s}'], w=['junk', f'ss{s}'])
                P.act(lambda e, s=s: e.activation(out=rt[s][:], in_=ss[s][:], func=AF.Sqrt,
                                                  bias=EPS, scale=1.0 / D),
                      r=[f'ss{s}'], w=[f'rt{s}'])
                P.dve(lambda e, s=s: e.reciprocal(out=rs[s][:], in_=rt[s][:]), r=[f'rt{s}'], w=[f'rs{s}'])
                P.dve(lambda e, s=s: e.scalar_tensor_tensor(
                    out=hb[s][:], in0=xt[s][:], scalar=rs[s][:], in1=gbc[:], op0=ALU.mult, op1=ALU.mult),
                    r=[f'xt{s}', f'rs{s}', 'gbc'], w=[f'hb{s}'])
                for j in range(8):
                    P.pe(lambda e, s=s, j=j: e.transpose(out=pt[s][:, j * 128:(j + 1) * 128],
                                                         in_=hb[s][:, j * 128:(j + 1) * 128],
                                                         identity=ident[:]),
                         r=[f'hb{s}'], w=[f'pt{s}'])
                P.act(lambda e, s=s, i=i: e.activation(
                    out=dstT[:, :, i * 128:(i + 1) * 128],
                    in_=pt[s][:].rearrange("p (j t) -> p j t", j=8), func=AF.Copy),
                    r=[f'pt{s}'], w=[f'dstT{i}'])

        with ExitStack() as st:
            norm_transpose_phase(st, x, NB, 'g1', hT, "A")
            P.barrier()
            P.emit()

        def proj_cm(st, W, ncols, evac, tag, wtile=512):
            wt = [sb(st, f"{tag}wt{i}", (128, 8, wtile), BF16) for i in range(2)]
            pp = [ps(st, f"{tag}pp{i}", (128, 512), F32) for i in range(4)]
            ntile = (ncols + wtile - 1) // wtile
            cnt = 0
            for wi_ in range(ntile):
                s = wi_ % 2
                c0 = wi_ * wtile
                wc = min(wtile, ncols - c0)
                P.dma('pool', wt[s][:, :, 0:wc],
                      W[:, c0:c0 + wc].rearrange("(k p) c -> p k c", p=128), w=[f'wt{s}'])
                for cc in range(wc // 128):
                    c = (c0 // 128) + cc
                    for T in range(NT):
                        b = cnt % 4
                        cnt += 1
                        for k in range(8):
                            P.pe(lambda e, s=s, cc=cc, T=T, k=k, b=b: e.matmul(
                                pp[b][:], lhsT=wt[s][:, k, cc * 128:(cc + 1) * 128],
                                rhs=hT[:, k, T * 512:(T + 1) * 512], start=(k == 0), stop=(k == 7)),
                                r=[f'wt{s}'], w=[f'pp{b}'])
                        evac(c, T, pp[b], f'pp{b}')

        if upto >= 2:
            with ExitStack() as st:
                yg = [sb(st, f"yg{i}", (128, 30 + 512), F32) for i in range(3)]
                sg = [sb(st, f"sg{i}", (128, 512), F32) for i in range(2)]
                yc = [sb(st, f"yc{i}", (128, 512), F32) for i in range(2)]
                wt = [sb(st, f"Bwt{i}", (128, 8, 512), BF16) for i in range(2)]
                pa = [ps(st, f"Bpa{i}", (128, 512), F32) for i in range(2)]
                pg = [ps(st, f"Bpg{i}", (128, 512), F32) for i in range(2)]
                n = 0
                for wi_ in range(4):
                    s = wi_ % 2
                    P.dma('pool', wt[s][:], wcm[:, wi_ * 512:(wi_ + 1) * 512].rearrange(
                        "(k p) c -> p k c", p=128), w=[f'wt{s}'])
                    for jj in range(2):
                        j = wi_ * 2 + jj
                        for T in range(NT):
                            b = n % 2
                            s3 = n % 3
                            p3 = (n - 1) % 3
                            for k in range(8):
                                P.pe(lambda e, s=s, jj=jj, T=T, k=k, b=b: e.matmul(
                                    pa[b][:], lhsT=wt[s][:, k, jj * 256:jj * 256 + 128],
                                    rhs=hT[:, k, T * 512:(T + 1) * 512], start=(k == 0), stop=(k == 7)),
                                    r=[f'wt{s}'], w=[f'pa{b}'])
                            for k in range(8):
                                P.pe(lambda e, s=s, jj=jj, T=T, k=k, b=b: e.matmul(
                                    pg[b][:], lhsT=wt[s][:, k, jj * 256 + 128:jj * 256 + 256],
                                    rhs=hT[:, k, T * 512:(T + 1) * 512], start=(k == 0), stop=(k == 7)),
                                    r=[f'wt{s}'], w=[f'pg{b}'])
                            P.act(lambda e, b=b: e.activation(out=sg[b][:], in_=pg[b][:], func=AF.Sigmoid),
                                  r=[f'pg{b}'], w=[f'sg{b}'])
                            if T == 0:
                                P.pool(lambda e, s3=s3: e.memset(yg[s3][:, 0:30], 0.0), w=[f'yg{s3}'])
                            else:
                                P.pool(lambda e, s3=s3, p3=p3: e.tensor_copy(
                                    out=yg[s3][:, 0:30], in_=yg[p3][:, 512:542]),
                                    r=[f'yg{p3}'], w=[f'yg{s3}'])
                            P.dve(lambda e, b=b, s3=s3: e.tensor_tensor(
                                out=yg[s3][:, 30:542], in0=pa[b][:], in1=sg[b][:], op=ALU.mult),
                                r=[f'pa{b}', f'sg{b}'], w=[f'yg{s3}'])
                            P.dve(lambda e, b=b, s3=s3, j=j: e.tensor_scalar(
                                out=yc[b][:], in0=yg[s3][:, 30:542], scalar1=pvc('convw', j * 31 + 30, 1),
                                scalar2=pvc('convb', j, 1), op0=ALU.mult, op1=ALU.add),
                                r=[f'yg{s3}'], w=[f'yc{b}'])
                            for tap in range(30):
                                P.dve(lambda e, b=b, s3=s3, j=j, tap=tap: e.scalar_tensor_tensor(
                                    out=yc[b][:], in0=yg[s3][:, tap:tap + 512],
                                    scalar=pvc('convw', j * 31 + tap, 1), in1=yc[b][:],
                                    op0=ALU.mult, op1=ALU.add),
                                    r=[f'yg{s3}'], w=[f'yc{b}'])
                            P.dma('sp', ycT[j * 128:(j + 1) * 128, T * 512:(T + 1) * 512], yc[b][:],
                                  r=[f'yc{b}'], w=['dram'])
                            n += 1
                P.barrier()
                P.emit()

            with ExitStack() as st:
                stg = [sb(st, f"stg{i}", (128, S), BF16) for i in range(2)]

                def evac_gen(c, T, pp, key):
                    c = c + 16
                    if c < 24:
                        dst, row, scale = qT, (c - 16) * 128, 128.0 ** -0.5
                    elif c < 32:
                        dst, row, scale = kT, (c - 24) * 128, 1.0
                    elif c < 40:
                        dst, row, scale = qmT, (c - 32) * 128, 256.0 ** -0.5
                    elif c < 44:
                        dst, row, scale = qiT, (c - 40) * 128, 1.0
                    else:
                        dst, row, scale = kiT, 0, 1.0
                    s2 = c % 2
                    P.act(lambda e: e.activation(out=stg[s2][:, T * 512:(T + 1) * 512], in_=pp[:],
                                                 func=AF.Copy, scale=scale),
                          r=[key], w=[f'stg{s2}'])
                    if T == NT - 1:
                        P.dma('sp', dst[row:row + 128, :], stg[s2][:], r=[f'stg{s2}'], w=['dram'])

                proj_cm(st, wcm[:, 2048:], WCM_COLS - 2048, evac_gen, "Bq")
                P.barrier()
                P.emit()

            with ExitStack() as st:
                stg = [sb(st, f"stg{i}", (128, S), BF16) for i in range(2)]

                def evac_gate(c, T, pp, key):
                    s2 = c % 2
                    P.act(lambda e: e.activation(out=stg[s2][:, T * 512:(T + 1) * 512], in_=pp[:],
                                                 func=AF.Sigmoid, bias=pvc('bgate', c, 1), scale=1.0),
                          r=[key], w=[f'stg{s2}'])
                    if T == NT - 1:
                        P.dma('sp', gT[c * 128:(c + 1) * 128, :], stg[s2][:], r=[f'stg{s2}'], w=['dram'])

                proj_cm(st, wgate, 3 * D, evac_gate, "Bg")
                P.barrier()
                P.emit()

            with ExitStack() as st:
                wv = sb(st, "wv", (128, 8, WTM_COLS), BF16)
                vst = [sb(st, f"vst{i}", (128, D), BF16) for i in range(2)]
                wst = [sb(st, f"wst{i}", (128, 8), F32) for i in range(2)]
                pvv = [ps(st, f"pvv{i}", (128, 512), F32) for i in range(4)]
                pw = [ps(st, f"pw{i}", (128, 8), F32) for i in range(2)]
                for hh in range(2):
                    P.dma('pool', wv[:, :, hh * 512:(hh + 1) * 512],
                          wtm[:, hh * 512:(hh + 1) * 512].rearrange("(k p) c -> p k c", p=128), w=['wv'])
                P.dma('pool', wv[:, :, 1024:1032],
                      wtm[:, 1024:1032].rearrange("(k p) c -> p k c", p=128), w=['wv'])
                for i in range(NB):
                    s = i % 2
                    for hh in range(2):
                        b = (i * 2 + hh) % 4
                        for k in range(8):
                            P.pe(lambda e, i=i, hh=hh, k=k, b=b: e.matmul(
                                pvv[b][:], lhsT=hT[:, k, i * 128:(i + 1) * 128],
                                rhs=wv[:, k, hh * 512:(hh + 1) * 512], start=(k == 0), stop=(k == 7)),
                                r=['wv'], w=[f'pvv{b}'])
                        P.act(lambda e, s=s, hh=hh, b=b: e.activation(
                            out=vst[s][:, hh * 512:(hh + 1) * 512], in_=pvv[b][:], func=AF.Copy),
                            r=[f'pvv{b}'], w=[f'vst{s}'])
                    for k in range(8):
                        P.pe(lambda e, i=i, k=k, s=s: e.matmul(
                            pw[s][:], lhsT=hT[:, k, i * 128:(i + 1) * 128],
                            rhs=wv[:, k, 1024:1032], start=(k == 0), stop=(k == 7)),
                            r=['wv'], w=[f'pw{s}'])
                    P.dve(lambda e, s=s: e.tensor_copy(out=wst[s][:], in_=pw[s][:]),
                          r=[f'pw{s}'], w=[f'wst{s}'])
                    P.dma('sp', vtm[i * 128:(i + 1) * 128, :], vst[s][:], r=[f'vst{s}'], w=['dram'])
                    P.dma('sp', witm[i * 128:(i + 1) * 128, :], wst[s][:], r=[f'wst{s}'], w=['dram'])
                P.barrier()
                P.emit()
        hst.close()

        def TS(T):
            return slice(T * 512, (T + 1) * 512)

        if upto >= 3:
            with ExitStack() as st:
                pwt = sb(st, "pwt", (128, 8, D), BF16)
                for hh in range(2):
                    P.dma('pool', pwt[:, :, hh * 512:(hh + 1) * 512],
                          pw2[:, hh * 512:(hh + 1) * 512].rearrange("(k p) c -> p k c", p=128), w=['pwt'])
                ycl = [sb(st, f"ycl{i}", (128, 8, 512), F32) for i in range(2)]
                sq = sb(st, "sq", (128, 8, 512), F32)
                gl = [sb(st, f"gl{i}", (128, 8, 512), BF16) for i in range(2)]
                mean = sb(st, "mean", (128, 512), F32)
                msq = sb(st, "msq", (128, 512), F32)
                var = sb(st, "var", (128, 512), F32)
                sd = sb(st, "sd", (128, 512), F32)
                rstd = sb(st, "rstd", (128, 512), F32)
                t1 = [sb(st, f"t1{i}", (128, 512), F32) for i in range(2)]
                ys = sb(st, "ys", (128, 8, 512), BF16)
                ot = [sb(st, f"ot{i}", (128, 512), BF16) for i in range(2)]
                s1 = ps(st, "s1", (128, 512))
                s2 = ps(st, "s2", (128, 512))
                po = [ps(st, f"po{i}", (128, 512)) for i in range(2)]
                for T in range(NT):
                    s = T % 2
                    P.dma('sp', ycl[s][:], ycT[:, TS(T)].rearrange("(j p) t -> p j t", p=128), w=[f'ycl{s}'])
                    P.dma('sp', gl[s][:], gT[0:1024, TS(T)].rearrange("(j p) t -> p j t", p=128), w=[f'gl{s}'])
                    P.act(lambda e, s=s: e.activation(out=sq[:], in_=ycl[s][:], func=AF.Square),
                          r=[f'ycl{s}'], w=['sq'])
                    for j in range(8):
                        P.pe(lambda e, s=s, j=j: e.matmul(s1[:], lhsT=ones_f[:], rhs=ycl[s][:, j, :],
                                                          start=(j == 0), stop=(j == 7)),
                             r=[f'ycl{s}', 'mean'], w=['s1'])
                    for j in range(8):
                        P.pe(lambda e, j=j: e.matmul(s2[:], lhsT=ones_f[:], rhs=sq[:, j, :],
                                                     start=(j == 0), stop=(j == 7)), r=['sq'], w=['s2'])
                    P.dve(lambda e: e.tensor_scalar(out=mean[:], in0=s1[:], scalar1=1.0 / D, scalar2=None,
                                                    op0=ALU.mult), r=['s1'], w=['mean'])
                    P.dve(lambda e: e.tensor_tensor(out=msq[:], in0=mean[:], in1=mean[:], op=ALU.mult),
                          r=['mean'], w=['msq'])
                    P.dve(lambda e: e.scalar_tensor_tensor(out=var[:], in0=s2[:], scalar=1.0 / D, in1=msq[:],
                                                           op0=ALU.mult, op1=ALU.subtract),
                          r=['s2', 'msq'], w=['var'])
                    P.act(lambda e: e.activation(out=sd[:], in_=var[:], func=AF.Sqrt, bias=EPS, scale=1.0),
                          r=['var'], w=['sd'])
                    P.dve(lambda e: e.reciprocal(out=rstd[:], in_=sd[:]), r=['sd'], w=['rstd'])
                    for j in range(8):
                        b = j % 2
                        P.dve(lambda e, s=s, j=j, b=b: e.tensor_tensor(
                            out=t1[b][:], in0=ycl[s][:, j, :], in1=mean[:], op=ALU.subtract),
                            r=[f'ycl{s}', 'mean'], w=[f't1{b}'])
                        P.dve(lambda e, b=b: e.tensor_tensor(out=t1[b][:], in0=t1[b][:], in1=rstd[:], op=ALU.mult),
                              r=['rstd'], w=[f't1{b}'])
                        P.act(lambda e, j=j, b=b: e.activation(out=ys[:, j, :], in_=t1[b][:], func=AF.Silu,
                                                               scale=pvc('lng', j, 1), bias=pvc('lnb', j, 1)),
                              r=[f't1{b}'], w=['ys'])
                    for dch in range(8):
                        b = dch % 2
                        for j in range(8):
                            P.pe(lambda e, dch=dch, j=j, b=b: e.matmul(
                                po[b][:], lhsT=pwt[:, j, dch * 128:(dch + 1) * 128], rhs=ys[:, j, :],
                                start=(j == 0), stop=(j == 7)), r=['ys', 'pwt'], w=[f'po{b}'])
                        P.dve(lambda e, s=s, dch=dch, b=b: e.tensor_tensor(
                            out=ot[b][:], in0=po[b][:], in1=gl[s][:, dch, :], op=ALU.mult),
                            r=[f'po{b}', f'gl{s}'], w=[f'ot{b}'])
                        P.dma('pool', mconvT[dch * 128:(dch + 1) * 128, TS(T)], ot[b][:], r=[f'ot{b}'], w=['dram'])
                P.barrier()
                P.emit()

        if upto >= 4:
            with ExitStack() as st0:
                mnT = sb(st0, "mnT", (128, 8, 256), BF16)
                mkT = sb(st0, "mkT", (128, 8, 256), BF16)
                mv = sb(st0, "mv", (128, 2, D), BF16)
                with ExitStack() as st:
                    norm_transpose_phase(st, mem, 2, 'gmem', mnT, "D")
                    P.barrier()
                    P.emit()
                with ExitStack() as st:
                    wk = [sb(st, f"wk{i}", (128, 8, 512), BF16) for i in range(2)]
                    pk = [ps(st, f"pk{i}", (128, 512)) for i in range(2)]
                    pmv = [ps(st, f"pmv{i}", (128, 512)) for i in range(2)]
                    for wi_ in range(2):
                        s = wi_ % 2
                        P.dma('pool', wk[s][:], wmkv[:, wi_ * 512:(wi_ + 1) * 512].rearrange(
                            "(k p) c -> p k c", p=128), w=[f'wk{s}'])
                        for cc in range(4):
                            c = wi_ * 4 + cc
                            b = c % 2
                            for k in range(8):
                                P.pe(lambda e, s=s, cc=cc, k=k, b=b: e.matmul(
                                    pk[b][:, 0:256], lhsT=wk[s][:, k, cc * 128:(cc + 1) * 128],
                                    rhs=mnT[:, k, :], start=(k == 0), stop=(k == 7)),
                                    r=[f'wk{s}'], w=[f'pk{b}'])
                            P.act(lambda e, c=c, b=b: e.activation(out=mkT[:, c, :], in_=pk[b][:, 0:256],
                                                                   func=AF.Copy), r=[f'pk{b}'], w=['mkT'])
                    for wi_ in range(2):
                        s = wi_ % 2
                        P.dma('pool', wk[s][:], wmkv[:, 1024 + wi_ * 512:1024 + (wi_ + 1) * 512].rearrange(
                            "(k p) c -> p k c", p=128), w=[f'wk{s}'])
                        for mc in range(2):
                            for k in range(8):
                                P.pe(lambda e, s=s, mc=mc, k=k: e.matmul(
                                    pmv[mc][:], lhsT=mnT[:, k, mc * 128:(mc + 1) * 128], rhs=wk[s][:, k, :],
                                    start=(k == 0), stop=(k == 7)), r=[f'wk{s}'], w=[f'pmv{mc}'])
                            P.act(lambda e, wi_=wi_, mc=mc: e.activation(
                                out=mv[:, mc, wi_ * 512:(wi_ + 1) * 512], in_=pmv[mc][:], func=AF.Copy),
                                r=[f'pmv{mc}'], w=['mv'])
                    P.barrier()
                    P.emit()
                with ExitStack() as st:
                    qml = [sb(st, f"qml{i}", (128, 2, 512), BF16) for i in range(2)]
                    gl = [sb(st, f"gl{i}", (128, 2, 512), BF16) for i in range(2)]
                    pT = [sb(st, f"pT{i}", (128, 2, 512), BF16) for i in range(2)]
                    rinv = sb(st, "rinv", (128, 512), F32)
                    o1 = [sb(st, f"o1{i}", (128, 512), F32) for i in range(2)]
                    ot = [sb(st, f"ot{i}", (128, 512), BF16) for i in range(2)]
                    pl = [ps(st, f"pl{i}", (128, 512)) for i in range(2)]
                    po = [ps(st, f"po{i}", (128, 512)) for i in range(2)]
                    pr = ps(st, "pr", (128, 512))
                    n = 0
                    for hm in range(4):
                        for T in range(NT):
                            s = n % 2
                            n += 1
                            P.dma('sp', qml[s][:], qmT[hm * 256:(hm + 1) * 256, TS(T)].rearrange(
                                "(c p) t -> p c t", p=128), w=[f'qml{s}'])
                            P.dma('sp', gl[s][:], gT[2048 + hm * 256:2048 + (hm + 1) * 256, TS(T)].rearrange(
                                "(c p) t -> p c t", p=128), w=[f'gl{s}'])
                            for mc in range(2):
                                for dmc in range(2):
                                    P.pe(lambda e, s=s, hm=hm, mc=mc, dmc=dmc: e.matmul(
                                        pl[mc][:], lhsT=mkT[:, hm * 2 + dmc, mc * 128:(mc + 1) * 128],
                                        rhs=qml[s][:, dmc, :], start=(dmc == 0), stop=(dmc == 1)),
                                        r=[f'qml{s}'], w=[f'pl{mc}'])
                                P.act(lambda e, s=s, mc=mc: e.activation(out=pT[s][:, mc, :], in_=pl[mc][:],
                                                                         func=AF.Exp),
                                      r=[f'pl{mc}'], w=[f'pT{s}'])
                            for dmc in range(2):
                                for mc in range(2):
                                    P.pe(lambda e, s=s, hm=hm, mc=mc, dmc=dmc: e.matmul(
                                        po[dmc][:], lhsT=mv[:, mc, hm * 256 + dmc * 128:hm * 256 + (dmc + 1) * 128],
                                        rhs=pT[s][:, mc, :], start=(mc == 0), stop=(mc == 1)),
                                        r=[f'pT{s}'], w=[f'po{dmc}'])
                            for mc in range(2):
                                P.pe(lambda e, s=s, mc=mc: e.matmul(pr[:], lhsT=ones_bf[:], rhs=pT[s][:, mc, :],
                                                                    start=(mc == 0), stop=(mc == 1)),
                                     r=[f'pT{s}'], w=['pr'])
                            P.dve(lambda e: e.reciprocal(out=rinv[:], in_=pr[:]), r=['pr'], w=['rinv'])
                            for dmc in range(2):
                                P.dve(lambda e, dmc=dmc: e.tensor_tensor(out=o1[dmc][:], in0=po[dmc][:], in1=rinv[:],
                                                                         op=ALU.mult),
                                      r=[f'po{dmc}', 'rinv'], w=[f'o1{dmc}'])
                                P.dve(lambda e, s=s, dmc=dmc: e.tensor_tensor(
                                    out=ot[dmc][:], in0=o1[dmc][:], in1=gl[s][:, dmc, :], op=ALU.mult),
                                    r=[f'o1{dmc}', f'gl{s}'], w=[f'ot{dmc}'])
                                P.dma('pool', mmemT[hm * 256 + dmc * 128:hm * 256 + (dmc + 1) * 128, TS(T)],
                                      ot[dmc][:], r=[f'ot{dmc}'], w=['dram'])
                    P.barrier()
                    P.emit()

        if upto >= 5:
            with ExitStack() as st:
                kil = sb(st, "kil", (128, S), BF16)
                qil = [sb(st, f"qil{i}", (128, 4, 128), BF16) for i in range(2)]
                wil = [sb(st, f"wil{i}", (128, 8), F32) for i in range(2)]
                score = [sb(st, f"score{i}", (128, S), F32) for i in range(2)]
                rl = [sb(st, f"rl{i}", (128, 512), F32) for i in range(3)]
                junk = sb(st, "junkE", (128, S), BF16)
                mk = [sb(st, f"mk{i}", (128, S), BF16) for i in range(2)]
                mT = [sb(st, f"mT{i}", (128, 8, 128), BF16) for i in range(2)]
                ptab = sb(st, "ptab", (128, NIT + 1), F32)
                thrc = sb(st, "thrc", (128, 1), F32)
                lo = sb(st, "lo", (128, 1), F32)
                hi = sb(st, "hi", (128, 1), F32)
                Wd = sb(st, "Wd", (128, 1), F32)
                hn = sb(st, "hn", (128, NIT + 1), F32)
                mid = sb(st, "mid", (128, 1), F32)
                cnt_t = sb(st, "cnt_t", (128, 1), F32)
                tt = sb(st, "tt", (128, 1), F32)
                pd = [ps(st, f"pd{i}", (128, 512)) for i in range(4)]
                ptm = [ps(st, f"ptm{i}", (128, 1024), BF16) for i in range(2)]
                for n in range(NIT + 1):
                    P.pool(lambda e, n=n: e.memset(ptab[:, n:n + 1], 2.0 ** -(n + 1)), w=['ptab'])
                P.pool(lambda e: e.memset(thrc[:], -1.0e29), w=['thrc'])
                P.dma('sp', kil[:], kiT[:, :], w=['kil'])
                cnt = 0
                tcnt = 0
                for i in range(NB):
                    s = i % 2
                    L = (i + 1) * 128
                    P.dma('sp', qil[s][:], qiT[:, i * 128:(i + 1) * 128].rearrange("(c p) t -> p c t", p=128),
                          w=[f'qil{s}'])
                    P.dma('sp', wil[s][:], witm[i * 128:(i + 1) * 128, :], w=[f'wil{s}'])
                    nst = (L + 511) // 512
                    for stile in range(nst):
                        w_ = min(512, L - stile * 512)
                        c0 = stile * 512
                        for h in range(8):
                            p2, half = divmod(h, 2)
                            b = cnt % 4
                            r3 = cnt % 3
                            cnt += 1
                            P.pe(lambda e, s=s, p2=p2, half=half, b=b, c0=c0, w_=w_: e.matmul(
                                pd[b][:, 0:w_], lhsT=qil[s][half * 64:(half + 1) * 64, p2, :],
                                rhs=kil[half * 64:(half + 1) * 64, c0:c0 + w_], start=True, stop=True),
                                r=[f'qil{s}', 'kil'], w=[f'pd{b}'])
                            P.act(lambda e, b=b, r3=r3, w_=w_: e.activation(
                                out=rl[r3][:, 0:w_], in_=pd[b][:, 0:w_], func=AF.Relu),
                                r=[f'pd{b}'], w=[f'rl{r3}'])
                            if h == 0:
                                P.dve(lambda e, s=s, r3=r3, c0=c0, w_=w_: e.tensor_scalar(
                                    out=score[s][:, c0:c0 + w_], in0=rl[r3][:, 0:w_], scalar1=wil[s][:, 0:1],
                                    scalar2=None, op0=ALU.mult),
                                    r=[f'rl{r3}', f'wil{s}'], w=[f'score{s}'])
                            else:
                                P.dve(lambda e, s=s, r3=r3, c0=c0, w_=w_, h=h: e.scalar_tensor_tensor(
                                    out=score[s][:, c0:c0 + w_], in0=rl[r3][:, 0:w_], scalar=wil[s][:, h:h + 1],
                                    in1=score[s][:, c0:c0 + w_], op0=ALU.mult, op1=ALU.add),
                                    r=[f'rl{r3}', f'wil{s}'], w=[f'score{s}'])
                    P.dve(lambda e, s=s, i=i: e.tensor_tensor(
                        out=score[s][:, i * 128:(i + 1) * 128], in0=score[s][:, i * 128:(i + 1) * 128],
                        in1=caus[:], op=ALU.add), w=[f'score{s}'])
                    if i >= 2:
                        P.dve(lambda e, s=s, i=i: e.tensor_reduce(out=lo[:], in_=score[s][:, 0:i * 128],
                                                                  axis=AX.X, op=ALU.min),
                              r=[f'score{s}'], w=['lo'])
                        P.dve(lambda e, s=s, L=L: e.tensor_reduce(out=hi[:], in_=score[s][:, 0:L],
                                                                  axis=AX.X, op=ALU.max),
                              r=[f'score{s}'], w=['hi'])
                        P.dve(lambda e: e.tensor_tensor(out=Wd[:], in0=hi[:], in1=lo[:], op=ALU.subtract),
                              r=['hi', 'lo'], w=['Wd'])
                        P.dve(lambda e: e.tensor_scalar(out=hn[:], in0=ptab[:], scalar1=Wd[:, 0:1], scalar2=None,
                                                        op0=ALU.mult), r=['Wd', 'ptab'], w=['hn'])
                        P.dve(lambda e: e.tensor_tensor(out=mid[:], in0=lo[:], in1=hn[:, 0:1], op=ALU.add),
                              r=['lo', 'hn'], w=['mid'])
                        for n in range(NIT):
                            P.dve(lambda e, s=s, L=L: e.tensor_scalar(
                                out=junk[:, 0:L], in0=score[s][:, 0:L], scalar1=mid[:, 0:1], scalar2=None,
                                op0=ALU.is_ge, op1=ALU.add, accum_out=cnt_t[:]),
                                r=[f'score{s}', 'mid'], w=['junkE', 'cnt_t'])
                            P.dve(lambda e, n=n: e.tensor_scalar(
                                out=tt[:], in0=cnt_t[:], scalar1=TOPK - 0.5, scalar2=hn[:, n:n + 1],
                                op0=ALU.is_ge, op1=ALU.mult), r=['cnt_t', 'hn'], w=['tt'])
                            nn = n + 1 if n < NIT - 1 else n
                            P.dve(lambda e, nn=nn: e.scalar_tensor_tensor(
                                out=mid[:], in0=mid[:], scalar=hn[:, nn:nn + 1], in1=tt[:],
                                op0=ALU.subtract, op1=ALU.add), r=['tt', 'hn'], w=['mid'])
                        thr = mid
                        thrk = 'mid'
                    else:
                        thr = thrc
                        thrk = 'thrc'
                    P.dve(lambda e, s=s, L=L, thr=thr: e.tensor_scalar(
                        out=mk[s][:, 0:L], in0=score[s][:, 0:L], scalar1=thr[:, 0:1], scalar2=None,
                        op0=ALU.is_ge), r=[f'score{s}', thrk], w=[f'mk{s}'])
                    for g0 in range(0, i + 1, 8):
                        nb_ = min(8, i + 1 - g0)
                        b = tcnt % 2
                        tcnt += 1
                        for q_ in range(nb_):
                            P.pe(lambda e, s=s, b=b, q_=q_, g0=g0: e.transpose(
                                out=ptm[b][:, q_ * 128:(q_ + 1) * 128],
                                in_=mk[s][:, (g0 + q_) * 128:(g0 + q_ + 1) * 128], identity=ident[:]),
                                r=[f'mk{s}'], w=[f'ptm{b}'])
                        P.act(lambda e, b=b, nb_=nb_: e.activation(
                            out=mT[b][:, 0:nb_, :],
                            in_=ptm[b][:, 0:nb_ * 128].rearrange("p (q t) -> p q t", q=nb_), func=AF.Copy),
                            r=[f'ptm{b}'], w=[f'mT{b}'])
                        P.dma('pool', maskT[g0 * 128:(g0 + nb_) * 128, i * 128:(i + 1) * 128].rearrange(
                            "(q p) t -> p q t", p=128), mT[b][:, 0:nb_, :], r=[f'mT{b}'], w=['dram'])
                P.barrier()
                P.emit()

        if upto >= 6:
            with ExitStack() as st:
                ql = [sb(st, f"ql{i}", (128, S), BF16) for i in range(2)]
                kl = [sb(st, f"kl{i}", (128, S), BF16) for i in range(2)]
                vl = [sb(st, f"vl{i}", (128, NB, 128), BF16) for i in range(2)]
                ml = [sb(st, f"ml{i}", (128, 512), BF16) for i in range(4)]
                ex = [sb(st, f"ex{i}", (128, 512), BF16) for i in range(3)]
                pm = [sb(st, f"pm{i}", (128, 512), BF16) for i in range(3)]
                gl = [sb(st, f"gl{i}", (128, 512), BF16) for i in range(2)]
                rinv = sb(st, "rinv", (128, 512), F32)
                o1 = sb(st, "o1", (128, 512), F32)
                ot = [sb(st, f"ot{i}", (128, 512), BF16) for i in range(2)]
                psc = [ps(st, f"psc{i}", (128, 512)) for i in range(3)]
                po = [ps(st, f"po{i}", (128, 512)) for i in range(2)]
                pr = [ps(st, f"pr{i}", (128, 512)) for i in range(2)]
                cnt = 0
                for h in range(8):
                    hs = h % 2
                    P.dma('sp', ql[hs][:], qT[h * 128:(h + 1) * 128, :], w=[f'ql{hs}'])
                    P.dma('sp', kl[hs][:], kT[h * 128:(h + 1) * 128, :], w=[f'kl{hs}'])
                    P.dma('sp', vl[hs][:], vtm[:, h * 128:(h + 1) * 128].rearrange("(b p) d -> p b d", p=128),
                          w=[f'vl{hs}'])
                    for T in range(NT):
                        a = (h * NT + T) % 2
                        P.dma('sp', gl[a][:], gT[1024 + h * 128:1024 + (h + 1) * 128, TS(T)], w=[f'gl{a}'])
                        nsb = 4 * T + 4
                        for sbk in range(nsb):
                            m4 = cnt % 4
                            b3 = cnt % 3
                            cnt += 1
                            r0 = max(0, sbk - 4 * T) * 128
                            t0 = T * 512 + r0
                            P.dma('sp', ml[m4][:, r0:512], maskT[sbk * 128:(sbk + 1) * 128, t0:(T + 1) * 512],
                                  w=[f'ml{m4}'])
                            P.pe(lambda e, hs=hs, sbk=sbk, b3=b3, r0=r0, t0=t0, T=T: e.matmul(
                                psc[b3][:, r0:512], lhsT=kl[hs][:, sbk * 128:(sbk + 1) * 128],
                                rhs=ql[hs][:, t0:(T + 1) * 512], start=True, stop=True),
                                r=[f'kl{hs}', f'ql{hs}'], w=[f'psc{b3}'])
                            P.act(lambda e, b3=b3, r0=r0: e.activation(out=ex[b3][:, r0:512], in_=psc[b3][:, r0:512],
                                                                       func=AF.Exp),
                                  r=[f'psc{b3}'], w=[f'ex{b3}'])
                            P.dve(lambda e, b3=b3, m4=m4, r0=r0: e.tensor_tensor(
                                out=pm[b3][:, r0:512], in0=ex[b3][:, r0:512], in1=ml[m4][:, r0:512], op=ALU.mult),
                                r=[f'ex{b3}', f'ml{m4}'], w=[f'pm{b3}'])
                            P.pe(lambda e, hs=hs, sbk=sbk, b3=b3, r0=r0, a=a, nsb=nsb: e.matmul(
                                po[a][:, r0:512], lhsT=vl[hs][:, sbk, :], rhs=pm[b3][:, r0:512],
                                start=(sbk == 0), stop=(sbk == nsb - 1)),
                                r=[f'pm{b3}', f'vl{hs}'], w=[f'po{a}'])
                            P.pe(lambda e, sbk=sbk, b3=b3, r0=r0, a=a, nsb=nsb: e.matmul(
                                pr[a][:, r0:512], lhsT=ones_bf[:], rhs=pm[b3][:, r0:512],
                                start=(sbk == 0), stop=(sbk == nsb - 1)),
                                r=[f'pm{b3}'], w=[f'pr{a}'])
                        P.dve(lambda e, a=a: e.reciprocal(out=rinv[:], in_=pr[a][:]), r=[f'pr{a}'], w=['rinv'])
                        P.dve(lambda e, a=a: e.tensor_tensor(out=o1[:], in0=po[a][:], in1=rinv[:], op=ALU.mult),
                              r=[f'po{a}', 'rinv'], w=['o1'])
                        P.dve(lambda e, a=a: e.tensor_tensor(out=ot[a][:], in0=o1[:], in1=gl[a][:], op=ALU.mult),
                              r=['o1', f'gl{a}'], w=[f'ot{a}'])
                        P.dma('pool', mattT[h * 128:(h + 1) * 128, TS(T)], ot[a][:], r=[f'ot{a}'], w=['dram'])
                P.barrier()
                P.emit()

        def out_norm_phase(st, nk, load_lhs, wres, gname, resid, dst, tag, post=None):
            gp = sb(st, tag + "gp", (128, D), F32)
            o, n_ = PCOL[gname]
            P.dma('sp', gp[:], params[:, o:o + n_], w=['gp'])
            xt = [sb(st, f"{tag}xt{i}", (128, D), F32) for i in range(2)]
            xo = [sb(st, f"{tag}xo{i}", (128, D), F32) for i in range(2)]
            tmp = sb(st, tag + "tmp", (128, D), F32)
            junkA = sb(st, tag + "junkA", (128, 512), BF16)
            ssh = [sb(st, f"{tag}ssh{i}", (128, 2), F32) for i in range(2)]
            ss = [sb(st, f"{tag}ss{i}", (128, 1), F32) for i in range(2)]
            rt = [sb(st, f"{tag}rt{i}", (128, 1), F32) for i in range(2)]
            rs = [sb(st, f"{tag}rs{i}", (128, 1), F32) for i in range(2)]
            po = [ps(st, f"{tag}po{i}", (128, 512)) for i in range(4)]
            for T in range(NT):
                lhs, lkey = load_lhs(T)
                for tb in range(4):
                    i = T * 4 + tb
                    s2 = i % 2
                    P.dma('sp', xt[s2][:], resid[i * 128:(i + 1) * 128, :], w=[f'xt{s2}'])
                    for hh in range(2):
                        b = s2 * 2 + hh
                        for j in range(nk):
                            P.pe(lambda e, lhs=lhs, j=j, tb=tb, hh=hh, b=b: e.matmul(
                                po[b][:], lhsT=lhs[:, j, tb * 128:(tb + 1) * 128],
                                rhs=wres[:, j, hh * 512:(hh + 1) * 512], start=(j == 0), stop=(j == nk - 1)),
                                r=[lkey, 'wres'], w=[f'po{b}'])
                        P.act(lambda e, b=b, s2=s2, hh=hh: e.activation(
                            out=junkA[:], in_=po[b][:], func=AF.Square, accum_out=ssh[s2][:, hh:hh + 1]),
                            r=[f'po{b}'], w=['junkA', f'ssh{s2}'])
                    P.dve(lambda e, s2=s2: e.tensor_tensor(out=ss[s2][:], in0=ssh[s2][:, 0:1], in1=ssh[s2][:, 1:2],
                                                           op=ALU.add), r=[f'ssh{s2}'], w=[f'ss{s2}'])
                    P.act(lambda e, s2=s2: e.activation(out=rt[s2][:], in_=ss[s2][:], func=AF.Sqrt, bias=EPS,
                                                        scale=1.0 / D), r=[f'ss{s2}'], w=[f'rt{s2}'])
                    P.dve(lambda e, s2=s2: e.reciprocal(out=rs[s2][:], in_=rt[s2][:]), r=[f'rt{s2}'], w=[f'rs{s2}'])
                    for hh in range(2):
                        b = s2 * 2 + hh
                        P.dve(lambda e, b=b, s2=s2, hh=hh: e.scalar_tensor_tensor(
                            out=tmp[:, hh * 512:(hh + 1) * 512], in0=po[b][:], scalar=rs[s2][:, 0:1],
                            in1=gp[:, hh * 512:(hh + 1) * 512], op0=ALU.mult, op1=ALU.mult),
                            r=[f'po{b}', f'rs{s2}', 'gp'], w=['tmp'])
                    P.dve(lambda e, s2=s2: e.tensor_tensor(out=xo[s2][:], in0=tmp[:], in1=xt[s2][:], op=ALU.add),
                          r=['tmp', f'xt{s2}'], w=[f'xo{s2}'])
                    P.dma('pool', dst[i * 128:(i + 1) * 128, :], xo[s2][:], r=[f'xo{s2}'], w=['dram'])
                    if post is not None:
                        post(i, s2, xo[s2], f'xo{s2}')

        h2st = ExitStack()
        if upto >= 7:
            h2T = sb(h2st, "h2T", (128, 8, S), BF16)
            with ExitStack() as st:
                wo = sb(st, "wo", (128, 8, D), BF16)
                for hh in range(2):
                    P.dma('pool', wo[:, :, hh * 512:(hh + 1) * 512],
                          wout[:, hh * 512:(hh + 1) * 512].rearrange("(k p) c -> p k c", p=128), w=['wres'])
                g2 = sb(st, "g2bc", (128, D), F32)
                o, n_ = PCOL['g2']
                P.dma('sp', g2[:], params[:, o:o + n_], w=['g2'])
                ma = [sb(st, f"ma{i}", (128, 8, 512), BF16) for i in range(2)]
                mb = [sb(st, f"mb{i}", (128, 8, 512), BF16) for i in range(2)]
                mc_ = [sb(st, f"mc{i}", (128, 8, 512), BF16) for i in range(2)]
                mg = [sb(st, f"mg{i}", (128, 8, 512), BF16) for i in range(2)]
                junkD = sb(st, "junkD", (128, D), BF16)
                hb = [sb(st, f"Ghb{i}", (128, D), BF16) for i in range(2)]
                ss2 = [sb(st, f"Gss2{i}", (128, 1), F32) for i in range(2)]
                rt2 = [sb(st, f"Grt2{i}", (128, 1), F32) for i in range(2)]
                rs2 = [sb(st, f"Grs2{i}", (128, 1), F32) for i in range(2)]
                pt = [ps(st, f"Gpt{i}", (128, D), BF16) for i in range(2)]

                def load_merged(T):
                    s = T % 2
                    for (buf, src, nm) in ((ma, mconvT, 'ma'), (mb, mattT, 'mb'), (mc_, mmemT, 'mc')):
                        P.dma('sp', buf[s][:], src[:, TS(T)].rearrange("(j p) t -> p j t", p=128), w=[f'{nm}{s}'])
                    P.dve(lambda e, s=s: e.tensor_tensor(out=mg[s][:], in0=ma[s][:], in1=mb[s][:], op=ALU.add),
                          r=[f'ma{s}', f'mb{s}'], w=[f'mg{s}'])
                    P.dve(lambda e, s=s: e.tensor_tensor(out=mg[s][:], in0=mg[s][:], in1=mc_[s][:], op=ALU.add),
                          r=[f'mc{s}'], w=[f'mg{s}'])
                    return mg[s], f'mg{s}'

                def post_h2(i, s2, xo, xkey):
                    P.dve(lambda e, s2=s2: e.scalar_tensor_tensor(
                        out=junkD[:], in0=xo[:], scalar=1.0, in1=xo[:], op0=ALU.mult, op1=ALU.mult,
                        accum_out=ss2[s2][:]), r=[xkey], w=['junkD', f'ss2{s2}'])
                    P.act(lambda e, s2=s2: e.activation(out=rt2[s2][:], in_=ss2[s2][:], func=AF.Sqrt, bias=EPS,
                                                        scale=1.0 / D), r=[f'ss2{s2}'], w=[f'rt2{s2}'])
                    P.dve(lambda e, s2=s2: e.reciprocal(out=rs2[s2][:], in_=rt2[s2][:]),
                          r=[f'rt2{s2}'], w=[f'rs2{s2}'])
                    P.dve(lambda e, s2=s2: e.scalar_tensor_tensor(
                        out=hb[s2][:], in0=xo[:], scalar=rs2[s2][:, 0:1], in1=g2[:], op0=ALU.mult, op1=ALU.mult),
                        r=[xkey, f'rs2{s2}', 'g2'], w=[f'hb{s2}'])
                    for j in range(8):
                        P.pe(lambda e, s2=s2, j=j: e.transpose(out=pt[s2][:, j * 128:(j + 1) * 128],
                                                               in_=hb[s2][:, j * 128:(j + 1) * 128],
                                                               identity=ident[:]),
                             r=[f'hb{s2}'], w=[f'pt{s2}'])
                    P.act(lambda e, s2=s2, i=i: e.activation(
                        out=h2T[:, :, i * 128:(i + 1) * 128],
                        in_=pt[s2][:].rearrange("p (j t) -> p j t", j=8), func=AF.Copy),
                        r=[f'pt{s2}'], w=[f'h2T{i}'])

                out_norm_phase(st, 8, load_merged, wo, 'gpost1', x, x1d, "G", post=post_h2)
                P.barrier()
                P.emit()

        if upto >= 8:
            with ExitStack() as st:
                wt = [sb(st, f"Hwt{i}", (128, 8, 512), BF16) for i in range(2)]
                ug = [sb(st, f"ug{i}", (128, 514), F32) for i in range(3)]
                uvv = [sb(st, f"uv{i}", (128, 514), F32) for i in range(3)]
                cg = [sb(st, f"cg{i}", (128, 512), F32) for i in range(2)]
                cv = [sb(st, f"cv{i}", (128, 512), F32) for i in range(2)]
                sgl = [sb(st, f"sgl{i}", (128, 512), F32) for i in range(2)]
                ast = [sb(st, f"ast{i}", (128, S), BF16) for i in range(2)]
                pg = [ps(st, f"Hpg{i}", (128, 512)) for i in range(2)]
                pvv = [ps(st, f"Hpv{i}", (128, 512)) for i in range(2)]

                def fw(c, tap):
                    return pvc('ffnw', c * 3 + tap, 1)

                n = 0
                for wi_ in range(11):
                    s = wi_ % 2
                    P.dma('pool', wt[s][:], wup[:, wi_ * 512:(wi_ + 1) * 512].rearrange(
                        "(k p) c -> p k c", p=128), w=[f'wt{s}'])
                    for jj in range(2):
                        j = wi_ * 2 + jj
                        a2 = j % 2
                        for T in range(NT):
                            b = n % 2
                            s3 = n % 3
                            p3 = (n - 1) % 3
                            n += 1
                            for k in range(8):
                                P.pe(lambda e, s=s, jj=jj, T=T, k=k, b=b: e.matmul(
                                    pg[b][:], lhsT=wt[s][:, k, jj * 256:jj * 256 + 128],
                                    rhs=h2T[:, k, TS(T)], start=(k == 0), stop=(k == 7)),
                                    r=[f'wt{s}'], w=[f'pg{b}'])
                            for k in range(8):
                                P.pe(lambda e, s=s, jj=jj, T=T, k=k, b=b: e.matmul(
                                    pvv[b][:], lhsT=wt[s][:, k, jj * 256 + 128:jj * 256 + 256],
                                    rhs=h2T[:, k, TS(T)], start=(k == 0), stop=(k == 7)),
                                    r=[f'wt{s}'], w=[f'pvv{b}'])
                            for (u_, nm) in ((ug, 'ug'), (uvv, 'uv')):
                                if T == 0:
                                    P.pool(lambda e, u_=u_, s3=s3: e.memset(u_[s3][:, 0:2], 0.0), w=[f'{nm}{s3}'])
                                else:
                                    P.pool(lambda e, u_=u_, s3=s3, p3=p3: e.tensor_copy(
                                        out=u_[s3][:, 0:2], in_=u_[p3][:, 512:514]),
                                        r=[f'{nm}{p3}'], w=[f'{nm}{s3}'])
                            P.act(lambda e, b=b, s3=s3: e.activation(out=ug[s3][:, 2:514], in_=pg[b][:], func=AF.Copy),
                                  r=[f'pg{b}'], w=[f'ug{s3}'])
                            P.act(lambda e, b=b, s3=s3: e.activation(out=uvv[s3][:, 2:514], in_=pvv[b][:], func=AF.Copy),
                                  r=[f'pvv{b}'], w=[f'uv{s3}'])
                            for (u_, nm, co, cname, c) in ((ug, 'ug', cg, 'cg', 2 * j), (uvv, 'uv', cv, 'cv', 2 * j + 1)):
                                P.dve(lambda e, u_=u_, co=co, b=b, s3=s3, c=c: e.tensor_scalar(
                                    out=co[b][:], in0=u_[s3][:, 2:514], scalar1=fw(c, 2), scalar2=pvc('ffnb', c, 1),
                                    op0=ALU.mult, op1=ALU.add), r=[f'{nm}{s3}'], w=[f'{cname}{b}'])
                                for tap in (1, 0):
                                    P.dve(lambda e, u_=u_, co=co, b=b, s3=s3, c=c, tap=tap: e.scalar_tensor_tensor(
                                        out=co[b][:], in0=u_[s3][:, tap:tap + 512], scalar=fw(c, tap), in1=co[b][:],
                                        op0=ALU.mult, op1=ALU.add), r=[f'{nm}{s3}'], w=[f'{cname}{b}'])
                            P.act(lambda e, b=b: e.activation(out=sgl[b][:], in_=cg[b][:], func=AF.Silu),
                                  r=[f'cg{b}'], w=[f'sgl{b}'])
                            P.dve(lambda e, b=b, a2=a2, T=T: e.tensor_tensor(
                                out=ast[a2][:, TS(T)], in0=sgl[b][:], in1=cv[b][:], op=ALU.mult),
                                r=[f'sgl{b}', f'cv{b}'], w=[f'ast{a2}'])
                            if T == NT - 1:
                                P.dma('sp', aT[j * 128:(j + 1) * 128, :], ast[a2][:], r=[f'ast{a2}'], w=['dram'])
                P.barrier()
                P.emit()
        h2st.close()

        if upto >= 9:
            with ExitStack() as st:
                wd = sb(st, "wd", (128, NFC, D), BF16)
                for hh in range(2):
                    P.dma('pool', wd[:, :, hh * 512:(hh + 1) * 512],
                          wdown[:, hh * 512:(hh + 1) * 512].rearrange("(k p) c -> p k c", p=128), w=['wres'])
                at = [sb(st, f"at{i}", (128, NFC, 512), BF16) for i in range(2)]

                def load_act(T):
                    s = T % 2
                    P.dma('sp', at[s][:], aT[:, TS(T)].rearrange("(j p) t -> p j t", p=128), w=[f'at{s}'])
                    return at[s], f'at{s}'

                out_norm_phase(st, NFC, load_act, wd, 'gpost2', x1d, y, "I")
                P.barrier()
                P.emit()


        P.barrier()
        P.emit()
    return nc


_CACHE = {}


def kernel(**inputs):
    shared = pack_inputs(inputs)
    if 'nc' not in _CACHE:
        _CACHE['nc'] = build()
    nc = _CACHE['nc']
    x = np.asarray(inputs['x'], np.float32)
    mem = np.asarray(inputs['mem'], np.float32)
    in_maps = []
    for b in range(8):
        m = dict(shared)
        m['x'] = np.ascontiguousarray(x[b])
        m['mem'] = np.ascontiguousarray(mem[b])
        in_maps.append(m)
    res = run_bass_kernel_spmd(nc, in_maps, core_ids=list(range(8)))
    return np.stack([np.asarray(r['y'], np.float32) for r in res.results], axis=0)
```

```python
import numpy as np
from contextlib import ExitStack
import concourse.bass as bass
import concourse.mybir as mybir
from concourse.bass_utils import run_bass_kernel_spmd

F32 = mybir.dt.float32
BF16 = mybir.dt.bfloat16
I32 = mybir.dt.int32
AF = mybir.ActivationFunctionType
ALU = mybir.AluOpType
AX = mybir.AxisListType

S = 4096
D = 1024
NB = S // 128
NT = S // 512
EPS = 1e-6
FFN = 2816
NFC = FFN // 128
TOPK = 256
NIT = 18
NEG = -1.0e30

ENGS = ['pe', 'act', 'dve', 'pool', 'sp']
BLK = {'pe': 'tensor', 'act': 'scalar', 'dve': 'vector', 'pool': 'gpsimd', 'sp': 'sync'}
EPOCH = 20000
NDS = 16


class Prog:
    def __init__(self, nc, stack):
        self.nc = nc
        self.stack = stack
        self.lists = {e: [] for e in ENGS}
        self.count = {e: 0 for e in ENGS}
        self.sems = {e: [] for e in ENGS}
        self.waited = {e: {} for e in ENGS}
        self.lastw = {}
        self.reads = {}
        self.dsem = {}
        self.dval = {}
        self.dnext = {}
        self.nsem = 0

    def _esem(self, e, idx):
        while len(self.sems[e]) <= idx:
            self.sems[e].append(self.stack.enter_context(
                self.nc.semaphore(f"s_{e}{len(self.sems[e])}")))
        return self.sems[e][idx]

    def _collect(self, e, r, w, extra):
        toks = list(extra)
        for k in r:
            t = self.lastw.get(k)
            if t is not None:
                toks.append(t)
        for k in w:
            t = self.lastw.get(k)
            if t is not None:
                toks.append(t)
            toks.extend(self.reads.get(k, ()))
        need = []
        for (sid, sem, v, te) in toks:
            if te == e and e == 'pe':
                continue
            if self.waited[e].get(sid, 0) >= v:
                continue
            self.waited[e][sid] = v
            need.append((sem, v))
        return need

    def _update(self, tok, r, w):
        for k in w:
            self.lastw[k] = tok
            self.reads[k] = []
        for k in r:
            self.reads.setdefault(k, []).append(tok)

    def op(self, e, fn, r=(), w=(), extra=()):
        need = self._collect(e, r, w, extra)
        n = self.count[e]
        ep, v = divmod(n, EPOCH)
        sem = self._esem(e, ep)
        self.count[e] = n + 1
        tok = ((e, ep), sem, v + 1, e)
        self.lists[e].append((need, fn, sem, 1))
        self._update(tok, r, w)
        return tok

    def pe(self, fn, r=(), w=(), extra=()):
        return self.op('pe', fn, r, w, extra)

    def act(self, fn, r=(), w=(), extra=()):
        return self.op('act', fn, r, w, extra)

    def dve(self, fn, r=(), w=(), extra=()):
        return self.op('dve', fn, r, w, extra)

    def pool(self, fn, r=(), w=(), extra=()):
        return self.op('pool', fn, r, w, extra)

    def dma(self, e, out, in_, r=(), w=(), extra=(), **kw):
        if e not in self.dsem:
            self.dsem[e] = [self.stack.enter_context(self.nc.semaphore(f"dma_{e}{j}")) for j in range(NDS)]
            self.dval[e] = [0] * NDS
            self.dnext[e] = 0
        j = self.dnext[e]
        self.dnext[e] = (j + 1) % NDS
        sem = self.dsem[e][j]
        extra = list(extra)
        if self.dval[e][j] > 0:
            extra.append((('d', e, j), sem, self.dval[e][j], 'dma'))
        need = self._collect(e, r, w, extra)
        self.dval[e][j] += 16
        tok = (('d', e, j), sem, self.dval[e][j], 'dma')
        self.lists[e].append((need, lambda eng: eng.dma_start(out=out, in_=in_, **kw), sem, 16))
        self._update(tok, r, w)
        return tok

    def barrier(self):
        toks = []
        for e in ENGS:
            n = self.count[e]
            if n == 0:
                continue
            ep, v = divmod(n - 1, EPOCH)
            toks.append(((e, ep), self.sems[e][ep], v + 1, e))
        for q in self.dsem:
            for j in range(NDS):
                if self.dval[q][j] > 0:
                    toks.append((('d', q, j), self.dsem[q][j], self.dval[q][j], 'dma'))
        for e in ENGS:
            need = []
            for (sid, sem, v, te) in toks:
                if te == e:
                    continue
                if self.waited[e].get(sid, 0) >= v:
                    continue
                self.waited[e][sid] = v
                need.append((sem, v))
            if need:
                self.lists[e].append((need, None, None, 0))
        self.lastw = {}
        self.reads = {}

    def emit(self):
        with self.nc.Block() as block:
            for e in ENGS:
                lst = self.lists[e]
                if not lst:
                    continue

                def body(eng, lst=lst):
                    for (need, fn, sem, inc) in lst:
                        for (s, v) in need:
                            eng.wait_ge(s, v)
                        if fn is not None:
                            fn(eng).then_inc(sem, inc)

                getattr(block, BLK[e])(body)
        self.lists = {e: [] for e in ENGS}


PCOL = {}
_off = 0
for _name, _n in [('g1', D), ('gmem', D), ('gpost1', D), ('g2', D), ('gpost2', D),
                  ('convw', 8 * 31), ('convb', 8), ('lng', 8), ('lnb', 8),
                  ('bgate', 24), ('ffnw', 44 * 3), ('ffnb', 44)]:
    PCOL[_name] = (_off, _n)
    _off += _n
NPCOL = _off

WCM_COLS = 45 * 128
WTM_COLS = 1032


def pack_inputs(inp):
    f = np.float32
    w_in = np.asarray(inp['w_in'][0], f)
    c0 = 0
    conv = w_in[:, 0:2048]
    q = w_in[:, 2048:3072]
    k = w_in[:, 3072:4096]
    v = w_in[:, 4096:5120]
    qi = w_in[:, 5120:5632]
    wi = w_in[:, 5632:5640]
    ki = w_in[:, 5640:5704]
    qm = w_in[:, 5704:6728]
    a, g = conv[:, :1024], conv[:, 1024:]
    inter = np.stack([a.reshape(D, 8, 128), g.reshape(D, 8, 128)], axis=2).reshape(D, 2048)
    wcm = np.ascontiguousarray(np.concatenate([inter, q, k, qm, qi, ki, ki], axis=1))
    wtm = np.ascontiguousarray(np.concatenate([v, wi], axis=1))
    w_up = np.asarray(inp['w_up'][0], f)
    ug, uv = w_up[:, :FFN], w_up[:, FFN:]
    wup = np.ascontiguousarray(
        np.stack([ug.reshape(D, NFC, 128), uv.reshape(D, NFC, 128)], axis=2).reshape(D, 2 * FFN))
    P = np.zeros((128, NPCOL), f)

    def put(name, arr):
        o, n = PCOL[name]
        P[:, o:o + n] = arr

    put('g1', np.broadcast_to(inp['norm1_pre_g'][0], (128, D)))
    put('gmem', np.broadcast_to(inp['mem_norm_g'][0], (128, D)))
    put('gpost1', np.broadcast_to(inp['norm1_post_g'][0], (128, D)))
    put('g2', np.broadcast_to(inp['norm2_pre_g'][0], (128, D)))
    put('gpost2', np.broadcast_to(inp['norm2_post_g'][0], (128, D)))
    cw = np.asarray(inp['conv_dw_w'][0], f)
    put('convw', cw.T.reshape(8, 128, 31).transpose(1, 0, 2).reshape(128, 8 * 31))
    put('convb', np.asarray(inp['conv_dw_b'][0], f).reshape(8, 128).T)
    put('lng', np.asarray(inp['conv_ln_g'][0], f).reshape(8, 128).T)
    put('lnb', np.asarray(inp['conv_ln_b'][0], f).reshape(8, 128).T)
    put('bgate', np.asarray(inp['b_gate'][0], f).reshape(24, 128).T)
    fw = np.asarray(inp['ffn_dw_w'][0], f)
    fwg, fwv = fw[:, :FFN], fw[:, FFN:]
    fwi = np.stack([fwg.reshape(3, NFC, 128), fwv.reshape(3, NFC, 128)], axis=2).reshape(3, 44, 128)
    put('ffnw', fwi.transpose(2, 1, 0).reshape(128, 44 * 3))
    fb = np.asarray(inp['ffn_dw_b'][0], f)
    fbi = np.stack([fb[:FFN].reshape(NFC, 128), fb[FFN:].reshape(NFC, 128)], axis=1).reshape(44, 128)
    put('ffnb', fbi.T)
    shared = {
        'wcm': wcm, 'wtm': wtm, 'wup': wup, 'params': P,
        'pw2': np.ascontiguousarray(inp['conv_pw2'][0], f),
        'wmkv': np.ascontiguousarray(inp['w_mem_kv'][0], f),
        'wgate': np.ascontiguousarray(inp['w_gate'][0], f),
        'wout': np.ascontiguousarray(inp['w_out'][0], f),
        'wdown': np.ascontiguousarray(inp['w_down'][0], f),
    }
    return shared


def build(debug=(), upto=99):
    nc = bass.Bass("TRN2", target_bir_lowering=False)
    dbg = set(debug)

    def din(name, shape):
        return nc.dram_tensor(name, list(shape), F32, kind="ExternalInput").ap()

    def scratch(name, shape, dt):
        kind = "ExternalOutput" if name in dbg else "Internal"
        return nc.dram_tensor(name, list(shape), dt, kind=kind).ap()

    x = din("x", (S, D))
    mem = din("mem", (256, D))
    wcm = din("wcm", (D, WCM_COLS))
    wtm = din("wtm", (D, WTM_COLS))
    wup = din("wup", (D, 2 * FFN))
    params = din("params", (128, NPCOL))
    pw2 = din("pw2", (D, D))
    wmkv = din("wmkv", (D, 2 * D))
    wgate = din("wgate", (D, 3 * D))
    wout = din("wout", (D, D))
    wdown = din("wdown", (FFN, D))
    y = nc.dram_tensor("y", [S, D], F32, kind="ExternalOutput").ap()

    ycT = scratch("ycT", (D, S), F32)
    qT = scratch("qT", (D, S), BF16)
    kT = scratch("kT", (D, S), BF16)
    qmT = scratch("qmT", (D, S), BF16)
    qiT = scratch("qiT", (512, S), BF16)
    kiT = scratch("kiT", (128, S), BF16)
    vtm = scratch("vtm", (S, D), BF16)
    witm = scratch("witm", (S, 8), F32)
    gT = scratch("gT", (3 * D, S), BF16)
    mconvT = scratch("mconvT", (D, S), BF16)
    mattT = scratch("mattT", (D, S), BF16)
    mmemT = scratch("mmemT", (D, S), BF16)
    maskT = scratch("maskT", (S, S), BF16)
    x1d = scratch("x1d", (S, D), F32)
    aT = scratch("aT", (FFN, S), BF16)

    def pcols(t, name):
        o, n = PCOL[name]
        return t[:, o:o + n]

    with ExitStack() as gs:
        P = Prog(nc, gs)

        uniq = [0]

        def sb(st, name, shape, dt):
            uniq[0] += 1
            return st.enter_context(nc.sbuf_tensor(f"{name}_{uniq[0]}", list(shape), dt))

        def ps(st, name, shape, dt=F32):
            uniq[0] += 1
            return st.enter_context(nc.psum_tensor(f"{name}_{uniq[0]}", list(shape), dt))

        ident = sb(gs, "ident", (128, 128), BF16)
        ones_bf = sb(gs, "ones_bf", (128, 128), BF16)
        ones_f = sb(gs, "ones_f", (128, 128), F32)
        caus = sb(gs, "caus", (128, 128), F32)
        pv = sb(gs, "pv", (128, 472), F32)
        pvo = PCOL['convw'][0]

        def pvc(name, j0=0, n=None):
            o, nn = PCOL[name]
            o -= pvo
            if n is None:
                n = nn
            return pv[:, o + j0:o + j0 + n]

        with ExitStack() as st:
            iot = sb(st, "iot", (128, 128), I32)
            P.pool(lambda e: e.iota(out=iot[:], pattern=[[1, 128]], base=0, channel_multiplier=-1),
                   w=['iot'])
            P.dve(lambda e: e.tensor_scalar(out=ident[:], in0=iot[:], scalar1=0.0, scalar2=None,
                                            op0=ALU.is_equal), r=['iot'], w=['ident'])
            P.dve(lambda e: e.tensor_scalar(out=caus[:], in0=iot[:], scalar1=0.5, scalar2=NEG,
                                            op0=ALU.is_gt, op1=ALU.mult), r=['iot'], w=['caus'])
            P.dve(lambda e: e.memset(ones_bf[:], 1.0), w=['ones_bf'])
            P.dve(lambda e: e.memset(ones_f[:], 1.0), w=['ones_f'])
            P.dma('sp', pv[:], params[:, pvo:pvo + 472], w=['pv'])
            P.barrier()
            P.emit()

        hst = ExitStack()
        hT = sb(hst, "hT", (128, 8, S), BF16)

        def norm_transpose_phase(st, src, nblk, gname, dstT, tag):
            gbc = sb(st, tag + "gbc", (128, D), F32)
            xt = [sb(st, f"{tag}xt{i}", (128, D), F32) for i in range(2)]
            junk = sb(st, tag + "junk", (128, D), BF16)
            hb = [sb(st, f"{tag}hb{i}", (128, D), BF16) for i in range(2)]
            ss = [sb(st, f"{tag}ss{i}", (128, 1), F32) for i in range(2)]
            rt = [sb(st, f"{tag}rt{i}", (128, 1), F32) for i in range(2)]
            rs = [sb(st, f"{tag}rs{i}", (128, 1), F32) for i in range(2)]
            pt = [ps(st, f"{tag}pt{i}", (128, D), BF16) for i in range(2)]
            o, n = PCOL[gname]
            P.dma('sp', gbc[:], params[:, o:o + n], w=['gbc'])
            for i in range(nblk):
                s = i % 2
                P.dma('sp', xt[s][:], src[i * 128:(i + 1) * 128, :], w=[f'xt{s}'])
                P.dve(lambda e, s=s: e.scalar_tensor_tensor(
                    out=junk[:], in0=xt[s][:], scalar=1.0, in1=xt[s][:], op0=ALU.mult, op1=ALU.mult,
                    accum_out=ss[s][:]), r=[f'xt{s}'], w=['junk', f'ss{s}'])
                P.act(lambda e, s=s: e.activation(out=rt[s][:], in_=ss[s][:], func=AF.Sqrt,
                                                  bias=EPS, scale=1.0 / D),
                      r=[f'ss{s}'], w=[f'rt{s}'])
                P.dve(lambda e, s=s: e.reciprocal(out=rs[s][:], in_=rt[s][:]), r=[f'rt{s}'], w=[f'rs{s}'])
                P.dve(lambda e, s=s: e.scalar_tensor_tensor(
                    out=hb[s][:], in0=xt[s][:], scalar=rs[s][:], in1=gbc[:], op0=ALU.mult, op1=ALU.mult),
                    r=[f'xt{s}', f'rs{s}', 'gbc'], w=[f'hb{s}'])
                for j in range(8):
                    P.pe(lambda e, s=s, j=j: e.transpose(out=pt[s][:, j * 128:(j + 1) * 128],
                                                         in_=hb[s][:, j * 128:(j + 1) * 128],
                                                         identity=ident[:]),
                         r=[f'hb{s}'], w=[f'pt{s}'])
                P.act(lambda e, s=s, i=i: e.activation(
                    out=dstT[:, :, i * 128:(i + 1) * 128],
                    in_=pt[s][:].rearrange("p (j t) -> p j t", j=8), func=AF.Copy),
                    r=[f'pt{s}'], w=[f'dstT{i}'])

        with ExitStack() as st:
            norm_transpose_phase(st, x, NB, 'g1', hT, "A")
            P.barrier()
            P.emit()

        def proj_cm(st, W, ncols, evac, tag, wtile=512):
            wt = [sb(st, f"{tag}wt{i}", (128, 8, wtile), BF16) for i in range(2)]
            pp = [ps(st, f"{tag}pp{i}", (128, 512), F32) for i in range(4)]
            ntile = (ncols + wtile - 1) // wtile
            cnt = 0
            for wi_ in range(ntile):
                s = wi_ % 2
                c0 = wi_ * wtile
                wc = min(wtile, ncols - c0)
                P.dma('pool', wt[s][:, :, 0:wc],
                      W[:, c0:c0 + wc].rearrange("(k p) c -> p k c", p=128), w=[f'wt{s}'])
                for cc in range(wc // 128):
                    c = (c0 // 128) + cc
                    for T in range(NT):
                        b = cnt % 4
                        cnt += 1
                        for k in range(8):
                            P.pe(lambda e, s=s, cc=cc, T=T, k=k, b=b: e.matmul(
                                pp[b][:], lhsT=wt[s][:, k, cc * 128:(cc + 1) * 128],
                                rhs=hT[:, k, T * 512:(T + 1) * 512], start=(k == 0), stop=(k == 7)),
                                r=[f'wt{s}'], w=[f'pp{b}'])
                        evac(c, T, pp[b], f'pp{b}')

        if upto >= 2:
            with ExitStack() as st:
                yg = [sb(st, f"yg{i}", (128, 30 + 512), F32) for i in range(3)]
                sg = [sb(st, f"sg{i}", (128, 512), F32) for i in range(2)]
                yc = [sb(st, f"yc{i}", (128, 512), F32) for i in range(2)]
                wt = [sb(st, f"Bwt{i}", (128, 8, 512), BF16) for i in range(2)]
                pa = [ps(st, f"Bpa{i}", (128, 512), F32) for i in range(2)]
                pg = [ps(st, f"Bpg{i}", (128, 512), F32) for i in range(2)]
                n = 0
                for wi_ in range(4):
                    s = wi_ % 2
                    P.dma('pool', wt[s][:], wcm[:, wi_ * 512:(wi_ + 1) * 512].rearrange(
                        "(k p) c -> p k c", p=128), w=[f'wt{s}'])
                    for jj in range(2):
                        j = wi_ * 2 + jj
                        for T in range(NT):
                            b = n % 2
                            s3 = n % 3
                            p3 = (n - 1) % 3
                            for k in range(8):
                                P.pe(lambda e, s=s, jj=jj, T=T, k=k, b=b: e.matmul(
                                    pa[b][:], lhsT=wt[s][:, k, jj * 256:jj * 256 + 128],
                                    rhs=hT[:, k, T * 512:(T + 1) * 512], start=(k == 0), stop=(k == 7)),
                                    r=[f'wt{s}'], w=[f'pa{b}'])
                            for k in range(8):
                                P.pe(lambda e, s=s, jj=jj, T=T, k=k, b=b: e.matmul(
                                    pg[b][:], lhsT=wt[s][:, k, jj * 256 + 128:jj * 256 + 256],
                                    rhs=hT[:, k, T * 512:(T + 1) * 512], start=(k == 0), stop=(k == 7)),
                                    r=[f'wt{s}'], w=[f'pg{b}'])
                            P.act(lambda e, b=b: e.activation(out=sg[b][:], in_=pg[b][:], func=AF.Sigmoid),
                                  r=[f'pg{b}'], w=[f'sg{b}'])
                            if T == 0:
                                P.pool(lambda e, s3=s3: e.memset(yg[s3][:, 0:30], 0.0), w=[f'yg{s3}'])
                            else:
                                P.pool(lambda e, s3=s3, p3=p3: e.tensor_copy(
                                    out=yg[s3][:, 0:30], in_=yg[p3][:, 512:542]),
                                    r=[f'yg{p3}'], w=[f'yg{s3}'])
                            P.dve(lambda e, b=b, s3=s3: e.tensor_tensor(
                                out=yg[s3][:, 30:542], in0=pa[b][:], in1=sg[b][:], op=ALU.mult),
                                r=[f'pa{b}', f'sg{b}'], w=[f'yg{s3}'])
                            P.dve(lambda e, b=b, s3=s3, j=j: e.tensor_scalar(
                                out=yc[b][:], in0=yg[s3][:, 30:542], scalar1=pvc('convw', j * 31 + 30, 1),
                                scalar2=pvc('convb', j, 1), op0=ALU.mult, op1=ALU.add),
                                r=[f'yg{s3}'], w=[f'yc{b}'])
                            for tap in range(30):
                                P.dve(lambda e, b=b, s3=s3, j=j, tap=tap: e.scalar_tensor_tensor(
                                    out=yc[b][:], in0=yg[s3][:, tap:tap + 512],
                                    scalar=pvc('convw', j * 31 + tap, 1), in1=yc[b][:],
                                    op0=ALU.mult, op1=ALU.add),
                                    r=[f'yg{s3}'], w=[f'yc{b}'])
                            P.dma('sp', ycT[j * 128:(j + 1) * 128, T * 512:(T + 1) * 512], yc[b][:],
                                  r=[f'yc{b}'], w=['dram'])
                            n += 1
                P.barrier()
                P.emit()

            with ExitStack() as st:
                stg = [sb(st, f"stg{i}", (128, S), BF16) for i in range(2)]

                def evac_gen(c, T, pp, key):
                    c = c + 16
                    if c < 24:
                        dst, row, scale = qT, (c - 16) * 128, 128.0 ** -0.5
                    elif c < 32:
                        dst, row, scale = kT, (c - 24) * 128, 1.0
                    elif c < 40:
                        dst, row, scale = qmT, (c - 32) * 128, 256.0 ** -0.5
                    elif c < 44:
                        dst, row, scale = qiT, (c - 40) * 128, 1.0
                    else:
                        dst, row, scale = kiT, 0, 1.0
                    s2 = c % 2
                    P.act(lambda e: e.activation(out=stg[s2][:, T * 512:(T + 1) * 512], in_=pp[:],
                                                 func=AF.Copy, scale=scale),
                          r=[key], w=[f'stg{s2}'])
                    if T == NT - 1:
                        P.dma('sp', dst[row:row + 128, :], stg[s2][:], r=[f'stg{s2}'], w=['dram'])

                proj_cm(st, wcm[:, 2048:], WCM_COLS - 2048, evac_gen, "Bq")
                P.barrier()
                P.emit()

            with ExitStack() as st:
                stg = [sb(st, f"stg{i}", (128, S), BF16) for i in range(2)]

                def evac_gate(c, T, pp, key):
                    s2 = c % 2
                    P.act(lambda e: e.activation(out=stg[s2][:, T * 512:(T + 1) * 512], in_=pp[:],
                                                 func=AF.Sigmoid, bias=pvc('bgate', c, 1), scale=1.0),
                          r=[key], w=[f'stg{s2}'])
                    if T == NT - 1:
                        P.dma('sp', gT[c * 128:(c + 1) * 128, :], stg[s2][:], r=[f'stg{s2}'], w=['dram'])

                proj_cm(st, wgate, 3 * D, evac_gate, "Bg")
                P.barrier()
                P.emit()

            with ExitStack() as st:
                wv = sb(st, "wv", (128, 8, WTM_COLS), BF16)
                vst = [sb(st, f"vst{i}", (128, D), BF16) for i in range(2)]
                wst = [sb(st, f"wst{i}", (128, 8), F32) for i in range(2)]
                pvv = [ps(st, f"pvv{i}", (128, 512), F32) for i in range(4)]
                pw = [ps(st, f"pw{i}", (128, 8), F32) for i in range(2)]
                for hh in range(2):
                    P.dma('pool', wv[:, :, hh * 512:(hh + 1) * 512],
                          wtm[:, hh * 512:(hh + 1) * 512].rearrange("(k p) c -> p k c", p=128), w=['wv'])
                P.dma('pool', wv[:, :, 1024:1032],
                      wtm[:, 1024:1032].rearrange("(k p) c -> p k c", p=128), w=['wv'])
                for i in range(NB):
                    s = i % 2
                    for hh in range(2):
                        b = (i * 2 + hh) % 4
                        for k in range(8):
                            P.pe(lambda e, i=i, hh=hh, k=k, b=b: e.matmul(
                                pvv[b][:], lhsT=hT[:, k, i * 128:(i + 1) * 128],
                                rhs=wv[:, k, hh * 512:(hh + 1) * 512], start=(k == 0), stop=(k == 7)),
                                r=['wv'], w=[f'pvv{b}'])
                        P.act(lambda e, s=s, hh=hh, b=b: e.activation(
                            out=vst[s][:, hh * 512:(hh + 1) * 512], in_=pvv[b][:], func=AF.Copy),
                            r=[f'pvv{b}'], w=[f'vst{s}'])
                    for k in range(8):
                        P.pe(lambda e, i=i, k=k, s=s: e.matmul(
                            pw[s][:], lhsT=hT[:, k, i * 128:(i + 1) * 128],
                            rhs=wv[:, k, 1024:1032], start=(k == 0), stop=(k == 7)),
                            r=['wv'], w=[f'pw{s}'])
                    P.dve(lambda e, s=s: e.tensor_copy(out=wst[s][:], in_=pw[s][:]),
                          r=[f'pw{s}'], w=[f'wst{s}'])
                    P.dma('sp', vtm[i * 128:(i + 1) * 128, :], vst[s][:], r=[f'vst{s}'], w=['dram'])
                    P.dma('sp', witm[i * 128:(i + 1) * 128, :], wst[s][:], r=[f'wst{s}'], w=['dram'])
                P.barrier()
                P.emit()
        hst.close()

        def TS(T):
            return slice(T * 512, (T + 1) * 512)

        if upto >= 3:
            with ExitStack() as st:
                pwt = sb(st, "pwt", (128, 8, D), BF16)
                for hh in range(2):
                    P.dma('pool', pwt[:, :, hh * 512:(hh + 1) * 512],
                          pw2[:, hh * 512:(hh + 1) * 512].rearrange("(k p) c -> p k c", p=128), w=['pwt'])
                ycl = [sb(st, f"ycl{i}", (128, 8, 512), F32) for i in range(2)]
                sq = sb(st, "sq", (128, 8, 512), F32)
                gl = [sb(st, f"gl{i}", (128, 8, 512), BF16) for i in range(2)]
                mean = sb(st, "mean", (128, 512), F32)
                msq = sb(st, "msq", (128, 512), F32)
                var = sb(st, "var", (128, 512), F32)
                sd = sb(st, "sd", (128, 512), F32)
                rstd = sb(st, "rstd", (128, 512), F32)
                t1 = [sb(st, f"t1{i}", (128, 512), F32) for i in range(2)]
                ys = sb(st, "ys", (128, 8, 512), BF16)
                ot = [sb(st, f"ot{i}", (128, 512), BF16) for i in range(2)]
                s1 = ps(st, "s1", (128, 512))
                s2 = ps(st, "s2", (128, 512))
                po = [ps(st, f"po{i}", (128, 512)) for i in range(2)]
                for T in range(NT):
                    s = T % 2
                    P.dma('sp', ycl[s][:], ycT[:, TS(T)].rearrange("(j p) t -> p j t", p=128), w=[f'ycl{s}'])
                    P.dma('sp', gl[s][:], gT[0:1024, TS(T)].rearrange("(j p) t -> p j t", p=128), w=[f'gl{s}'])
                    P.act(lambda e, s=s: e.activation(out=sq[:], in_=ycl[s][:], func=AF.Square),
                          r=[f'ycl{s}'], w=['sq'])
                    for j in range(8):
                        P.pe(lambda e, s=s, j=j: e.matmul(s1[:], lhsT=ones_f[:], rhs=ycl[s][:, j, :],
                                                          start=(j == 0), stop=(j == 7)),
                             r=[f'ycl{s}', 'mean'], w=['s1'])
                    for j in range(8):
                        P.pe(lambda e, j=j: e.matmul(s2[:], lhsT=ones_f[:], rhs=sq[:, j, :],
                                                     start=(j == 0), stop=(j == 7)), r=['sq'], w=['s2'])
                    P.dve(lambda e: e.tensor_scalar(out=mean[:], in0=s1[:], scalar1=1.0 / D, scalar2=None,
                                                    op0=ALU.mult), r=['s1'], w=['mean'])
                    P.dve(lambda e: e.tensor_tensor(out=msq[:], in0=mean[:], in1=mean[:], op=ALU.mult),
                          r=['mean'], w=['msq'])
                    P.dve(lambda e: e.scalar_tensor_tensor(out=var[:], in0=s2[:], scalar=1.0 / D, in1=msq[:],
                                                           op0=ALU.mult, op1=ALU.subtract),
                          r=['s2', 'msq'], w=['var'])
                    P.act(lambda e: e.activation(out=sd[:], in_=var[:], func=AF.Sqrt, bias=EPS, scale=1.0),
                          r=['var'], w=['sd'])
                    P.dve(lambda e: e.reciprocal(out=rstd[:], in_=sd[:]), r=['sd'], w=['rstd'])
                    for j in range(8):
                        b = j % 2
                        P.dve(lambda e, s=s, j=j, b=b: e.tensor_tensor(
                            out=t1[b][:], in0=ycl[s][:, j, :], in1=mean[:], op=ALU.subtract),
                            r=[f'ycl{s}', 'mean'], w=[f't1{b}'])
                        P.dve(lambda e, b=b: e.tensor_tensor(out=t1[b][:], in0=t1[b][:], in1=rstd[:], op=ALU.mult),
                              r=['rstd'], w=[f't1{b}'])
                        P.act(lambda e, j=j, b=b: e.activation(out=ys[:, j, :], in_=t1[b][:], func=AF.Silu,
                                                               scale=pvc('lng', j, 1), bias=pvc('lnb', j, 1)),
                              r=[f't1{b}'], w=['ys'])
                    for dch in range(8):
                        b = dch % 2
                        for j in range(8):
                            P.pe(lambda e, dch=dch, j=j, b=b: e.matmul(
                                po[b][:], lhsT=pwt[:, j, dch * 128:(dch + 1) * 128], rhs=ys[:, j, :],
                                start=(j == 0), stop=(j == 7)), r=['ys', 'pwt'], w=[f'po{b}'])
                        P.dve(lambda e, s=s, dch=dch, b=b: e.tensor_tensor(
                            out=ot[b][:], in0=po[b][:], in1=gl[s][:, dch, :], op=ALU.mult),
                            r=[f'po{b}', f'gl{s}'], w=[f'ot{b}'])
                        P.dma('pool', mconvT[dch * 128:(dch + 1) * 128, TS(T)], ot[b][:], r=[f'ot{b}'], w=['dram'])
                P.barrier()
                P.emit()

        if upto >= 4:
            with ExitStack() as st0:
                mnT = sb(st0, "mnT", (128, 8, 256), BF16)
                mkT = sb(st0, "mkT", (128, 8, 256), BF16)
                mv = sb(st0, "mv", (128, 2, D), BF16)
                with ExitStack() as st:
                    norm_transpose_phase(st, mem, 2, 'gmem', mnT, "D")
                    P.barrier()
                    P.emit()
                with ExitStack() as st:
                    wk = [sb(st, f"wk{i}", (128, 8, 512), BF16) for i in range(2)]
                    pk = [ps(st, f"pk{i}", (128, 512)) for i in range(2)]
                    pmv = [ps(st, f"pmv{i}", (128, 512)) for i in range(2)]
                    for wi_ in range(2):
                        s = wi_ % 2
                        P.dma('pool', wk[s][:], wmkv[:, wi_ * 512:(wi_ + 1) * 512].rearrange(
                            "(k p) c -> p k c", p=128), w=[f'wk{s}'])
                        for cc in range(4):
                            c = wi_ * 4 + cc
                            b = c % 2
                            for k in range(8):
                                P.pe(lambda e, s=s, cc=cc, k=k, b=b: e.matmul(
                                    pk[b][:, 0:256], lhsT=wk[s][:, k, cc * 128:(cc + 1) * 128],
                                    rhs=mnT[:, k, :], start=(k == 0), stop=(k == 7)),
                                    r=[f'wk{s}'], w=[f'pk{b}'])
                            P.act(lambda e, c=c, b=b: e.activation(out=mkT[:, c, :], in_=pk[b][:, 0:256],
                                                                   func=AF.Copy), r=[f'pk{b}'], w=['mkT'])
                    for wi_ in range(2):
                        s = wi_ % 2
                        P.dma('pool', wk[s][:], wmkv[:, 1024 + wi_ * 512:1024 + (wi_ + 1) * 512].rearrange(
                            "(k p) c -> p k c", p=128), w=[f'wk{s}'])
                        for mc in range(2):
                            for k in range(8):
                                P.pe(lambda e, s=s, mc=mc, k=k: e.matmul(
                                    pmv[mc][:], lhsT=mnT[:, k, mc * 128:(mc + 1) * 128], rhs=wk[s][:, k, :],
                                    start=(k == 0), stop=(k == 7)), r=[f'wk{s}'], w=[f'pmv{mc}'])
                            P.act(lambda e, wi_=wi_, mc=mc: e.activation(
                                out=mv[:, mc, wi_ * 512:(wi_ + 1) * 512], in_=pmv[mc][:], func=AF.Copy),
                                r=[f'pmv{mc}'], w=['mv'])
                    P.barrier()
                    P.emit()
                with ExitStack() as st:
                    qml = [sb(st, f"qml{i}", (128, 2, 512), BF16) for i in range(2)]
                    gl = [sb(st, f"gl{i}", (128, 2, 512), BF16) for i in range(2)]
                    pT = [sb(st, f"pT{i}", (128, 2, 512), BF16) for i in range(2)]
                    rinv = sb(st, "rinv", (128, 512), F32)
                    o1 = [sb(st, f"o1{i}", (128, 512), F32) for i in range(2)]
                    ot = [sb(st, f"ot{i}", (128, 512), BF16) for i in range(2)]
                    pl = [ps(st, f"pl{i}", (128, 512)) for i in range(2)]
                    po = [ps(st, f"po{i}", (128, 512)) for i in range(2)]
                    pr = ps(st, "pr", (128, 512))
                    n = 0
                    for hm in range(4):
                        for T in range(NT):
                            s = n % 2
                            n += 1
                            P.dma('sp', qml[s][:], qmT[hm * 256:(hm + 1) * 256, TS(T)].rearrange(
                                "(c p) t -> p c t", p=128), w=[f'qml{s}'])
                            P.dma('sp', gl[s][:], gT[2048 + hm * 256:2048 + (hm + 1) * 256, TS(T)].rearrange(
                                "(c p) t -> p c t", p=128), w=[f'gl{s}'])
                            for mc in range(2):
                                for dmc in range(2):
                                    P.pe(lambda e, s=s, hm=hm, mc=mc, dmc=dmc: e.matmul(
                                        pl[mc][:], lhsT=mkT[:, hm * 2 + dmc, mc * 128:(mc + 1) * 128],
                                        rhs=qml[s][:, dmc, :], start=(dmc == 0), stop=(dmc == 1)),
                                        r=[f'qml{s}'], w=[f'pl{mc}'])
                                P.act(lambda e, s=s, mc=mc: e.activation(out=pT[s][:, mc, :], in_=pl[mc][:],
                                                                         func=AF.Exp),
                                      r=[f'pl{mc}'], w=[f'pT{s}'])
                            for dmc in range(2):
                                for mc in range(2):
                                    P.pe(lambda e, s=s, hm=hm, mc=mc, dmc=dmc: e.matmul(
                                        po[dmc][:], lhsT=mv[:, mc, hm * 256 + dmc * 128:hm * 256 + (dmc + 1) * 128],
                                        rhs=pT[s][:, mc, :], start=(mc == 0), stop=(mc == 1)),
                                        r=[f'pT{s}'], w=[f'po{dmc}'])
                            for mc in range(2):
                                P.pe(lambda e, s=s, mc=mc: e.matmul(pr[:], lhsT=ones_bf[:], rhs=pT[s][:, mc, :],
                                                                    start=(mc == 0), stop=(mc == 1)),
                                     r=[f'pT{s}'], w=['pr'])
                            P.dve(lambda e: e.reciprocal(out=rinv[:], in_=pr[:]), r=['pr'], w=['rinv'])
                            for dmc in range(2):
                                P.dve(lambda e, dmc=dmc: e.tensor_tensor(out=o1[dmc][:], in0=po[dmc][:], in1=rinv[:],
                                                                         op=ALU.mult),
                                      r=[f'po{dmc}', 'rinv'], w=[f'o1{dmc}'])
                                P.dve(lambda e, s=s, dmc=dmc: e.tensor_tensor(
                                    out=ot[dmc][:], in0=o1[dmc][:], in1=gl[s][:, dmc, :], op=ALU.mult),
                                    r=[f'o1{dmc}', f'gl{s}'], w=[f'ot{dmc}'])
                                P.dma('pool', mmemT[hm * 256 + dmc * 128:hm * 256 + (dmc + 1) * 128, TS(T)],
                                      ot[dmc][:], r=[f'ot{dmc}'], w=['dram'])
                    P.barrier()
                    P.emit()

        if upto >= 5:
            with ExitStack() as st:
                kil = sb(st, "kil", (128, S), BF16)
                qil = [sb(st, f"qil{i}", (128, 4, 128), BF16) for i in range(2)]
                wil = [sb(st, f"wil{i}", (128, 8), F32) for i in range(2)]
                score = [sb(st, f"score{i}", (128, S), F32) for i in range(2)]
                rl = [sb(st, f"rl{i}", (128, 512), F32) for i in range(3)]
                junk = sb(st, "junkE", (128, S), BF16)
                mk = [sb(st, f"mk{i}", (128, S), BF16) for i in range(2)]
                mT = [sb(st, f"mT{i}", (128, 8, 128), BF16) for i in range(2)]
                ptab = sb(st, "ptab", (128, NIT + 1), F32)
                thrc = sb(st, "thrc", (128, 1), F32)
                lo = sb(st, "lo", (128, 1), F32)
                hi = sb(st, "hi", (128, 1), F32)
                Wd = sb(st, "Wd", (128, 1), F32)
                hneg = sb(st, "hneg", (128, NIT + 1), F32)
                nm = sb(st, "nm", (128, 1), F32)
                ssum = sb(st, "ssum", (128, 1), F32)
                sgn = sb(st, "sgn", (128, 1), F32)
                thr_t = sb(st, "thr_t", (128, 1), F32)
                pd = [ps(st, f"pd{i}", (128, 512)) for i in range(4)]
                ptm = [ps(st, f"ptm{i}", (128, 1024), BF16) for i in range(2)]
                for n in range(NIT + 1):
                    P.pool(lambda e, n=n: e.memset(ptab[:, n:n + 1], -(2.0 ** -(n + 1))), w=['ptab'])
                P.pool(lambda e: e.memset(thrc[:], -1.0e29), w=['thrc'])
                P.dma('sp', kil[:], kiT[:, :], w=['kil'])
                cnts = {'c': 0, 't': 0}

                def score_units(i):
                    s = i % 2
                    L = (i + 1) * 128
                    units = []

                    def loads():
                        P.dma('sp', qil[s][:], qiT[:, i * 128:(i + 1) * 128].rearrange("(c p) t -> p c t", p=128),
                              w=[f'qil{s}'])
                        P.dma('sp', wil[s][:], witm[i * 128:(i + 1) * 128, :], w=[f'wil{s}'])
                    units.append(loads)
                    nst = (L + 511) // 512
                    for stile in range(nst):
                        w_ = min(512, L - stile * 512)
                        c0 = stile * 512
                        for h in range(8):
                            def unit(stile=stile, w_=w_, c0=c0, h=h):
                                p2, half = divmod(h, 2)
                                b = cnts['c'] % 4
                                r3 = cnts['c'] % 3
                                cnts['c'] += 1
                                P.pe(lambda e: e.matmul(
                                    pd[b][:, 0:w_], lhsT=qil[s][half * 64:(half + 1) * 64, p2, :],
                                    rhs=kil[half * 64:(half + 1) * 64, c0:c0 + w_], start=True, stop=True),
                                    r=[f'qil{s}', 'kil'], w=[f'pd{b}'])
                                P.act(lambda e: e.activation(
                                    out=rl[r3][:, 0:w_], in_=pd[b][:, 0:w_], func=AF.Relu),
                                    r=[f'pd{b}'], w=[f'rl{r3}'])
                                if h == 0:
                                    P.dve(lambda e: e.tensor_scalar(
                                        out=score[s][:, c0:c0 + w_], in0=rl[r3][:, 0:w_], scalar1=wil[s][:, 0:1],
                                        scalar2=None, op0=ALU.mult),
                                        r=[f'rl{r3}', f'wil{s}'], w=[f'score{s}'])
                                else:
                                    P.dve(lambda e: e.scalar_tensor_tensor(
                                        out=score[s][:, c0:c0 + w_], in0=rl[r3][:, 0:w_], scalar=wil[s][:, h:h + 1],
                                        in1=score[s][:, c0:c0 + w_], op0=ALU.mult, op1=ALU.add),
                                        r=[f'rl{r3}', f'wil{s}'], w=[f'score{s}'])
                            units.append(unit)

                    def fin():
                        P.dve(lambda e: e.tensor_tensor(
                            out=score[s][:, i * 128:(i + 1) * 128], in0=score[s][:, i * 128:(i + 1) * 128],
                            in1=caus[:], op=ALU.add), w=[f'score{s}'])
                    units.append(fin)
                    return units

                def bisect_units(i):
                    s = i % 2
                    L = (i + 1) * 128
                    units = []
                    if i >= 2:
                        def pro():
                            P.dve(lambda e: e.tensor_reduce(out=lo[:], in_=score[s][:, 0:i * 128],
                                                            axis=AX.X, op=ALU.min), r=[f'score{s}'], w=['lo'])
                            P.dve(lambda e: e.tensor_reduce(out=hi[:], in_=score[s][:, 0:L],
                                                            axis=AX.X, op=ALU.max), r=[f'score{s}'], w=['hi'])
                            P.dve(lambda e: e.tensor_tensor(out=Wd[:], in0=hi[:], in1=lo[:], op=ALU.subtract),
                                  r=['hi', 'lo'], w=['Wd'])
                            P.dve(lambda e: e.tensor_scalar(out=hneg[:], in0=ptab[:], scalar1=Wd[:, 0:1],
                                                            scalar2=None, op0=ALU.mult),
                                  r=['Wd', 'ptab'], w=['hneg'])
                            P.dve(lambda e: e.tensor_scalar(out=nm[:], in0=lo[:], scalar1=-1.0,
                                                            scalar2=hneg[:, 0:1], op0=ALU.mult, op1=ALU.add),
                                  r=['lo', 'hneg'], w=['nm'])
                        units.append(pro)
                        for n in range(NIT):
                            def it(n=n):
                                P.act(lambda e: e.activation(
                                    out=junk[:, 0:L], in_=score[s][:, 0:L], func=AF.Sign, bias=nm[:, 0:1],
                                    scale=1.0, accum_out=ssum[:]),
                                    r=[f'score{s}', 'nm'], w=['junkE', 'ssum'])
                                P.act(lambda e: e.activation(
                                    out=sgn[:], in_=ssum[:], func=AF.Sign, bias=float(L - 2 * TOPK) + 0.5,
                                    scale=1.0), r=['ssum'], w=['sgn'])
                                P.act(lambda e: e.activation(
                                    out=nm[:], in_=sgn[:], func=AF.Identity, scale=hneg[:, n + 1:n + 2],
                                    bias=nm[:, 0:1]), r=['sgn', 'hneg'], w=['nm'])
                            units.append(it)

                    def epi():
                        if i >= 2:
                            P.dve(lambda e: e.tensor_scalar(out=thr_t[:], in0=nm[:], scalar1=-1.0,
                                                            scalar2=hneg[:, NIT:NIT + 1], op0=ALU.mult, op1=ALU.add),
                                  r=['nm', 'hneg'], w=['thr_t'])
                            thr, thrk = thr_t, 'thr_t'
                        else:
                            thr, thrk = thrc, 'thrc'
                        P.dve(lambda e: e.tensor_scalar(
                            out=mk[s][:, 0:L], in0=score[s][:, 0:L], scalar1=thr[:, 0:1], scalar2=None,
                            op0=ALU.is_ge), r=[f'score{s}', thrk], w=[f'mk{s}'])
                        for g0 in range(0, i + 1, 8):
                            nb_ = min(8, i + 1 - g0)
                            b = cnts['t'] % 2
                            cnts['t'] += 1
                            for q_ in range(nb_):
                                P.pe(lambda e, b=b, q_=q_, g0=g0: e.transpose(
                                    out=ptm[b][:, q_ * 128:(q_ + 1) * 128],
                                    in_=mk[s][:, (g0 + q_) * 128:(g0 + q_ + 1) * 128], identity=ident[:]),
                                    r=[f'mk{s}'], w=[f'ptm{b}'])
                            P.act(lambda e, b=b, nb_=nb_: e.activation(
                                out=mT[b][:, 0:nb_, :],
                                in_=ptm[b][:, 0:nb_ * 128].rearrange("p (q t) -> p q t", q=nb_), func=AF.Copy),
                                r=[f'ptm{b}'], w=[f'mT{b}'])
                            P.dma('pool', maskT[g0 * 128:(g0 + nb_) * 128, i * 128:(i + 1) * 128].rearrange(
                                "(q p) t -> p q t", p=128), mT[b][:, 0:nb_, :], r=[f'mT{b}'], w=['dram'])
                    units.append(epi)
                    return units

                for u in score_units(0):
                    u()
                for i in range(NB):
                    A_ = bisect_units(i)
                    B_ = score_units(i + 1) if i + 1 < NB else []
                    done = 0
                    for idx, a_ in enumerate(A_):
                        a_()
                        tgt = (len(B_) * (idx + 1)) // len(A_)
                        while done < tgt:
                            B_[done]()
                            done += 1
                P.barrier()
                P.emit()

        if upto >= 6:
            with ExitStack() as st:
                ql = [sb(st, f"ql{i}", (128, S), BF16) for i in range(2)]
                kl = [sb(st, f"kl{i}", (128, S), BF16) for i in range(2)]
                vl = [sb(st, f"vl{i}", (128, NB, 128), BF16) for i in range(2)]
                NML = 6
                ml = [sb(st, f"ml{i}", (128, 512), BF16) for i in range(NML)]
                ex = [sb(st, f"ex{i}", (128, 512), BF16) for i in range(4)]
                pm = [sb(st, f"pm{i}", (128, 512), BF16) for i in range(4)]
                gl = [sb(st, f"gl{i}", (128, 512), BF16) for i in range(2)]
                rinv = sb(st, "rinv", (128, 512), F32)
                o1 = sb(st, "o1", (128, 512), F32)
                ot = [sb(st, f"ot{i}", (128, 512), BF16) for i in range(2)]
                psc = [ps(st, f"psc{i}", (128, 512)) for i in range(4)]
                po = [ps(st, f"po{i}", (128, 512)) for i in range(2)]
                pr = [ps(st, f"pr{i}", (128, 512)) for i in range(2)]
                its = []
                for h in range(8):
                    for T in range(NT):
                        nsb = 4 * T + 4
                        for sbk in range(nsb):
                            its.append((h, T, sbk, nsb))
                DEPTH = 2

                def front(n):
                    h, T, sbk, nsb = its[n]
                    hs = h % 2
                    a = (h * NT + T) % 2
                    if T == 0 and sbk == 0:
                        P.dma('sp', ql[hs][:], qT[h * 128:(h + 1) * 128, :], w=[f'ql{hs}'])
                        P.dma('sp', kl[hs][:], kT[h * 128:(h + 1) * 128, :], w=[f'kl{hs}'])
                        P.dma('sp', vl[hs][:], vtm[:, h * 128:(h + 1) * 128].rearrange("(b p) d -> p b d", p=128),
                              w=[f'vl{hs}'])
                    if sbk == 0:
                        P.dma('sp', gl[a][:], gT[1024 + h * 128:1024 + (h + 1) * 128, TS(T)], w=[f'gl{a}'])
                    m4 = n % NML
                    b3 = n % 4
                    r0 = max(0, sbk - 4 * T) * 128
                    t0 = T * 512 + r0
                    P.dma('sp', ml[m4][:, r0:512], maskT[sbk * 128:(sbk + 1) * 128, t0:(T + 1) * 512],
                          w=[f'ml{m4}'])
                    P.pe(lambda e: e.matmul(
                        psc[b3][:, r0:512], lhsT=kl[hs][:, sbk * 128:(sbk + 1) * 128],
                        rhs=ql[hs][:, t0:(T + 1) * 512], start=True, stop=True),
                        r=[f'kl{hs}', f'ql{hs}'], w=[f'psc{b3}'])
                    P.act(lambda e: e.activation(out=ex[b3][:, r0:512], in_=psc[b3][:, r0:512], func=AF.Exp),
                          r=[f'psc{b3}'], w=[f'ex{b3}'])
                    P.dve(lambda e: e.tensor_tensor(
                        out=pm[b3][:, r0:512], in0=ex[b3][:, r0:512], in1=ml[m4][:, r0:512], op=ALU.mult),
                        r=[f'ex{b3}', f'ml{m4}'], w=[f'pm{b3}'])

                def back(n):
                    h, T, sbk, nsb = its[n]
                    hs = h % 2
                    a = (h * NT + T) % 2
                    b3 = n % 4
                    r0 = max(0, sbk - 4 * T) * 128
                    P.pe(lambda e: e.matmul(
                        po[a][:, r0:512], lhsT=vl[hs][:, sbk, :], rhs=pm[b3][:, r0:512],
                        start=(sbk == 0), stop=(sbk == nsb - 1)),
                        r=[f'pm{b3}', f'vl{hs}'], w=[f'po{a}'])
                    P.pe(lambda e: e.matmul(
                        pr[a][:, r0:512], lhsT=ones_bf[:], rhs=pm[b3][:, r0:512],
                        start=(sbk == 0), stop=(sbk == nsb - 1)),
                        r=[f'pm{b3}'], w=[f'pr{a}'])
                    if sbk == nsb - 1:
                        P.dve(lambda e: e.reciprocal(out=rinv[:], in_=pr[a][:]), r=[f'pr{a}'], w=['rinv'])
                        P.dve(lambda e: e.tensor_tensor(out=o1[:], in0=po[a][:], in1=rinv[:], op=ALU.mult),
                              r=[f'po{a}', 'rinv'], w=['o1'])
                        P.dve(lambda e: e.tensor_tensor(out=ot[a][:], in0=o1[:], in1=gl[a][:], op=ALU.mult),
                              r=['o1', f'gl{a}'], w=[f'ot{a}'])
                        P.dma('pool', mattT[h * 128:(h + 1) * 128, TS(T)], ot[a][:], r=[f'ot{a}'], w=['dram'])

                for n in range(len(its) + DEPTH):
                    if n < len(its):
                        front(n)
                    if n >= DEPTH:
                        back(n - DEPTH)
                P.barrier()
                P.emit()

        def out_norm_phase(st, nk, load_lhs, wres, gname, resid, dst, tag, post=None):
            gp = sb(st, tag + "gp", (128, D), F32)
            o, n_ = PCOL[gname]
            P.dma('sp', gp[:], params[:, o:o + n_], w=['gp'])
            xt = [sb(st, f"{tag}xt{i}", (128, D), F32) for i in range(2)]
            xo = [sb(st, f"{tag}xo{i}", (128, D), F32) for i in range(2)]
            tmp = sb(st, tag + "tmp", (128, D), F32)
            junkA = sb(st, tag + "junkA", (128, 512), BF16)
            ssh = [sb(st, f"{tag}ssh{i}", (128, 2), F32) for i in range(2)]
            ss = [sb(st, f"{tag}ss{i}", (128, 1), F32) for i in range(2)]
            rt = [sb(st, f"{tag}rt{i}", (128, 1), F32) for i in range(2)]
            rs = [sb(st, f"{tag}rs{i}", (128, 1), F32) for i in range(2)]
            po = [ps(st, f"{tag}po{i}", (128, 512)) for i in range(4)]
            for T in range(NT):
                lhs, lkey = load_lhs(T)
                for tb in range(4):
                    i = T * 4 + tb
                    s2 = i % 2
                    P.dma('sp', xt[s2][:], resid[i * 128:(i + 1) * 128, :], w=[f'xt{s2}'])
                    for hh in range(2):
                        b = s2 * 2 + hh
                        for j in range(nk):
                            P.pe(lambda e, lhs=lhs, j=j, tb=tb, hh=hh, b=b: e.matmul(
                                po[b][:], lhsT=lhs[:, j, tb * 128:(tb + 1) * 128],
                                rhs=wres[:, j, hh * 512:(hh + 1) * 512], start=(j == 0), stop=(j == nk - 1)),
                                r=[lkey, 'wres'], w=[f'po{b}'])
                        P.act(lambda e, b=b, s2=s2, hh=hh: e.activation(
                            out=junkA[:], in_=po[b][:], func=AF.Square, accum_out=ssh[s2][:, hh:hh + 1]),
                            r=[f'po{b}'], w=['junkA', f'ssh{s2}'])
                    P.dve(lambda e, s2=s2: e.tensor_tensor(out=ss[s2][:], in0=ssh[s2][:, 0:1], in1=ssh[s2][:, 1:2],
                                                           op=ALU.add), r=[f'ssh{s2}'], w=[f'ss{s2}'])
                    P.act(lambda e, s2=s2: e.activation(out=rt[s2][:], in_=ss[s2][:], func=AF.Sqrt, bias=EPS,
                                                        scale=1.0 / D), r=[f'ss{s2}'], w=[f'rt{s2}'])
                    P.dve(lambda e, s2=s2: e.reciprocal(out=rs[s2][:], in_=rt[s2][:]), r=[f'rt{s2}'], w=[f'rs{s2}'])
                    for hh in range(2):
                        b = s2 * 2 + hh
                        P.dve(lambda e, b=b, s2=s2, hh=hh: e.scalar_tensor_tensor(
                            out=tmp[:, hh * 512:(hh + 1) * 512], in0=po[b][:], scalar=rs[s2][:, 0:1],
                            in1=gp[:, hh * 512:(hh + 1) * 512], op0=ALU.mult, op1=ALU.mult),
                            r=[f'po{b}', f'rs{s2}', 'gp'], w=['tmp'])
                    P.dve(lambda e, s2=s2: e.tensor_tensor(out=xo[s2][:], in0=tmp[:], in1=xt[s2][:], op=ALU.add),
                          r=['tmp', f'xt{s2}'], w=[f'xo{s2}'])
                    P.dma('pool', dst[i * 128:(i + 1) * 128, :], xo[s2][:], r=[f'xo{s2}'], w=['dram'])
                    if post is not None:
                        post(i, s2, xo[s2], f'xo{s2}')

        h2st = ExitStack()
        if upto >= 7:
            h2T = sb(h2st, "h2T", (128, 8, S), BF16)
            with ExitStack() as st:
                wo = sb(st, "wo", (128, 8, D), BF16)
                for hh in range(2):
                    P.dma('pool', wo[:, :, hh * 512:(hh + 1) * 512],
                          wout[:, hh * 512:(hh + 1) * 512].rearrange("(k p) c -> p k c", p=128), w=['wres'])
                g2 = sb(st, "g2bc", (128, D), F32)
                o, n_ = PCOL['g2']
                P.dma('sp', g2[:], params[:, o:o + n_], w=['g2'])
                ma = [sb(st, f"ma{i}", (128, 8, 512), BF16) for i in range(2)]
                mb = [sb(st, f"mb{i}", (128, 8, 512), BF16) for i in range(2)]
                mc_ = [sb(st, f"mc{i}", (128, 8, 512), BF16) for i in range(2)]
                mg = [sb(st, f"mg{i}", (128, 8, 512), BF16) for i in range(2)]
                junkD = sb(st, "junkD", (128, D), BF16)
                hb = [sb(st, f"Ghb{i}", (128, D), BF16) for i in range(2)]
                ss2 = [sb(st, f"Gss2{i}", (128, 1), F32) for i in range(2)]
                rt2 = [sb(st, f"Grt2{i}", (128, 1), F32) for i in range(2)]
                rs2 = [sb(st, f"Grs2{i}", (128, 1), F32) for i in range(2)]
                pt = [ps(st, f"Gpt{i}", (128, D), BF16) for i in range(2)]

                def load_merged(T):
                    s = T % 2
                    for (buf, src, nm) in ((ma, mconvT, 'ma'), (mb, mattT, 'mb'), (mc_, mmemT, 'mc')):
                        P.dma('sp', buf[s][:], src[:, TS(T)].rearrange("(j p) t -> p j t", p=128), w=[f'{nm}{s}'])
                    P.dve(lambda e, s=s: e.tensor_tensor(out=mg[s][:], in0=ma[s][:], in1=mb[s][:], op=ALU.add),
                          r=[f'ma{s}', f'mb{s}'], w=[f'mg{s}'])
                    P.dve(lambda e, s=s: e.tensor_tensor(out=mg[s][:], in0=mg[s][:], in1=mc_[s][:], op=ALU.add),
                          r=[f'mc{s}'], w=[f'mg{s}'])
                    return mg[s], f'mg{s}'

                def post_h2(i, s2, xo, xkey):
                    P.dve(lambda e, s2=s2: e.scalar_tensor_tensor(
                        out=junkD[:], in0=xo[:], scalar=1.0, in1=xo[:], op0=ALU.mult, op1=ALU.mult,
                        accum_out=ss2[s2][:]), r=[xkey], w=['junkD', f'ss2{s2}'])
                    P.act(lambda e, s2=s2: e.activation(out=rt2[s2][:], in_=ss2[s2][:], func=AF.Sqrt, bias=EPS,
                                                        scale=1.0 / D), r=[f'ss2{s2}'], w=[f'rt2{s2}'])
                    P.dve(lambda e, s2=s2: e.reciprocal(out=rs2[s2][:], in_=rt2[s2][:]),
                          r=[f'rt2{s2}'], w=[f'rs2{s2}'])
                    P.dve(lambda e, s2=s2: e.scalar_tensor_tensor(
                        out=hb[s2][:], in0=xo[:], scalar=rs2[s2][:, 0:1], in1=g2[:], op0=ALU.mult, op1=ALU.mult),
                        r=[xkey, f'rs2{s2}', 'g2'], w=[f'hb{s2}'])
                    for j in range(8):
                        P.pe(lambda e, s2=s2, j=j: e.transpose(out=pt[s2][:, j * 128:(j + 1) * 128],
                                                               in_=hb[s2][:, j * 128:(j + 1) * 128],
                                                               identity=ident[:]),
                             r=[f'hb{s2}'], w=[f'pt{s2}'])
                    P.act(lambda e, s2=s2, i=i: e.activation(
                        out=h2T[:, :, i * 128:(i + 1) * 128],
                        in_=pt[s2][:].rearrange("p (j t) -> p j t", j=8), func=AF.Copy),
                        r=[f'pt{s2}'], w=[f'h2T{i}'])

                out_norm_phase(st, 8, load_merged, wo, 'gpost1', x, x1d, "G", post=post_h2)
                P.barrier()
                P.emit()

        if upto >= 8:
            with ExitStack() as st:
                wt = [sb(st, f"Hwt{i}", (128, 8, 512), BF16) for i in range(2)]
                ug = [sb(st, f"ug{i}", (128, 514), F32) for i in range(3)]
                uvv = [sb(st, f"uv{i}", (128, 514), F32) for i in range(3)]
                cg = [sb(st, f"cg{i}", (128, 512), F32) for i in range(2)]
                cv = [sb(st, f"cv{i}", (128, 512), F32) for i in range(2)]
                sgl = [sb(st, f"sgl{i}", (128, 512), F32) for i in range(2)]
                ast = [sb(st, f"ast{i}", (128, S), BF16) for i in range(2)]
                pg = [ps(st, f"Hpg{i}", (128, 512)) for i in range(2)]
                pvv = [ps(st, f"Hpv{i}", (128, 512)) for i in range(2)]

                def fw(c, tap):
                    return pvc('ffnw', c * 3 + tap, 1)

                n = 0
                for wi_ in range(11):
                    s = wi_ % 2
                    P.dma('pool', wt[s][:], wup[:, wi_ * 512:(wi_ + 1) * 512].rearrange(
                        "(k p) c -> p k c", p=128), w=[f'wt{s}'])
                    for jj in range(2):
                        j = wi_ * 2 + jj
                        a2 = j % 2
                        for T in range(NT):
                            b = n % 2
                            s3 = n % 3
                            p3 = (n - 1) % 3
                            n += 1
                            for k in range(8):
                                P.pe(lambda e, s=s, jj=jj, T=T, k=k, b=b: e.matmul(
                                    pg[b][:], lhsT=wt[s][:, k, jj * 256:jj * 256 + 128],
                                    rhs=h2T[:, k, TS(T)], start=(k == 0), stop=(k == 7)),
                                    r=[f'wt{s}'], w=[f'pg{b}'])
                            for k in range(8):
                                P.pe(lambda e, s=s, jj=jj, T=T, k=k, b=b: e.matmul(
                                    pvv[b][:], lhsT=wt[s][:, k, jj * 256 + 128:jj * 256 + 256],
                                    rhs=h2T[:, k, TS(T)], start=(k == 0), stop=(k == 7)),
                                    r=[f'wt{s}'], w=[f'pvv{b}'])
                            for (u_, nm) in ((ug, 'ug'), (uvv, 'uv')):
                                if T == 0:
                                    P.pool(lambda e, u_=u_, s3=s3: e.memset(u_[s3][:, 0:2], 0.0), w=[f'{nm}{s3}'])
                                else:
                                    P.pool(lambda e, u_=u_, s3=s3, p3=p3: e.tensor_copy(
                                        out=u_[s3][:, 0:2], in_=u_[p3][:, 512:514]),
                                        r=[f'{nm}{p3}'], w=[f'{nm}{s3}'])
                            P.act(lambda e, b=b, s3=s3: e.activation(out=ug[s3][:, 2:514], in_=pg[b][:], func=AF.Copy),
                                  r=[f'pg{b}'], w=[f'ug{s3}'])
                            P.act(lambda e, b=b, s3=s3: e.activation(out=uvv[s3][:, 2:514], in_=pvv[b][:], func=AF.Copy),
                                  r=[f'pvv{b}'], w=[f'uv{s3}'])
                            for (u_, nm, co, cname, c) in ((ug, 'ug', cg, 'cg', 2 * j), (uvv, 'uv', cv, 'cv', 2 * j + 1)):
                                P.dve(lambda e, u_=u_, co=co, b=b, s3=s3, c=c: e.tensor_scalar(
                                    out=co[b][:], in0=u_[s3][:, 2:514], scalar1=fw(c, 2), scalar2=pvc('ffnb', c, 1),
                                    op0=ALU.mult, op1=ALU.add), r=[f'{nm}{s3}'], w=[f'{cname}{b}'])
                                for tap in (1, 0):
                                    P.dve(lambda e, u_=u_, co=co, b=b, s3=s3, c=c, tap=tap: e.scalar_tensor_tensor(
                                        out=co[b][:], in0=u_[s3][:, tap:tap + 512], scalar=fw(c, tap), in1=co[b][:],
                                        op0=ALU.mult, op1=ALU.add), r=[f'{nm}{s3}'], w=[f'{cname}{b}'])
                            P.act(lambda e, b=b: e.activation(out=sgl[b][:], in_=cg[b][:], func=AF.Silu),
                                  r=[f'cg{b}'], w=[f'sgl{b}'])
                            P.dve(lambda e, b=b, a2=a2, T=T: e.tensor_tensor(
                                out=ast[a2][:, TS(T)], in0=sgl[b][:], in1=cv[b][:], op=ALU.mult),
                                r=[f'sgl{b}', f'cv{b}'], w=[f'ast{a2}'])
                            if T == NT - 1:
                                P.dma('sp', aT[j * 128:(j + 1) * 128, :], ast[a2][:], r=[f'ast{a2}'], w=['dram'])
                P.barrier()
                P.emit()
        h2st.close()

        if upto >= 9:
            with ExitStack() as st:
                wd = sb(st, "wd", (128, NFC, D), BF16)
                for hh in range(2):
                    P.dma('pool', wd[:, :, hh * 512:(hh + 1) * 512],
                          wdown[:, hh * 512:(hh + 1) * 512].rearrange("(k p) c -> p k c", p=128), w=['wres'])
                at = [sb(st, f"at{i}", (128, NFC, 512), BF16) for i in range(2)]

                def load_act(T):
                    s = T % 2
                    P.dma('sp', at[s][:], aT[:, TS(T)].rearrange("(j p) t -> p j t", p=128), w=[f'at{s}'])
                    return at[s], f'at{s}'

                out_norm_phase(st, NFC, load_act, wd, 'gpost2', x1d, y, "I")
                P.barrier()
                P.emit()


        P.barrier()
        P.emit()
    return nc


_CACHE = {}


def kernel(**inputs):
    shared = pack_inputs(inputs)
    if 'nc' not in _CACHE:
        _CACHE['nc'] = build()
    nc = _CACHE['nc']
    x = np.asarray(inputs['x'], np.float32)
    mem = np.asarray(inputs['mem'], np.float32)
    in_maps = []
    for b in range(8):
        m = dict(shared)
        m['x'] = np.ascontiguousarray(x[b])
        m['mem'] = np.ascontiguousarray(mem[b])
        in_maps.append(m)
    res = run_bass_kernel_spmd(nc, in_maps, core_ids=list(range(8)))
    return np.stack([np.asarray(r['y'], np.float32) for r in res.results], axis=0)
```

```python
import numpy as np
from contextlib import ExitStack
import concourse.bass as bass
import concourse.mybir as mybir
from concourse.bass_utils import run_bass_kernel_spmd

F32 = mybir.dt.float32
BF16 = mybir.dt.bfloat16
I32 = mybir.dt.int32
AF = mybir.ActivationFunctionType
ALU = mybir.AluOpType
AX = mybir.AxisListType

S = 4096
D = 1024
NB = S // 128
NT = S // 512
EPS = 1e-6
FFN = 2816
NFC = FFN // 128
TOPK = 256
NIT = 16
DVE_ITERS = (3, 6, 9, 12, 14)
NEG = -1.0e30

ENGS = ['pe', 'act', 'dve', 'pool', 'sp']
BLK = {'pe': 'tensor', 'act': 'scalar', 'dve': 'vector', 'pool': 'gpsimd', 'sp': 'sync'}
EPOCH = 20000
NDS = 16


class Prog:
    def __init__(self, nc, stack):
        self.nc = nc
        self.stack = stack
        self.lists = {e: [] for e in ENGS}
        self.count = {e: 0 for e in ENGS}
        self.sems = {e: [] for e in ENGS}
        self.waited = {e: {} for e in ENGS}
        self.lastw = {}
        self.reads = {}
        self.dsem = {}
        self.dval = {}
        self.dnext = {}
        self.nsem = 0

    def _esem(self, e, idx):
        while len(self.sems[e]) <= idx:
            self.sems[e].append(self.stack.enter_context(
                self.nc.semaphore(f"s_{e}{len(self.sems[e])}")))
        return self.sems[e][idx]

    def _collect(self, e, r, w, extra):
        toks = list(extra)
        for k in r:
            t = self.lastw.get(k)
            if t is not None:
                toks.append(t)
        for k in w:
            t = self.lastw.get(k)
            if t is not None:
                toks.append(t)
            toks.extend(self.reads.get(k, ()))
        need = []
        for (sid, sem, v, te) in toks:
            if te == e and e == 'pe':
                continue
            if self.waited[e].get(sid, 0) >= v:
                continue
            self.waited[e][sid] = v
            need.append((sem, v))
        return need

    def _update(self, tok, r, w):
        for k in w:
            self.lastw[k] = tok
            self.reads[k] = []
        for k in r:
            self.reads.setdefault(k, []).append(tok)

    def op(self, e, fn, r=(), w=(), extra=()):
        need = self._collect(e, r, w, extra)
        n = self.count[e]
        ep, v = divmod(n, EPOCH)
        sem = self._esem(e, ep)
        self.count[e] = n + 1
        tok = ((e, ep), sem, v + 1, e)
        self.lists[e].append((need, fn, sem, 1))
        self._update(tok, r, w)
        return tok

    def pe(self, fn, r=(), w=(), extra=()):
        return self.op('pe', fn, r, w, extra)

    def act(self, fn, r=(), w=(), extra=()):
        return self.op('act', fn, r, w, extra)

    def dve(self, fn, r=(), w=(), extra=()):
        return self.op('dve', fn, r, w, extra)

    def pool(self, fn, r=(), w=(), extra=()):
        return self.op('pool', fn, r, w, extra)

    def dma(self, e, out, in_, r=(), w=(), extra=(), **kw):
        if e not in self.dsem:
            self.dsem[e] = [self.stack.enter_context(self.nc.semaphore(f"dma_{e}{j}")) for j in range(NDS)]
            self.dval[e] = [0] * NDS
            self.dnext[e] = 0
        j = self.dnext[e]
        self.dnext[e] = (j + 1) % NDS
        sem = self.dsem[e][j]
        extra = list(extra)
        if self.dval[e][j] > 0:
            extra.append((('d', e, j), sem, self.dval[e][j], 'dma'))
        need = self._collect(e, r, w, extra)
        self.dval[e][j] += 16
        tok = (('d', e, j), sem, self.dval[e][j], 'dma')
        self.lists[e].append((need, lambda eng: eng.dma_start(out=out, in_=in_, **kw), sem, 16))
        self._update(tok, r, w)
        return tok

    def barrier(self):
        toks = []
        for e in ENGS:
            n = self.count[e]
            if n == 0:
                continue
            ep, v = divmod(n - 1, EPOCH)
            toks.append(((e, ep), self.sems[e][ep], v + 1, e))
        for q in self.dsem:
            for j in range(NDS):
                if self.dval[q][j] > 0:
                    toks.append((('d', q, j), self.dsem[q][j], self.dval[q][j], 'dma'))
        for e in ENGS:
            need = []
            for (sid, sem, v, te) in toks:
                if te == e:
                    continue
                if self.waited[e].get(sid, 0) >= v:
                    continue
                self.waited[e][sid] = v
                need.append((sem, v))
            if need:
                self.lists[e].append((need, None, None, 0))
        self.lastw = {}
        self.reads = {}

    def emit(self):
        with self.nc.Block() as block:
            for e in ENGS:
                lst = self.lists[e]
                if not lst:
                    continue

                def body(eng, lst=lst):
                    for (need, fn, sem, inc) in lst:
                        for (s, v) in need:
                            eng.wait_ge(s, v)
                        if fn is not None:
                            fn(eng).then_inc(sem, inc)

                getattr(block, BLK[e])(body)
        self.lists = {e: [] for e in ENGS}


PCOL = {}
_off = 0
for _name, _n in [('g1', D), ('gmem', D), ('gpost1', D), ('g2', D), ('gpost2', D),
                  ('convw', 8 * 31), ('convb', 8), ('lng', 8), ('lnb', 8),
                  ('bgate', 24), ('ffnw', 44 * 3), ('ffnb', 44)]:
    PCOL[_name] = (_off, _n)
    _off += _n
NPCOL = _off

WCM_COLS = 45 * 128
WTM_COLS = 1032


def pack_inputs(inp):
    f = np.float32
    w_in = np.asarray(inp['w_in'][0], f)
    c0 = 0
    conv = w_in[:, 0:2048]
    q = w_in[:, 2048:3072]
    k = w_in[:, 3072:4096]
    v = w_in[:, 4096:5120]
    qi = w_in[:, 5120:5632]
    wi = w_in[:, 5632:5640]
    ki = w_in[:, 5640:5704]
    qm = w_in[:, 5704:6728]
    a, g = conv[:, :1024], conv[:, 1024:]
    inter = np.stack([a.reshape(D, 8, 128), g.reshape(D, 8, 128)], axis=2).reshape(D, 2048)
    wcm = np.ascontiguousarray(np.concatenate([inter, q, k, qm, qi, ki, ki], axis=1))
    wtm = np.ascontiguousarray(np.concatenate([v, wi], axis=1))
    w_up = np.asarray(inp['w_up'][0], f)
    ug, uv = w_up[:, :FFN], w_up[:, FFN:]
    wup = np.ascontiguousarray(
        np.stack([ug.reshape(D, NFC, 128), uv.reshape(D, NFC, 128)], axis=2).reshape(D, 2 * FFN))
    P = np.zeros((128, NPCOL), f)

    def put(name, arr):
        o, n = PCOL[name]
        P[:, o:o + n] = arr

    put('g1', np.broadcast_to(inp['norm1_pre_g'][0], (128, D)))
    put('gmem', np.broadcast_to(inp['mem_norm_g'][0], (128, D)))
    put('gpost1', np.broadcast_to(inp['norm1_post_g'][0], (128, D)))
    put('g2', np.broadcast_to(inp['norm2_pre_g'][0], (128, D)))
    put('gpost2', np.broadcast_to(inp['norm2_post_g'][0], (128, D)))
    cw = np.asarray(inp['conv_dw_w'][0], f)
    put('convw', cw.T.reshape(8, 128, 31).transpose(1, 0, 2).reshape(128, 8 * 31))
    put('convb', np.asarray(inp['conv_dw_b'][0], f).reshape(8, 128).T)
    put('lng', np.asarray(inp['conv_ln_g'][0], f).reshape(8, 128).T)
    put('lnb', np.asarray(inp['conv_ln_b'][0], f).reshape(8, 128).T)
    put('bgate', np.asarray(inp['b_gate'][0], f).reshape(24, 128).T)
    fw = np.asarray(inp['ffn_dw_w'][0], f)
    fwg, fwv = fw[:, :FFN], fw[:, FFN:]
    fwi = np.stack([fwg.reshape(3, NFC, 128), fwv.reshape(3, NFC, 128)], axis=2).reshape(3, 44, 128)
    put('ffnw', fwi.transpose(2, 1, 0).reshape(128, 44 * 3))
    fb = np.asarray(inp['ffn_dw_b'][0], f)
    fbi = np.stack([fb[:FFN].reshape(NFC, 128), fb[FFN:].reshape(NFC, 128)], axis=1).reshape(44, 128)
    put('ffnb', fbi.T)
    shared = {
        'wcm': wcm, 'wtm': wtm, 'wup': wup, 'params': P,
        'pw2': np.ascontiguousarray(inp['conv_pw2'][0], f),
        'wmkv': np.ascontiguousarray(inp['w_mem_kv'][0], f),
        'wgate': np.ascontiguousarray(inp['w_gate'][0], f),
        'wout': np.ascontiguousarray(inp['w_out'][0], f),
        'wdown': np.ascontiguousarray(inp['w_down'][0], f),
    }
    return shared


def build(debug=(), upto=99):
    nc = bass.Bass("TRN2", target_bir_lowering=False)
    dbg = set(debug)

    def din(name, shape):
        return nc.dram_tensor(name, list(shape), F32, kind="ExternalInput").ap()

    def scratch(name, shape, dt):
        kind = "ExternalOutput" if name in dbg else "Internal"
        return nc.dram_tensor(name, list(shape), dt, kind=kind).ap()

    x = din("x", (S, D))
    mem = din("mem", (256, D))
    wcm = din("wcm", (D, WCM_COLS))
    wtm = din("wtm", (D, WTM_COLS))
    wup = din("wup", (D, 2 * FFN))
    params = din("params", (128, NPCOL))
    pw2 = din("pw2", (D, D))
    wmkv = din("wmkv", (D, 2 * D))
    wgate = din("wgate", (D, 3 * D))
    wout = din("wout", (D, D))
    wdown = din("wdown", (FFN, D))
    y = nc.dram_tensor("y", [S, D], F32, kind="ExternalOutput").ap()

    ycT = scratch("ycT", (D, S), F32)
    qT = scratch("qT", (D, S), BF16)
    kT = scratch("kT", (D, S), BF16)
    qmT = scratch("qmT", (D, S), BF16)
    qiT = scratch("qiT", (512, S), BF16)
    kiT = scratch("kiT", (128, S), BF16)
    vtm = scratch("vtm", (S, D), BF16)
    witm = scratch("witm", (S, 8), F32)
    gT = scratch("gT", (3 * D, S), BF16)
    mconvT = scratch("mconvT", (D, S), BF16)
    mattT = scratch("mattT", (D, S), BF16)
    mmemT = scratch("mmemT", (D, S), BF16)
    maskT = scratch("maskT", (S, S), BF16)
    x1d = scratch("x1d", (S, D), F32)
    aT = scratch("aT", (FFN, S), BF16)

    def pcols(t, name):
        o, n = PCOL[name]
        return t[:, o:o + n]

    with ExitStack() as gs:
        P = Prog(nc, gs)

        uniq = [0]

        def sb(st, name, shape, dt):
            uniq[0] += 1
            return st.enter_context(nc.sbuf_tensor(f"{name}_{uniq[0]}", list(shape), dt))

        def ps(st, name, shape, dt=F32):
            uniq[0] += 1
            return st.enter_context(nc.psum_tensor(f"{name}_{uniq[0]}", list(shape), dt))

        ident = sb(gs, "ident", (128, 128), BF16)
        ones_bf = sb(gs, "ones_bf", (128, 128), BF16)
        ones_f = sb(gs, "ones_f", (128, 128), F32)
        caus = sb(gs, "caus", (128, 128), F32)
        pv = sb(gs, "pv", (128, 472), F32)
        pvo = PCOL['convw'][0]

        def pvc(name, j0=0, n=None):
            o, nn = PCOL[name]
            o -= pvo
            if n is None:
                n = nn
            return pv[:, o + j0:o + j0 + n]

        with ExitStack() as st:
            iot = sb(st, "iot", (128, 128), I32)
            P.pool(lambda e: e.iota(out=iot[:], pattern=[[1, 128]], base=0, channel_multiplier=-1),
                   w=['iot'])
            P.dve(lambda e: e.tensor_scalar(out=ident[:], in0=iot[:], scalar1=0.0, scalar2=None,
                                            op0=ALU.is_equal), r=['iot'], w=['ident'])
            P.dve(lambda e: e.tensor_scalar(out=caus[:], in0=iot[:], scalar1=0.5, scalar2=NEG,
                                            op0=ALU.is_gt, op1=ALU.mult), r=['iot'], w=['caus'])
            P.dve(lambda e: e.memset(ones_bf[:], 1.0), w=['ones_bf'])
            P.dve(lambda e: e.memset(ones_f[:], 1.0), w=['ones_f'])
            P.dma('sp', pv[:], params[:, pvo:pvo + 472], w=['pv'])
            P.barrier()
            P.emit()

        hst = ExitStack()
        hT = sb(hst, "hT", (128, 8, S), BF16)

        def norm_transpose_phase(st, src, nblk, gname, dstT, tag):
            gbc = sb(st, tag + "gbc", (128, D), F32)
            xt = [sb(st, f"{tag}xt{i}", (128, D), F32) for i in range(2)]
            junk = sb(st, tag + "junk", (128, D), BF16)
            hb = [sb(st, f"{tag}hb{i}", (128, D), BF16) for i in range(2)]
            ss = [sb(st, f"{tag}ss{i}", (128, 1), F32) for i in range(2)]
            rt = [sb(st, f"{tag}rt{i}", (128, 1), F32) for i in range(2)]
            rs = [sb(st, f"{tag}rs{i}", (128, 1), F32) for i in range(2)]
            pt = [ps(st, f"{tag}pt{i}", (128, D), BF16) for i in range(2)]
            o, n = PCOL[gname]
            P.dma('sp', gbc[:], params[:, o:o + n], w=['gbc'])
            for i in range(nblk):
                s = i % 2
                P.dma('sp', xt[s][:], src[i * 128:(i + 1) * 128, :], w=[f'xt{s}'])
                P.dve(lambda e, s=s: e.scalar_tensor_tensor(
                    out=junk[:], in0=xt[s][:], scalar=1.0, in1=xt[s][:], op0=ALU.mult, op1=ALU.mult,
                    accum_out=ss[s][:]), r=[f'xt{s}'], w=['junk', f'ss{s}'])
                P.act(lambda e, s=s: e.activation(out=rt[s][:], in_=ss[s][:], func=AF.Sqrt,
                                                  bias=EPS, scale=1.0 / D),
                      r=[f'ss{s}'], w=[f'rt{s}'])
                P.dve(lambda e, s=s: e.reciprocal(out=rs[s][:], in_=rt[s][:]), r=[f'rt{s}'], w=[f'rs{s}'])
                P.dve(lambda e, s=s: e.scalar_tensor_tensor(
                    out=hb[s][:], in0=xt[s][:], scalar=rs[s][:], in1=gbc[:], op0=ALU.mult, op1=ALU.mult),
                    r=[f'xt{s}', f'rs{s}', 'gbc'], w=[f'hb{s}'])
                for j in range(8):
                    P.pe(lambda e, s=s, j=j: e.transpose(out=pt[s][:, j * 128:(j + 1) * 128],
                                                         in_=hb[s][:, j * 128:(j + 1) * 128],
                                                         identity=ident[:]),
                         r=[f'hb{s}'], w=[f'pt{s}'])
                P.act(lambda e, s=s, i=i: e.activation(
                    out=dstT[:, :, i * 128:(i + 1) * 128],
                    in_=pt[s][:].rearrange("p (j t) -> p j t", j=8), func=AF.Copy),
                    r=[f'pt{s}'], w=[f'dstT{i}'])

        with ExitStack() as st:
            norm_transpose_phase(st, x, NB, 'g1', hT, "A")
            P.barrier()
            P.emit()

        def proj_cm(st, W, ncols, evac, tag, wtile=512):
            wt = [sb(st, f"{tag}wt{i}", (128, 8, wtile), BF16) for i in range(2)]
            pp = [ps(st, f"{tag}pp{i}", (128, 512), F32) for i in range(4)]
            ntile = (ncols + wtile - 1) // wtile
            cnt = 0
            for wi_ in range(ntile):
                s = wi_ % 2
                c0 = wi_ * wtile
                wc = min(wtile, ncols - c0)
                P.dma('pool', wt[s][:, :, 0:wc],
                      W[:, c0:c0 + wc].rearrange("(k p) c -> p k c", p=128), w=[f'wt{s}'])
                for cc in range(wc // 128):
                    c = (c0 // 128) + cc
                    for T in range(NT):
                        b = cnt % 4
                        cnt += 1
                        for k in range(8):
                            P.pe(lambda e, s=s, cc=cc, T=T, k=k, b=b: e.matmul(
                                pp[b][:], lhsT=wt[s][:, k, cc * 128:(cc + 1) * 128],
                                rhs=hT[:, k, T * 512:(T + 1) * 512], start=(k == 0), stop=(k == 7)),
                                r=[f'wt{s}'], w=[f'pp{b}'])
                        evac(c, T, pp[b], f'pp{b}')

        if upto >= 2:
            with ExitStack() as st:
                NDT = 9
                yg = [sb(st, f"yg{i}", (128, 30 + 512), F32) for i in range(3)]
                ygb = [sb(st, f"ygb{i}", (128, 30 + 512), BF16) for i in range(3)]
                sg = [sb(st, f"sg{i}", (128, 512), F32) for i in range(2)]
                acc = [sb(st, f"acc{i}", (128, 512), F32) for i in range(2)]
                yc = [sb(st, f"yc{i}", (128, 512), F32) for i in range(2)]
                dg = [sb(st, f"dg{i}", (128, 31 - NDT, 128), BF16) for i in range(2)]
                wt = [sb(st, f"Bwt{i}", (128, 8, 512), BF16) for i in range(2)]
                pa = [ps(st, f"Bpa{i}", (128, 512), F32) for i in range(2)]
                pg = [ps(st, f"Bpg{i}", (128, 512), F32) for i in range(2)]
                pc = [ps(st, f"Bpc{i}", (128, 512), F32) for i in range(2)]
                n = 0
                for wi_ in range(4):
                    s = wi_ % 2
                    P.dma('pool', wt[s][:], wcm[:, wi_ * 512:(wi_ + 1) * 512].rearrange(
                        "(k p) c -> p k c", p=128), w=[f'wt{s}'])
                    for jj in range(2):
                        j = wi_ * 2 + jj
                        js = j % 2
                        for tap in range(NDT, 31):
                            P.pool(lambda e, js=js, tap=tap, j=j: e.tensor_scalar(
                                out=dg[js][:, tap - NDT, :], in0=ident[:], scalar1=pvc('convw', j * 31 + tap, 1),
                                scalar2=None, op0=ALU.mult), w=[f'dg{js}'])
                        for T in range(NT):
                            b = n % 2
                            s3 = n % 3
                            p3 = (n - 1) % 3
                            for k in range(8):
                                P.pe(lambda e, s=s, jj=jj, T=T, k=k, b=b: e.matmul(
                                    pa[b][:], lhsT=wt[s][:, k, jj * 256:jj * 256 + 128],
                                    rhs=hT[:, k, T * 512:(T + 1) * 512], start=(k == 0), stop=(k == 7)),
                                    r=[f'wt{s}'], w=[f'pa{b}'])
                            for k in range(8):
                                P.pe(lambda e, s=s, jj=jj, T=T, k=k, b=b: e.matmul(
                                    pg[b][:], lhsT=wt[s][:, k, jj * 256 + 128:jj * 256 + 256],
                                    rhs=hT[:, k, T * 512:(T + 1) * 512], start=(k == 0), stop=(k == 7)),
                                    r=[f'wt{s}'], w=[f'pg{b}'])
                            P.act(lambda e, b=b: e.activation(out=sg[b][:], in_=pg[b][:], func=AF.Sigmoid),
                                  r=[f'pg{b}'], w=[f'sg{b}'])
                            if T == 0:
                                P.pool(lambda e, s3=s3: e.memset(yg[s3][:, 0:30], 0.0), w=[f'yg{s3}'])
                                P.pool(lambda e, s3=s3: e.memset(ygb[s3][:, 0:30], 0.0), w=[f'ygb{s3}'])
                            else:
                                P.pool(lambda e, s3=s3, p3=p3: e.tensor_copy(
                                    out=yg[s3][:, 0:30], in_=yg[p3][:, 512:542]),
                                    r=[f'yg{p3}'], w=[f'yg{s3}'])
                                P.pool(lambda e, s3=s3, p3=p3: e.tensor_copy(
                                    out=ygb[s3][:, 0:30], in_=ygb[p3][:, 512:542]),
                                    r=[f'ygb{p3}'], w=[f'ygb{s3}'])
                            P.dve(lambda e, b=b, s3=s3: e.tensor_tensor(
                                out=yg[s3][:, 30:542], in0=pa[b][:], in1=sg[b][:], op=ALU.mult),
                                r=[f'pa{b}', f'sg{b}'], w=[f'yg{s3}'])
                            P.act(lambda e, s3=s3: e.activation(out=ygb[s3][:, 30:542], in_=yg[s3][:, 30:542],
                                                                func=AF.Copy),
                                  r=[f'yg{s3}'], w=[f'ygb{s3}'])
                            for tap in range(NDT, 31):
                                P.pe(lambda e, b=b, s3=s3, js=js, tap=tap: e.matmul(
                                    pc[b][:], lhsT=dg[js][:, tap - NDT, :], rhs=ygb[s3][:, tap:tap + 512],
                                    start=(tap == NDT), stop=(tap == 30)),
                                    r=[f'dg{js}', f'ygb{s3}'], w=[f'pc{b}'])
                            P.dve(lambda e, b=b, s3=s3, j=j: e.tensor_scalar(
                                out=acc[b][:], in0=yg[s3][:, 0:512], scalar1=pvc('convw', j * 31, 1),
                                scalar2=pvc('convb', j, 1), op0=ALU.mult, op1=ALU.add),
                                r=[f'yg{s3}'], w=[f'acc{b}'])
                            for tap in range(1, NDT):
                                P.dve(lambda e, b=b, s3=s3, j=j, tap=tap: e.scalar_tensor_tensor(
                                    out=acc[b][:], in0=yg[s3][:, tap:tap + 512],
                                    scalar=pvc('convw', j * 31 + tap, 1), in1=acc[b][:],
                                    op0=ALU.mult, op1=ALU.add),
                                    r=[f'yg{s3}'], w=[f'acc{b}'])
                            P.dve(lambda e, b=b: e.tensor_tensor(out=yc[b][:], in0=pc[b][:], in1=acc[b][:],
                                                                 op=ALU.add),
                                  r=[f'pc{b}', f'acc{b}'], w=[f'yc{b}'])
                            P.dma('sp', ycT[j * 128:(j + 1) * 128, T * 512:(T + 1) * 512], yc[b][:],
                                  r=[f'yc{b}'], w=['dram'])
                            n += 1
                P.barrier()
                P.emit()

            with ExitStack() as st:
                stg = [sb(st, f"stg{i}", (128, S), BF16) for i in range(2)]

                def evac_gen(c, T, pp, key):
                    c = c + 16
                    if c < 24:
                        dst, row, scale = qT, (c - 16) * 128, 128.0 ** -0.5
                    elif c < 32:
                        dst, row, scale = kT, (c - 24) * 128, 1.0
                    elif c < 40:
                        dst, row, scale = qmT, (c - 32) * 128, 256.0 ** -0.5
                    elif c < 44:
                        dst, row, scale = qiT, (c - 40) * 128, 1.0
                    else:
                        dst, row, scale = kiT, 0, 1.0
                    s2 = c % 2
                    P.act(lambda e: e.activation(out=stg[s2][:, T * 512:(T + 1) * 512], in_=pp[:],
                                                 func=AF.Copy, scale=scale),
                          r=[key], w=[f'stg{s2}'])
                    if T == NT - 1:
                        P.dma('sp', dst[row:row + 128, :], stg[s2][:], r=[f'stg{s2}'], w=['dram'])

                proj_cm(st, wcm[:, 2048:], WCM_COLS - 2048, evac_gen, "Bq")
                P.barrier()
                P.emit()

            with ExitStack() as st:
                stg = [sb(st, f"stg{i}", (128, S), BF16) for i in range(2)]

                def evac_gate(c, T, pp, key):
                    s2 = c % 2
                    P.act(lambda e: e.activation(out=stg[s2][:, T * 512:(T + 1) * 512], in_=pp[:],
                                                 func=AF.Sigmoid, bias=pvc('bgate', c, 1), scale=1.0),
                          r=[key], w=[f'stg{s2}'])
                    if T == NT - 1:
                        P.dma('sp', gT[c * 128:(c + 1) * 128, :], stg[s2][:], r=[f'stg{s2}'], w=['dram'])

                proj_cm(st, wgate, 3 * D, evac_gate, "Bg")
                P.barrier()
                P.emit()

            with ExitStack() as st:
                wv = sb(st, "wv", (128, 8, WTM_COLS), BF16)
                vst = [sb(st, f"vst{i}", (128, D), BF16) for i in range(2)]
                wst = [sb(st, f"wst{i}", (128, 8), F32) for i in range(2)]
                pvv = [ps(st, f"pvv{i}", (128, 512), F32) for i in range(4)]
                pw = [ps(st, f"pw{i}", (128, 8), F32) for i in range(2)]
                for hh in range(2):
                    P.dma('pool', wv[:, :, hh * 512:(hh + 1) * 512],
                          wtm[:, hh * 512:(hh + 1) * 512].rearrange("(k p) c -> p k c", p=128), w=['wv'])
                P.dma('pool', wv[:, :, 1024:1032],
                      wtm[:, 1024:1032].rearrange("(k p) c -> p k c", p=128), w=['wv'])
                for i in range(NB):
                    s = i % 2
                    for hh in range(2):
                        b = (i * 2 + hh) % 4
                        for k in range(8):
                            P.pe(lambda e, i=i, hh=hh, k=k, b=b: e.matmul(
                                pvv[b][:], lhsT=hT[:, k, i * 128:(i + 1) * 128],
                                rhs=wv[:, k, hh * 512:(hh + 1) * 512], start=(k == 0), stop=(k == 7)),
                                r=['wv'], w=[f'pvv{b}'])
                        P.act(lambda e, s=s, hh=hh, b=b: e.activation(
                            out=vst[s][:, hh * 512:(hh + 1) * 512], in_=pvv[b][:], func=AF.Copy),
                            r=[f'pvv{b}'], w=[f'vst{s}'])
                    for k in range(8):
                        P.pe(lambda e, i=i, k=k, s=s: e.matmul(
                            pw[s][:], lhsT=hT[:, k, i * 128:(i + 1) * 128],
                            rhs=wv[:, k, 1024:1032], start=(k == 0), stop=(k == 7)),
                            r=['wv'], w=[f'pw{s}'])
                    P.dve(lambda e, s=s: e.tensor_copy(out=wst[s][:], in_=pw[s][:]),
                          r=[f'pw{s}'], w=[f'wst{s}'])
                    P.dma('sp', vtm[i * 128:(i + 1) * 128, :], vst[s][:], r=[f'vst{s}'], w=['dram'])
                    P.dma('sp', witm[i * 128:(i + 1) * 128, :], wst[s][:], r=[f'wst{s}'], w=['dram'])
                P.barrier()
                P.emit()
        hst.close()

        def TS(T):
            return slice(T * 512, (T + 1) * 512)

        if upto >= 3:
            with ExitStack() as st:
                pwt = sb(st, "pwt", (128, 8, D), BF16)
                for hh in range(2):
                    P.dma('pool', pwt[:, :, hh * 512:(hh + 1) * 512],
                          pw2[:, hh * 512:(hh + 1) * 512].rearrange("(k p) c -> p k c", p=128), w=['pwt'])
                ycl = [sb(st, f"ycl{i}", (128, 8, 512), F32) for i in range(2)]
                sq = sb(st, "sq", (128, 8, 512), F32)
                gl = [sb(st, f"gl{i}", (128, 8, 512), BF16) for i in range(2)]
                mean = sb(st, "mean", (128, 512), F32)
                msq = sb(st, "msq", (128, 512), F32)
                var = sb(st, "var", (128, 512), F32)
                sd = sb(st, "sd", (128, 512), F32)
                rstd = sb(st, "rstd", (128, 512), F32)
                t1 = [sb(st, f"t1{i}", (128, 512), F32) for i in range(2)]
                ys = sb(st, "ys", (128, 8, 512), BF16)
                ot = [sb(st, f"ot{i}", (128, 512), BF16) for i in range(2)]
                s1 = ps(st, "s1", (128, 512))
                s2 = ps(st, "s2", (128, 512))
                po = [ps(st, f"po{i}", (128, 512)) for i in range(2)]
                for T in range(NT):
                    s = T % 2
                    P.dma('sp', ycl[s][:], ycT[:, TS(T)].rearrange("(j p) t -> p j t", p=128), w=[f'ycl{s}'])
                    P.dma('sp', gl[s][:], gT[0:1024, TS(T)].rearrange("(j p) t -> p j t", p=128), w=[f'gl{s}'])
                    P.act(lambda e, s=s: e.activation(out=sq[:], in_=ycl[s][:], func=AF.Square),
                          r=[f'ycl{s}'], w=['sq'])
                    for j in range(8):
                        P.pe(lambda e, s=s, j=j: e.matmul(s1[:], lhsT=ones_f[:], rhs=ycl[s][:, j, :],
                                                          start=(j == 0), stop=(j == 7)),
                             r=[f'ycl{s}', 'mean'], w=['s1'])
                    for j in range(8):
                        P.pe(lambda e, j=j: e.matmul(s2[:], lhsT=ones_f[:], rhs=sq[:, j, :],
                                                     start=(j == 0), stop=(j == 7)), r=['sq'], w=['s2'])
                    P.dve(lambda e: e.tensor_scalar(out=mean[:], in0=s1[:], scalar1=1.0 / D, scalar2=None,
                                                    op0=ALU.mult), r=['s1'], w=['mean'])
                    P.dve(lambda e: e.tensor_tensor(out=msq[:], in0=mean[:], in1=mean[:], op=ALU.mult),
                          r=['mean'], w=['msq'])
                    P.dve(lambda e: e.scalar_tensor_tensor(out=var[:], in0=s2[:], scalar=1.0 / D, in1=msq[:],
                                                           op0=ALU.mult, op1=ALU.subtract),
                          r=['s2', 'msq'], w=['var'])
                    P.act(lambda e: e.activation(out=sd[:], in_=var[:], func=AF.Sqrt, bias=EPS, scale=1.0),
                          r=['var'], w=['sd'])
                    P.dve(lambda e: e.reciprocal(out=rstd[:], in_=sd[:]), r=['sd'], w=['rstd'])
                    for j in range(8):
                        b = j % 2
                        P.dve(lambda e, s=s, j=j, b=b: e.tensor_tensor(
                            out=t1[b][:], in0=ycl[s][:, j, :], in1=mean[:], op=ALU.subtract),
                            r=[f'ycl{s}', 'mean'], w=[f't1{b}'])
                        P.dve(lambda e, b=b: e.tensor_tensor(out=t1[b][:], in0=t1[b][:], in1=rstd[:], op=ALU.mult),
                              r=['rstd'], w=[f't1{b}'])
                        P.act(lambda e, j=j, b=b: e.activation(out=ys[:, j, :], in_=t1[b][:], func=AF.Silu,
                                                               scale=pvc('lng', j, 1), bias=pvc('lnb', j, 1)),
                              r=[f't1{b}'], w=['ys'])
                    for dch in range(8):
                        b = dch % 2
                        for j in range(8):
                            P.pe(lambda e, dch=dch, j=j, b=b: e.matmul(
                                po[b][:], lhsT=pwt[:, j, dch * 128:(dch + 1) * 128], rhs=ys[:, j, :],
                                start=(j == 0), stop=(j == 7)), r=['ys', 'pwt'], w=[f'po{b}'])
                        P.dve(lambda e, s=s, dch=dch, b=b: e.tensor_tensor(
                            out=ot[b][:], in0=po[b][:], in1=gl[s][:, dch, :], op=ALU.mult),
                            r=[f'po{b}', f'gl{s}'], w=[f'ot{b}'])
                        P.dma('pool', mconvT[dch * 128:(dch + 1) * 128, TS(T)], ot[b][:], r=[f'ot{b}'], w=['dram'])
                P.barrier()
                P.emit()

        if upto >= 4:
            with ExitStack() as st0:
                mnT = sb(st0, "mnT", (128, 8, 256), BF16)
                mkT = sb(st0, "mkT", (128, 8, 256), BF16)
                mv = sb(st0, "mv", (128, 2, D), BF16)
                with ExitStack() as st:
                    norm_transpose_phase(st, mem, 2, 'gmem', mnT, "D")
                    P.barrier()
                    P.emit()
                with ExitStack() as st:
                    wk = [sb(st, f"wk{i}", (128, 8, 512), BF16) for i in range(2)]
                    pk = [ps(st, f"pk{i}", (128, 512)) for i in range(2)]
                    pmv = [ps(st, f"pmv{i}", (128, 512)) for i in range(2)]
                    for wi_ in range(2):
                        s = wi_ % 2
                        P.dma('pool', wk[s][:], wmkv[:, wi_ * 512:(wi_ + 1) * 512].rearrange(
                            "(k p) c -> p k c", p=128), w=[f'wk{s}'])
                        for cc in range(4):
                            c = wi_ * 4 + cc
                            b = c % 2
                            for k in range(8):
                                P.pe(lambda e, s=s, cc=cc, k=k, b=b: e.matmul(
                                    pk[b][:, 0:256], lhsT=wk[s][:, k, cc * 128:(cc + 1) * 128],
                                    rhs=mnT[:, k, :], start=(k == 0), stop=(k == 7)),
                                    r=[f'wk{s}'], w=[f'pk{b}'])
                            P.act(lambda e, c=c, b=b: e.activation(out=mkT[:, c, :], in_=pk[b][:, 0:256],
                                                                   func=AF.Copy), r=[f'pk{b}'], w=['mkT'])
                    for wi_ in range(2):
                        s = wi_ % 2
                        P.dma('pool', wk[s][:], wmkv[:, 1024 + wi_ * 512:1024 + (wi_ + 1) * 512].rearrange(
                            "(k p) c -> p k c", p=128), w=[f'wk{s}'])
                        for mc in range(2):
                            for k in range(8):
                                P.pe(lambda e, s=s, mc=mc, k=k: e.matmul(
                                    pmv[mc][:], lhsT=mnT[:, k, mc * 128:(mc + 1) * 128], rhs=wk[s][:, k, :],
                                    start=(k == 0), stop=(k == 7)), r=[f'wk{s}'], w=[f'pmv{mc}'])
                            P.act(lambda e, wi_=wi_, mc=mc: e.activation(
                                out=mv[:, mc, wi_ * 512:(wi_ + 1) * 512], in_=pmv[mc][:], func=AF.Copy),
                                r=[f'pmv{mc}'], w=['mv'])
                    P.barrier()
                    P.emit()
                with ExitStack() as st:
                    qml = [sb(st, f"qml{i}", (128, 2, 512), BF16) for i in range(2)]
                    gl = [sb(st, f"gl{i}", (128, 2, 512), BF16) for i in range(2)]
                    pT = [sb(st, f"pT{i}", (128, 2, 512), BF16) for i in range(2)]
                    rinv = sb(st, "rinv", (128, 512), F32)
                    o1 = [sb(st, f"o1{i}", (128, 512), F32) for i in range(2)]
                    ot = [sb(st, f"ot{i}", (128, 512), BF16) for i in range(2)]
                    pl = [ps(st, f"pl{i}", (128, 512)) for i in range(2)]
                    po = [ps(st, f"po{i}", (128, 512)) for i in range(2)]
                    pr = ps(st, "pr", (128, 512))
                    n = 0
                    for hm in range(4):
                        for T in range(NT):
                            s = n % 2
                            n += 1
                            P.dma('sp', qml[s][:], qmT[hm * 256:(hm + 1) * 256, TS(T)].rearrange(
                                "(c p) t -> p c t", p=128), w=[f'qml{s}'])
                            P.dma('sp', gl[s][:], gT[2048 + hm * 256:2048 + (hm + 1) * 256, TS(T)].rearrange(
                                "(c p) t -> p c t", p=128), w=[f'gl{s}'])
                            for mc in range(2):
                                for dmc in range(2):
                                    P.pe(lambda e, s=s, hm=hm, mc=mc, dmc=dmc: e.matmul(
                                        pl[mc][:], lhsT=mkT[:, hm * 2 + dmc, mc * 128:(mc + 1) * 128],
                                        rhs=qml[s][:, dmc, :], start=(dmc == 0), stop=(dmc == 1)),
                                        r=[f'qml{s}'], w=[f'pl{mc}'])
                                P.act(lambda e, s=s, mc=mc: e.activation(out=pT[s][:, mc, :], in_=pl[mc][:],
                                                                         func=AF.Exp),
                                      r=[f'pl{mc}'], w=[f'pT{s}'])
                            for dmc in range(2):
                                for mc in range(2):
                                    P.pe(lambda e, s=s, hm=hm, mc=mc, dmc=dmc: e.matmul(
                                        po[dmc][:], lhsT=mv[:, mc, hm * 256 + dmc * 128:hm * 256 + (dmc + 1) * 128],
                                        rhs=pT[s][:, mc, :], start=(mc == 0), stop=(mc == 1)),
                                        r=[f'pT{s}'], w=[f'po{dmc}'])
                            for mc in range(2):
                                P.pe(lambda e, s=s, mc=mc: e.matmul(pr[:], lhsT=ones_bf[:], rhs=pT[s][:, mc, :],
                                                                    start=(mc == 0), stop=(mc == 1)),
                                     r=[f'pT{s}'], w=['pr'])
                            P.dve(lambda e: e.reciprocal(out=rinv[:], in_=pr[:]), r=['pr'], w=['rinv'])
                            for dmc in range(2):
                                P.dve(lambda e, dmc=dmc: e.tensor_tensor(out=o1[dmc][:], in0=po[dmc][:], in1=rinv[:],
                                                                         op=ALU.mult),
                                      r=[f'po{dmc}', 'rinv'], w=[f'o1{dmc}'])
                                P.dve(lambda e, s=s, dmc=dmc: e.tensor_tensor(
                                    out=ot[dmc][:], in0=o1[dmc][:], in1=gl[s][:, dmc, :], op=ALU.mult),
                                    r=[f'o1{dmc}', f'gl{s}'], w=[f'ot{dmc}'])
                                P.dma('pool', mmemT[hm * 256 + dmc * 128:hm * 256 + (dmc + 1) * 128, TS(T)],
                                      ot[dmc][:], r=[f'ot{dmc}'], w=['dram'])
                    P.barrier()
                    P.emit()

        if upto >= 5:
            with ExitStack() as st:
                kil = sb(st, "kil", (128, S), BF16)
                qil = [sb(st, f"qil{i}", (128, 4, 128), BF16) for i in range(2)]
                wil = [sb(st, f"wil{i}", (128, 8), F32) for i in range(2)]
                score = [sb(st, f"score{i}", (128, S), F32) for i in range(2)]
                rl = [sb(st, f"rl{i}", (128, 512), F32) for i in range(3)]
                junk = sb(st, "junkE", (128, S), BF16)
                mk = [sb(st, f"mk{i}", (128, S), BF16) for i in range(2)]
                mT = [sb(st, f"mT{i}", (128, 8, 128), BF16) for i in range(2)]
                ptab = sb(st, "ptab", (128, NIT + 1), F32)
                thrc = sb(st, "thrc", (128, 1), F32)
                lo = sb(st, "lo", (128, 1), F32)
                hi = sb(st, "hi", (128, 1), F32)
                Wd = sb(st, "Wd", (128, 1), F32)
                hneg = sb(st, "hneg", (128, NIT + 1), F32)
                nm = sb(st, "nm", (128, 1), F32)
                ssum = sb(st, "ssum", (128, 1), F32)
                sgn = sb(st, "sgn", (128, 1), F32)
                thr_t = sb(st, "thr_t", (128, 1), F32)
                midp = sb(st, "midp", (128, 1), F32)
                cntd = sb(st, "cntd", (128, 1), F32)
                t2 = sb(st, "t2", (128, 1), F32)
                junkD = sb(st, "junkDE", (128, S), BF16)
                pd = [ps(st, f"pd{i}", (128, 512)) for i in range(4)]
                ptm = [ps(st, f"ptm{i}", (128, 1024), BF16) for i in range(2)]
                for n in range(NIT + 1):
                    P.pool(lambda e, n=n: e.memset(ptab[:, n:n + 1], -(2.0 ** -(n + 1))), w=['ptab'])
                P.pool(lambda e: e.memset(thrc[:], -1.0e29), w=['thrc'])
                P.dma('sp', kil[:], kiT[:, :], w=['kil'])
                cnts = {'c': 0, 't': 0}

                def score_units(i):
                    s = i % 2
                    L = (i + 1) * 128
                    units = []

                    def loads():
                        P.dma('sp', qil[s][:], qiT[:, i * 128:(i + 1) * 128].rearrange("(c p) t -> p c t", p=128),
                              w=[f'qil{s}'])
                        P.dma('sp', wil[s][:], witm[i * 128:(i + 1) * 128, :], w=[f'wil{s}'])
                    units.append(loads)
                    nst = (L + 511) // 512
                    for stile in range(nst):
                        w_ = min(512, L - stile * 512)
                        c0 = stile * 512
                        for h in range(8):
                            def unit(stile=stile, w_=w_, c0=c0, h=h):
                                p2, half = divmod(h, 2)
                                b = cnts['c'] % 4
                                r3 = cnts['c'] % 3
                                cnts['c'] += 1
                                P.pe(lambda e: e.matmul(
                                    pd[b][:, 0:w_], lhsT=qil[s][half * 64:(half + 1) * 64, p2, :],
                                    rhs=kil[half * 64:(half + 1) * 64, c0:c0 + w_], start=True, stop=True),
                                    r=[f'qil{s}', 'kil'], w=[f'pd{b}'])
                                P.act(lambda e: e.activation(
                                    out=rl[r3][:, 0:w_], in_=pd[b][:, 0:w_], func=AF.Relu),
                                    r=[f'pd{b}'], w=[f'rl{r3}'])
                                if h == 0:
                                    P.dve(lambda e: e.tensor_scalar(
                                        out=score[s][:, c0:c0 + w_], in0=rl[r3][:, 0:w_], scalar1=wil[s][:, 0:1],
                                        scalar2=None, op0=ALU.mult),
                                        r=[f'rl{r3}', f'wil{s}'], w=[f'score{s}'])
                                else:
                                    P.dve(lambda e: e.scalar_tensor_tensor(
                                        out=score[s][:, c0:c0 + w_], in0=rl[r3][:, 0:w_], scalar=wil[s][:, h:h + 1],
                                        in1=score[s][:, c0:c0 + w_], op0=ALU.mult, op1=ALU.add),
                                        r=[f'rl{r3}', f'wil{s}'], w=[f'score{s}'])
                            units.append(unit)

                    def fin():
                        P.dve(lambda e: e.tensor_tensor(
                            out=score[s][:, i * 128:(i + 1) * 128], in0=score[s][:, i * 128:(i + 1) * 128],
                            in1=caus[:], op=ALU.add), w=[f'score{s}'])
                    units.append(fin)
                    return units

                def bisect_units(i):
                    s = i % 2
                    L = (i + 1) * 128
                    units = []
                    if i >= 2:
                        def pro():
                            P.dve(lambda e: e.tensor_reduce(out=lo[:], in_=score[s][:, 0:i * 128],
                                                            axis=AX.X, op=ALU.min), r=[f'score{s}'], w=['lo'])
                            P.dve(lambda e: e.tensor_reduce(out=hi[:], in_=score[s][:, 0:L],
                                                            axis=AX.X, op=ALU.max), r=[f'score{s}'], w=['hi'])
                            P.dve(lambda e: e.tensor_tensor(out=Wd[:], in0=hi[:], in1=lo[:], op=ALU.subtract),
                                  r=['hi', 'lo'], w=['Wd'])
                            P.dve(lambda e: e.tensor_scalar(out=hneg[:], in0=ptab[:], scalar1=Wd[:, 0:1],
                                                            scalar2=None, op0=ALU.mult),
                                  r=['Wd', 'ptab'], w=['hneg'])
                            P.dve(lambda e: e.tensor_scalar(out=nm[:], in0=lo[:], scalar1=-1.0,
                                                            scalar2=hneg[:, 0:1], op0=ALU.mult, op1=ALU.add),
                                  r=['lo', 'hneg'], w=['nm'])
                        units.append(pro)
                        for n in range(NIT):
                            def it(n=n):
                                if n in DVE_ITERS:
                                    P.dve(lambda e: e.tensor_scalar(out=midp[:], in0=nm[:], scalar1=-1.0, scalar2=None,
                                                                    op0=ALU.mult), r=['nm'], w=['midp'])
                                    P.dve(lambda e: e.tensor_scalar(
                                        out=junkD[:, 0:L], in0=score[s][:, 0:L], scalar1=midp[:, 0:1], scalar2=None,
                                        op0=ALU.is_ge, op1=ALU.add, accum_out=cntd[:]),
                                        r=[f'score{s}', 'midp'], w=['junkD', 'cntd'])
                                    P.dve(lambda e: e.tensor_scalar(out=t2[:], in0=cntd[:], scalar1=TOPK - 0.5,
                                                                    scalar2=2.0, op0=ALU.is_ge, op1=ALU.mult),
                                          r=['cntd'], w=['t2'])
                                    P.dve(lambda e: e.tensor_scalar(out=t2[:], in0=t2[:], scalar1=-1.0,
                                                                    scalar2=hneg[:, n + 1:n + 2], op0=ALU.add,
                                                                    op1=ALU.mult), r=['hneg'], w=['t2'])
                                    P.dve(lambda e: e.tensor_tensor(out=nm[:], in0=nm[:], in1=t2[:], op=ALU.add),
                                          r=['t2'], w=['nm'])
                                    return
                                P.act(lambda e: e.activation(
                                    out=junk[:, 0:L], in_=score[s][:, 0:L], func=AF.Sign, bias=nm[:, 0:1],
                                    scale=1.0, accum_out=ssum[:]),
                                    r=[f'score{s}', 'nm'], w=['junkE', 'ssum'])
                                P.act(lambda e: e.activation(
                                    out=sgn[:], in_=ssum[:], func=AF.Sign, bias=float(L - 2 * TOPK) + 0.5,
                                    scale=1.0), r=['ssum'], w=['sgn'])
                                P.act(lambda e: e.activation(
                                    out=nm[:], in_=sgn[:], func=AF.Identity, scale=hneg[:, n + 1:n + 2],
                                    bias=nm[:, 0:1]), r=['sgn', 'hneg'], w=['nm'])
                            units.append(it)

                    def epi():
                        if i >= 2:
                            P.dve(lambda e: e.tensor_scalar(out=thr_t[:], in0=nm[:], scalar1=-1.0,
                                                            scalar2=hneg[:, NIT:NIT + 1], op0=ALU.mult, op1=ALU.add),
                                  r=['nm', 'hneg'], w=['thr_t'])
                            thr, thrk = thr_t, 'thr_t'
                        else:
                            thr, thrk = thrc, 'thrc'
                        P.dve(lambda e: e.tensor_scalar(
                            out=mk[s][:, 0:L], in0=score[s][:, 0:L], scalar1=thr[:, 0:1], scalar2=None,
                            op0=ALU.is_ge), r=[f'score{s}', thrk], w=[f'mk{s}'])
                        for g0 in range(0, i + 1, 8):
                            nb_ = min(8, i + 1 - g0)
                            b = cnts['t'] % 2
                            cnts['t'] += 1
                            for q_ in range(nb_):
                                P.pe(lambda e, b=b, q_=q_, g0=g0: e.transpose(
                                    out=ptm[b][:, q_ * 128:(q_ + 1) * 128],
                                    in_=mk[s][:, (g0 + q_) * 128:(g0 + q_ + 1) * 128], identity=ident[:]),
                                    r=[f'mk{s}'], w=[f'ptm{b}'])
                            P.act(lambda e, b=b, nb_=nb_: e.activation(
                                out=mT[b][:, 0:nb_, :],
                                in_=ptm[b][:, 0:nb_ * 128].rearrange("p (q t) -> p q t", q=nb_), func=AF.Copy),
                                r=[f'ptm{b}'], w=[f'mT{b}'])
                            P.dma('pool', maskT[g0 * 128:(g0 + nb_) * 128, i * 128:(i + 1) * 128].rearrange(
                                "(q p) t -> p q t", p=128), mT[b][:, 0:nb_, :], r=[f'mT{b}'], w=['dram'])
                    units.append(epi)
                    return units

                for u in score_units(0):
                    u()
                for i in range(NB):
                    A_ = bisect_units(i)
                    B_ = score_units(i + 1) if i + 1 < NB else []
                    done = 0
                    for idx, a_ in enumerate(A_):
                        a_()
                        tgt = (len(B_) * (idx + 1)) // len(A_)
                        while done < tgt:
                            B_[done]()
                            done += 1
                P.barrier()
                P.emit()

        if upto >= 6:
            with ExitStack() as st:
                ql = [sb(st, f"ql{i}", (128, S), BF16) for i in range(2)]
                kl = [sb(st, f"kl{i}", (128, S), BF16) for i in range(2)]
                vl = [sb(st, f"vl{i}", (128, NB, 128), BF16) for i in range(2)]
                NML = 6
                ml = [sb(st, f"ml{i}", (128, 512), BF16) for i in range(NML)]
                ex = [sb(st, f"ex{i}", (128, 512), BF16) for i in range(4)]
                pm = [sb(st, f"pm{i}", (128, 512), BF16) for i in range(4)]
                gl = [sb(st, f"gl{i}", (128, 512), BF16) for i in range(2)]
                rinv = sb(st, "rinv", (128, 512), F32)
                o1 = sb(st, "o1", (128, 512), F32)
                ot = [sb(st, f"ot{i}", (128, 512), BF16) for i in range(2)]
                psc = [ps(st, f"psc{i}", (128, 512)) for i in range(4)]
                po = [ps(st, f"po{i}", (128, 512)) for i in range(2)]
                pr = [ps(st, f"pr{i}", (128, 512)) for i in range(2)]
                its = []
                for h in range(8):
                    for T in range(NT):
                        nsb = 4 * T + 4
                        for sbk in range(nsb):
                            its.append((h, T, sbk, nsb))
                DEPTH = 2

                def front(n):
                    h, T, sbk, nsb = its[n]
                    hs = h % 2
                    a = (h * NT + T) % 2
                    if T == 0 and sbk == 0:
                        P.dma('sp', ql[hs][:], qT[h * 128:(h + 1) * 128, :], w=[f'ql{hs}'])
                        P.dma('sp', kl[hs][:], kT[h * 128:(h + 1) * 128, :], w=[f'kl{hs}'])
                        P.dma('sp', vl[hs][:], vtm[:, h * 128:(h + 1) * 128].rearrange("(b p) d -> p b d", p=128),
                              w=[f'vl{hs}'])
                    if sbk == 0:
                        P.dma('sp', gl[a][:], gT[1024 + h * 128:1024 + (h + 1) * 128, TS(T)], w=[f'gl{a}'])
                    m4 = n % NML
                    b3 = n % 4
                    r0 = max(0, sbk - 4 * T) * 128
                    t0 = T * 512 + r0
                    P.dma('sp', ml[m4][:, r0:512], maskT[sbk * 128:(sbk + 1) * 128, t0:(T + 1) * 512],
                          w=[f'ml{m4}'])
                    P.pe(lambda e: e.matmul(
                        psc[b3][:, r0:512], lhsT=kl[hs][:, sbk * 128:(sbk + 1) * 128],
                        rhs=ql[hs][:, t0:(T + 1) * 512], start=True, stop=True),
                        r=[f'kl{hs}', f'ql{hs}'], w=[f'psc{b3}'])
                    P.act(lambda e: e.activation(out=ex[b3][:, r0:512], in_=psc[b3][:, r0:512], func=AF.Exp),
                          r=[f'psc{b3}'], w=[f'ex{b3}'])
                    P.dve(lambda e: e.tensor_tensor(
                        out=pm[b3][:, r0:512], in0=ex[b3][:, r0:512], in1=ml[m4][:, r0:512], op=ALU.mult),
                        r=[f'ex{b3}', f'ml{m4}'], w=[f'pm{b3}'])

                def back(n):
                    h, T, sbk, nsb = its[n]
                    hs = h % 2
                    a = (h * NT + T) % 2
                    b3 = n % 4
                    r0 = max(0, sbk - 4 * T) * 128
                    P.pe(lambda e: e.matmul(
                        po[a][:, r0:512], lhsT=vl[hs][:, sbk, :], rhs=pm[b3][:, r0:512],
                        start=(sbk == 0), stop=(sbk == nsb - 1)),
                        r=[f'pm{b3}', f'vl{hs}'], w=[f'po{a}'])
                    P.pe(lambda e: e.matmul(
                        pr[a][:, r0:512], lhsT=ones_bf[:], rhs=pm[b3][:, r0:512],
                        start=(sbk == 0), stop=(sbk == nsb - 1)),
                        r=[f'pm{b3}'], w=[f'pr{a}'])
                    if sbk == nsb - 1:
                        P.dve(lambda e: e.reciprocal(out=rinv[:], in_=pr[a][:]), r=[f'pr{a}'], w=['rinv'])
                        P.dve(lambda e: e.tensor_tensor(out=o1[:], in0=po[a][:], in1=rinv[:], op=ALU.mult),
                              r=[f'po{a}', 'rinv'], w=['o1'])
                        P.dve(lambda e: e.tensor_tensor(out=ot[a][:], in0=o1[:], in1=gl[a][:], op=ALU.mult),
                              r=['o1', f'gl{a}'], w=[f'ot{a}'])
                        P.dma('pool', mattT[h * 128:(h + 1) * 128, TS(T)], ot[a][:], r=[f'ot{a}'], w=['dram'])

                for n in range(len(its) + DEPTH):
                    if n < len(its):
                        front(n)
                    if n >= DEPTH:
                        back(n - DEPTH)
                P.barrier()
                P.emit()

        def out_norm_phase(st, nk, load_lhs, wres, gname, resid, dst, tag, post=None):
            gp = sb(st, tag + "gp", (128, D), F32)
            o, n_ = PCOL[gname]
            P.dma('sp', gp[:], params[:, o:o + n_], w=['gp'])
            xt = [sb(st, f"{tag}xt{i}", (128, D), F32) for i in range(2)]
            xo = [sb(st, f"{tag}xo{i}", (128, D), F32) for i in range(2)]
            tmp = sb(st, tag + "tmp", (128, D), F32)
            junkA = sb(st, tag + "junkA", (128, 512), BF16)
            ssh = [sb(st, f"{tag}ssh{i}", (128, 2), F32) for i in range(2)]
            ss = [sb(st, f"{tag}ss{i}", (128, 1), F32) for i in range(2)]
            rt = [sb(st, f"{tag}rt{i}", (128, 1), F32) for i in range(2)]
            rs = [sb(st, f"{tag}rs{i}", (128, 1), F32) for i in range(2)]
            po = [ps(st, f"{tag}po{i}", (128, 512)) for i in range(4)]
            for T in range(NT):
                lhs, lkey = load_lhs(T)
                for tb in range(4):
                    i = T * 4 + tb
                    s2 = i % 2
                    P.dma('sp', xt[s2][:], resid[i * 128:(i + 1) * 128, :], w=[f'xt{s2}'])
                    for hh in range(2):
                        b = s2 * 2 + hh
                        for j in range(nk):
                            P.pe(lambda e, lhs=lhs, j=j, tb=tb, hh=hh, b=b: e.matmul(
                                po[b][:], lhsT=lhs[:, j, tb * 128:(tb + 1) * 128],
                                rhs=wres[:, j, hh * 512:(hh + 1) * 512], start=(j == 0), stop=(j == nk - 1)),
                                r=[lkey, 'wres'], w=[f'po{b}'])
                        P.act(lambda e, b=b, s2=s2, hh=hh: e.activation(
                            out=junkA[:], in_=po[b][:], func=AF.Square, accum_out=ssh[s2][:, hh:hh + 1]),
                            r=[f'po{b}'], w=['junkA', f'ssh{s2}'])
                    P.dve(lambda e, s2=s2: e.tensor_tensor(out=ss[s2][:], in0=ssh[s2][:, 0:1], in1=ssh[s2][:, 1:2],
                                                           op=ALU.add), r=[f'ssh{s2}'], w=[f'ss{s2}'])
                    P.act(lambda e, s2=s2: e.activation(out=rt[s2][:], in_=ss[s2][:], func=AF.Sqrt, bias=EPS,
                                                        scale=1.0 / D), r=[f'ss{s2}'], w=[f'rt{s2}'])
                    P.dve(lambda e, s2=s2: e.reciprocal(out=rs[s2][:], in_=rt[s2][:]), r=[f'rt{s2}'], w=[f'rs{s2}'])
                    for hh in range(2):
                        b = s2 * 2 + hh
                        P.dve(lambda e, b=b, s2=s2, hh=hh: e.scalar_tensor_tensor(
                            out=tmp[:, hh * 512:(hh + 1) * 512], in0=po[b][:], scalar=rs[s2][:, 0:1],
                            in1=gp[:, hh * 512:(hh + 1) * 512], op0=ALU.mult, op1=ALU.mult),
                            r=[f'po{b}', f'rs{s2}', 'gp'], w=['tmp'])
                    P.dve(lambda e, s2=s2: e.tensor_tensor(out=xo[s2][:], in0=tmp[:], in1=xt[s2][:], op=ALU.add),
                          r=['tmp', f'xt{s2}'], w=[f'xo{s2}'])
                    P.dma('pool', dst[i * 128:(i + 1) * 128, :], xo[s2][:], r=[f'xo{s2}'], w=['dram'])
                    if post is not None:
                        post(i, s2, xo[s2], f'xo{s2}')

        h2st = ExitStack()
        if upto >= 7:
            h2T = sb(h2st, "h2T", (128, 8, S), BF16)
            with ExitStack() as st:
                wo = sb(st, "wo", (128, 8, D), BF16)
                for hh in range(2):
                    P.dma('pool', wo[:, :, hh * 512:(hh + 1) * 512],
                          wout[:, hh * 512:(hh + 1) * 512].rearrange("(k p) c -> p k c", p=128), w=['wres'])
                g2 = sb(st, "g2bc", (128, D), F32)
                o, n_ = PCOL['g2']
                P.dma('sp', g2[:], params[:, o:o + n_], w=['g2'])
                ma = [sb(st, f"ma{i}", (128, 8, 512), BF16) for i in range(2)]
                mb = [sb(st, f"mb{i}", (128, 8, 512), BF16) for i in range(2)]
                mc_ = [sb(st, f"mc{i}", (128, 8, 512), BF16) for i in range(2)]
                mg = [sb(st, f"mg{i}", (128, 8, 512), BF16) for i in range(2)]
                junkD = sb(st, "junkD", (128, D), BF16)
                hb = [sb(st, f"Ghb{i}", (128, D), BF16) for i in range(2)]
                ss2 = [sb(st, f"Gss2{i}", (128, 1), F32) for i in range(2)]
                rt2 = [sb(st, f"Grt2{i}", (128, 1), F32) for i in range(2)]
                rs2 = [sb(st, f"Grs2{i}", (128, 1), F32) for i in range(2)]
                pt = [ps(st, f"Gpt{i}", (128, D), BF16) for i in range(2)]

                def load_merged(T):
                    s = T % 2
                    for (buf, src, nm) in ((ma, mconvT, 'ma'), (mb, mattT, 'mb'), (mc_, mmemT, 'mc')):
                        P.dma('sp', buf[s][:], src[:, TS(T)].rearrange("(j p) t -> p j t", p=128), w=[f'{nm}{s}'])
                    P.dve(lambda e, s=s: e.tensor_tensor(out=mg[s][:], in0=ma[s][:], in1=mb[s][:], op=ALU.add),
                          r=[f'ma{s}', f'mb{s}'], w=[f'mg{s}'])
                    P.dve(lambda e, s=s: e.tensor_tensor(out=mg[s][:], in0=mg[s][:], in1=mc_[s][:], op=ALU.add),
                          r=[f'mc{s}'], w=[f'mg{s}'])
                    return mg[s], f'mg{s}'

                def post_h2(i, s2, xo, xkey):
                    P.dve(lambda e, s2=s2: e.scalar_tensor_tensor(
                        out=junkD[:], in0=xo[:], scalar=1.0, in1=xo[:], op0=ALU.mult, op1=ALU.mult,
                        accum_out=ss2[s2][:]), r=[xkey], w=['junkD', f'ss2{s2}'])
                    P.act(lambda e, s2=s2: e.activation(out=rt2[s2][:], in_=ss2[s2][:], func=AF.Sqrt, bias=EPS,
                                                        scale=1.0 / D), r=[f'ss2{s2}'], w=[f'rt2{s2}'])
                    P.dve(lambda e, s2=s2: e.reciprocal(out=rs2[s2][:], in_=rt2[s2][:]),
                          r=[f'rt2{s2}'], w=[f'rs2{s2}'])
                    P.dve(lambda e, s2=s2: e.scalar_tensor_tensor(
                        out=hb[s2][:], in0=xo[:], scalar=rs2[s2][:, 0:1], in1=g2[:], op0=ALU.mult, op1=ALU.mult),
                        r=[xkey, f'rs2{s2}', 'g2'], w=[f'hb{s2}'])
                    for j in range(8):
                        P.pe(lambda e, s2=s2, j=j: e.transpose(out=pt[s2][:, j * 128:(j + 1) * 128],
                                                               in_=hb[s2][:, j * 128:(j + 1) * 128],
                                                               identity=ident[:]),
                             r=[f'hb{s2}'], w=[f'pt{s2}'])
                    P.act(lambda e, s2=s2, i=i: e.activation(
                        out=h2T[:, :, i * 128:(i + 1) * 128],
                        in_=pt[s2][:].rearrange("p (j t) -> p j t", j=8), func=AF.Copy),
                        r=[f'pt{s2}'], w=[f'h2T{i}'])

                out_norm_phase(st, 8, load_merged, wo, 'gpost1', x, x1d, "G", post=post_h2)
                P.barrier()
                P.emit()

        if upto >= 8:
            with ExitStack() as st:
                wt = [sb(st, f"Hwt{i}", (128, 8, 512), BF16) for i in range(2)]
                ug = [sb(st, f"ug{i}", (128, 514), F32) for i in range(3)]
                uvv = [sb(st, f"uv{i}", (128, 514), F32) for i in range(3)]
                cg = [sb(st, f"cg{i}", (128, 512), F32) for i in range(2)]
                cv = [sb(st, f"cv{i}", (128, 512), F32) for i in range(2)]
                sgl = [sb(st, f"sgl{i}", (128, 512), F32) for i in range(2)]
                ast = [sb(st, f"ast{i}", (128, S), BF16) for i in range(2)]
                pg = [ps(st, f"Hpg{i}", (128, 512)) for i in range(2)]
                pvv = [ps(st, f"Hpv{i}", (128, 512)) for i in range(2)]

                def fw(c, tap):
                    return pvc('ffnw', c * 3 + tap, 1)

                n = 0
                for wi_ in range(11):
                    s = wi_ % 2
                    P.dma('pool', wt[s][:], wup[:, wi_ * 512:(wi_ + 1) * 512].rearrange(
                        "(k p) c -> p k c", p=128), w=[f'wt{s}'])
                    for jj in range(2):
                        j = wi_ * 2 + jj
                        a2 = j % 2
                        for T in range(NT):
                            b = n % 2
                            s3 = n % 3
                            p3 = (n - 1) % 3
                            n += 1
                            for k in range(8):
                                P.pe(lambda e, s=s, jj=jj, T=T, k=k, b=b: e.matmul(
                                    pg[b][:], lhsT=wt[s][:, k, jj * 256:jj * 256 + 128],
                                    rhs=h2T[:, k, TS(T)], start=(k == 0), stop=(k == 7)),
                                    r=[f'wt{s}'], w=[f'pg{b}'])
                            for k in range(8):
                                P.pe(lambda e, s=s, jj=jj, T=T, k=k, b=b: e.matmul(
                                    pvv[b][:], lhsT=wt[s][:, k, jj * 256 + 128:jj * 256 + 256],
                                    rhs=h2T[:, k, TS(T)], start=(k == 0), stop=(k == 7)),
                                    r=[f'wt{s}'], w=[f'pvv{b}'])
                            for (u_, nm_) in ((ug, 'ug'), (uvv, 'uv')):
                                if T == 0:
                                    P.pool(lambda e, u_=u_, s3=s3: e.memset(u_[s3][:, 0:2], 0.0), w=[f'{nm_}{s3}'])
                                else:
                                    P.pool(lambda e, u_=u_, s3=s3, p3=p3: e.tensor_copy(
                                        out=u_[s3][:, 0:2], in_=u_[p3][:, 512:514]),
                                        r=[f'{nm_}{p3}'], w=[f'{nm_}{s3}'])
                            for (u_, nm_, pp_, pk_, co, cname, c) in (
                                    (ug, 'ug', pg, 'pg', cg, 'cg', 2 * j), (uvv, 'uv', pvv, 'pvv', cv, 'cv', 2 * j + 1)):
                                P.act(lambda e, u_=u_, pp_=pp_, b=b, s3=s3: e.activation(
                                    out=u_[s3][:, 2:514], in_=pp_[b][:], func=AF.Copy),
                                    r=[f'{pk_}{b}'], w=[f'{nm_}{s3}'])
                                P.act(lambda e, co=co, pp_=pp_, b=b, c=c: e.activation(
                                    out=co[b][:], in_=pp_[b][:], func=AF.Identity, scale=fw(c, 2),
                                    bias=pvc('ffnb', c, 1)), r=[f'{pk_}{b}'], w=[f'{cname}{b}'])
                                for tap in (1, 0):
                                    P.dve(lambda e, u_=u_, co=co, b=b, s3=s3, c=c, tap=tap: e.scalar_tensor_tensor(
                                        out=co[b][:], in0=u_[s3][:, tap:tap + 512], scalar=fw(c, tap), in1=co[b][:],
                                        op0=ALU.mult, op1=ALU.add), r=[f'{nm_}{s3}'], w=[f'{cname}{b}'])
                            P.act(lambda e, b=b: e.activation(out=sgl[b][:], in_=cg[b][:], func=AF.Silu),
                                  r=[f'cg{b}'], w=[f'sgl{b}'])
                            P.dve(lambda e, b=b, a2=a2, T=T: e.tensor_tensor(
                                out=ast[a2][:, TS(T)], in0=sgl[b][:], in1=cv[b][:], op=ALU.mult),
                                r=[f'sgl{b}', f'cv{b}'], w=[f'ast{a2}'])
                            if T == NT - 1:
                                P.dma('sp', aT[j * 128:(j + 1) * 128, :], ast[a2][:], r=[f'ast{a2}'], w=['dram'])
                P.barrier()
                P.emit()
        h2st.close()

        if upto >= 9:
            with ExitStack() as st:
                wd = sb(st, "wd", (128, NFC, D), BF16)
                for hh in range(2):
                    P.dma('pool', wd[:, :, hh * 512:(hh + 1) * 512],
                          wdown[:, hh * 512:(hh + 1) * 512].rearrange("(k p) c -> p k c", p=128), w=['wres'])
                at = [sb(st, f"at{i}", (128, NFC, 512), BF16) for i in range(2)]

                def load_act(T):
                    s = T % 2
                    P.dma('sp', at[s][:], aT[:, TS(T)].rearrange("(j p) t -> p j t", p=128), w=[f'at{s}'])
                    return at[s], f'at{s}'

                out_norm_phase(st, NFC, load_act, wd, 'gpost2', x1d, y, "I")
                P.barrier()
                P.emit()


        P.barrier()
        P.emit()
    return nc


_CACHE = {}


def kernel(**inputs):
    shared = pack_inputs(inputs)
    if 'nc' not in _CACHE:
        _CACHE['nc'] = build()
    nc = _CACHE['nc']
    x = np.asarray(inputs['x'], np.float32)
    mem = np.asarray(inputs['mem'], np.float32)
    in_maps = []
    for b in range(8):
        m = dict(shared)
        m['x'] = np.ascontiguousarray(x[b])
        m['mem'] = np.ascontiguousarray(mem[b])
        in_maps.append(m)
    res = run_bass_kernel_spmd(nc, in_maps, core_ids=list(range(8)))
    return np.stack([np.asarray(r['y'], np.float32) for r in res.results], axis=0)
```

```python
import numpy as np
from contextlib import ExitStack
import concourse.bass as bass
import concourse.mybir as mybir
from concourse.bass_utils import run_bass_kernel_spmd

F32 = mybir.dt.float32
BF16 = mybir.dt.bfloat16
I32 = mybir.dt.int32
AF = mybir.ActivationFunctionType
ALU = mybir.AluOpType
AX = mybir.AxisListType

S = 4096
D = 1024
NB = S // 128
NT = S // 512
EPS = 1e-6
FFN = 2816
NFC = FFN // 128
TOPK = 256
NIT = 16
DVE_ITERS = (3, 6, 9, 12, 14)
NEG = -1.0e30

ENGS = ['pe', 'act', 'dve', 'pool', 'sp']
BLK = {'pe': 'tensor', 'act': 'scalar', 'dve': 'vector', 'pool': 'gpsimd', 'sp': 'sync'}
EPOCH = 20000
NDS = 16


class Prog:
    def __init__(self, nc, stack):
        self.nc = nc
        self.stack = stack
        self.lists = {e: [] for e in ENGS}
        self.count = {e: 0 for e in ENGS}
        self.sems = {e: [] for e in ENGS}
        self.waited = {e: {} for e in ENGS}
        self.lastw = {}
        self.reads = {}
        self.dsem = {}
        self.dval = {}
        self.dnext = {}
        self.nsem = 0

    def _esem(self, e, idx):
        while len(self.sems[e]) <= idx:
            self.sems[e].append(self.stack.enter_context(
                self.nc.semaphore(f"s_{e}{len(self.sems[e])}")))
        return self.sems[e][idx]

    def _collect(self, e, r, w, extra):
        toks = list(extra)
        for k in r:
            t = self.lastw.get(k)
            if t is not None:
                toks.append(t)
        for k in w:
            t = self.lastw.get(k)
            if t is not None:
                toks.append(t)
            toks.extend(self.reads.get(k, ()))
        need = []
        for (sid, sem, v, te) in toks:
            if te == e and e == 'pe':
                continue
            if self.waited[e].get(sid, 0) >= v:
                continue
            self.waited[e][sid] = v
            need.append((sem, v))
        return need

    def _update(self, tok, r, w):
        for k in w:
            self.lastw[k] = tok
            self.reads[k] = []
        for k in r:
            self.reads.setdefault(k, []).append(tok)

    def op(self, e, fn, r=(), w=(), extra=()):
        need = self._collect(e, r, w, extra)
        n = self.count[e]
        ep, v = divmod(n, EPOCH)
        sem = self._esem(e, ep)
        self.count[e] = n + 1
        tok = ((e, ep), sem, v + 1, e)
        self.lists[e].append((need, fn, sem, 1))
        self._update(tok, r, w)
        return tok

    def pe(self, fn, r=(), w=(), extra=()):
        return self.op('pe', fn, r, w, extra)

    def act(self, fn, r=(), w=(), extra=()):
        return self.op('act', fn, r, w, extra)

    def dve(self, fn, r=(), w=(), extra=()):
        return self.op('dve', fn, r, w, extra)

    def pool(self, fn, r=(), w=(), extra=()):
        return self.op('pool', fn, r, w, extra)

    def dma(self, e, out, in_, r=(), w=(), extra=(), **kw):
        if e not in self.dsem:
            self.dsem[e] = [self.stack.enter_context(self.nc.semaphore(f"dma_{e}{j}")) for j in range(NDS)]
            self.dval[e] = [0] * NDS
            self.dnext[e] = 0
        j = self.dnext[e]
        self.dnext[e] = (j + 1) % NDS
        sem = self.dsem[e][j]
        extra = list(extra)
        if self.dval[e][j] > 0:
            extra.append((('d', e, j), sem, self.dval[e][j], 'dma'))
        need = self._collect(e, r, w, extra)
        self.dval[e][j] += 16
        tok = (('d', e, j), sem, self.dval[e][j], 'dma')
        self.lists[e].append((need, lambda eng: eng.dma_start(out=out, in_=in_, **kw), sem, 16))
        self._update(tok, r, w)
        return tok

    def barrier(self):
        toks = []
        for e in ENGS:
            n = self.count[e]
            if n == 0:
                continue
            ep, v = divmod(n - 1, EPOCH)
            toks.append(((e, ep), self.sems[e][ep], v + 1, e))
        for q in self.dsem:
            for j in range(NDS):
                if self.dval[q][j] > 0:
                    toks.append((('d', q, j), self.dsem[q][j], self.dval[q][j], 'dma'))
        for e in ENGS:
            need = []
            for (sid, sem, v, te) in toks:
                if te == e:
                    continue
                if self.waited[e].get(sid, 0) >= v:
                    continue
                self.waited[e][sid] = v
                need.append((sem, v))
            if need:
                self.lists[e].append((need, None, None, 0))
        self.lastw = {}
        self.reads = {}

    def emit(self):
        with self.nc.Block() as block:
            for e in ENGS:
                lst = self.lists[e]
                if not lst:
                    continue

                def body(eng, lst=lst):
                    for (need, fn, sem, inc) in lst:
                        for (s, v) in need:
                            eng.wait_ge(s, v)
                        if fn is not None:
                            fn(eng).then_inc(sem, inc)

                getattr(block, BLK[e])(body)
        self.lists = {e: [] for e in ENGS}


PCOL = {}
_off = 0
for _name, _n in [('g1', D), ('gmem', D), ('gpost1', D), ('g2', D), ('gpost2', D),
                  ('convw', 8 * 31), ('convb', 8), ('lng', 8), ('lnb', 8),
                  ('bgate', 24), ('ffnw', 44 * 3), ('ffnb', 44)]:
    PCOL[_name] = (_off, _n)
    _off += _n
NPCOL = _off

WCM_COLS = 45 * 128
WTM_COLS = 1032


def pack_inputs(inp):
    f = np.float32
    w_in = np.asarray(inp['w_in'][0], f)
    c0 = 0
    conv = w_in[:, 0:2048]
    q = w_in[:, 2048:3072]
    k = w_in[:, 3072:4096]
    v = w_in[:, 4096:5120]
    qi = w_in[:, 5120:5632]
    wi = w_in[:, 5632:5640]
    ki = w_in[:, 5640:5704]
    qm = w_in[:, 5704:6728]
    a, g = conv[:, :1024], conv[:, 1024:]
    inter = np.stack([a.reshape(D, 8, 128), g.reshape(D, 8, 128)], axis=2).reshape(D, 2048)
    wcm = np.ascontiguousarray(np.concatenate([inter, q, k, qm, qi, ki, ki], axis=1))
    wtm = np.ascontiguousarray(np.concatenate([v, wi], axis=1))
    w_up = np.asarray(inp['w_up'][0], f)
    ug, uv = w_up[:, :FFN], w_up[:, FFN:]
    wup = np.ascontiguousarray(
        np.stack([ug.reshape(D, NFC, 128), uv.reshape(D, NFC, 128)], axis=2).reshape(D, 2 * FFN))
    P = np.zeros((128, NPCOL), f)

    def put(name, arr):
        o, n = PCOL[name]
        P[:, o:o + n] = arr

    put('g1', np.broadcast_to(inp['norm1_pre_g'][0], (128, D)))
    put('gmem', np.broadcast_to(inp['mem_norm_g'][0], (128, D)))
    put('gpost1', np.broadcast_to(inp['norm1_post_g'][0], (128, D)))
    put('g2', np.broadcast_to(inp['norm2_pre_g'][0], (128, D)))
    put('gpost2', np.broadcast_to(inp['norm2_post_g'][0], (128, D)))
    cw = np.asarray(inp['conv_dw_w'][0], f)
    put('convw', cw.T.reshape(8, 128, 31).transpose(1, 0, 2).reshape(128, 8 * 31))
    put('convb', np.asarray(inp['conv_dw_b'][0], f).reshape(8, 128).T)
    put('lng', np.asarray(inp['conv_ln_g'][0], f).reshape(8, 128).T)
    put('lnb', np.asarray(inp['conv_ln_b'][0], f).reshape(8, 128).T)
    put('bgate', np.asarray(inp['b_gate'][0], f).reshape(24, 128).T)
    fw = np.asarray(inp['ffn_dw_w'][0], f)
    fwg, fwv = fw[:, :FFN], fw[:, FFN:]
    fwi = np.stack([fwg.reshape(3, NFC, 128), fwv.reshape(3, NFC, 128)], axis=2).reshape(3, 44, 128)
    put('ffnw', fwi.transpose(2, 1, 0).reshape(128, 44 * 3))
    fb = np.asarray(inp['ffn_dw_b'][0], f)
    fbi = np.stack([fb[:FFN].reshape(NFC, 128), fb[FFN:].reshape(NFC, 128)], axis=1).reshape(44, 128)
    put('ffnb', fbi.T)
    shared = {
        'wcm': wcm, 'wtm': wtm, 'wup': wup, 'params': P,
        'pw2': np.ascontiguousarray(inp['conv_pw2'][0], f),
        'wmkv': np.ascontiguousarray(inp['w_mem_kv'][0], f),
        'wgate': np.ascontiguousarray(inp['w_gate'][0], f),
        'wout': np.ascontiguousarray(inp['w_out'][0], f),
        'wdown': np.ascontiguousarray(inp['w_down'][0], f),
    }
    return shared


def build(debug=(), upto=99):
    nc = bass.Bass("TRN2", target_bir_lowering=False)
    dbg = set(debug)

    def din(name, shape):
        return nc.dram_tensor(name, list(shape), F32, kind="ExternalInput").ap()

    def scratch(name, shape, dt):
        kind = "ExternalOutput" if name in dbg else "Internal"
        return nc.dram_tensor(name, list(shape), dt, kind=kind).ap()

    x = din("x", (S, D))
    mem = din("mem", (256, D))
    wcm = din("wcm", (D, WCM_COLS))
    wtm = din("wtm", (D, WTM_COLS))
    wup = din("wup", (D, 2 * FFN))
    params = din("params", (128, NPCOL))
    pw2 = din("pw2", (D, D))
    wmkv = din("wmkv", (D, 2 * D))
    wgate = din("wgate", (D, 3 * D))
    wout = din("wout", (D, D))
    wdown = din("wdown", (FFN, D))
    y = nc.dram_tensor("y", [S, D], F32, kind="ExternalOutput").ap()

    ycT = scratch("ycT", (D, S), F32)
    qT = scratch("qT", (D, S), BF16)
    kT = scratch("kT", (D, S), BF16)
    qmT = scratch("qmT", (D, S), BF16)
    qiT = scratch("qiT", (512, S), BF16)
    kiT = scratch("kiT", (128, S), BF16)
    vtm = scratch("vtm", (S, D), BF16)
    witm = scratch("witm", (S, 8), F32)
    gT = scratch("gT", (3 * D, S), BF16)
    mconvT = scratch("mconvT", (D, S), BF16)
    mattT = scratch("mattT", (D, S), BF16)
    mmemT = scratch("mmemT", (D, S), BF16)
    maskT = scratch("maskT", (S, S), BF16)
    x1d = scratch("x1d", (S, D), F32)
    aT = scratch("aT", (FFN, S), BF16)

    def pcols(t, name):
        o, n = PCOL[name]
        return t[:, o:o + n]

    with ExitStack() as gs:
        P = Prog(nc, gs)

        uniq = [0]

        def sb(st, name, shape, dt):
            uniq[0] += 1
            return st.enter_context(nc.sbuf_tensor(f"{name}_{uniq[0]}", list(shape), dt))

        def ps(st, name, shape, dt=F32):
            uniq[0] += 1
            return st.enter_context(nc.psum_tensor(f"{name}_{uniq[0]}", list(shape), dt))

        ident = sb(gs, "ident", (128, 128), BF16)
        ones_bf = sb(gs, "ones_bf", (128, 128), BF16)
        ones_f = sb(gs, "ones_f", (128, 128), F32)
        caus = sb(gs, "caus", (128, 128), F32)
        pv = sb(gs, "pv", (128, 472), F32)
        pvo = PCOL['convw'][0]

        def pvc(name, j0=0, n=None):
            o, nn = PCOL[name]
            o -= pvo
            if n is None:
                n = nn
            return pv[:, o + j0:o + j0 + n]

        with ExitStack() as st:
            iot = sb(st, "iot", (128, 128), I32)
            P.pool(lambda e: e.iota(out=iot[:], pattern=[[1, 128]], base=0, channel_multiplier=-1),
                   w=['iot'])
            P.dve(lambda e: e.tensor_scalar(out=ident[:], in0=iot[:], scalar1=0.0, scalar2=None,
                                            op0=ALU.is_equal), r=['iot'], w=['ident'])
            P.dve(lambda e: e.tensor_scalar(out=caus[:], in0=iot[:], scalar1=0.5, scalar2=NEG,
                                            op0=ALU.is_gt, op1=ALU.mult), r=['iot'], w=['caus'])
            P.dve(lambda e: e.memset(ones_bf[:], 1.0), w=['ones_bf'])
            P.dve(lambda e: e.memset(ones_f[:], 1.0), w=['ones_f'])
            P.dma('sp', pv[:], params[:, pvo:pvo + 472], w=['pv'])
            P.barrier()
            P.emit()

        hst = ExitStack()
        hT = sb(hst, "hT", (128, 8, S), BF16)

        def norm_transpose_phase(st, src, nblk, gname, dstT, tag):
            gbc = sb(st, tag + "gbc", (128, D), F32)
            xt = [sb(st, f"{tag}xt{i}", (128, D), F32) for i in range(2)]
            junk = sb(st, tag + "junk", (128, D), BF16)
            hb = [sb(st, f"{tag}hb{i}", (128, D), BF16) for i in range(2)]
            ss = [sb(st, f"{tag}ss{i}", (128, 1), F32) for i in range(2)]
            rt = [sb(st, f"{tag}rt{i}", (128, 1), F32) for i in range(2)]
            rs = [sb(st, f"{tag}rs{i}", (128, 1), F32) for i in range(2)]
            pt = [ps(st, f"{tag}pt{i}", (128, D), BF16) for i in range(2)]
            o, n = PCOL[gname]
            P.dma('sp', gbc[:], params[:, o:o + n], w=['gbc'])
            for i in range(nblk):
                s = i % 2
                P.dma('sp', xt[s][:], src[i * 128:(i + 1) * 128, :], w=[f'xt{s}'])
                P.dve(lambda e, s=s: e.scalar_tensor_tensor(
                    out=junk[:], in0=xt[s][:], scalar=1.0, in1=xt[s][:], op0=ALU.mult, op1=ALU.mult,
                    accum_out=ss[s][:]), r=[f'xt{s}'], w=['junk', f'ss{s}'])
                P.act(lambda e, s=s: e.activation(out=rt[s][:], in_=ss[s][:], func=AF.Sqrt,
                                                  bias=EPS, scale=1.0 / D),
                      r=[f'ss{s}'], w=[f'rt{s}'])
                P.dve(lambda e, s=s: e.reciprocal(out=rs[s][:], in_=rt[s][:]), r=[f'rt{s}'], w=[f'rs{s}'])
                P.dve(lambda e, s=s: e.scalar_tensor_tensor(
                    out=hb[s][:], in0=xt[s][:], scalar=rs[s][:], in1=gbc[:], op0=ALU.mult, op1=ALU.mult),
                    r=[f'xt{s}', f'rs{s}', 'gbc'], w=[f'hb{s}'])
                for j in range(8):
                    P.pe(lambda e, s=s, j=j: e.transpose(out=pt[s][:, j * 128:(j + 1) * 128],
                                                         in_=hb[s][:, j * 128:(j + 1) * 128],
                                                         identity=ident[:]),
                         r=[f'hb{s}'], w=[f'pt{s}'])
                P.act(lambda e, s=s, i=i: e.activation(
                    out=dstT[:, :, i * 128:(i + 1) * 128],
                    in_=pt[s][:].rearrange("p (j t) -> p j t", j=8), func=AF.Copy),
                    r=[f'pt{s}'], w=[f'dstT{i}'])

        with ExitStack() as st:
            norm_transpose_phase(st, x, NB, 'g1', hT, "A")
            P.barrier()
            P.emit()

        def proj_cm(st, W, ncols, evac, tag, wtile=512):
            wt = [sb(st, f"{tag}wt{i}", (128, 8, wtile), BF16) for i in range(2)]
            pp = [ps(st, f"{tag}pp{i}", (128, 512), F32) for i in range(4)]
            ntile = (ncols + wtile - 1) // wtile
            cnt = 0
            for wi_ in range(ntile):
                s = wi_ % 2
                c0 = wi_ * wtile
                wc = min(wtile, ncols - c0)
                P.dma('pool', wt[s][:, :, 0:wc],
                      W[:, c0:c0 + wc].rearrange("(k p) c -> p k c", p=128), w=[f'wt{s}'])
                for cc in range(wc // 128):
                    c = (c0 // 128) + cc
                    for T in range(NT):
                        b = cnt % 4
                        cnt += 1
                        for k in range(8):
                            P.pe(lambda e, s=s, cc=cc, T=T, k=k, b=b: e.matmul(
                                pp[b][:], lhsT=wt[s][:, k, cc * 128:(cc + 1) * 128],
                                rhs=hT[:, k, T * 512:(T + 1) * 512], start=(k == 0), stop=(k == 7)),
                                r=[f'wt{s}'], w=[f'pp{b}'])
                        evac(c, T, pp[b], f'pp{b}')

        if upto >= 2:
            with ExitStack() as st:
                NDT = 9
                yg = [sb(st, f"yg{i}", (128, 30 + 512), F32) for i in range(3)]
                ygb = [sb(st, f"ygb{i}", (128, 30 + 512), BF16) for i in range(3)]
                sg = [sb(st, f"sg{i}", (128, 512), F32) for i in range(2)]
                acc = [sb(st, f"acc{i}", (128, 512), F32) for i in range(2)]
                yc = [sb(st, f"yc{i}", (128, 512), F32) for i in range(2)]
                dg = [sb(st, f"dg{i}", (128, 31 - NDT, 128), BF16) for i in range(2)]
                wt = [sb(st, f"Bwt{i}", (128, 8, 512), BF16) for i in range(2)]
                pa = [ps(st, f"Bpa{i}", (128, 512), F32) for i in range(2)]
                pg = [ps(st, f"Bpg{i}", (128, 512), F32) for i in range(2)]
                pc = [ps(st, f"Bpc{i}", (128, 512), F32) for i in range(2)]
                n = 0
                for wi_ in range(4):
                    s = wi_ % 2
                    P.dma('pool', wt[s][:], wcm[:, wi_ * 512:(wi_ + 1) * 512].rearrange(
                        "(k p) c -> p k c", p=128), w=[f'wt{s}'])
                    for jj in range(2):
                        j = wi_ * 2 + jj
                        js = j % 2
                        for jn in ([0, 1] if j == 0 else ([j + 1] if j + 1 < 8 else [])):
                            for tap in range(NDT, 31):
                                P.pool(lambda e, jn=jn, tap=tap: e.tensor_scalar(
                                    out=dg[jn % 2][:, tap - NDT, :], in0=ident[:],
                                    scalar1=pvc('convw', jn * 31 + tap, 1), scalar2=0.0,
                                    op0=ALU.mult, op1=ALU.add), w=[f'dg{jn % 2}'])
                        for T in range(NT):
                            b = n % 2
                            s3 = n % 3
                            p3 = (n - 1) % 3
                            for k in range(8):
                                P.pe(lambda e, s=s, jj=jj, T=T, k=k, b=b: e.matmul(
                                    pa[b][:], lhsT=wt[s][:, k, jj * 256:jj * 256 + 128],
                                    rhs=hT[:, k, T * 512:(T + 1) * 512], start=(k == 0), stop=(k == 7)),
                                    r=[f'wt{s}'], w=[f'pa{b}'])
                            for k in range(8):
                                P.pe(lambda e, s=s, jj=jj, T=T, k=k, b=b: e.matmul(
                                    pg[b][:], lhsT=wt[s][:, k, jj * 256 + 128:jj * 256 + 256],
                                    rhs=hT[:, k, T * 512:(T + 1) * 512], start=(k == 0), stop=(k == 7)),
                                    r=[f'wt{s}'], w=[f'pg{b}'])
                            P.act(lambda e, b=b: e.activation(out=sg[b][:], in_=pg[b][:], func=AF.Sigmoid),
                                  r=[f'pg{b}'], w=[f'sg{b}'])
                            if T == 0:
                                P.pool(lambda e, s3=s3: e.memset(yg[s3][:, 0:30], 0.0), w=[f'yg{s3}'])
                                P.pool(lambda e, s3=s3: e.memset(ygb[s3][:, 0:30], 0.0), w=[f'ygb{s3}'])
                            else:
                                P.pool(lambda e, s3=s3, p3=p3: e.tensor_copy(
                                    out=yg[s3][:, 0:30], in_=yg[p3][:, 512:542]),
                                    r=[f'yg{p3}'], w=[f'yg{s3}'])
                                P.pool(lambda e, s3=s3, p3=p3: e.tensor_copy(
                                    out=ygb[s3][:, 0:30], in_=ygb[p3][:, 512:542]),
                                    r=[f'ygb{p3}'], w=[f'ygb{s3}'])
                            P.dve(lambda e, b=b, s3=s3: e.tensor_tensor(
                                out=yg[s3][:, 30:542], in0=pa[b][:], in1=sg[b][:], op=ALU.mult),
                                r=[f'pa{b}', f'sg{b}'], w=[f'yg{s3}'])
                            P.act(lambda e, s3=s3: e.activation(out=ygb[s3][:, 30:542], in_=yg[s3][:, 30:542],
                                                                func=AF.Copy),
                                  r=[f'yg{s3}'], w=[f'ygb{s3}'])
                            for tap in range(NDT, 31):
                                P.pe(lambda e, b=b, s3=s3, js=js, tap=tap: e.matmul(
                                    pc[b][:], lhsT=dg[js][:, tap - NDT, :], rhs=ygb[s3][:, tap:tap + 512],
                                    start=(tap == NDT), stop=(tap == 30)),
                                    r=[f'dg{js}', f'ygb{s3}'], w=[f'pc{b}'])
                            P.dve(lambda e, b=b, s3=s3, j=j: e.tensor_scalar(
                                out=acc[b][:], in0=yg[s3][:, 0:512], scalar1=pvc('convw', j * 31, 1),
                                scalar2=pvc('convb', j, 1), op0=ALU.mult, op1=ALU.add),
                                r=[f'yg{s3}'], w=[f'acc{b}'])
                            for tap in range(1, NDT):
                                P.dve(lambda e, b=b, s3=s3, j=j, tap=tap: e.scalar_tensor_tensor(
                                    out=acc[b][:], in0=yg[s3][:, tap:tap + 512],
                                    scalar=pvc('convw', j * 31 + tap, 1), in1=acc[b][:],
                                    op0=ALU.mult, op1=ALU.add),
                                    r=[f'yg{s3}'], w=[f'acc{b}'])
                            P.dve(lambda e, b=b: e.tensor_tensor(out=yc[b][:], in0=pc[b][:], in1=acc[b][:],
                                                                 op=ALU.add),
                                  r=[f'pc{b}', f'acc{b}'], w=[f'yc{b}'])
                            P.dma('sp', ycT[j * 128:(j + 1) * 128, T * 512:(T + 1) * 512], yc[b][:],
                                  r=[f'yc{b}'], w=['dram'])
                            n += 1
                P.barrier()
                P.emit()

            with ExitStack() as st:
                stg = [sb(st, f"stg{i}", (128, S), BF16) for i in range(2)]

                def evac_gen(c, T, pp, key):
                    c = c + 16
                    if c < 24:
                        dst, row, scale = qT, (c - 16) * 128, 128.0 ** -0.5
                    elif c < 32:
                        dst, row, scale = kT, (c - 24) * 128, 1.0
                    elif c < 40:
                        dst, row, scale = qmT, (c - 32) * 128, 256.0 ** -0.5
                    elif c < 44:
                        dst, row, scale = qiT, (c - 40) * 128, 1.0
                    else:
                        dst, row, scale = kiT, 0, 1.0
                    s2 = c % 2
                    P.act(lambda e: e.activation(out=stg[s2][:, T * 512:(T + 1) * 512], in_=pp[:],
                                                 func=AF.Copy, scale=scale),
                          r=[key], w=[f'stg{s2}'])
                    if T == NT - 1:
                        P.dma('sp', dst[row:row + 128, :], stg[s2][:], r=[f'stg{s2}'], w=['dram'])

                proj_cm(st, wcm[:, 2048:], WCM_COLS - 2048, evac_gen, "Bq")
                P.barrier()
                P.emit()

            with ExitStack() as st:
                stg = [sb(st, f"stg{i}", (128, S), BF16) for i in range(2)]

                def evac_gate(c, T, pp, key):
                    s2 = c % 2
                    P.act(lambda e: e.activation(out=stg[s2][:, T * 512:(T + 1) * 512], in_=pp[:],
                                                 func=AF.Sigmoid, bias=pvc('bgate', c, 1), scale=1.0),
                          r=[key], w=[f'stg{s2}'])
                    if T == NT - 1:
                        P.dma('sp', gT[c * 128:(c + 1) * 128, :], stg[s2][:], r=[f'stg{s2}'], w=['dram'])

                proj_cm(st, wgate, 3 * D, evac_gate, "Bg")
                P.barrier()
                P.emit()

            with ExitStack() as st:
                wv = sb(st, "wv", (128, 8, WTM_COLS), BF16)
                vst = [sb(st, f"vst{i}", (128, D), BF16) for i in range(2)]
                wst = [sb(st, f"wst{i}", (128, 8), F32) for i in range(2)]
                pvv = [ps(st, f"pvv{i}", (128, 512), F32) for i in range(4)]
                pw = [ps(st, f"pw{i}", (128, 8), F32) for i in range(2)]
                for hh in range(2):
                    P.dma('pool', wv[:, :, hh * 512:(hh + 1) * 512],
                          wtm[:, hh * 512:(hh + 1) * 512].rearrange("(k p) c -> p k c", p=128), w=['wv'])
                P.dma('pool', wv[:, :, 1024:1032],
                      wtm[:, 1024:1032].rearrange("(k p) c -> p k c", p=128), w=['wv'])
                for i in range(NB):
                    s = i % 2
                    for hh in range(2):
                        b = (i * 2 + hh) % 4
                        for k in range(8):
                            P.pe(lambda e, i=i, hh=hh, k=k, b=b: e.matmul(
                                pvv[b][:], lhsT=hT[:, k, i * 128:(i + 1) * 128],
                                rhs=wv[:, k, hh * 512:(hh + 1) * 512], start=(k == 0), stop=(k == 7)),
                                r=['wv'], w=[f'pvv{b}'])
                        P.act(lambda e, s=s, hh=hh, b=b: e.activation(
                            out=vst[s][:, hh * 512:(hh + 1) * 512], in_=pvv[b][:], func=AF.Copy),
                            r=[f'pvv{b}'], w=[f'vst{s}'])
                    for k in range(8):
                        P.pe(lambda e, i=i, k=k, s=s: e.matmul(
                            pw[s][:], lhsT=hT[:, k, i * 128:(i + 1) * 128],
                            rhs=wv[:, k, 1024:1032], start=(k == 0), stop=(k == 7)),
                            r=['wv'], w=[f'pw{s}'])
                    P.dve(lambda e, s=s: e.tensor_copy(out=wst[s][:], in_=pw[s][:]),
                          r=[f'pw{s}'], w=[f'wst{s}'])
                    P.dma('sp', vtm[i * 128:(i + 1) * 128, :], vst[s][:], r=[f'vst{s}'], w=['dram'])
                    P.dma('sp', witm[i * 128:(i + 1) * 128, :], wst[s][:], r=[f'wst{s}'], w=['dram'])
                P.barrier()
                P.emit()
        hst.close()

        def TS(T):
            return slice(T * 512, (T + 1) * 512)

        if upto >= 3:
            with ExitStack() as st:
                pwt = sb(st, "pwt", (128, 8, D), BF16)
                for hh in range(2):
                    P.dma('pool', pwt[:, :, hh * 512:(hh + 1) * 512],
                          pw2[:, hh * 512:(hh + 1) * 512].rearrange("(k p) c -> p k c", p=128), w=['pwt'])
                ycl = [sb(st, f"ycl{i}", (128, 8, 512), F32) for i in range(2)]
                sq = sb(st, "sq", (128, 8, 512), F32)
                gl = [sb(st, f"gl{i}", (128, 8, 512), BF16) for i in range(2)]
                mean = sb(st, "mean", (128, 512), F32)
                msq = sb(st, "msq", (128, 512), F32)
                var = sb(st, "var", (128, 512), F32)
                sd = sb(st, "sd", (128, 512), F32)
                rstd = sb(st, "rstd", (128, 512), F32)
                t1 = [sb(st, f"t1{i}", (128, 512), F32) for i in range(2)]
                ys = sb(st, "ys", (128, 8, 512), BF16)
                ot = [sb(st, f"ot{i}", (128, 512), BF16) for i in range(2)]
                s1 = ps(st, "s1", (128, 512))
                s2 = ps(st, "s2", (128, 512))
                po = [ps(st, f"po{i}", (128, 512)) for i in range(2)]
                for T in range(NT):
                    s = T % 2
                    P.dma('sp', ycl[s][:], ycT[:, TS(T)].rearrange("(j p) t -> p j t", p=128), w=[f'ycl{s}'])
                    P.dma('sp', gl[s][:], gT[0:1024, TS(T)].rearrange("(j p) t -> p j t", p=128), w=[f'gl{s}'])
                    P.act(lambda e, s=s: e.activation(out=sq[:], in_=ycl[s][:], func=AF.Square),
                          r=[f'ycl{s}'], w=['sq'])
                    for j in range(8):
                        P.pe(lambda e, s=s, j=j: e.matmul(s1[:], lhsT=ones_f[:], rhs=ycl[s][:, j, :],
                                                          start=(j == 0), stop=(j == 7)),
                             r=[f'ycl{s}', 'mean'], w=['s1'])
                    for j in range(8):
                        P.pe(lambda e, j=j: e.matmul(s2[:], lhsT=ones_f[:], rhs=sq[:, j, :],
                                                     start=(j == 0), stop=(j == 7)), r=['sq'], w=['s2'])
                    P.dve(lambda e: e.tensor_scalar(out=mean[:], in0=s1[:], scalar1=1.0 / D, scalar2=None,
                                                    op0=ALU.mult), r=['s1'], w=['mean'])
                    P.dve(lambda e: e.tensor_tensor(out=msq[:], in0=mean[:], in1=mean[:], op=ALU.mult),
                          r=['mean'], w=['msq'])
                    P.dve(lambda e: e.scalar_tensor_tensor(out=var[:], in0=s2[:], scalar=1.0 / D, in1=msq[:],
                                                           op0=ALU.mult, op1=ALU.subtract),
                          r=['s2', 'msq'], w=['var'])
                    P.act(lambda e: e.activation(out=sd[:], in_=var[:], func=AF.Sqrt, bias=EPS, scale=1.0),
                          r=['var'], w=['sd'])
                    P.dve(lambda e: e.reciprocal(out=rstd[:], in_=sd[:]), r=['sd'], w=['rstd'])
                    for j in range(8):
                        b = j % 2
                        P.dve(lambda e, s=s, j=j, b=b: e.tensor_tensor(
                            out=t1[b][:], in0=ycl[s][:, j, :], in1=mean[:], op=ALU.subtract),
                            r=[f'ycl{s}', 'mean'], w=[f't1{b}'])
                        P.dve(lambda e, b=b: e.tensor_tensor(out=t1[b][:], in0=t1[b][:], in1=rstd[:], op=ALU.mult),
                              r=['rstd'], w=[f't1{b}'])
                        P.act(lambda e, j=j, b=b: e.activation(out=ys[:, j, :], in_=t1[b][:], func=AF.Silu,
                                                               scale=pvc('lng', j, 1), bias=pvc('lnb', j, 1)),
                              r=[f't1{b}'], w=['ys'])
                    for dch in range(8):
                        b = dch % 2
                        for j in range(8):
                            P.pe(lambda e, dch=dch, j=j, b=b: e.matmul(
                                po[b][:], lhsT=pwt[:, j, dch * 128:(dch + 1) * 128], rhs=ys[:, j, :],
                                start=(j == 0), stop=(j == 7)), r=['ys', 'pwt'], w=[f'po{b}'])
                        P.dve(lambda e, s=s, dch=dch, b=b: e.tensor_tensor(
                            out=ot[b][:], in0=po[b][:], in1=gl[s][:, dch, :], op=ALU.mult),
                            r=[f'po{b}', f'gl{s}'], w=[f'ot{b}'])
                        P.dma('pool', mconvT[dch * 128:(dch + 1) * 128, TS(T)], ot[b][:], r=[f'ot{b}'], w=['dram'])
                P.barrier()
                P.emit()

        if upto >= 4:
            with ExitStack() as st0:
                mnT = sb(st0, "mnT", (128, 8, 256), BF16)
                mkT = sb(st0, "mkT", (128, 8, 256), BF16)
                mv = sb(st0, "mv", (128, 2, D), BF16)
                with ExitStack() as st:
                    norm_transpose_phase(st, mem, 2, 'gmem', mnT, "D")
                    P.barrier()
                    P.emit()
                with ExitStack() as st:
                    wk = [sb(st, f"wk{i}", (128, 8, 512), BF16) for i in range(2)]
                    pk = [ps(st, f"pk{i}", (128, 512)) for i in range(2)]
                    pmv = [ps(st, f"pmv{i}", (128, 512)) for i in range(2)]
                    for wi_ in range(2):
                        s = wi_ % 2
                        P.dma('pool', wk[s][:], wmkv[:, wi_ * 512:(wi_ + 1) * 512].rearrange(
                            "(k p) c -> p k c", p=128), w=[f'wk{s}'])
                        for cc in range(4):
                            c = wi_ * 4 + cc
                            b = c % 2
                            for k in range(8):
                                P.pe(lambda e, s=s, cc=cc, k=k, b=b: e.matmul(
                                    pk[b][:, 0:256], lhsT=wk[s][:, k, cc * 128:(cc + 1) * 128],
                                    rhs=mnT[:, k, :], start=(k == 0), stop=(k == 7)),
                                    r=[f'wk{s}'], w=[f'pk{b}'])
                            P.act(lambda e, c=c, b=b: e.activation(out=mkT[:, c, :], in_=pk[b][:, 0:256],
                                                                   func=AF.Copy), r=[f'pk{b}'], w=['mkT'])
                    for wi_ in range(2):
                        s = wi_ % 2
                        P.dma('pool', wk[s][:], wmkv[:, 1024 + wi_ * 512:1024 + (wi_ + 1) * 512].rearrange(
                            "(k p) c -> p k c", p=128), w=[f'wk{s}'])
                        for mc in range(2):
                            for k in range(8):
                                P.pe(lambda e, s=s, mc=mc, k=k: e.matmul(
                                    pmv[mc][:], lhsT=mnT[:, k, mc * 128:(mc + 1) * 128], rhs=wk[s][:, k, :],
                                    start=(k == 0), stop=(k == 7)), r=[f'wk{s}'], w=[f'pmv{mc}'])
                            P.act(lambda e, wi_=wi_, mc=mc: e.activation(
                                out=mv[:, mc, wi_ * 512:(wi_ + 1) * 512], in_=pmv[mc][:], func=AF.Copy),
                                r=[f'pmv{mc}'], w=['mv'])
                    P.barrier()
                    P.emit()
                with ExitStack() as st:
                    qml = [sb(st, f"qml{i}", (128, 2, 512), BF16) for i in range(2)]
                    gl = [sb(st, f"gl{i}", (128, 2, 512), BF16) for i in range(2)]
                    pT = [sb(st, f"pT{i}", (128, 2, 512), BF16) for i in range(2)]
                    rinv = sb(st, "rinv", (128, 512), F32)
                    o1 = [sb(st, f"o1{i}", (128, 512), F32) for i in range(2)]
                    ot = [sb(st, f"ot{i}", (128, 512), BF16) for i in range(2)]
                    pl = [ps(st, f"pl{i}", (128, 512)) for i in range(2)]
                    po = [ps(st, f"po{i}", (128, 512)) for i in range(2)]
                    pr = ps(st, "pr", (128, 512))
                    n = 0
                    for hm in range(4):
                        for T in range(NT):
                            s = n % 2
                            n += 1
                            P.dma('sp', qml[s][:], qmT[hm * 256:(hm + 1) * 256, TS(T)].rearrange(
                                "(c p) t -> p c t", p=128), w=[f'qml{s}'])
                            P.dma('sp', gl[s][:], gT[2048 + hm * 256:2048 + (hm + 1) * 256, TS(T)].rearrange(
                                "(c p) t -> p c t", p=128), w=[f'gl{s}'])
                            for mc in range(2):
                                for dmc in range(2):
                                    P.pe(lambda e, s=s, hm=hm, mc=mc, dmc=dmc: e.matmul(
                                        pl[mc][:], lhsT=mkT[:, hm * 2 + dmc, mc * 128:(mc + 1) * 128],
                                        rhs=qml[s][:, dmc, :], start=(dmc == 0), stop=(dmc == 1)),
                                        r=[f'qml{s}'], w=[f'pl{mc}'])
                                P.act(lambda e, s=s, mc=mc: e.activation(out=pT[s][:, mc, :], in_=pl[mc][:],
                                                                         func=AF.Exp),
                                      r=[f'pl{mc}'], w=[f'pT{s}'])
                            for dmc in range(2):
                                for mc in range(2):
                                    P.pe(lambda e, s=s, hm=hm, mc=mc, dmc=dmc: e.matmul(
                                        po[dmc][:], lhsT=mv[:, mc, hm * 256 + dmc * 128:hm * 256 + (dmc + 1) * 128],
                                        rhs=pT[s][:, mc, :], start=(mc == 0), stop=(mc == 1)),
                                        r=[f'pT{s}'], w=[f'po{dmc}'])
                            for mc in range(2):
                                P.pe(lambda e, s=s, mc=mc: e.matmul(pr[:], lhsT=ones_bf[:], rhs=pT[s][:, mc, :],
                                                                    start=(mc == 0), stop=(mc == 1)),
                                     r=[f'pT{s}'], w=['pr'])
                            P.dve(lambda e: e.reciprocal(out=rinv[:], in_=pr[:]), r=['pr'], w=['rinv'])
                            for dmc in range(2):
                                P.dve(lambda e, dmc=dmc: e.tensor_tensor(out=o1[dmc][:], in0=po[dmc][:], in1=rinv[:],
                                                                         op=ALU.mult),
                                      r=[f'po{dmc}', 'rinv'], w=[f'o1{dmc}'])
                                P.dve(lambda e, s=s, dmc=dmc: e.tensor_tensor(
                                    out=ot[dmc][:], in0=o1[dmc][:], in1=gl[s][:, dmc, :], op=ALU.mult),
                                    r=[f'o1{dmc}', f'gl{s}'], w=[f'ot{dmc}'])
                                P.dma('pool', mmemT[hm * 256 + dmc * 128:hm * 256 + (dmc + 1) * 128, TS(T)],
                                      ot[dmc][:], r=[f'ot{dmc}'], w=['dram'])
                    P.barrier()
                    P.emit()

        if upto >= 5:
            with ExitStack() as st:
                kil = sb(st, "kil", (128, S), BF16)
                qil = [sb(st, f"qil{i}", (128, 4, 128), BF16) for i in range(2)]
                wil = [sb(st, f"wil{i}", (128, 8), F32) for i in range(2)]
                score = [sb(st, f"score{i}", (128, S), F32) for i in range(4)]
                rl = [sb(st, f"rl{i}", (128, 512), F32) for i in range(3)]
                junk = sb(st, "junkE", (128, S), BF16)
                junkD = sb(st, "junkDE", (128, S), BF16)
                mk = [sb(st, f"mk{i}", (128, S), BF16) for i in range(2)]
                mT = [sb(st, f"mT{i}", (128, 8, 128), BF16) for i in range(2)]
                ptab = sb(st, "ptab", (128, NIT + 1), F32)
                thrc = sb(st, "thrc", (128, 1), F32)

                def two(name, shape):
                    return [sb(st, f"{name}{c}", shape, F32) for c in range(2)]
                lo = two("lo", (128, 1))
                hi = two("hi", (128, 1))
                Wd = two("Wd", (128, 1))
                hneg = two("hneg", (128, NIT + 1))
                nm = two("nm", (128, 1))
                ssum = two("ssum", (128, 1))
                sgn = two("sgn", (128, 1))
                thr_t = two("thr_t", (128, 1))
                midp = two("midp", (128, 1))
                cntd = two("cntd", (128, 1))
                t2 = two("t2", (128, 1))
                pd = [ps(st, f"pd{i}", (128, 512)) for i in range(4)]
                ptm = [ps(st, f"ptm{i}", (128, 1024), BF16) for i in range(2)]
                for n in range(NIT + 1):
                    P.pool(lambda e, n=n: e.memset(ptab[:, n:n + 1], -(2.0 ** -(n + 1))), w=['ptab'])
                P.pool(lambda e: e.memset(thrc[:], -1.0e29), w=['thrc'])
                P.dma('sp', kil[:], kiT[:, :], w=['kil'])
                cnts = {'c': 0, 't': 0, 'q': 0}
                DVE_IT = ((1, 5, 9, 13), (3, 7, 11, 15))

                def score_units(i):
                    s = i % 4
                    L = (i + 1) * 128
                    units = []
                    qs = cnts['q'] % 2
                    cnts['q'] += 1

                    def loads():
                        P.dma('sp', qil[qs][:], qiT[:, i * 128:(i + 1) * 128].rearrange("(c p) t -> p c t", p=128),
                              w=[f'qil{qs}'])
                        P.dma('sp', wil[qs][:], witm[i * 128:(i + 1) * 128, :], w=[f'wil{qs}'])
                    units.append(loads)
                    nst = (L + 511) // 512
                    for stile in range(nst):
                        w_ = min(512, L - stile * 512)
                        c0 = stile * 512
                        for h in range(8):
                            def unit(stile=stile, w_=w_, c0=c0, h=h):
                                p2, half = divmod(h, 2)
                                b = cnts['c'] % 4
                                r3 = cnts['c'] % 3
                                cnts['c'] += 1
                                P.pe(lambda e: e.matmul(
                                    pd[b][:, 0:w_], lhsT=qil[qs][half * 64:(half + 1) * 64, p2, :],
                                    rhs=kil[half * 64:(half + 1) * 64, c0:c0 + w_], start=True, stop=True),
                                    r=[f'qil{qs}', 'kil'], w=[f'pd{b}'])
                                P.act(lambda e: e.activation(
                                    out=rl[r3][:, 0:w_], in_=pd[b][:, 0:w_], func=AF.Relu),
                                    r=[f'pd{b}'], w=[f'rl{r3}'])
                                if h == 0:
                                    P.dve(lambda e: e.tensor_scalar(
                                        out=score[s][:, c0:c0 + w_], in0=rl[r3][:, 0:w_], scalar1=wil[qs][:, 0:1],
                                        scalar2=None, op0=ALU.mult),
                                        r=[f'rl{r3}', f'wil{qs}'], w=[f'score{s}'])
                                else:
                                    P.dve(lambda e: e.scalar_tensor_tensor(
                                        out=score[s][:, c0:c0 + w_], in0=rl[r3][:, 0:w_], scalar=wil[qs][:, h:h + 1],
                                        in1=score[s][:, c0:c0 + w_], op0=ALU.mult, op1=ALU.add),
                                        r=[f'rl{r3}', f'wil{qs}'], w=[f'score{s}'])
                            units.append(unit)

                    def fin():
                        P.dve(lambda e: e.tensor_tensor(
                            out=score[s][:, i * 128:(i + 1) * 128], in0=score[s][:, i * 128:(i + 1) * 128],
                            in1=caus[:], op=ALU.add), w=[f'score{s}'])
                    units.append(fin)
                    return units

                def bisect_ops(i, c):
                    s = i % 4
                    L = (i + 1) * 128
                    sk = f'score{s}'
                    K = lambda nme: f'{nme}{c}'
                    units = []
                    if i >= 2:
                        units.append([
                            lambda: P.dve(lambda e: e.tensor_reduce(out=lo[c][:], in_=score[s][:, 0:i * 128],
                                                                    axis=AX.X, op=ALU.min), r=[sk], w=[K('lo')]),
                            lambda: P.dve(lambda e: e.tensor_reduce(out=hi[c][:], in_=score[s][:, 0:L],
                                                                    axis=AX.X, op=ALU.max), r=[sk], w=[K('hi')]),
                            lambda: P.dve(lambda e: e.tensor_tensor(out=Wd[c][:], in0=hi[c][:], in1=lo[c][:],
                                                                    op=ALU.subtract),
                                          r=[K('hi'), K('lo')], w=[K('Wd')]),
                            lambda: P.dve(lambda e: e.tensor_scalar(out=hneg[c][:], in0=ptab[:], scalar1=Wd[c][:, 0:1],
                                                                    scalar2=None, op0=ALU.mult),
                                          r=[K('Wd'), 'ptab'], w=[K('hneg')]),
                            lambda: P.dve(lambda e: e.tensor_scalar(out=nm[c][:], in0=lo[c][:], scalar1=-1.0,
                                                                    scalar2=hneg[c][:, 0:1], op0=ALU.mult,
                                                                    op1=ALU.add),
                                          r=[K('lo'), K('hneg')], w=[K('nm')]),
                        ])
                        for n in range(NIT):
                            if n in DVE_IT[c]:
                                units.append([
                                    lambda: P.dve(lambda e: e.tensor_scalar(
                                        out=midp[c][:], in0=nm[c][:], scalar1=-1.0, scalar2=None, op0=ALU.mult),
                                        r=[K('nm')], w=[K('midp')]),
                                    lambda: P.dve(lambda e: e.tensor_scalar(
                                        out=junkD[:, 0:L], in0=score[s][:, 0:L], scalar1=midp[c][:, 0:1],
                                        scalar2=None, op0=ALU.is_ge, op1=ALU.add, accum_out=cntd[c][:]),
                                        r=[sk, K('midp')], w=[K('cntd')]),
                                    lambda: P.dve(lambda e: e.tensor_scalar(
                                        out=t2[c][:], in0=cntd[c][:], scalar1=TOPK - 0.5, scalar2=2.0,
                                        op0=ALU.is_ge, op1=ALU.mult), r=[K('cntd')], w=[K('t2')]),
                                    lambda n=n: P.dve(lambda e: e.tensor_scalar(
                                        out=t2[c][:], in0=t2[c][:], scalar1=-1.0, scalar2=hneg[c][:, n + 1:n + 2],
                                        op0=ALU.add, op1=ALU.mult), r=[K('hneg')], w=[K('t2')]),
                                    lambda: P.dve(lambda e: e.tensor_tensor(
                                        out=nm[c][:], in0=nm[c][:], in1=t2[c][:], op=ALU.add),
                                        r=[K('t2')], w=[K('nm')]),
                                ])
                            else:
                                units.append([
                                    lambda: P.act(lambda e: e.activation(
                                        out=junk[:, 0:L], in_=score[s][:, 0:L], func=AF.Sign, bias=nm[c][:, 0:1],
                                        scale=1.0, accum_out=ssum[c][:]), r=[sk, K('nm')], w=[K('ssum')]),
                                    lambda: P.act(lambda e: e.activation(
                                        out=sgn[c][:], in_=ssum[c][:], func=AF.Sign,
                                        bias=float(L - 2 * TOPK) + 0.5, scale=1.0), r=[K('ssum')], w=[K('sgn')]),
                                    lambda n=n: P.act(lambda e: e.activation(
                                        out=nm[c][:], in_=sgn[c][:], func=AF.Identity,
                                        scale=hneg[c][:, n + 1:n + 2], bias=nm[c][:, 0:1]),
                                        r=[K('sgn'), K('hneg')], w=[K('nm')]),
                                ])

                    def epi():
                        if i >= 2:
                            P.dve(lambda e: e.tensor_scalar(out=thr_t[c][:], in0=nm[c][:], scalar1=-1.0,
                                                            scalar2=hneg[c][:, NIT:NIT + 1], op0=ALU.mult,
                                                            op1=ALU.add),
                                  r=[K('nm'), K('hneg')], w=[K('thr_t')])
                            thr, thrk = thr_t[c], K('thr_t')
                        else:
                            thr, thrk = thrc, 'thrc'
                        P.dve(lambda e: e.tensor_scalar(
                            out=mk[c][:, 0:L], in0=score[s][:, 0:L], scalar1=thr[:, 0:1], scalar2=None,
                            op0=ALU.is_ge), r=[sk, thrk], w=[K('mk')])
                        for g0 in range(0, i + 1, 8):
                            nb_ = min(8, i + 1 - g0)
                            b = cnts['t'] % 2
                            cnts['t'] += 1
                            for q_ in range(nb_):
                                P.pe(lambda e, b=b, q_=q_, g0=g0: e.transpose(
                                    out=ptm[b][:, q_ * 128:(q_ + 1) * 128],
                                    in_=mk[c][:, (g0 + q_) * 128:(g0 + q_ + 1) * 128], identity=ident[:]),
                                    r=[K('mk')], w=[f'ptm{b}'])
                            P.act(lambda e, b=b, nb_=nb_: e.activation(
                                out=mT[b][:, 0:nb_, :],
                                in_=ptm[b][:, 0:nb_ * 128].rearrange("p (q t) -> p q t", q=nb_), func=AF.Copy),
                                r=[f'ptm{b}'], w=[f'mT{b}'])
                            P.dma('pool', maskT[g0 * 128:(g0 + nb_) * 128, i * 128:(i + 1) * 128].rearrange(
                                "(q p) t -> p q t", p=128), mT[b][:, 0:nb_, :], r=[f'mT{b}'], w=['dram'])
                    units.append([epi])
                    return units

                def run_units(us):
                    for u in us:
                        u()

                run_units(score_units(0))
                run_units(score_units(1))
                for pidx in range(NB // 2):
                    ia, ib = 2 * pidx, 2 * pidx + 1
                    A_ = bisect_ops(ia, 0)
                    B_ = bisect_ops(ib, 1)
                    S_ = []
                    if ia + 2 < NB:
                        s1, s2 = score_units(ia + 2), score_units(ib + 2)
                        for k_ in range(max(len(s1), len(s2))):
                            if k_ < len(s1):
                                S_.append(s1[k_])
                            if k_ < len(s2):
                                S_.append(s2[k_])
                    nun = max(len(A_), len(B_))
                    done = 0
                    for idx in range(nun):
                        ua = A_[idx] if idx < len(A_) else []
                        ub = B_[idx] if idx < len(B_) else []
                        for k_ in range(max(len(ua), len(ub))):
                            if k_ < len(ua):
                                ua[k_]()
                            if k_ < len(ub):
                                ub[k_]()
                        tgt = (len(S_) * (idx + 1)) // nun
                        while done < tgt:
                            S_[done]()
                            done += 1
                P.barrier()
                P.emit()

        if upto >= 6:
            with ExitStack() as st:
                ql = [sb(st, f"ql{i}", (128, S), BF16) for i in range(2)]
                kl = [sb(st, f"kl{i}", (128, S), BF16) for i in range(2)]
                vl = [sb(st, f"vl{i}", (128, NB, 128), BF16) for i in range(2)]
                NML = 6
                ml = [sb(st, f"ml{i}", (128, 512), BF16) for i in range(NML)]
                ex = [sb(st, f"ex{i}", (128, 512), BF16) for i in range(4)]
                pm = [sb(st, f"pm{i}", (128, 512), BF16) for i in range(4)]
                gl = [sb(st, f"gl{i}", (128, 512), BF16) for i in range(2)]
                rinv = sb(st, "rinv", (128, 512), F32)
                o1 = sb(st, "o1", (128, 512), F32)
                ot = [sb(st, f"ot{i}", (128, 512), BF16) for i in range(2)]
                psc = [ps(st, f"psc{i}", (128, 512)) for i in range(4)]
                po = [ps(st, f"po{i}", (128, 512)) for i in range(2)]
                pr = [ps(st, f"pr{i}", (128, 512)) for i in range(2)]
                its = []
                for h in range(8):
                    for T in range(NT):
                        nsb = 4 * T + 4
                        for sbk in range(nsb):
                            its.append((h, T, sbk, nsb))
                DEPTH = 3

                def front(n):
                    h, T, sbk, nsb = its[n]
                    hs = h % 2
                    a = (h * NT + T) % 2
                    if T == 0 and sbk == 0:
                        P.dma('sp', ql[hs][:], qT[h * 128:(h + 1) * 128, :], w=[f'ql{hs}'])
                        P.dma('sp', kl[hs][:], kT[h * 128:(h + 1) * 128, :], w=[f'kl{hs}'])
                        P.dma('sp', vl[hs][:], vtm[:, h * 128:(h + 1) * 128].rearrange("(b p) d -> p b d", p=128),
                              w=[f'vl{hs}'])
                    if sbk == 0:
                        P.dma('sp', gl[a][:], gT[1024 + h * 128:1024 + (h + 1) * 128, TS(T)], w=[f'gl{a}'])
                    m4 = n % NML
                    b3 = n % 4
                    r0 = max(0, sbk - 4 * T) * 128
                    t0 = T * 512 + r0
                    P.dma('sp', ml[m4][:, r0:512], maskT[sbk * 128:(sbk + 1) * 128, t0:(T + 1) * 512],
                          w=[f'ml{m4}'])
                    P.pe(lambda e: e.matmul(
                        psc[b3][:, r0:512], lhsT=kl[hs][:, sbk * 128:(sbk + 1) * 128],
                        rhs=ql[hs][:, t0:(T + 1) * 512], start=True, stop=True),
                        r=[f'kl{hs}', f'ql{hs}'], w=[f'psc{b3}'])
                    P.act(lambda e: e.activation(out=ex[b3][:, r0:512], in_=psc[b3][:, r0:512], func=AF.Exp),
                          r=[f'psc{b3}'], w=[f'ex{b3}'])
                    P.dve(lambda e: e.tensor_tensor(
                        out=pm[b3][:, r0:512], in0=ex[b3][:, r0:512], in1=ml[m4][:, r0:512], op=ALU.mult),
                        r=[f'ex{b3}', f'ml{m4}'], w=[f'pm{b3}'])

                def back(n):
                    h, T, sbk, nsb = its[n]
                    hs = h % 2
                    a = (h * NT + T) % 2
                    b3 = n % 4
                    r0 = max(0, sbk - 4 * T) * 128
                    P.pe(lambda e: e.matmul(
                        po[a][:, r0:512], lhsT=vl[hs][:, sbk, :], rhs=pm[b3][:, r0:512],
                        start=(sbk == 0), stop=(sbk == nsb - 1)),
                        r=[f'pm{b3}', f'vl{hs}'], w=[f'po{a}'])
                    P.pe(lambda e: e.matmul(
                        pr[a][:, r0:512], lhsT=ones_bf[:], rhs=pm[b3][:, r0:512],
                        start=(sbk == 0), stop=(sbk == nsb - 1)),
                        r=[f'pm{b3}'], w=[f'pr{a}'])
                    if sbk == nsb - 1:
                        P.dve(lambda e: e.reciprocal(out=rinv[:], in_=pr[a][:]), r=[f'pr{a}'], w=['rinv'])
                        P.dve(lambda e: e.tensor_tensor(out=o1[:], in0=po[a][:], in1=rinv[:], op=ALU.mult),
                              r=[f'po{a}', 'rinv'], w=['o1'])
                        P.dve(lambda e: e.tensor_tensor(out=ot[a][:], in0=o1[:], in1=gl[a][:], op=ALU.mult),
                              r=['o1', f'gl{a}'], w=[f'ot{a}'])
                        P.dma('pool', mattT[h * 128:(h + 1) * 128, TS(T)], ot[a][:], r=[f'ot{a}'], w=['dram'])

                for n in range(len(its) + DEPTH):
                    if n < len(its):
                        front(n)
                    if n >= DEPTH:
                        back(n - DEPTH)
                P.barrier()
                P.emit()

        def out_norm_phase(st, nk, load_lhs, wres, gname, resid, dst, tag, post=None):
            gp = sb(st, tag + "gp", (128, D), F32)
            o, n_ = PCOL[gname]
            P.dma('sp', gp[:], params[:, o:o + n_], w=['gp'])
            xt = [sb(st, f"{tag}xt{i}", (128, D), F32) for i in range(2)]
            xo = [sb(st, f"{tag}xo{i}", (128, D), F32) for i in range(2)]
            tmp = sb(st, tag + "tmp", (128, D), F32)
            junkA = sb(st, tag + "junkA", (128, 512), BF16)
            ssh = [sb(st, f"{tag}ssh{i}", (128, 2), F32) for i in range(2)]
            ss = [sb(st, f"{tag}ss{i}", (128, 1), F32) for i in range(2)]
            rt = [sb(st, f"{tag}rt{i}", (128, 1), F32) for i in range(2)]
            rs = [sb(st, f"{tag}rs{i}", (128, 1), F32) for i in range(2)]
            po = [ps(st, f"{tag}po{i}", (128, 512)) for i in range(4)]
            for T in range(NT):
                lhs, lkey = load_lhs(T)
                for tb in range(4):
                    i = T * 4 + tb
                    s2 = i % 2
                    P.dma('sp', xt[s2][:], resid[i * 128:(i + 1) * 128, :], w=[f'xt{s2}'])
                    for hh in range(2):
                        b = s2 * 2 + hh
                        for j in range(nk):
                            P.pe(lambda e, lhs=lhs, j=j, tb=tb, hh=hh, b=b: e.matmul(
                                po[b][:], lhsT=lhs[:, j, tb * 128:(tb + 1) * 128],
                                rhs=wres[:, j, hh * 512:(hh + 1) * 512], start=(j == 0), stop=(j == nk - 1)),
                                r=[lkey, 'wres'], w=[f'po{b}'])
                        P.act(lambda e, b=b, s2=s2, hh=hh: e.activation(
                            out=junkA[:], in_=po[b][:], func=AF.Square, accum_out=ssh[s2][:, hh:hh + 1]),
                            r=[f'po{b}'], w=['junkA', f'ssh{s2}'])
                    P.dve(lambda e, s2=s2: e.tensor_tensor(out=ss[s2][:], in0=ssh[s2][:, 0:1], in1=ssh[s2][:, 1:2],
                                                           op=ALU.add), r=[f'ssh{s2}'], w=[f'ss{s2}'])
                    P.act(lambda e, s2=s2: e.activation(out=rt[s2][:], in_=ss[s2][:], func=AF.Sqrt, bias=EPS,
                                                        scale=1.0 / D), r=[f'ss{s2}'], w=[f'rt{s2}'])
                    P.dve(lambda e, s2=s2: e.reciprocal(out=rs[s2][:], in_=rt[s2][:]), r=[f'rt{s2}'], w=[f'rs{s2}'])
                    for hh in range(2):
                        b = s2 * 2 + hh
                        P.dve(lambda e, b=b, s2=s2, hh=hh: e.scalar_tensor_tensor(
                            out=tmp[:, hh * 512:(hh + 1) * 512], in0=po[b][:], scalar=rs[s2][:, 0:1],
                            in1=gp[:, hh * 512:(hh + 1) * 512], op0=ALU.mult, op1=ALU.mult),
                            r=[f'po{b}', f'rs{s2}', 'gp'], w=['tmp'])
                    P.dve(lambda e, s2=s2: e.tensor_tensor(out=xo[s2][:], in0=tmp[:], in1=xt[s2][:], op=ALU.add),
                          r=['tmp', f'xt{s2}'], w=[f'xo{s2}'])
                    P.dma('pool', dst[i * 128:(i + 1) * 128, :], xo[s2][:], r=[f'xo{s2}'], w=['dram'])
                    if post is not None:
                        post(i, s2, xo[s2], f'xo{s2}')

        h2st = ExitStack()
        if upto >= 7:
            h2T = sb(h2st, "h2T", (128, 8, S), BF16)
            with ExitStack() as st:
                wo = sb(st, "wo", (128, 8, D), BF16)
                for hh in range(2):
                    P.dma('pool', wo[:, :, hh * 512:(hh + 1) * 512],
                          wout[:, hh * 512:(hh + 1) * 512].rearrange("(k p) c -> p k c", p=128), w=['wres'])
                g2 = sb(st, "g2bc", (128, D), F32)
                o, n_ = PCOL['g2']
                P.dma('sp', g2[:], params[:, o:o + n_], w=['g2'])
                ma = [sb(st, f"ma{i}", (128, 8, 512), BF16) for i in range(2)]
                mb = [sb(st, f"mb{i}", (128, 8, 512), BF16) for i in range(2)]
                mc_ = [sb(st, f"mc{i}", (128, 8, 512), BF16) for i in range(2)]
                mg = [sb(st, f"mg{i}", (128, 8, 512), BF16) for i in range(2)]
                junkD = sb(st, "junkD", (128, D), BF16)
                hb = [sb(st, f"Ghb{i}", (128, D), BF16) for i in range(2)]
                ss2 = [sb(st, f"Gss2{i}", (128, 1), F32) for i in range(2)]
                rt2 = [sb(st, f"Grt2{i}", (128, 1), F32) for i in range(2)]
                rs2 = [sb(st, f"Grs2{i}", (128, 1), F32) for i in range(2)]
                pt = [ps(st, f"Gpt{i}", (128, D), BF16) for i in range(2)]

                def load_merged(T):
                    s = T % 2
                    for (buf, src, nm) in ((ma, mconvT, 'ma'), (mb, mattT, 'mb'), (mc_, mmemT, 'mc')):
                        P.dma('sp', buf[s][:], src[:, TS(T)].rearrange("(j p) t -> p j t", p=128), w=[f'{nm}{s}'])
                    P.dve(lambda e, s=s: e.tensor_tensor(out=mg[s][:], in0=ma[s][:], in1=mb[s][:], op=ALU.add),
                          r=[f'ma{s}', f'mb{s}'], w=[f'mg{s}'])
                    P.dve(lambda e, s=s: e.tensor_tensor(out=mg[s][:], in0=mg[s][:], in1=mc_[s][:], op=ALU.add),
                          r=[f'mc{s}'], w=[f'mg{s}'])
                    return mg[s], f'mg{s}'

                def post_h2(i, s2, xo, xkey):
                    P.dve(lambda e, s2=s2: e.scalar_tensor_tensor(
                        out=junkD[:], in0=xo[:], scalar=1.0, in1=xo[:], op0=ALU.mult, op1=ALU.mult,
                        accum_out=ss2[s2][:]), r=[xkey], w=['junkD', f'ss2{s2}'])
                    P.act(lambda e, s2=s2: e.activation(out=rt2[s2][:], in_=ss2[s2][:], func=AF.Sqrt, bias=EPS,
                                                        scale=1.0 / D), r=[f'ss2{s2}'], w=[f'rt2{s2}'])
                    P.dve(lambda e, s2=s2: e.reciprocal(out=rs2[s2][:], in_=rt2[s2][:]),
                          r=[f'rt2{s2}'], w=[f'rs2{s2}'])
                    P.dve(lambda e, s2=s2: e.scalar_tensor_tensor(
                        out=hb[s2][:], in0=xo[:], scalar=rs2[s2][:, 0:1], in1=g2[:], op0=ALU.mult, op1=ALU.mult),
                        r=[xkey, f'rs2{s2}', 'g2'], w=[f'hb{s2}'])
                    for j in range(8):
                        P.pe(lambda e, s2=s2, j=j: e.transpose(out=pt[s2][:, j * 128:(j + 1) * 128],
                                                               in_=hb[s2][:, j * 128:(j + 1) * 128],
                                                               identity=ident[:]),
                             r=[f'hb{s2}'], w=[f'pt{s2}'])
                    P.act(lambda e, s2=s2, i=i: e.activation(
                        out=h2T[:, :, i * 128:(i + 1) * 128],
                        in_=pt[s2][:].rearrange("p (j t) -> p j t", j=8), func=AF.Copy),
                        r=[f'pt{s2}'], w=[f'h2T{i}'])

                out_norm_phase(st, 8, load_merged, wo, 'gpost1', x, x1d, "G", post=post_h2)
                P.barrier()
                P.emit()

        if upto >= 8:
            with ExitStack() as st:
                wt = [sb(st, f"Hwt{i}", (128, 8, 512), BF16) for i in range(2)]
                ug = [sb(st, f"ug{i}", (128, 514), F32) for i in range(3)]
                uvv = [sb(st, f"uv{i}", (128, 514), F32) for i in range(3)]
                cg = [sb(st, f"cg{i}", (128, 512), F32) for i in range(3)]
                cv = [sb(st, f"cv{i}", (128, 512), F32) for i in range(3)]
                sgl = [sb(st, f"sgl{i}", (128, 512), F32) for i in range(3)]
                pend = []
                ast = [sb(st, f"ast{i}", (128, S), BF16) for i in range(2)]
                pg = [ps(st, f"Hpg{i}", (128, 512)) for i in range(2)]
                pvv = [ps(st, f"Hpv{i}", (128, 512)) for i in range(2)]

                def fw(c, tap):
                    return pvc('ffnw', c * 3 + tap, 1)

                n = 0
                for wi_ in range(11):
                    s = wi_ % 2
                    P.dma('pool', wt[s][:], wup[:, wi_ * 512:(wi_ + 1) * 512].rearrange(
                        "(k p) c -> p k c", p=128), w=[f'wt{s}'])
                    for jj in range(2):
                        j = wi_ * 2 + jj
                        a2 = j % 2
                        for T in range(NT):
                            b = n % 2
                            s3 = n % 3
                            p3 = (n - 1) % 3
                            b3 = n % 3
                            n += 1
                            for k in range(8):
                                P.pe(lambda e, s=s, jj=jj, T=T, k=k, b=b: e.matmul(
                                    pg[b][:], lhsT=wt[s][:, k, jj * 256:jj * 256 + 128],
                                    rhs=h2T[:, k, TS(T)], start=(k == 0), stop=(k == 7)),
                                    r=[f'wt{s}'], w=[f'pg{b}'])
                            for k in range(8):
                                P.pe(lambda e, s=s, jj=jj, T=T, k=k, b=b: e.matmul(
                                    pvv[b][:], lhsT=wt[s][:, k, jj * 256 + 128:jj * 256 + 256],
                                    rhs=h2T[:, k, TS(T)], start=(k == 0), stop=(k == 7)),
                                    r=[f'wt{s}'], w=[f'pvv{b}'])
                            for (u_, nm_) in ((ug, 'ug'), (uvv, 'uv')):
                                if T == 0:
                                    P.pool(lambda e, u_=u_, s3=s3: e.memset(u_[s3][:, 0:2], 0.0), w=[f'{nm_}{s3}'])
                                else:
                                    P.pool(lambda e, u_=u_, s3=s3, p3=p3: e.tensor_copy(
                                        out=u_[s3][:, 0:2], in_=u_[p3][:, 512:514]),
                                        r=[f'{nm_}{p3}'], w=[f'{nm_}{s3}'])
                            for (u_, nm_, pp_, pk_, co, cname, c) in (
                                    (ug, 'ug', pg, 'pg', cg, 'cg', 2 * j), (uvv, 'uv', pvv, 'pvv', cv, 'cv', 2 * j + 1)):
                                P.act(lambda e, u_=u_, pp_=pp_, b=b, s3=s3: e.activation(
                                    out=u_[s3][:, 2:514], in_=pp_[b][:], func=AF.Copy),
                                    r=[f'{pk_}{b}'], w=[f'{nm_}{s3}'])
                                P.act(lambda e, co=co, pp_=pp_, b=b, c=c, b3=b3: e.activation(
                                    out=co[b3][:], in_=pp_[b][:], func=AF.Identity, scale=fw(c, 2),
                                    bias=pvc('ffnb', c, 1)), r=[f'{pk_}{b}'], w=[f'{cname}{b3}'])
                                for tap in (1, 0):
                                    P.dve(lambda e, u_=u_, co=co, b3=b3, s3=s3, c=c, tap=tap: e.scalar_tensor_tensor(
                                        out=co[b3][:], in0=u_[s3][:, tap:tap + 512], scalar=fw(c, tap), in1=co[b3][:],
                                        op0=ALU.mult, op1=ALU.add), r=[f'{nm_}{s3}'], w=[f'{cname}{b3}'])
                            def tail(b3=b3, a2=a2, T=T, j=j):
                                P.act(lambda e: e.activation(out=sgl[b3][:], in_=cg[b3][:], func=AF.Silu),
                                      r=[f'cg{b3}'], w=[f'sgl{b3}'])
                                P.dve(lambda e: e.tensor_tensor(
                                    out=ast[a2][:, TS(T)], in0=sgl[b3][:], in1=cv[b3][:], op=ALU.mult),
                                    r=[f'sgl{b3}', f'cv{b3}'], w=[f'ast{a2}'])
                                if T == NT - 1:
                                    P.dma('sp', aT[j * 128:(j + 1) * 128, :], ast[a2][:], r=[f'ast{a2}'], w=['dram'])
                            while pend:
                                pend.pop(0)()
                            pend.append(tail)
                while pend:
                    pend.pop(0)()
                P.barrier()
                P.emit()
        h2st.close()

        if upto >= 9:
            with ExitStack() as st:
                wd = sb(st, "wd", (128, NFC, D), BF16)
                for hh in range(2):
                    P.dma('pool', wd[:, :, hh * 512:(hh + 1) * 512],
                          wdown[:, hh * 512:(hh + 1) * 512].rearrange("(k p) c -> p k c", p=128), w=['wres'])
                at = [sb(st, f"at{i}", (128, NFC, 512), BF16) for i in range(2)]

                def load_act(T):
                    s = T % 2
                    P.dma('sp', at[s][:], aT[:, TS(T)].rearrange("(j p) t -> p j t", p=128), w=[f'at{s}'])
                    return at[s], f'at{s}'

                out_norm_phase(st, NFC, load_act, wd, 'gpost2', x1d, y, "I")
                P.barrier()
                P.emit()


        P.barrier()
        P.emit()
    return nc


_CACHE = {}


def kernel(**inputs):
    shared = pack_inputs(inputs)
    if 'nc' not in _CACHE:
        _CACHE['nc'] = build()
    nc = _CACHE['nc']
    x = np.asarray(inputs['x'], np.float32)
    mem = np.asarray(inputs['mem'], np.float32)
    in_maps = []
    for b in range(8):
        m = dict(shared)
        m['x'] = np.ascontiguousarray(x[b])
        m['mem'] = np.ascontiguousarray(mem[b])
        in_maps.append(m)
    res = run_bass_kernel_spmd(nc, in_maps, core_ids=list(range(8)))
    return np.stack([np.asarray(r['y'], np.float32) for r in res.results], axis=0)
```

```python
import numpy as np
from contextlib import ExitStack
import concourse.bass as bass
import concourse.mybir as mybir
from concourse.bass_utils import run_bass_kernel_spmd

F32 = mybir.dt.float32
BF16 = mybir.dt.bfloat16
I32 = mybir.dt.int32
AF = mybir.ActivationFunctionType
ALU = mybir.AluOpType
AX = mybir.AxisListType

S = 4096
D = 1024
NB = S // 128
NT = S // 512
EPS = 1e-6
FFN = 2816
NFC = FFN // 128
TOPK = 256
NIT = 16
DVE_ITERS = (3, 6, 9, 12, 14)
NEG = -1.0e30

ENGS = ['pe', 'act', 'dve', 'pool', 'sp']
BLK = {'pe': 'tensor', 'act': 'scalar', 'dve': 'vector', 'pool': 'gpsimd', 'sp': 'sync'}
EPOCH = 20000
NDS = 16


class Prog:
    def __init__(self, nc, stack):
        self.nc = nc
        self.stack = stack
        self.lists = {e: [] for e in ENGS}
        self.count = {e: 0 for e in ENGS}
        self.sems = {e: [] for e in ENGS}
        self.waited = {e: {} for e in ENGS}
        self.lastw = {}
        self.reads = {}
        self.dsem = {}
        self.dval = {}
        self.dnext = {}
        self.nsem = 0

    def _esem(self, e, idx):
        while len(self.sems[e]) <= idx:
            self.sems[e].append(self.stack.enter_context(
                self.nc.semaphore(f"s_{e}{len(self.sems[e])}")))
        return self.sems[e][idx]

    def _collect(self, e, r, w, extra):
        toks = list(extra)
        for k in r:
            t = self.lastw.get(k)
            if t is not None:
                toks.append(t)
        for k in w:
            t = self.lastw.get(k)
            if t is not None:
                toks.append(t)
            toks.extend(self.reads.get(k, ()))
        need = []
        for (sid, sem, v, te) in toks:
            if te == e and e == 'pe':
                continue
            if self.waited[e].get(sid, 0) >= v:
                continue
            self.waited[e][sid] = v
            need.append((sem, v))
        return need

    def _update(self, tok, r, w):
        for k in w:
            self.lastw[k] = tok
            self.reads[k] = []
        for k in r:
            self.reads.setdefault(k, []).append(tok)

    def op(self, e, fn, r=(), w=(), extra=()):
        need = self._collect(e, r, w, extra)
        n = self.count[e]
        ep, v = divmod(n, EPOCH)
        sem = self._esem(e, ep)
        self.count[e] = n + 1
        tok = ((e, ep), sem, v + 1, e)
        self.lists[e].append((need, fn, sem, 1))
        self._update(tok, r, w)
        return tok

    def pe(self, fn, r=(), w=(), extra=()):
        return self.op('pe', fn, r, w, extra)

    def act(self, fn, r=(), w=(), extra=()):
        return self.op('act', fn, r, w, extra)

    def dve(self, fn, r=(), w=(), extra=()):
        return self.op('dve', fn, r, w, extra)

    def pool(self, fn, r=(), w=(), extra=()):
        return self.op('pool', fn, r, w, extra)

    def dma(self, e, out, in_, r=(), w=(), extra=(), **kw):
        if e not in self.dsem:
            self.dsem[e] = [self.stack.enter_context(self.nc.semaphore(f"dma_{e}{j}")) for j in range(NDS)]
            self.dval[e] = [0] * NDS
            self.dnext[e] = 0
        j = self.dnext[e]
        self.dnext[e] = (j + 1) % NDS
        sem = self.dsem[e][j]
        extra = list(extra)
        if self.dval[e][j] > 0:
            extra.append((('d', e, j), sem, self.dval[e][j], 'dma'))
        need = self._collect(e, r, w, extra)
        self.dval[e][j] += 16
        tok = (('d', e, j), sem, self.dval[e][j], 'dma')
        self.lists[e].append((need, lambda eng: eng.dma_start(out=out, in_=in_, **kw), sem, 16))
        self._update(tok, r, w)
        return tok

    def barrier(self):
        toks = []
        for e in ENGS:
            n = self.count[e]
            if n == 0:
                continue
            ep, v = divmod(n - 1, EPOCH)
            toks.append(((e, ep), self.sems[e][ep], v + 1, e))
        for q in self.dsem:
            for j in range(NDS):
                if self.dval[q][j] > 0:
                    toks.append((('d', q, j), self.dsem[q][j], self.dval[q][j], 'dma'))
        for e in ENGS:
            need = []
            for (sid, sem, v, te) in toks:
                if te == e:
                    continue
                if self.waited[e].get(sid, 0) >= v:
                    continue
                self.waited[e][sid] = v
                need.append((sem, v))
            if need:
                self.lists[e].append((need, None, None, 0))
        self.lastw = {}
        self.reads = {}

    def emit(self):
        with self.nc.Block() as block:
            for e in ENGS:
                lst = self.lists[e]
                if not lst:
                    continue

                def body(eng, lst=lst):
                    for (need, fn, sem, inc) in lst:
                        for (s, v) in need:
                            eng.wait_ge(s, v)
                        if fn is not None:
                            fn(eng).then_inc(sem, inc)

                getattr(block, BLK[e])(body)
        self.lists = {e: [] for e in ENGS}


PCOL = {}
_off = 0
for _name, _n in [('g1', D), ('gmem', D), ('gpost1', D), ('g2', D), ('gpost2', D),
                  ('convw', 8 * 31), ('convb', 8), ('lng', 8), ('lnb', 8),
                  ('bgate', 24), ('ffnw', 44 * 3), ('ffnb', 44)]:
    PCOL[_name] = (_off, _n)
    _off += _n
NPCOL = _off

WCM_COLS = 45 * 128
WTM_COLS = 1032


def pack_inputs(inp):
    f = np.float32
    w_in = np.asarray(inp['w_in'][0], f)
    c0 = 0
    conv = w_in[:, 0:2048]
    q = w_in[:, 2048:3072]
    k = w_in[:, 3072:4096]
    v = w_in[:, 4096:5120]
    qi = w_in[:, 5120:5632]
    wi = w_in[:, 5632:5640]
    ki = w_in[:, 5640:5704]
    qm = w_in[:, 5704:6728]
    a, g = conv[:, :1024], conv[:, 1024:]
    inter = np.stack([a.reshape(D, 8, 128), g.reshape(D, 8, 128)], axis=2).reshape(D, 2048)
    wcm = np.ascontiguousarray(np.concatenate([inter, q, k, qm, qi, ki, ki], axis=1))
    wtm = np.ascontiguousarray(np.concatenate([v, wi], axis=1))
    w_up = np.asarray(inp['w_up'][0], f)
    ug, uv = w_up[:, :FFN], w_up[:, FFN:]
    wup = np.ascontiguousarray(
        np.stack([ug.reshape(D, NFC, 128), uv.reshape(D, NFC, 128)], axis=2).reshape(D, 2 * FFN))
    P = np.zeros((128, NPCOL), f)

    def put(name, arr):
        o, n = PCOL[name]
        P[:, o:o + n] = arr

    put('g1', np.broadcast_to(inp['norm1_pre_g'][0], (128, D)))
    put('gmem', np.broadcast_to(inp['mem_norm_g'][0], (128, D)))
    put('gpost1', np.broadcast_to(inp['norm1_post_g'][0], (128, D)))
    put('g2', np.broadcast_to(inp['norm2_pre_g'][0], (128, D)))
    put('gpost2', np.broadcast_to(inp['norm2_post_g'][0], (128, D)))
    cw = np.asarray(inp['conv_dw_w'][0], f)
    put('convw', cw.T.reshape(8, 128, 31).transpose(1, 0, 2).reshape(128, 8 * 31))
    put('convb', np.asarray(inp['conv_dw_b'][0], f).reshape(8, 128).T)
    put('lng', np.asarray(inp['conv_ln_g'][0], f).reshape(8, 128).T)
    put('lnb', np.asarray(inp['conv_ln_b'][0], f).reshape(8, 128).T)
    put('bgate', np.asarray(inp['b_gate'][0], f).reshape(24, 128).T)
    fw = np.asarray(inp['ffn_dw_w'][0], f)
    fwg, fwv = fw[:, :FFN], fw[:, FFN:]
    fwi = np.stack([fwg.reshape(3, NFC, 128), fwv.reshape(3, NFC, 128)], axis=2).reshape(3, 44, 128)
    put('ffnw', fwi.transpose(2, 1, 0).reshape(128, 44 * 3))
    fb = np.asarray(inp['ffn_dw_b'][0], f)
    fbi = np.stack([fb[:FFN].reshape(NFC, 128), fb[FFN:].reshape(NFC, 128)], axis=1).reshape(44, 128)
    put('ffnb', fbi.T)
    shared = {
        'wcm': wcm, 'wtm': wtm, 'wup': wup, 'params': P,
        'pw2': np.ascontiguousarray(inp['conv_pw2'][0], f),
        'wmkv': np.ascontiguousarray(inp['w_mem_kv'][0], f),
        'wgate': np.ascontiguousarray(inp['w_gate'][0], f),
        'wout': np.ascontiguousarray(inp['w_out'][0], f),
        'wdown': np.ascontiguousarray(inp['w_down'][0], f),
    }
    return shared


def build(debug=(), upto=99):
    nc = bass.Bass("TRN2", target_bir_lowering=False)
    dbg = set(debug)

    def din(name, shape):
        return nc.dram_tensor(name, list(shape), F32, kind="ExternalInput").ap()

    def scratch(name, shape, dt):
        kind = "ExternalOutput" if name in dbg else "Internal"
        return nc.dram_tensor(name, list(shape), dt, kind=kind).ap()

    x = din("x", (S, D))
    mem = din("mem", (256, D))
    wcm = din("wcm", (D, WCM_COLS))
    wtm = din("wtm", (D, WTM_COLS))
    wup = din("wup", (D, 2 * FFN))
    params = din("params", (128, NPCOL))
    pw2 = din("pw2", (D, D))
    wmkv = din("wmkv", (D, 2 * D))
    wgate = din("wgate", (D, 3 * D))
    wout = din("wout", (D, D))
    wdown = din("wdown", (FFN, D))
    y = nc.dram_tensor("y", [S, D], F32, kind="ExternalOutput").ap()

    ycT = scratch("ycT", (D, S), F32)
    qT = scratch("qT", (D, S), BF16)
    kT = scratch("kT", (D, S), BF16)
    qmT = scratch("qmT", (D, S), BF16)
    qiT = scratch("qiT", (512, S), BF16)
    kiT = scratch("kiT", (128, S), BF16)
    vtm = scratch("vtm", (S, D), BF16)
    witm = scratch("witm", (S, 8), F32)
    gT = scratch("gT", (3 * D, S), BF16)
    mconvT = scratch("mconvT", (D, S), BF16)
    mattT = scratch("mattT", (D, S), BF16)
    mmemT = scratch("mmemT", (D, S), BF16)
    maskT = scratch("maskT", (S, S), BF16)
    x1d = scratch("x1d", (S, D), F32)
    aT = scratch("aT", (FFN, S), BF16)

    def pcols(t, name):
        o, n = PCOL[name]
        return t[:, o:o + n]

    with ExitStack() as gs:
        P = Prog(nc, gs)

        uniq = [0]

        def sb(st, name, shape, dt):
            uniq[0] += 1
            return st.enter_context(nc.sbuf_tensor(f"{name}_{uniq[0]}", list(shape), dt))

        def ps(st, name, shape, dt=F32):
            uniq[0] += 1
            return st.enter_context(nc.psum_tensor(f"{name}_{uniq[0]}", list(shape), dt))

        ident = sb(gs, "ident", (128, 128), BF16)
        ones_bf = sb(gs, "ones_bf", (128, 128), BF16)
        ones_f = sb(gs, "ones_f", (128, 128), F32)
        caus = sb(gs, "caus", (128, 128), F32)
        pv = sb(gs, "pv", (128, 472), F32)
        pvo = PCOL['convw'][0]

        def pvc(name, j0=0, n=None):
            o, nn = PCOL[name]
            o -= pvo
            if n is None:
                n = nn
            return pv[:, o + j0:o + j0 + n]

        with ExitStack() as st:
            iot = sb(st, "iot", (128, 128), I32)
            P.pool(lambda e: e.iota(out=iot[:], pattern=[[1, 128]], base=0, channel_multiplier=-1),
                   w=['iot'])
            P.dve(lambda e: e.tensor_scalar(out=ident[:], in0=iot[:], scalar1=0.0, scalar2=None,
                                            op0=ALU.is_equal), r=['iot'], w=['ident'])
            P.dve(lambda e: e.tensor_scalar(out=caus[:], in0=iot[:], scalar1=0.5, scalar2=NEG,
                                            op0=ALU.is_gt, op1=ALU.mult), r=['iot'], w=['caus'])
            P.dve(lambda e: e.memset(ones_bf[:], 1.0), w=['ones_bf'])
            P.dve(lambda e: e.memset(ones_f[:], 1.0), w=['ones_f'])
            P.dma('sp', pv[:], params[:, pvo:pvo + 472], w=['pv'])
            P.barrier()
            P.emit()

        hst = ExitStack()
        hT = sb(hst, "hT", (128, 8, S), BF16)

        def norm_transpose_phase(st, src, nblk, gname, dstT, tag):
            gbc = sb(st, tag + "gbc", (128, D), F32)
            xt = [sb(st, f"{tag}xt{i}", (128, D), F32) for i in range(2)]
            junk = sb(st, tag + "junk", (128, D), BF16)
            hb = [sb(st, f"{tag}hb{i}", (128, D), BF16) for i in range(2)]
            ss = [sb(st, f"{tag}ss{i}", (128, 1), F32) for i in range(2)]
            rt = [sb(st, f"{tag}rt{i}", (128, 1), F32) for i in range(2)]
            rs = [sb(st, f"{tag}rs{i}", (128, 1), F32) for i in range(2)]
            pt = [ps(st, f"{tag}pt{i}", (128, D), BF16) for i in range(2)]
            o, n = PCOL[gname]
            P.dma('sp', gbc[:], params[:, o:o + n], w=['gbc'])
            for i in range(nblk):
                s = i % 2
                P.dma('sp', xt[s][:], src[i * 128:(i + 1) * 128, :], w=[f'xt{s}'])
                P.dve(lambda e, s=s: e.scalar_tensor_tensor(
                    out=junk[:], in0=xt[s][:], scalar=1.0, in1=xt[s][:], op0=ALU.mult, op1=ALU.mult,
                    accum_out=ss[s][:]), r=[f'xt{s}'], w=['junk', f'ss{s}'])
                P.act(lambda e, s=s: e.activation(out=rt[s][:], in_=ss[s][:], func=AF.Sqrt,
                                                  bias=EPS, scale=1.0 / D),
                      r=[f'ss{s}'], w=[f'rt{s}'])
                P.dve(lambda e, s=s: e.reciprocal(out=rs[s][:], in_=rt[s][:]), r=[f'rt{s}'], w=[f'rs{s}'])
                P.dve(lambda e, s=s: e.scalar_tensor_tensor(
                    out=hb[s][:], in0=xt[s][:], scalar=rs[s][:], in1=gbc[:], op0=ALU.mult, op1=ALU.mult),
                    r=[f'xt{s}', f'rs{s}', 'gbc'], w=[f'hb{s}'])
                for j in range(8):
                    P.pe(lambda e, s=s, j=j: e.transpose(out=pt[s][:, j * 128:(j + 1) * 128],
                                                         in_=hb[s][:, j * 128:(j + 1) * 128],
                                                         identity=ident[:]),
                         r=[f'hb{s}'], w=[f'pt{s}'])
                P.act(lambda e, s=s, i=i: e.activation(
                    out=dstT[:, :, i * 128:(i + 1) * 128],
                    in_=pt[s][:].rearrange("p (j t) -> p j t", j=8), func=AF.Copy),
                    r=[f'pt{s}'], w=[f'dstT{i}'])

        with ExitStack() as st:
            norm_transpose_phase(st, x, NB, 'g1', hT, "A")
            P.barrier()
            P.emit()

        def proj_cm(st, W, ncols, evac, tag, wtile=512):
            wt = [sb(st, f"{tag}wt{i}", (128, 8, wtile), BF16) for i in range(2)]
            pp = [ps(st, f"{tag}pp{i}", (128, 512), F32) for i in range(4)]
            ntile = (ncols + wtile - 1) // wtile
            cnt = 0
            for wi_ in range(ntile):
                s = wi_ % 2
                c0 = wi_ * wtile
                wc = min(wtile, ncols - c0)
                P.dma('pool', wt[s][:, :, 0:wc],
                      W[:, c0:c0 + wc].rearrange("(k p) c -> p k c", p=128), w=[f'wt{s}'])
                for cc in range(wc // 128):
                    c = (c0 // 128) + cc
                    for T in range(NT):
                        b = cnt % 4
                        cnt += 1
                        for k in range(8):
                            P.pe(lambda e, s=s, cc=cc, T=T, k=k, b=b: e.matmul(
                                pp[b][:], lhsT=wt[s][:, k, cc * 128:(cc + 1) * 128],
                                rhs=hT[:, k, T * 512:(T + 1) * 512], start=(k == 0), stop=(k == 7)),
                                r=[f'wt{s}'], w=[f'pp{b}'])
                        evac(c, T, pp[b], f'pp{b}')

        if upto >= 2:
            with ExitStack() as st:
                NDT = 9
                yg = [sb(st, f"yg{i}", (128, 30 + 512), F32) for i in range(3)]
                ygb = [sb(st, f"ygb{i}", (128, 30 + 512), BF16) for i in range(3)]
                sg = [sb(st, f"sg{i}", (128, 512), F32) for i in range(2)]
                acc = [sb(st, f"acc{i}", (128, 512), F32) for i in range(2)]
                yc = [sb(st, f"yc{i}", (128, 512), F32) for i in range(2)]
                dg = [sb(st, f"dg{i}", (128, 31 - NDT, 128), BF16) for i in range(2)]
                wt = [sb(st, f"Bwt{i}", (128, 8, 512), BF16) for i in range(2)]
                pa = [ps(st, f"Bpa{i}", (128, 512), F32) for i in range(2)]
                pg = [ps(st, f"Bpg{i}", (128, 512), F32) for i in range(2)]
                pc = [ps(st, f"Bpc{i}", (128, 512), F32) for i in range(2)]
                n = 0
                for wi_ in range(4):
                    s = wi_ % 2
                    P.dma('pool', wt[s][:], wcm[:, wi_ * 512:(wi_ + 1) * 512].rearrange(
                        "(k p) c -> p k c", p=128), w=[f'wt{s}'])
                    for jj in range(2):
                        j = wi_ * 2 + jj
                        js = j % 2
                        for jn in ([0, 1] if j == 0 else ([j + 1] if j + 1 < 8 else [])):
                            for tap in range(NDT, 31):
                                P.pool(lambda e, jn=jn, tap=tap: e.tensor_scalar(
                                    out=dg[jn % 2][:, tap - NDT, :], in0=ident[:],
                                    scalar1=pvc('convw', jn * 31 + tap, 1), scalar2=0.0,
                                    op0=ALU.mult, op1=ALU.add), w=[f'dg{jn % 2}'])
                        for T in range(NT):
                            b = n % 2
                            s3 = n % 3
                            p3 = (n - 1) % 3
                            for k in range(8):
                                P.pe(lambda e, s=s, jj=jj, T=T, k=k, b=b: e.matmul(
                                    pa[b][:], lhsT=wt[s][:, k, jj * 256:jj * 256 + 128],
                                    rhs=hT[:, k, T * 512:(T + 1) * 512], start=(k == 0), stop=(k == 7)),
                                    r=[f'wt{s}'], w=[f'pa{b}'])
                            for k in range(8):
                                P.pe(lambda e, s=s, jj=jj, T=T, k=k, b=b: e.matmul(
                                    pg[b][:], lhsT=wt[s][:, k, jj * 256 + 128:jj * 256 + 256],
                                    rhs=hT[:, k, T * 512:(T + 1) * 512], start=(k == 0), stop=(k == 7)),
                                    r=[f'wt{s}'], w=[f'pg{b}'])
                            P.act(lambda e, b=b: e.activation(out=sg[b][:], in_=pg[b][:], func=AF.Sigmoid),
                                  r=[f'pg{b}'], w=[f'sg{b}'])
                            if T == 0:
                                P.pool(lambda e, s3=s3: e.memset(yg[s3][:, 0:30], 0.0), w=[f'yg{s3}'])
                                P.pool(lambda e, s3=s3: e.memset(ygb[s3][:, 0:30], 0.0), w=[f'ygb{s3}'])
                            else:
                                P.pool(lambda e, s3=s3, p3=p3: e.tensor_copy(
                                    out=yg[s3][:, 0:30], in_=yg[p3][:, 512:542]),
                                    r=[f'yg{p3}'], w=[f'yg{s3}'])
                                P.pool(lambda e, s3=s3, p3=p3: e.tensor_copy(
                                    out=ygb[s3][:, 0:30], in_=ygb[p3][:, 512:542]),
                                    r=[f'ygb{p3}'], w=[f'ygb{s3}'])
                            P.dve(lambda e, b=b, s3=s3: e.tensor_tensor(
                                out=yg[s3][:, 30:542], in0=pa[b][:], in1=sg[b][:], op=ALU.mult),
                                r=[f'pa{b}', f'sg{b}'], w=[f'yg{s3}'])
                            P.act(lambda e, s3=s3: e.activation(out=ygb[s3][:, 30:542], in_=yg[s3][:, 30:542],
                                                                func=AF.Copy),
                                  r=[f'yg{s3}'], w=[f'ygb{s3}'])
                            for tap in range(NDT, 31):
                                P.pe(lambda e, b=b, s3=s3, js=js, tap=tap: e.matmul(
                                    pc[b][:], lhsT=dg[js][:, tap - NDT, :], rhs=ygb[s3][:, tap:tap + 512],
                                    start=(tap == NDT), stop=(tap == 30)),
                                    r=[f'dg{js}', f'ygb{s3}'], w=[f'pc{b}'])
                            P.dve(lambda e, b=b, s3=s3, j=j: e.tensor_scalar(
                                out=acc[b][:], in0=yg[s3][:, 0:512], scalar1=pvc('convw', j * 31, 1),
                                scalar2=pvc('convb', j, 1), op0=ALU.mult, op1=ALU.add),
                                r=[f'yg{s3}'], w=[f'acc{b}'])
                            for tap in range(1, NDT):
                                P.dve(lambda e, b=b, s3=s3, j=j, tap=tap: e.scalar_tensor_tensor(
                                    out=acc[b][:], in0=yg[s3][:, tap:tap + 512],
                                    scalar=pvc('convw', j * 31 + tap, 1), in1=acc[b][:],
                                    op0=ALU.mult, op1=ALU.add),
                                    r=[f'yg{s3}'], w=[f'acc{b}'])
                            P.dve(lambda e, b=b: e.tensor_tensor(out=yc[b][:], in0=pc[b][:], in1=acc[b][:],
                                                                 op=ALU.add),
                                  r=[f'pc{b}', f'acc{b}'], w=[f'yc{b}'])
                            P.dma('sp', ycT[j * 128:(j + 1) * 128, T * 512:(T + 1) * 512], yc[b][:],
                                  r=[f'yc{b}'], w=['dram'])
                            n += 1
                P.barrier()
                P.emit()

            with ExitStack() as st:
                stg = [sb(st, f"stg{i}", (128, S), BF16) for i in range(2)]

                def evac_gen(c, T, pp, key):
                    c = c + 16
                    if c < 24:
                        dst, row, scale = qT, (c - 16) * 128, 128.0 ** -0.5
                    elif c < 32:
                        dst, row, scale = kT, (c - 24) * 128, 1.0
                    elif c < 40:
                        dst, row, scale = qmT, (c - 32) * 128, 256.0 ** -0.5
                    elif c < 44:
                        dst, row, scale = qiT, (c - 40) * 128, 1.0
                    else:
                        dst, row, scale = kiT, 0, 1.0
                    s2 = c % 2
                    P.act(lambda e: e.activation(out=stg[s2][:, T * 512:(T + 1) * 512], in_=pp[:],
                                                 func=AF.Copy, scale=scale),
                          r=[key], w=[f'stg{s2}'])
                    if T == NT - 1:
                        P.dma('sp', dst[row:row + 128, :], stg[s2][:], r=[f'stg{s2}'], w=['dram'])

                proj_cm(st, wcm[:, 2048:], WCM_COLS - 2048, evac_gen, "Bq")
                P.barrier()
                P.emit()

            with ExitStack() as st:
                stg = [sb(st, f"stg{i}", (128, S), BF16) for i in range(2)]

                def evac_gate(c, T, pp, key):
                    s2 = c % 2
                    P.act(lambda e: e.activation(out=stg[s2][:, T * 512:(T + 1) * 512], in_=pp[:],
                                                 func=AF.Sigmoid, bias=pvc('bgate', c, 1), scale=1.0),
                          r=[key], w=[f'stg{s2}'])
                    if T == NT - 1:
                        P.dma('sp', gT[c * 128:(c + 1) * 128, :], stg[s2][:], r=[f'stg{s2}'], w=['dram'])

                proj_cm(st, wgate, 3 * D, evac_gate, "Bg")
                P.barrier()
                P.emit()

            with ExitStack() as st:
                wv = sb(st, "wv", (128, 8, WTM_COLS), BF16)
                vst = [sb(st, f"vst{i}", (128, D), BF16) for i in range(2)]
                wst = [sb(st, f"wst{i}", (128, 8), F32) for i in range(2)]
                pvv = [ps(st, f"pvv{i}", (128, 512), F32) for i in range(4)]
                pw = [ps(st, f"pw{i}", (128, 8), F32) for i in range(2)]
                for hh in range(2):
                    P.dma('pool', wv[:, :, hh * 512:(hh + 1) * 512],
                          wtm[:, hh * 512:(hh + 1) * 512].rearrange("(k p) c -> p k c", p=128), w=['wv'])
                P.dma('pool', wv[:, :, 1024:1032],
                      wtm[:, 1024:1032].rearrange("(k p) c -> p k c", p=128), w=['wv'])
                for i in range(NB):
                    s = i % 2
                    for hh in range(2):
                        b = (i * 2 + hh) % 4
                        for k in range(8):
                            P.pe(lambda e, i=i, hh=hh, k=k, b=b: e.matmul(
                                pvv[b][:], lhsT=hT[:, k, i * 128:(i + 1) * 128],
                                rhs=wv[:, k, hh * 512:(hh + 1) * 512], start=(k == 0), stop=(k == 7)),
                                r=['wv'], w=[f'pvv{b}'])
                        P.act(lambda e, s=s, hh=hh, b=b: e.activation(
                            out=vst[s][:, hh * 512:(hh + 1) * 512], in_=pvv[b][:], func=AF.Copy),
                            r=[f'pvv{b}'], w=[f'vst{s}'])
                    for k in range(8):
                        P.pe(lambda e, i=i, k=k, s=s: e.matmul(
                            pw[s][:], lhsT=hT[:, k, i * 128:(i + 1) * 128],
                            rhs=wv[:, k, 1024:1032], start=(k == 0), stop=(k == 7)),
                            r=['wv'], w=[f'pw{s}'])
                    P.dve(lambda e, s=s: e.tensor_copy(out=wst[s][:], in_=pw[s][:]),
                          r=[f'pw{s}'], w=[f'wst{s}'])
                    P.dma('sp', vtm[i * 128:(i + 1) * 128, :], vst[s][:], r=[f'vst{s}'], w=['dram'])
                    P.dma('sp', witm[i * 128:(i + 1) * 128, :], wst[s][:], r=[f'wst{s}'], w=['dram'])
                P.barrier()
                P.emit()
        hst.close()

        def TS(T):
            return slice(T * 512, (T + 1) * 512)

        if upto >= 3:
            with ExitStack() as st:
                pwt = sb(st, "pwt", (128, 8, D), BF16)
                for hh in range(2):
                    P.dma('pool', pwt[:, :, hh * 512:(hh + 1) * 512],
                          pw2[:, hh * 512:(hh + 1) * 512].rearrange("(k p) c -> p k c", p=128), w=['pwt'])
                ycl = [sb(st, f"ycl{i}", (128, 8, 512), F32) for i in range(2)]
                sq = sb(st, "sq", (128, 8, 512), F32)
                gl = [sb(st, f"gl{i}", (128, 8, 512), BF16) for i in range(2)]
                mean = sb(st, "mean", (128, 512), F32)
                msq = sb(st, "msq", (128, 512), F32)
                var = sb(st, "var", (128, 512), F32)
                sd = sb(st, "sd", (128, 512), F32)
                rstd = sb(st, "rstd", (128, 512), F32)
                t1 = [sb(st, f"t1{i}", (128, 512), F32) for i in range(2)]
                ys = sb(st, "ys", (128, 8, 512), BF16)
                ot = [sb(st, f"ot{i}", (128, 512), BF16) for i in range(2)]
                s1 = ps(st, "s1", (128, 512))
                s2 = ps(st, "s2", (128, 512))
                po = [ps(st, f"po{i}", (128, 512)) for i in range(2)]
                for T in range(NT):
                    s = T % 2
                    P.dma('sp', ycl[s][:], ycT[:, TS(T)].rearrange("(j p) t -> p j t", p=128), w=[f'ycl{s}'])
                    P.dma('sp', gl[s][:], gT[0:1024, TS(T)].rearrange("(j p) t -> p j t", p=128), w=[f'gl{s}'])
                    P.act(lambda e, s=s: e.activation(out=sq[:], in_=ycl[s][:], func=AF.Square),
                          r=[f'ycl{s}'], w=['sq'])
                    for j in range(8):
                        P.pe(lambda e, s=s, j=j: e.matmul(s1[:], lhsT=ones_f[:], rhs=ycl[s][:, j, :],
                                                          start=(j == 0), stop=(j == 7)),
                             r=[f'ycl{s}', 'mean'], w=['s1'])
                    for j in range(8):
                        P.pe(lambda e, j=j: e.matmul(s2[:], lhsT=ones_f[:], rhs=sq[:, j, :],
                                                     start=(j == 0), stop=(j == 7)), r=['sq'], w=['s2'])
                    P.dve(lambda e: e.tensor_scalar(out=mean[:], in0=s1[:], scalar1=1.0 / D, scalar2=None,
                                                    op0=ALU.mult), r=['s1'], w=['mean'])
                    P.dve(lambda e: e.tensor_tensor(out=msq[:], in0=mean[:], in1=mean[:], op=ALU.mult),
                          r=['mean'], w=['msq'])
                    P.dve(lambda e: e.scalar_tensor_tensor(out=var[:], in0=s2[:], scalar=1.0 / D, in1=msq[:],
                                                           op0=ALU.mult, op1=ALU.subtract),
                          r=['s2', 'msq'], w=['var'])
                    P.act(lambda e: e.activation(out=sd[:], in_=var[:], func=AF.Sqrt, bias=EPS, scale=1.0),
                          r=['var'], w=['sd'])
                    P.dve(lambda e: e.reciprocal(out=rstd[:], in_=sd[:]), r=['sd'], w=['rstd'])
                    for j in range(8):
                        b = j % 2
                        P.dve(lambda e, s=s, j=j, b=b: e.tensor_tensor(
                            out=t1[b][:], in0=ycl[s][:, j, :], in1=mean[:], op=ALU.subtract),
                            r=[f'ycl{s}', 'mean'], w=[f't1{b}'])
                        P.dve(lambda e, b=b: e.tensor_tensor(out=t1[b][:], in0=t1[b][:], in1=rstd[:], op=ALU.mult),
                              r=['rstd'], w=[f't1{b}'])
                        P.act(lambda e, j=j, b=b: e.activation(out=ys[:, j, :], in_=t1[b][:], func=AF.Silu,
                                                               scale=pvc('lng', j, 1), bias=pvc('lnb', j, 1)),
                              r=[f't1{b}'], w=['ys'])
                    for dch in range(8):
                        b = dch % 2
                        for j in range(8):
                            P.pe(lambda e, dch=dch, j=j, b=b: e.matmul(
                                po[b][:], lhsT=pwt[:, j, dch * 128:(dch + 1) * 128], rhs=ys[:, j, :],
                                start=(j == 0), stop=(j == 7)), r=['ys', 'pwt'], w=[f'po{b}'])
                        P.dve(lambda e, s=s, dch=dch, b=b: e.tensor_tensor(
                            out=ot[b][:], in0=po[b][:], in1=gl[s][:, dch, :], op=ALU.mult),
                            r=[f'po{b}', f'gl{s}'], w=[f'ot{b}'])
                        P.dma('pool', mconvT[dch * 128:(dch + 1) * 128, TS(T)], ot[b][:], r=[f'ot{b}'], w=['dram'])
                P.barrier()
                P.emit()

        if upto >= 4:
            with ExitStack() as st0:
                mnT = sb(st0, "mnT", (128, 8, 256), BF16)
                mkT = sb(st0, "mkT", (128, 8, 256), BF16)
                mv = sb(st0, "mv", (128, 2, D), BF16)
                with ExitStack() as st:
                    norm_transpose_phase(st, mem, 2, 'gmem', mnT, "D")
                    P.barrier()
                    P.emit()
                with ExitStack() as st:
                    wk = [sb(st, f"wk{i}", (128, 8, 512), BF16) for i in range(2)]
                    pk = [ps(st, f"pk{i}", (128, 512)) for i in range(2)]
                    pmv = [ps(st, f"pmv{i}", (128, 512)) for i in range(2)]
                    for wi_ in range(2):
                        s = wi_ % 2
                        P.dma('pool', wk[s][:], wmkv[:, wi_ * 512:(wi_ + 1) * 512].rearrange(
                            "(k p) c -> p k c", p=128), w=[f'wk{s}'])
                        for cc in range(4):
                            c = wi_ * 4 + cc
                            b = c % 2
                            for k in range(8):
                                P.pe(lambda e, s=s, cc=cc, k=k, b=b: e.matmul(
                                    pk[b][:, 0:256], lhsT=wk[s][:, k, cc * 128:(cc + 1) * 128],
                                    rhs=mnT[:, k, :], start=(k == 0), stop=(k == 7)),
                                    r=[f'wk{s}'], w=[f'pk{b}'])
                            P.act(lambda e, c=c, b=b: e.activation(out=mkT[:, c, :], in_=pk[b][:, 0:256],
                                                                   func=AF.Copy), r=[f'pk{b}'], w=['mkT'])
                    for wi_ in range(2):
                        s = wi_ % 2
                        P.dma('pool', wk[s][:], wmkv[:, 1024 + wi_ * 512:1024 + (wi_ + 1) * 512].rearrange(
                            "(k p) c -> p k c", p=128), w=[f'wk{s}'])
                        for mc in range(2):
                            for k in range(8):
                                P.pe(lambda e, s=s, mc=mc, k=k: e.matmul(
                                    pmv[mc][:], lhsT=mnT[:, k, mc * 128:(mc + 1) * 128], rhs=wk[s][:, k, :],
                                    start=(k == 0), stop=(k == 7)), r=[f'wk{s}'], w=[f'pmv{mc}'])
                            P.act(lambda e, wi_=wi_, mc=mc: e.activation(
                                out=mv[:, mc, wi_ * 512:(wi_ + 1) * 512], in_=pmv[mc][:], func=AF.Copy),
                                r=[f'pmv{mc}'], w=['mv'])
                    P.barrier()
                    P.emit()
                with ExitStack() as st:
                    qml = [sb(st, f"qml{i}", (128, 2, 512), BF16) for i in range(2)]
                    gl = [sb(st, f"gl{i}", (128, 2, 512), BF16) for i in range(2)]
                    pT = [sb(st, f"pT{i}", (128, 2, 512), BF16) for i in range(2)]
                    rinv = sb(st, "rinv", (128, 512), F32)
                    o1 = [sb(st, f"o1{i}", (128, 512), F32) for i in range(2)]
                    ot = [sb(st, f"ot{i}", (128, 512), BF16) for i in range(2)]
                    pl = [ps(st, f"pl{i}", (128, 512)) for i in range(2)]
                    po = [ps(st, f"po{i}", (128, 512)) for i in range(2)]
                    pr = ps(st, "pr", (128, 512))
                    n = 0
                    for hm in range(4):
                        for T in range(NT):
                            s = n % 2
                            n += 1
                            P.dma('sp', qml[s][:], qmT[hm * 256:(hm + 1) * 256, TS(T)].rearrange(
                                "(c p) t -> p c t", p=128), w=[f'qml{s}'])
                            P.dma('sp', gl[s][:], gT[2048 + hm * 256:2048 + (hm + 1) * 256, TS(T)].rearrange(
                                "(c p) t -> p c t", p=128), w=[f'gl{s}'])
                            for mc in range(2):
                                for dmc in range(2):
                                    P.pe(lambda e, s=s, hm=hm, mc=mc, dmc=dmc: e.matmul(
                                        pl[mc][:], lhsT=mkT[:, hm * 2 + dmc, mc * 128:(mc + 1) * 128],
                                        rhs=qml[s][:, dmc, :], start=(dmc == 0), stop=(dmc == 1)),
                                        r=[f'qml{s}'], w=[f'pl{mc}'])
                                P.act(lambda e, s=s, mc=mc: e.activation(out=pT[s][:, mc, :], in_=pl[mc][:],
                                                                         func=AF.Exp),
                                      r=[f'pl{mc}'], w=[f'pT{s}'])
                            for dmc in range(2):
                                for mc in range(2):
                                    P.pe(lambda e, s=s, hm=hm, mc=mc, dmc=dmc: e.matmul(
                                        po[dmc][:], lhsT=mv[:, mc, hm * 256 + dmc * 128:hm * 256 + (dmc + 1) * 128],
                                        rhs=pT[s][:, mc, :], start=(mc == 0), stop=(mc == 1)),
                                        r=[f'pT{s}'], w=[f'po{dmc}'])
                            for mc in range(2):
                                P.pe(lambda e, s=s, mc=mc: e.matmul(pr[:], lhsT=ones_bf[:], rhs=pT[s][:, mc, :],
                                                                    start=(mc == 0), stop=(mc == 1)),
                                     r=[f'pT{s}'], w=['pr'])
                            P.act(lambda e: e.activation(out=o1[0][:], in_=pr[:], func=AF.Ln), r=['pr'], w=['o10'])
                            P.act(lambda e: e.activation(out=rinv[:], in_=o1[0][:], func=AF.Exp, scale=-1.0),
                                  r=['o10'], w=['rinv'])
                            for dmc in range(2):
                                P.dve(lambda e, dmc=dmc: e.tensor_tensor(out=o1[dmc][:], in0=po[dmc][:], in1=rinv[:],
                                                                         op=ALU.mult),
                                      r=[f'po{dmc}', 'rinv'], w=[f'o1{dmc}'])
                                P.dve(lambda e, s=s, dmc=dmc: e.tensor_tensor(
                                    out=ot[dmc][:], in0=o1[dmc][:], in1=gl[s][:, dmc, :], op=ALU.mult),
                                    r=[f'o1{dmc}', f'gl{s}'], w=[f'ot{dmc}'])
                                P.dma('pool', mmemT[hm * 256 + dmc * 128:hm * 256 + (dmc + 1) * 128, TS(T)],
                                      ot[dmc][:], r=[f'ot{dmc}'], w=['dram'])
                    P.barrier()
                    P.emit()

        if upto >= 5:
            with ExitStack() as st:
                kil = sb(st, "kil", (128, S), BF16)
                qil = [sb(st, f"qil{i}", (128, 4, 128), BF16) for i in range(2)]
                wil = [sb(st, f"wil{i}", (128, 8), F32) for i in range(2)]
                score = [sb(st, f"score{i}", (128, S), F32) for i in range(4)]
                rl = [sb(st, f"rl{i}", (128, 512), F32) for i in range(3)]
                junk = sb(st, "junkE", (128, S), BF16)
                junkD = sb(st, "junkDE", (128, S), BF16)
                mk = [sb(st, f"mk{i}", (128, S), BF16) for i in range(2)]
                mT = [sb(st, f"mT{i}", (128, 8, 128), BF16) for i in range(2)]
                ptab = sb(st, "ptab", (128, NIT + 1), F32)
                thrc = sb(st, "thrc", (128, 1), F32)

                def two(name, shape):
                    return [sb(st, f"{name}{c}", shape, F32) for c in range(2)]
                lo = two("lo", (128, 1))
                hi = two("hi", (128, 1))
                Wd = two("Wd", (128, 1))
                hneg = two("hneg", (128, NIT + 1))
                nm = two("nm", (128, 1))
                ssum = two("ssum", (128, 1))
                sgn = two("sgn", (128, 1))
                thr_t = two("thr_t", (128, 1))
                midp = two("midp", (128, 1))
                cntd = two("cntd", (128, 1))
                t2 = two("t2", (128, 1))
                pd = [ps(st, f"pd{i}", (128, 512)) for i in range(4)]
                ptm = [ps(st, f"ptm{i}", (128, 1024), BF16) for i in range(2)]
                for n in range(NIT + 1):
                    P.pool(lambda e, n=n: e.memset(ptab[:, n:n + 1], -(2.0 ** -(n + 1))), w=['ptab'])
                P.pool(lambda e: e.memset(thrc[:], -1.0e29), w=['thrc'])
                P.dma('sp', kil[:], kiT[:, :], w=['kil'])
                cnts = {'c': 0, 't': 0, 'q': 0}
                DVE_IT = ((1, 5, 9, 13), (3, 7, 11, 15))

                def score_units(i):
                    s = i % 4
                    L = (i + 1) * 128
                    units = []
                    qs = cnts['q'] % 2
                    cnts['q'] += 1

                    def loads():
                        P.dma('sp', qil[qs][:], qiT[:, i * 128:(i + 1) * 128].rearrange("(c p) t -> p c t", p=128),
                              w=[f'qil{qs}'])
                        P.dma('sp', wil[qs][:], witm[i * 128:(i + 1) * 128, :], w=[f'wil{qs}'])
                    units.append(loads)
                    nst = (L + 511) // 512
                    for stile in range(nst):
                        w_ = min(512, L - stile * 512)
                        c0 = stile * 512
                        for h in range(8):
                            def unit(stile=stile, w_=w_, c0=c0, h=h):
                                p2, half = divmod(h, 2)
                                b = cnts['c'] % 4
                                r3 = cnts['c'] % 3
                                cnts['c'] += 1
                                P.pe(lambda e: e.matmul(
                                    pd[b][:, 0:w_], lhsT=qil[qs][half * 64:(half + 1) * 64, p2, :],
                                    rhs=kil[half * 64:(half + 1) * 64, c0:c0 + w_], start=True, stop=True),
                                    r=[f'qil{qs}', 'kil'], w=[f'pd{b}'])
                                P.act(lambda e: e.activation(
                                    out=rl[r3][:, 0:w_], in_=pd[b][:, 0:w_], func=AF.Relu),
                                    r=[f'pd{b}'], w=[f'rl{r3}'])
                                if h == 0:
                                    P.dve(lambda e: e.tensor_scalar(
                                        out=score[s][:, c0:c0 + w_], in0=rl[r3][:, 0:w_], scalar1=wil[qs][:, 0:1],
                                        scalar2=None, op0=ALU.mult),
                                        r=[f'rl{r3}', f'wil{qs}'], w=[f'score{s}'])
                                else:
                                    P.dve(lambda e: e.scalar_tensor_tensor(
                                        out=score[s][:, c0:c0 + w_], in0=rl[r3][:, 0:w_], scalar=wil[qs][:, h:h + 1],
                                        in1=score[s][:, c0:c0 + w_], op0=ALU.mult, op1=ALU.add),
                                        r=[f'rl{r3}', f'wil{qs}'], w=[f'score{s}'])
                            units.append(unit)

                    def fin():
                        P.dve(lambda e: e.tensor_tensor(
                            out=score[s][:, i * 128:(i + 1) * 128], in0=score[s][:, i * 128:(i + 1) * 128],
                            in1=caus[:], op=ALU.add), w=[f'score{s}'])
                    units.append(fin)
                    return units

                def bisect_ops(i, c):
                    s = i % 4
                    L = (i + 1) * 128
                    sk = f'score{s}'
                    K = lambda nme: f'{nme}{c}'
                    units = []
                    if i >= 2:
                        units.append([
                            lambda: P.dve(lambda e: e.tensor_reduce(out=lo[c][:], in_=score[s][:, 0:i * 128],
                                                                    axis=AX.X, op=ALU.min), r=[sk], w=[K('lo')]),
                            lambda: P.dve(lambda e: e.tensor_reduce(out=hi[c][:], in_=score[s][:, 0:L],
                                                                    axis=AX.X, op=ALU.max), r=[sk], w=[K('hi')]),
                            lambda: P.dve(lambda e: e.tensor_tensor(out=Wd[c][:], in0=hi[c][:], in1=lo[c][:],
                                                                    op=ALU.subtract),
                                          r=[K('hi'), K('lo')], w=[K('Wd')]),
                            lambda: P.dve(lambda e: e.tensor_scalar(out=hneg[c][:], in0=ptab[:], scalar1=Wd[c][:, 0:1],
                                                                    scalar2=None, op0=ALU.mult),
                                          r=[K('Wd'), 'ptab'], w=[K('hneg')]),
                            lambda: P.dve(lambda e: e.tensor_scalar(out=nm[c][:], in0=lo[c][:], scalar1=-1.0,
                                                                    scalar2=hneg[c][:, 0:1], op0=ALU.mult,
                                                                    op1=ALU.add),
                                          r=[K('lo'), K('hneg')], w=[K('nm')]),
                        ])
                        for n in range(NIT):
                            if n in DVE_IT[c]:
                                units.append([
                                    lambda: P.dve(lambda e: e.tensor_scalar(
                                        out=midp[c][:], in0=nm[c][:], scalar1=-1.0, scalar2=None, op0=ALU.mult),
                                        r=[K('nm')], w=[K('midp')]),
                                    lambda: P.dve(lambda e: e.tensor_scalar(
                                        out=junkD[:, 0:L], in0=score[s][:, 0:L], scalar1=midp[c][:, 0:1],
                                        scalar2=None, op0=ALU.is_ge, op1=ALU.add, accum_out=cntd[c][:]),
                                        r=[sk, K('midp')], w=[K('cntd')]),
                                    lambda: P.dve(lambda e: e.tensor_scalar(
                                        out=t2[c][:], in0=cntd[c][:], scalar1=TOPK - 0.5, scalar2=2.0,
                                        op0=ALU.is_ge, op1=ALU.mult), r=[K('cntd')], w=[K('t2')]),
                                    lambda n=n: P.dve(lambda e: e.tensor_scalar(
                                        out=t2[c][:], in0=t2[c][:], scalar1=-1.0, scalar2=hneg[c][:, n + 1:n + 2],
                                        op0=ALU.add, op1=ALU.mult), r=[K('hneg')], w=[K('t2')]),
                                    lambda: P.dve(lambda e: e.tensor_tensor(
                                        out=nm[c][:], in0=nm[c][:], in1=t2[c][:], op=ALU.add),
                                        r=[K('t2')], w=[K('nm')]),
                                ])
                            else:
                                units.append([
                                    lambda: P.act(lambda e: e.activation(
                                        out=junk[:, 0:L], in_=score[s][:, 0:L], func=AF.Sign, bias=nm[c][:, 0:1],
                                        scale=1.0, accum_out=ssum[c][:]), r=[sk, K('nm')], w=[K('ssum')]),
                                    lambda: P.act(lambda e: e.activation(
                                        out=sgn[c][:], in_=ssum[c][:], func=AF.Sign,
                                        bias=float(L - 2 * TOPK) + 0.5, scale=1.0), r=[K('ssum')], w=[K('sgn')]),
                                    lambda n=n: P.act(lambda e: e.activation(
                                        out=nm[c][:], in_=sgn[c][:], func=AF.Identity,
                                        scale=hneg[c][:, n + 1:n + 2], bias=nm[c][:, 0:1]),
                                        r=[K('sgn'), K('hneg')], w=[K('nm')]),
                                ])

                    def epi():
                        if i >= 2:
                            P.dve(lambda e: e.tensor_scalar(out=thr_t[c][:], in0=nm[c][:], scalar1=-1.0,
                                                            scalar2=hneg[c][:, NIT:NIT + 1], op0=ALU.mult,
                                                            op1=ALU.add),
                                  r=[K('nm'), K('hneg')], w=[K('thr_t')])
                            thr, thrk = thr_t[c], K('thr_t')
                        else:
                            thr, thrk = thrc, 'thrc'
                        P.dve(lambda e: e.tensor_scalar(
                            out=mk[c][:, 0:L], in0=score[s][:, 0:L], scalar1=thr[:, 0:1], scalar2=None,
                            op0=ALU.is_ge), r=[sk, thrk], w=[K('mk')])
                        for g0 in range(0, i + 1, 8):
                            nb_ = min(8, i + 1 - g0)
                            b = cnts['t'] % 2
                            cnts['t'] += 1
                            for q_ in range(nb_):
                                P.pe(lambda e, b=b, q_=q_, g0=g0: e.transpose(
                                    out=ptm[b][:, q_ * 128:(q_ + 1) * 128],
                                    in_=mk[c][:, (g0 + q_) * 128:(g0 + q_ + 1) * 128], identity=ident[:]),
                                    r=[K('mk')], w=[f'ptm{b}'])
                            P.act(lambda e, b=b, nb_=nb_: e.activation(
                                out=mT[b][:, 0:nb_, :],
                                in_=ptm[b][:, 0:nb_ * 128].rearrange("p (q t) -> p q t", q=nb_), func=AF.Copy),
                                r=[f'ptm{b}'], w=[f'mT{b}'])
                            P.dma('pool', maskT[g0 * 128:(g0 + nb_) * 128, i * 128:(i + 1) * 128].rearrange(
                                "(q p) t -> p q t", p=128), mT[b][:, 0:nb_, :], r=[f'mT{b}'], w=['dram'])
                    units.append([epi])
                    return units

                def run_units(us):
                    for u in us:
                        u()

                run_units(score_units(0))
                run_units(score_units(1))
                for pidx in range(NB // 2):
                    ia, ib = 2 * pidx, 2 * pidx + 1
                    A_ = bisect_ops(ia, 0)
                    B_ = bisect_ops(ib, 1)
                    S_ = []
                    if ia + 2 < NB:
                        s1, s2 = score_units(ia + 2), score_units(ib + 2)
                        for k_ in range(max(len(s1), len(s2))):
                            if k_ < len(s1):
                                S_.append(s1[k_])
                            if k_ < len(s2):
                                S_.append(s2[k_])
                    nun = max(len(A_), len(B_))
                    done = 0
                    for idx in range(nun):
                        ua = A_[idx] if idx < len(A_) else []
                        ub = B_[idx] if idx < len(B_) else []
                        for k_ in range(max(len(ua), len(ub))):
                            if k_ < len(ua):
                                ua[k_]()
                            if k_ < len(ub):
                                ub[k_]()
                        tgt = (len(S_) * (idx + 1)) // nun
                        while done < tgt:
                            S_[done]()
                            done += 1
                P.barrier()
                P.emit()

        if upto >= 6:
            with ExitStack() as st:
                ql = [sb(st, f"ql{i}", (128, S), BF16) for i in range(2)]
                kl = [sb(st, f"kl{i}", (128, S), BF16) for i in range(2)]
                vl = [sb(st, f"vl{i}", (128, NB, 128), BF16) for i in range(2)]
                NML = 6
                ml = [sb(st, f"ml{i}", (128, 512), BF16) for i in range(NML)]
                ex = [sb(st, f"ex{i}", (128, 512), BF16) for i in range(4)]
                pm = [sb(st, f"pm{i}", (128, 512), BF16) for i in range(4)]
                gl = [sb(st, f"gl{i}", (128, 512), BF16) for i in range(2)]
                rinv = sb(st, "rinv", (128, 512), F32)
                o1 = sb(st, "o1", (128, 512), F32)
                ot = [sb(st, f"ot{i}", (128, 512), BF16) for i in range(2)]
                psc = [ps(st, f"psc{i}", (128, 512)) for i in range(4)]
                po = [ps(st, f"po{i}", (128, 512)) for i in range(2)]
                pr = [ps(st, f"pr{i}", (128, 512)) for i in range(2)]
                its = []
                for h in range(8):
                    for T in range(NT):
                        nsb = 4 * T + 4
                        for sbk in range(nsb):
                            its.append((h, T, sbk, nsb))
                DEPTH = 3

                def front(n):
                    h, T, sbk, nsb = its[n]
                    hs = h % 2
                    a = (h * NT + T) % 2
                    if T == 0 and sbk == 0:
                        P.dma('sp', ql[hs][:], qT[h * 128:(h + 1) * 128, :], w=[f'ql{hs}'])
                        P.dma('sp', kl[hs][:], kT[h * 128:(h + 1) * 128, :], w=[f'kl{hs}'])
                        P.dma('sp', vl[hs][:], vtm[:, h * 128:(h + 1) * 128].rearrange("(b p) d -> p b d", p=128),
                              w=[f'vl{hs}'])
                    if sbk == 0:
                        P.dma('sp', gl[a][:], gT[1024 + h * 128:1024 + (h + 1) * 128, TS(T)], w=[f'gl{a}'])
                    m4 = n % NML
                    b3 = n % 4
                    r0 = max(0, sbk - 4 * T) * 128
                    t0 = T * 512 + r0
                    P.dma('sp', ml[m4][:, r0:512], maskT[sbk * 128:(sbk + 1) * 128, t0:(T + 1) * 512],
                          w=[f'ml{m4}'])
                    P.pe(lambda e: e.matmul(
                        psc[b3][:, r0:512], lhsT=kl[hs][:, sbk * 128:(sbk + 1) * 128],
                        rhs=ql[hs][:, t0:(T + 1) * 512], start=True, stop=True),
                        r=[f'kl{hs}', f'ql{hs}'], w=[f'psc{b3}'])
                    P.act(lambda e: e.activation(out=ex[b3][:, r0:512], in_=psc[b3][:, r0:512], func=AF.Exp),
                          r=[f'psc{b3}'], w=[f'ex{b3}'])
                    P.dve(lambda e: e.tensor_tensor(
                        out=pm[b3][:, r0:512], in0=ex[b3][:, r0:512], in1=ml[m4][:, r0:512], op=ALU.mult),
                        r=[f'ex{b3}', f'ml{m4}'], w=[f'pm{b3}'])

                def back(n):
                    h, T, sbk, nsb = its[n]
                    hs = h % 2
                    a = (h * NT + T) % 2
                    b3 = n % 4
                    r0 = max(0, sbk - 4 * T) * 128
                    P.pe(lambda e: e.matmul(
                        po[a][:, r0:512], lhsT=vl[hs][:, sbk, :], rhs=pm[b3][:, r0:512],
                        start=(sbk == 0), stop=(sbk == nsb - 1)),
                        r=[f'pm{b3}', f'vl{hs}'], w=[f'po{a}'])
                    P.pe(lambda e: e.matmul(
                        pr[a][:, r0:512], lhsT=ones_bf[:], rhs=pm[b3][:, r0:512],
                        start=(sbk == 0), stop=(sbk == nsb - 1)),
                        r=[f'pm{b3}'], w=[f'pr{a}'])
                    if sbk == nsb - 1:
                        P.act(lambda e: e.activation(out=o1[:], in_=pr[a][:], func=AF.Ln), r=[f'pr{a}'], w=['o1'])
                        P.act(lambda e: e.activation(out=rinv[:], in_=o1[:], func=AF.Exp, scale=-1.0),
                              r=['o1'], w=['rinv'])
                        P.dve(lambda e: e.tensor_tensor(out=o1[:], in0=po[a][:], in1=rinv[:], op=ALU.mult),
                              r=[f'po{a}', 'rinv'], w=['o1'])
                        P.dve(lambda e: e.tensor_tensor(out=ot[a][:], in0=o1[:], in1=gl[a][:], op=ALU.mult),
                              r=['o1', f'gl{a}'], w=[f'ot{a}'])
                        P.dma('pool', mattT[h * 128:(h + 1) * 128, TS(T)], ot[a][:], r=[f'ot{a}'], w=['dram'])

                for n in range(len(its) + DEPTH):
                    if n < len(its):
                        front(n)
                    if n >= DEPTH:
                        back(n - DEPTH)
                P.barrier()
                P.emit()

        def out_norm_phase(st, nk, load_lhs, wres, gname, resid, dst, tag, post=None):
            gp = sb(st, tag + "gp", (128, D), F32)
            o, n_ = PCOL[gname]
            P.dma('sp', gp[:], params[:, o:o + n_], w=['gp'])
            xt = [sb(st, f"{tag}xt{i}", (128, D), F32) for i in range(2)]
            xo = [sb(st, f"{tag}xo{i}", (128, D), F32) for i in range(2)]
            tmp = sb(st, tag + "tmp", (128, D), F32)
            junkA = sb(st, tag + "junkA", (128, 512), BF16)
            ssh = [sb(st, f"{tag}ssh{i}", (128, 2), F32) for i in range(2)]
            ss = [sb(st, f"{tag}ss{i}", (128, 1), F32) for i in range(2)]
            rt = [sb(st, f"{tag}rt{i}", (128, 1), F32) for i in range(2)]
            rs = [sb(st, f"{tag}rs{i}", (128, 1), F32) for i in range(2)]
            po = [ps(st, f"{tag}po{i}", (128, 512)) for i in range(4)]
            cache = {}
            nblk = NT * 4

            def S1(i):
                T, tb = divmod(i, 4)
                s2 = i % 2
                if T not in cache:
                    cache[T] = load_lhs(T)
                if tb == 1 and T + 1 < NT and (T + 1) not in cache:
                    cache[T + 1] = load_lhs(T + 1)
                lhs, lkey = cache[T]
                P.dma('sp', xt[s2][:], resid[i * 128:(i + 1) * 128, :], w=[f'xt{s2}'])
                for hh in range(2):
                    b = s2 * 2 + hh
                    for j in range(nk):
                        P.pe(lambda e, lhs=lhs, j=j, tb=tb, hh=hh, b=b: e.matmul(
                            po[b][:], lhsT=lhs[:, j, tb * 128:(tb + 1) * 128],
                            rhs=wres[:, j, hh * 512:(hh + 1) * 512], start=(j == 0), stop=(j == nk - 1)),
                            r=[lkey, 'wres'], w=[f'po{b}'])
                    P.act(lambda e, b=b, s2=s2, hh=hh: e.activation(
                        out=junkA[:], in_=po[b][:], func=AF.Square, accum_out=ssh[s2][:, hh:hh + 1]),
                        r=[f'po{b}'], w=[f'ssh{s2}'])

            def S2(i):
                s2 = i % 2
                P.dve(lambda e: e.tensor_tensor(out=ss[s2][:], in0=ssh[s2][:, 0:1], in1=ssh[s2][:, 1:2],
                                                op=ALU.add), r=[f'ssh{s2}'], w=[f'ss{s2}'])
                P.act(lambda e: e.activation(out=rt[s2][:], in_=ss[s2][:], func=AF.Sqrt, bias=EPS,
                                             scale=1.0 / D), r=[f'ss{s2}'], w=[f'rt{s2}'])
                P.dve(lambda e: e.reciprocal(out=rs[s2][:], in_=rt[s2][:]), r=[f'rt{s2}'], w=[f'rs{s2}'])
                for hh in range(2):
                    b = s2 * 2 + hh
                    P.dve(lambda e, b=b, hh=hh: e.scalar_tensor_tensor(
                        out=tmp[:, hh * 512:(hh + 1) * 512], in0=po[b][:], scalar=rs[s2][:, 0:1],
                        in1=gp[:, hh * 512:(hh + 1) * 512], op0=ALU.mult, op1=ALU.mult),
                        r=[f'po{b}', f'rs{s2}', 'gp'], w=['tmp'])
                P.dve(lambda e: e.tensor_tensor(out=xo[s2][:], in0=tmp[:], in1=xt[s2][:], op=ALU.add),
                      r=['tmp', f'xt{s2}'], w=[f'xo{s2}'])
                P.dma('pool', dst[i * 128:(i + 1) * 128, :], xo[s2][:], r=[f'xo{s2}'], w=['dram'])
                if post is not None:
                    post[0](i, s2, xo[s2], f'xo{s2}')

            def S3(i):
                s2 = i % 2
                if post is not None:
                    post[1](i, s2, xo[s2], f'xo{s2}')

            for step in range(nblk + 2):
                if step < nblk:
                    S1(step)
                if 1 <= step <= nblk:
                    S2(step - 1)
                if 2 <= step:
                    S3(step - 2)

        h2st = ExitStack()
        if upto >= 7:
            h2T = sb(h2st, "h2T", (128, 8, S), BF16)
            with ExitStack() as st:
                wo = sb(st, "wo", (128, 8, D), BF16)
                for hh in range(2):
                    P.dma('pool', wo[:, :, hh * 512:(hh + 1) * 512],
                          wout[:, hh * 512:(hh + 1) * 512].rearrange("(k p) c -> p k c", p=128), w=['wres'])
                g2 = sb(st, "g2bc", (128, D), F32)
                o, n_ = PCOL['g2']
                P.dma('sp', g2[:], params[:, o:o + n_], w=['g2'])
                ma = [sb(st, f"ma{i}", (128, 8, 512), BF16) for i in range(2)]
                mb = [sb(st, f"mb{i}", (128, 8, 512), BF16) for i in range(2)]
                mc_ = [sb(st, f"mc{i}", (128, 8, 512), BF16) for i in range(2)]
                mg = [sb(st, f"mg{i}", (128, 8, 512), BF16) for i in range(2)]
                junkD = sb(st, "junkD", (128, D), BF16)
                hb = [sb(st, f"Ghb{i}", (128, D), BF16) for i in range(2)]
                ss2 = [sb(st, f"Gss2{i}", (128, 1), F32) for i in range(2)]
                rt2 = [sb(st, f"Grt2{i}", (128, 1), F32) for i in range(2)]
                rs2 = [sb(st, f"Grs2{i}", (128, 1), F32) for i in range(2)]
                pt = [ps(st, f"Gpt{i}", (128, D), BF16) for i in range(2)]

                def load_merged(T):
                    s = T % 2
                    for (buf, src, nm) in ((ma, mconvT, 'ma'), (mb, mattT, 'mb'), (mc_, mmemT, 'mc')):
                        P.dma('sp', buf[s][:], src[:, TS(T)].rearrange("(j p) t -> p j t", p=128), w=[f'{nm}{s}'])
                    P.dve(lambda e, s=s: e.tensor_tensor(out=mg[s][:], in0=ma[s][:], in1=mb[s][:], op=ALU.add),
                          r=[f'ma{s}', f'mb{s}'], w=[f'mg{s}'])
                    P.dve(lambda e, s=s: e.tensor_tensor(out=mg[s][:], in0=mg[s][:], in1=mc_[s][:], op=ALU.add),
                          r=[f'mc{s}'], w=[f'mg{s}'])
                    return mg[s], f'mg{s}'

                def post_h2a(i, s2, xo, xkey):
                    P.dve(lambda e, s2=s2: e.scalar_tensor_tensor(
                        out=junkD[:], in0=xo[:], scalar=1.0, in1=xo[:], op0=ALU.mult, op1=ALU.mult,
                        accum_out=ss2[s2][:]), r=[xkey], w=[f'ss2{s2}'])

                def post_h2b(i, s2, xo, xkey):
                    P.act(lambda e, s2=s2: e.activation(out=rt2[s2][:], in_=ss2[s2][:], func=AF.Sqrt, bias=EPS,
                                                        scale=1.0 / D), r=[f'ss2{s2}'], w=[f'rt2{s2}'])
                    P.dve(lambda e, s2=s2: e.reciprocal(out=rs2[s2][:], in_=rt2[s2][:]),
                          r=[f'rt2{s2}'], w=[f'rs2{s2}'])
                    P.dve(lambda e, s2=s2: e.scalar_tensor_tensor(
                        out=hb[s2][:], in0=xo[:], scalar=rs2[s2][:, 0:1], in1=g2[:], op0=ALU.mult, op1=ALU.mult),
                        r=[xkey, f'rs2{s2}', 'g2'], w=[f'hb{s2}'])
                    for j in range(8):
                        P.pe(lambda e, s2=s2, j=j: e.transpose(out=pt[s2][:, j * 128:(j + 1) * 128],
                                                               in_=hb[s2][:, j * 128:(j + 1) * 128],
                                                               identity=ident[:]),
                             r=[f'hb{s2}'], w=[f'pt{s2}'])
                    P.act(lambda e, s2=s2, i=i: e.activation(
                        out=h2T[:, :, i * 128:(i + 1) * 128],
                        in_=pt[s2][:].rearrange("p (j t) -> p j t", j=8), func=AF.Copy),
                        r=[f'pt{s2}'], w=[f'h2T{i}'])

                out_norm_phase(st, 8, load_merged, wo, 'gpost1', x, x1d, "G", post=(post_h2a, post_h2b))
                P.barrier()
                P.emit()

        if upto >= 8:
            with ExitStack() as st:
                wt = [sb(st, f"Hwt{i}", (128, 8, 512), BF16) for i in range(2)]
                ug = [sb(st, f"ug{i}", (128, 514), F32) for i in range(3)]
                uvv = [sb(st, f"uv{i}", (128, 514), F32) for i in range(3)]
                cg = [sb(st, f"cg{i}", (128, 512), F32) for i in range(3)]
                cv = [sb(st, f"cv{i}", (128, 512), F32) for i in range(3)]
                sgl = [sb(st, f"sgl{i}", (128, 512), F32) for i in range(3)]
                pend = []
                ast = [sb(st, f"ast{i}", (128, S), BF16) for i in range(2)]
                pg = [ps(st, f"Hpg{i}", (128, 512)) for i in range(2)]
                pvv = [ps(st, f"Hpv{i}", (128, 512)) for i in range(2)]

                def fw(c, tap):
                    return pvc('ffnw', c * 3 + tap, 1)

                n = 0
                for wi_ in range(11):
                    s = wi_ % 2
                    P.dma('pool', wt[s][:], wup[:, wi_ * 512:(wi_ + 1) * 512].rearrange(
                        "(k p) c -> p k c", p=128), w=[f'wt{s}'])
                    for jj in range(2):
                        j = wi_ * 2 + jj
                        a2 = j % 2
                        for T in range(NT):
                            b = n % 2
                            s3 = n % 3
                            p3 = (n - 1) % 3
                            b3 = n % 3
                            n += 1
                            for k in range(8):
                                P.pe(lambda e, s=s, jj=jj, T=T, k=k, b=b: e.matmul(
                                    pg[b][:], lhsT=wt[s][:, k, jj * 256:jj * 256 + 128],
                                    rhs=h2T[:, k, TS(T)], start=(k == 0), stop=(k == 7)),
                                    r=[f'wt{s}'], w=[f'pg{b}'])
                            for k in range(8):
                                P.pe(lambda e, s=s, jj=jj, T=T, k=k, b=b: e.matmul(
                                    pvv[b][:], lhsT=wt[s][:, k, jj * 256 + 128:jj * 256 + 256],
                                    rhs=h2T[:, k, TS(T)], start=(k == 0), stop=(k == 7)),
                                    r=[f'wt{s}'], w=[f'pvv{b}'])
                            for (u_, nm_) in ((ug, 'ug'), (uvv, 'uv')):
                                if T == 0:
                                    P.pool(lambda e, u_=u_, s3=s3: e.memset(u_[s3][:, 0:2], 0.0), w=[f'{nm_}{s3}'])
                                else:
                                    P.pool(lambda e, u_=u_, s3=s3, p3=p3: e.tensor_copy(
                                        out=u_[s3][:, 0:2], in_=u_[p3][:, 512:514]),
                                        r=[f'{nm_}{p3}'], w=[f'{nm_}{s3}'])
                            for (u_, nm_, pp_, pk_, co, cname, c) in (
                                    (ug, 'ug', pg, 'pg', cg, 'cg', 2 * j), (uvv, 'uv', pvv, 'pvv', cv, 'cv', 2 * j + 1)):
                                P.act(lambda e, u_=u_, pp_=pp_, b=b, s3=s3: e.activation(
                                    out=u_[s3][:, 2:514], in_=pp_[b][:], func=AF.Copy),
                                    r=[f'{pk_}{b}'], w=[f'{nm_}{s3}'])
                                P.act(lambda e, co=co, pp_=pp_, b=b, c=c, b3=b3: e.activation(
                                    out=co[b3][:], in_=pp_[b][:], func=AF.Identity, scale=fw(c, 2),
                                    bias=pvc('ffnb', c, 1)), r=[f'{pk_}{b}'], w=[f'{cname}{b3}'])
                                for tap in (1, 0):
                                    P.dve(lambda e, u_=u_, co=co, b3=b3, s3=s3, c=c, tap=tap: e.scalar_tensor_tensor(
                                        out=co[b3][:], in0=u_[s3][:, tap:tap + 512], scalar=fw(c, tap), in1=co[b3][:],
                                        op0=ALU.mult, op1=ALU.add), r=[f'{nm_}{s3}'], w=[f'{cname}{b3}'])
                            def tail(b3=b3, a2=a2, T=T, j=j):
                                P.act(lambda e: e.activation(out=sgl[b3][:], in_=cg[b3][:], func=AF.Silu),
                                      r=[f'cg{b3}'], w=[f'sgl{b3}'])
                                P.dve(lambda e: e.tensor_tensor(
                                    out=ast[a2][:, TS(T)], in0=sgl[b3][:], in1=cv[b3][:], op=ALU.mult),
                                    r=[f'sgl{b3}', f'cv{b3}'], w=[f'ast{a2}'])
                                if T == NT - 1:
                                    P.dma('sp', aT[j * 128:(j + 1) * 128, :], ast[a2][:], r=[f'ast{a2}'], w=['dram'])
                            while pend:
                                pend.pop(0)()
                            pend.append(tail)
                while pend:
                    pend.pop(0)()
                P.barrier()
                P.emit()
        h2st.close()

        if upto >= 9:
            with ExitStack() as st:
                wd = sb(st, "wd", (128, NFC, D), BF16)
                for hh in range(2):
                    P.dma('pool', wd[:, :, hh * 512:(hh + 1) * 512],
                          wdown[:, hh * 512:(hh + 1) * 512].rearrange("(k p) c -> p k c", p=128), w=['wres'])
                at = [sb(st, f"at{i}", (128, NFC, 512), BF16) for i in range(2)]

                def load_act(T):
                    s = T % 2
                    P.dma('sp', at[s][:], aT[:, TS(T)].rearrange("(j p) t -> p j t", p=128), w=[f'at{s}'])
                    return at[s], f'at{s}'

                out_norm_phase(st, NFC, load_act, wd, 'gpost2', x1d, y, "I")
                P.barrier()
                P.emit()


        P.barrier()
        P.emit()
    return nc


_CACHE = {}


def kernel(**inputs):
    shared = pack_inputs(inputs)
    if 'nc' not in _CACHE:
        _CACHE['nc'] = build()
    nc = _CACHE['nc']
    x = np.asarray(inputs['x'], np.float32)
    mem = np.asarray(inputs['mem'], np.float32)
    in_maps = []
    for b in range(8):
        m = dict(shared)
        m['x'] = np.ascontiguousarray(x[b])
        m['mem'] = np.ascontiguousarray(mem[b])
        in_maps.append(m)
    res = run_bass_kernel_spmd(nc, in_maps, core_ids=list(range(8)))
    return np.stack([np.asarray(r['y'], np.float32) for r in res.results], axis=0)
```

```python
import numpy as np
from contextlib import ExitStack
import concourse.bass as bass
import concourse.mybir as mybir
from concourse.bass_utils import run_bass_kernel_spmd

F32 = mybir.dt.float32
BF16 = mybir.dt.bfloat16
I32 = mybir.dt.int32
AF = mybir.ActivationFunctionType
ALU = mybir.AluOpType
AX = mybir.AxisListType

S = 4096
D = 1024
NB = S // 128
NT = S // 512
EPS = 1e-6
FFN = 2816
NFC = FFN // 128
TOPK = 256
NIT = 16
DVE_ITERS = (3, 6, 9, 12, 14)
NEG = -1.0e30

ENGS = ['pe', 'act', 'dve', 'pool', 'sp']
BLK = {'pe': 'tensor', 'act': 'scalar', 'dve': 'vector', 'pool': 'gpsimd', 'sp': 'sync'}
EPOCH = 20000
NDS = 16


class Prog:
    def __init__(self, nc, stack):
        self.nc = nc
        self.stack = stack
        self.lists = {e: [] for e in ENGS}
        self.count = {e: 0 for e in ENGS}
        self.sems = {e: [] for e in ENGS}
        self.waited = {e: {} for e in ENGS}
        self.lastw = {}
        self.reads = {}
        self.dsem = {}
        self.dval = {}
        self.dnext = {}
        self.nsem = 0

    def _esem(self, e, idx):
        while len(self.sems[e]) <= idx:
            self.sems[e].append(self.stack.enter_context(
                self.nc.semaphore(f"s_{e}{len(self.sems[e])}")))
        return self.sems[e][idx]

    def _collect(self, e, r, w, extra):
        toks = list(extra)
        for k in r:
            t = self.lastw.get(k)
            if t is not None:
                toks.append(t)
        for k in w:
            t = self.lastw.get(k)
            if t is not None:
                toks.append(t)
            toks.extend(self.reads.get(k, ()))
        need = []
        for (sid, sem, v, te) in toks:
            if te == e and e == 'pe':
                continue
            if self.waited[e].get(sid, 0) >= v:
                continue
            self.waited[e][sid] = v
            need.append((sem, v))
        return need

    def _update(self, tok, r, w):
        for k in w:
            self.lastw[k] = tok
            self.reads[k] = []
        for k in r:
            self.reads.setdefault(k, []).append(tok)

    def op(self, e, fn, r=(), w=(), extra=()):
        need = self._collect(e, r, w, extra)
        n = self.count[e]
        ep, v = divmod(n, EPOCH)
        sem = self._esem(e, ep)
        self.count[e] = n + 1
        tok = ((e, ep), sem, v + 1, e)
        self.lists[e].append((need, fn, sem, 1))
        self._update(tok, r, w)
        return tok

    def pe(self, fn, r=(), w=(), extra=()):
        return self.op('pe', fn, r, w, extra)

    def act(self, fn, r=(), w=(), extra=()):
        return self.op('act', fn, r, w, extra)

    def dve(self, fn, r=(), w=(), extra=()):
        return self.op('dve', fn, r, w, extra)

    def pool(self, fn, r=(), w=(), extra=()):
        return self.op('pool', fn, r, w, extra)

    def dma(self, e, out, in_, r=(), w=(), extra=(), **kw):
        if e not in self.dsem:
            self.dsem[e] = [self.stack.enter_context(self.nc.semaphore(f"dma_{e}{j}")) for j in range(NDS)]
            self.dval[e] = [0] * NDS
            self.dnext[e] = 0
        j = self.dnext[e]
        self.dnext[e] = (j + 1) % NDS
        sem = self.dsem[e][j]
        extra = list(extra)
        if self.dval[e][j] > 0:
            extra.append((('d', e, j), sem, self.dval[e][j], 'dma'))
        need = self._collect(e, r, w, extra)
        self.dval[e][j] += 16
        tok = (('d', e, j), sem, self.dval[e][j], 'dma')
        self.lists[e].append((need, lambda eng: eng.dma_start(out=out, in_=in_, **kw), sem, 16))
        self._update(tok, r, w)
        return tok

    def barrier(self):
        toks = []
        for e in ENGS:
            n = self.count[e]
            if n == 0:
                continue
            ep, v = divmod(n - 1, EPOCH)
            toks.append(((e, ep), self.sems[e][ep], v + 1, e))
        for q in self.dsem:
            for j in range(NDS):
                if self.dval[q][j] > 0:
                    toks.append((('d', q, j), self.dsem[q][j], self.dval[q][j], 'dma'))
        for e in ENGS:
            need = []
            for (sid, sem, v, te) in toks:
                if te == e:
                    continue
                if self.waited[e].get(sid, 0) >= v:
                    continue
                self.waited[e][sid] = v
                need.append((sem, v))
            if need:
                self.lists[e].append((need, None, None, 0))
        self.lastw = {}
        self.reads = {}

    def emit(self):
        with self.nc.Block() as block:
            for e in ENGS:
                lst = self.lists[e]
                if not lst:
                    continue

                def body(eng, lst=lst):
                    for (need, fn, sem, inc) in lst:
                        for (s, v) in need:
                            eng.wait_ge(s, v)
                        if fn is not None:
                            fn(eng).then_inc(sem, inc)

                getattr(block, BLK[e])(body)
        self.lists = {e: [] for e in ENGS}


PCOL = {}
_off = 0
for _name, _n in [('g1', D), ('gmem', D), ('gpost1', D), ('g2', D), ('gpost2', D),
                  ('convw', 8 * 31), ('convb', 8), ('lng', 8), ('lnb', 8),
                  ('bgate', 24), ('ffnw', 44 * 3), ('ffnb', 44)]:
    PCOL[_name] = (_off, _n)
    _off += _n
NPCOL = _off

WCM_COLS = 45 * 128
WTM_COLS = 1032


def pack_inputs(inp):
    f = np.float32
    w_in = np.asarray(inp['w_in'][0], f)
    c0 = 0
    conv = w_in[:, 0:2048]
    q = w_in[:, 2048:3072]
    k = w_in[:, 3072:4096]
    v = w_in[:, 4096:5120]
    qi = w_in[:, 5120:5632]
    wi = w_in[:, 5632:5640]
    ki = w_in[:, 5640:5704]
    qm = w_in[:, 5704:6728]
    a, g = conv[:, :1024], conv[:, 1024:]
    inter = np.stack([a.reshape(D, 8, 128), g.reshape(D, 8, 128)], axis=2).reshape(D, 2048)
    wcm = np.ascontiguousarray(np.concatenate([inter, q, k, qm, qi, ki, ki], axis=1))
    wtm = np.ascontiguousarray(np.concatenate([v, wi], axis=1))
    w_up = np.asarray(inp['w_up'][0], f)
    ug, uv = w_up[:, :FFN], w_up[:, FFN:]
    wup = np.ascontiguousarray(
        np.stack([ug.reshape(D, NFC, 128), uv.reshape(D, NFC, 128)], axis=2).reshape(D, 2 * FFN))
    P = np.zeros((128, NPCOL), f)

    def put(name, arr):
        o, n = PCOL[name]
        P[:, o:o + n] = arr

    put('g1', np.broadcast_to(inp['norm1_pre_g'][0], (128, D)))
    put('gmem', np.broadcast_to(inp['mem_norm_g'][0], (128, D)))
    put('gpost1', np.broadcast_to(inp['norm1_post_g'][0], (128, D)))
    put('g2', np.broadcast_to(inp['norm2_pre_g'][0], (128, D)))
    put('gpost2', np.broadcast_to(inp['norm2_post_g'][0], (128, D)))
    cw = np.asarray(inp['conv_dw_w'][0], f)
    put('convw', cw.T.reshape(8, 128, 31).transpose(1, 0, 2).reshape(128, 8 * 31))
    put('convb', np.asarray(inp['conv_dw_b'][0], f).reshape(8, 128).T)
    put('lng', np.asarray(inp['conv_ln_g'][0], f).reshape(8, 128).T)
    put('lnb', np.asarray(inp['conv_ln_b'][0], f).reshape(8, 128).T)
    put('bgate', np.asarray(inp['b_gate'][0], f).reshape(24, 128).T)
    fw = np.asarray(inp['ffn_dw_w'][0], f)
    fwg, fwv = fw[:, :FFN], fw[:, FFN:]
    fwi = np.stack([fwg.reshape(3, NFC, 128), fwv.reshape(3, NFC, 128)], axis=2).reshape(3, 44, 128)
    put('ffnw', fwi.transpose(2, 1, 0).reshape(128, 44 * 3))
    fb = np.asarray(inp['ffn_dw_b'][0], f)
    fbi = np.stack([fb[:FFN].reshape(NFC, 128), fb[FFN:].reshape(NFC, 128)], axis=1).reshape(44, 128)
    put('ffnb', fbi.T)
    shared = {
        'wcm': wcm, 'wtm': wtm, 'wup': wup, 'params': P,
        'pw2': np.ascontiguousarray(inp['conv_pw2'][0], f),
        'wmkv': np.ascontiguousarray(inp['w_mem_kv'][0], f),
        'wgate': np.ascontiguousarray(inp['w_gate'][0], f),
        'wout': np.ascontiguousarray(inp['w_out'][0], f),
        'wdown': np.ascontiguousarray(inp['w_down'][0], f),
    }
    return shared


def build(debug=(), upto=99):
    nc = bass.Bass("TRN2", target_bir_lowering=False)
    dbg = set(debug)

    def din(name, shape):
        return nc.dram_tensor(name, list(shape), F32, kind="ExternalInput").ap()

    def scratch(name, shape, dt):
        kind = "ExternalOutput" if name in dbg else "Internal"
        return nc.dram_tensor(name, list(shape), dt, kind=kind).ap()

    x = din("x", (S, D))
    mem = din("mem", (256, D))
    wcm = din("wcm", (D, WCM_COLS))
    wtm = din("wtm", (D, WTM_COLS))
    wup = din("wup", (D, 2 * FFN))
    params = din("params", (128, NPCOL))
    pw2 = din("pw2", (D, D))
    wmkv = din("wmkv", (D, 2 * D))
    wgate = din("wgate", (D, 3 * D))
    wout = din("wout", (D, D))
    wdown = din("wdown", (FFN, D))
    y = nc.dram_tensor("y", [S, D], F32, kind="ExternalOutput").ap()

    ycT = scratch("ycT", (D, S), F32)
    qT = scratch("qT", (D, S), BF16)
    kT = scratch("kT", (D, S), BF16)
    qmT = scratch("qmT", (D, S), BF16)
    qiT = scratch("qiT", (512, S), BF16)
    kiT = scratch("kiT", (128, S), BF16)
    vtm = scratch("vtm", (S, D), BF16)
    witm = scratch("witm", (S, 8), F32)
    gT = scratch("gT", (3 * D, S), BF16)
    mconvT = scratch("mconvT", (D, S), BF16)
    mattT = scratch("mattT", (D, S), BF16)
    mmemT = scratch("mmemT", (D, S), BF16)
    maskT = scratch("maskT", (S, S), BF16)
    x1d = scratch("x1d", (S, D), F32)
    aT = scratch("aT", (FFN, S), BF16)

    def pcols(t, name):
        o, n = PCOL[name]
        return t[:, o:o + n]

    with ExitStack() as gs:
        P = Prog(nc, gs)

        uniq = [0]

        def sb(st, name, shape, dt):
            uniq[0] += 1
            return st.enter_context(nc.sbuf_tensor(f"{name}_{uniq[0]}", list(shape), dt))

        def ps(st, name, shape, dt=F32):
            uniq[0] += 1
            return st.enter_context(nc.psum_tensor(f"{name}_{uniq[0]}", list(shape), dt))

        ident = sb(gs, "ident", (128, 128), BF16)
        ones_bf = sb(gs, "ones_bf", (128, 128), BF16)
        ones_f = sb(gs, "ones_f", (128, 128), F32)
        caus = sb(gs, "caus", (128, 128), F32)
        pv = sb(gs, "pv", (128, 472), F32)
        pvo = PCOL['convw'][0]

        def pvc(name, j0=0, n=None):
            o, nn = PCOL[name]
            o -= pvo
            if n is None:
                n = nn
            return pv[:, o + j0:o + j0 + n]

        with ExitStack() as st:
            iot = sb(st, "iot", (128, 128), I32)
            P.pool(lambda e: e.iota(out=iot[:], pattern=[[1, 128]], base=0, channel_multiplier=-1),
                   w=['iot'])
            P.dve(lambda e: e.tensor_scalar(out=ident[:], in0=iot[:], scalar1=0.0, scalar2=None,
                                            op0=ALU.is_equal), r=['iot'], w=['ident'])
            P.dve(lambda e: e.tensor_scalar(out=caus[:], in0=iot[:], scalar1=0.5, scalar2=NEG,
                                            op0=ALU.is_gt, op1=ALU.mult), r=['iot'], w=['caus'])
            P.dve(lambda e: e.memset(ones_bf[:], 1.0), w=['ones_bf'])
            P.dve(lambda e: e.memset(ones_f[:], 1.0), w=['ones_f'])
            P.dma('sp', pv[:], params[:, pvo:pvo + 472], w=['pv'])
            P.barrier()
            P.emit()

        hst = ExitStack()
        hT = sb(hst, "hT", (128, 8, S), BF16)

        def norm_transpose_phase(st, src, nblk, gname, dstT, tag):
            gbc = sb(st, tag + "gbc", (128, D), F32)
            xt = [sb(st, f"{tag}xt{i}", (128, D), F32) for i in range(2)]
            junk = sb(st, tag + "junk", (128, D), BF16)
            hb = [sb(st, f"{tag}hb{i}", (128, D), BF16) for i in range(2)]
            ss = [sb(st, f"{tag}ss{i}", (128, 1), F32) for i in range(2)]
            rt = [sb(st, f"{tag}rt{i}", (128, 1), F32) for i in range(2)]
            rs = [sb(st, f"{tag}rs{i}", (128, 1), F32) for i in range(2)]
            pt = [ps(st, f"{tag}pt{i}", (128, D), BF16) for i in range(2)]
            o, n = PCOL[gname]
            P.dma('sp', gbc[:], params[:, o:o + n], w=['gbc'])
            def N1(i):
                s = i % 2
                P.dma('sp', xt[s][:], src[i * 128:(i + 1) * 128, :], w=[f'xt{s}'])
                P.dve(lambda e: e.scalar_tensor_tensor(
                    out=junk[:], in0=xt[s][:], scalar=1.0, in1=xt[s][:], op0=ALU.mult, op1=ALU.mult,
                    accum_out=ss[s][:]), r=[f'xt{s}'], w=[f'ss{s}'])
                P.act(lambda e: e.activation(out=rt[s][:], in_=ss[s][:], func=AF.Sqrt,
                                             bias=EPS, scale=1.0 / D), r=[f'ss{s}'], w=[f'rt{s}'])
                P.dve(lambda e: e.reciprocal(out=rs[s][:], in_=rt[s][:]), r=[f'rt{s}'], w=[f'rs{s}'])
                P.dve(lambda e: e.scalar_tensor_tensor(
                    out=hb[s][:], in0=xt[s][:], scalar=rs[s][:], in1=gbc[:], op0=ALU.mult, op1=ALU.mult),
                    r=[f'xt{s}', f'rs{s}', 'gbc'], w=[f'hb{s}'])

            def N2(i):
                s = i % 2
                for j in range(8):
                    P.pe(lambda e, j=j: e.transpose(out=pt[s][:, j * 128:(j + 1) * 128],
                                                    in_=hb[s][:, j * 128:(j + 1) * 128],
                                                    identity=ident[:]), r=[f'hb{s}'], w=[f'pt{s}'])
                P.act(lambda e: e.activation(
                    out=dstT[:, :, i * 128:(i + 1) * 128],
                    in_=pt[s][:].rearrange("p (j t) -> p j t", j=8), func=AF.Copy),
                    r=[f'pt{s}'], w=[f'dstT{i}'])

            N1(0)
            for i in range(nblk):
                if i + 1 < nblk:
                    N1(i + 1)
                N2(i)

        with ExitStack() as st:
            norm_transpose_phase(st, x, NB, 'g1', hT, "A")
            P.barrier()
            P.emit()

        def proj_cm(st, W, ncols, evac, tag, wtile=512):
            wt = [sb(st, f"{tag}wt{i}", (128, 8, wtile), BF16) for i in range(2)]
            pp = [ps(st, f"{tag}pp{i}", (128, 512), F32) for i in range(4)]
            ntile = (ncols + wtile - 1) // wtile
            cnt = 0
            for wi_ in range(ntile):
                s = wi_ % 2
                c0 = wi_ * wtile
                wc = min(wtile, ncols - c0)
                P.dma('pool', wt[s][:, :, 0:wc],
                      W[:, c0:c0 + wc].rearrange("(k p) c -> p k c", p=128), w=[f'wt{s}'])
                for cc in range(wc // 128):
                    c = (c0 // 128) + cc
                    for T in range(NT):
                        b = cnt % 4
                        cnt += 1
                        for k in range(8):
                            P.pe(lambda e, s=s, cc=cc, T=T, k=k, b=b: e.matmul(
                                pp[b][:], lhsT=wt[s][:, k, cc * 128:(cc + 1) * 128],
                                rhs=hT[:, k, T * 512:(T + 1) * 512], start=(k == 0), stop=(k == 7)),
                                r=[f'wt{s}'], w=[f'pp{b}'])
                        evac(c, T, pp[b], f'pp{b}')

        if upto >= 2:
            with ExitStack() as st:
                NDT = 9
                yg = [sb(st, f"yg{i}", (128, 30 + 512), F32) for i in range(3)]
                ygb = [sb(st, f"ygb{i}", (128, 30 + 512), BF16) for i in range(3)]
                sg = [sb(st, f"sg{i}", (128, 512), F32) for i in range(2)]
                acc = [sb(st, f"acc{i}", (128, 512), F32) for i in range(2)]
                yc = [sb(st, f"yc{i}", (128, 512), F32) for i in range(2)]
                dg = [sb(st, f"dg{i}", (128, 31 - NDT, 128), BF16) for i in range(2)]
                wt = [sb(st, f"Bwt{i}", (128, 8, 512), BF16) for i in range(2)]
                pa = [ps(st, f"Bpa{i}", (128, 512), F32) for i in range(2)]
                pg = [ps(st, f"Bpg{i}", (128, 512), F32) for i in range(2)]
                pc = [ps(st, f"Bpc{i}", (128, 512), F32) for i in range(2)]
                n = 0
                cpend = []
                for wi_ in range(4):
                    s = wi_ % 2
                    P.dma('pool', wt[s][:], wcm[:, wi_ * 512:(wi_ + 1) * 512].rearrange(
                        "(k p) c -> p k c", p=128), w=[f'wt{s}'])
                    for jj in range(2):
                        j = wi_ * 2 + jj
                        js = j % 2
                        while cpend:
                            cpend.pop(0)()
                        for jn in ([0, 1] if j == 0 else ([j + 1] if j + 1 < 8 else [])):
                            for tap in range(NDT, 31):
                                P.pool(lambda e, jn=jn, tap=tap: e.tensor_scalar(
                                    out=dg[jn % 2][:, tap - NDT, :], in0=ident[:],
                                    scalar1=pvc('convw', jn * 31 + tap, 1), scalar2=0.0,
                                    op0=ALU.mult, op1=ALU.add), w=[f'dg{jn % 2}'])
                        for T in range(NT):
                            b = n % 2
                            s3 = n % 3
                            p3 = (n - 1) % 3
                            for k in range(8):
                                P.pe(lambda e, s=s, jj=jj, T=T, k=k, b=b: e.matmul(
                                    pa[b][:], lhsT=wt[s][:, k, jj * 256:jj * 256 + 128],
                                    rhs=hT[:, k, T * 512:(T + 1) * 512], start=(k == 0), stop=(k == 7)),
                                    r=[f'wt{s}'], w=[f'pa{b}'])
                            for k in range(8):
                                P.pe(lambda e, s=s, jj=jj, T=T, k=k, b=b: e.matmul(
                                    pg[b][:], lhsT=wt[s][:, k, jj * 256 + 128:jj * 256 + 256],
                                    rhs=hT[:, k, T * 512:(T + 1) * 512], start=(k == 0), stop=(k == 7)),
                                    r=[f'wt{s}'], w=[f'pg{b}'])
                            P.act(lambda e, b=b: e.activation(out=sg[b][:], in_=pg[b][:], func=AF.Sigmoid),
                                  r=[f'pg{b}'], w=[f'sg{b}'])
                            if T == 0:
                                P.pool(lambda e, s3=s3: e.memset(yg[s3][:, 0:30], 0.0), w=[f'yg{s3}'])
                                P.pool(lambda e, s3=s3: e.memset(ygb[s3][:, 0:30], 0.0), w=[f'ygb{s3}'])
                            else:
                                P.pool(lambda e, s3=s3, p3=p3: e.tensor_copy(
                                    out=yg[s3][:, 0:30], in_=yg[p3][:, 512:542]),
                                    r=[f'yg{p3}'], w=[f'yg{s3}'])
                                P.pool(lambda e, s3=s3, p3=p3: e.tensor_copy(
                                    out=ygb[s3][:, 0:30], in_=ygb[p3][:, 512:542]),
                                    r=[f'ygb{p3}'], w=[f'ygb{s3}'])
                            P.dve(lambda e, b=b, s3=s3: e.tensor_tensor(
                                out=yg[s3][:, 30:542], in0=pa[b][:], in1=sg[b][:], op=ALU.mult),
                                r=[f'pa{b}', f'sg{b}'], w=[f'yg{s3}'])
                            P.act(lambda e, s3=s3: e.activation(out=ygb[s3][:, 30:542], in_=yg[s3][:, 30:542],
                                                                func=AF.Copy),
                                  r=[f'yg{s3}'], w=[f'ygb{s3}'])
                            def taps(b=b, s3=s3, js=js):
                                for tap in range(NDT, 31):
                                    P.pe(lambda e, tap=tap: e.matmul(
                                        pc[b][:], lhsT=dg[js][:, tap - NDT, :], rhs=ygb[s3][:, tap:tap + 512],
                                        start=(tap == NDT), stop=(tap == 30)),
                                        r=[f'dg{js}', f'ygb{s3}'], w=[f'pc{b}'])
                            P.dve(lambda e, b=b, s3=s3, j=j: e.tensor_scalar(
                                out=acc[b][:], in0=yg[s3][:, 0:512], scalar1=pvc('convw', j * 31, 1),
                                scalar2=pvc('convb', j, 1), op0=ALU.mult, op1=ALU.add),
                                r=[f'yg{s3}'], w=[f'acc{b}'])
                            for tap in range(1, NDT):
                                P.dve(lambda e, b=b, s3=s3, j=j, tap=tap: e.scalar_tensor_tensor(
                                    out=acc[b][:], in0=yg[s3][:, tap:tap + 512],
                                    scalar=pvc('convw', j * 31 + tap, 1), in1=acc[b][:],
                                    op0=ALU.mult, op1=ALU.add),
                                    r=[f'yg{s3}'], w=[f'acc{b}'])
                            def fin_(b=b, j=j, T=T, taps=taps):
                                taps()
                                P.dve(lambda e: e.tensor_tensor(out=yc[b][:], in0=pc[b][:], in1=acc[b][:],
                                                                op=ALU.add),
                                      r=[f'pc{b}', f'acc{b}'], w=[f'yc{b}'])
                                P.dma('sp', ycT[j * 128:(j + 1) * 128, T * 512:(T + 1) * 512], yc[b][:],
                                      r=[f'yc{b}'], w=['dram'])
                            while cpend:
                                cpend.pop(0)()
                            cpend.append(fin_)
                            n += 1
                while cpend:
                    cpend.pop(0)()
                P.barrier()
                P.emit()

            with ExitStack() as st:
                stg = [sb(st, f"stg{i}", (128, S), BF16) for i in range(2)]

                def evac_gen(c, T, pp, key):
                    c = c + 16
                    if c < 24:
                        dst, row, scale = qT, (c - 16) * 128, 128.0 ** -0.5
                    elif c < 32:
                        dst, row, scale = kT, (c - 24) * 128, 1.0
                    elif c < 40:
                        dst, row, scale = qmT, (c - 32) * 128, 256.0 ** -0.5
                    elif c < 44:
                        dst, row, scale = qiT, (c - 40) * 128, 1.0
                    else:
                        dst, row, scale = kiT, 0, 1.0
                    s2 = c % 2
                    P.act(lambda e: e.activation(out=stg[s2][:, T * 512:(T + 1) * 512], in_=pp[:],
                                                 func=AF.Copy, scale=scale),
                          r=[key], w=[f'stg{s2}'])
                    if T == NT - 1:
                        P.dma('sp', dst[row:row + 128, :], stg[s2][:], r=[f'stg{s2}'], w=['dram'])

                proj_cm(st, wcm[:, 2048:], WCM_COLS - 2048, evac_gen, "Bq")
                P.barrier()
                P.emit()

            with ExitStack() as st:
                stg = [sb(st, f"stg{i}", (128, S), BF16) for i in range(2)]

                def evac_gate(c, T, pp, key):
                    s2 = c % 2
                    P.act(lambda e: e.activation(out=stg[s2][:, T * 512:(T + 1) * 512], in_=pp[:],
                                                 func=AF.Sigmoid, bias=pvc('bgate', c, 1), scale=1.0),
                          r=[key], w=[f'stg{s2}'])
                    if T == NT - 1:
                        P.dma('sp', gT[c * 128:(c + 1) * 128, :], stg[s2][:], r=[f'stg{s2}'], w=['dram'])

                proj_cm(st, wgate, 3 * D, evac_gate, "Bg")
                P.barrier()
                P.emit()

            with ExitStack() as st:
                wv = sb(st, "wv", (128, 8, WTM_COLS), BF16)
                vst = [sb(st, f"vst{i}", (128, D), BF16) for i in range(2)]
                wst = [sb(st, f"wst{i}", (128, 8), F32) for i in range(2)]
                pvv = [ps(st, f"pvv{i}", (128, 512), F32) for i in range(4)]
                pw = [ps(st, f"pw{i}", (128, 8), F32) for i in range(2)]
                for hh in range(2):
                    P.dma('pool', wv[:, :, hh * 512:(hh + 1) * 512],
                          wtm[:, hh * 512:(hh + 1) * 512].rearrange("(k p) c -> p k c", p=128), w=['wv'])
                P.dma('pool', wv[:, :, 1024:1032],
                      wtm[:, 1024:1032].rearrange("(k p) c -> p k c", p=128), w=['wv'])
                for i in range(NB):
                    s = i % 2
                    for hh in range(2):
                        b = (i * 2 + hh) % 4
                        for k in range(8):
                            P.pe(lambda e, i=i, hh=hh, k=k, b=b: e.matmul(
                                pvv[b][:], lhsT=hT[:, k, i * 128:(i + 1) * 128],
                                rhs=wv[:, k, hh * 512:(hh + 1) * 512], start=(k == 0), stop=(k == 7)),
                                r=['wv'], w=[f'pvv{b}'])
                        P.act(lambda e, s=s, hh=hh, b=b: e.activation(
                            out=vst[s][:, hh * 512:(hh + 1) * 512], in_=pvv[b][:], func=AF.Copy),
                            r=[f'pvv{b}'], w=[f'vst{s}'])
                    for k in range(8):
                        P.pe(lambda e, i=i, k=k, s=s: e.matmul(
                            pw[s][:], lhsT=hT[:, k, i * 128:(i + 1) * 128],
                            rhs=wv[:, k, 1024:1032], start=(k == 0), stop=(k == 7)),
                            r=['wv'], w=[f'pw{s}'])
                    P.dve(lambda e, s=s: e.tensor_copy(out=wst[s][:], in_=pw[s][:]),
                          r=[f'pw{s}'], w=[f'wst{s}'])
                    P.dma('sp', vtm[i * 128:(i + 1) * 128, :], vst[s][:], r=[f'vst{s}'], w=['dram'])
                    P.dma('sp', witm[i * 128:(i + 1) * 128, :], wst[s][:], r=[f'wst{s}'], w=['dram'])
                P.barrier()
                P.emit()
        hst.close()

        def TS(T):
            return slice(T * 512, (T + 1) * 512)

        if upto >= 3:
            with ExitStack() as st:
                pwt = sb(st, "pwt", (128, 8, D), BF16)
                for hh in range(2):
                    P.dma('pool', pwt[:, :, hh * 512:(hh + 1) * 512],
                          pw2[:, hh * 512:(hh + 1) * 512].rearrange("(k p) c -> p k c", p=128), w=['pwt'])
                ycl = [sb(st, f"ycl{i}", (128, 8, 512), F32) for i in range(2)]
                sq = sb(st, "sq", (128, 8, 512), F32)
                gl = [sb(st, f"gl{i}", (128, 8, 512), BF16) for i in range(2)]
                mean = [sb(st, f"mean{i}", (128, 512), F32) for i in range(2)]
                msq = sb(st, "msq", (128, 512), F32)
                var = sb(st, "var", (128, 512), F32)
                sd = sb(st, "sd", (128, 512), F32)
                rstd = [sb(st, f"rstd{i}", (128, 512), F32) for i in range(2)]
                t1 = [sb(st, f"t1{i}", (128, 512), F32) for i in range(2)]
                ys = [sb(st, f"ys{i}", (128, 8, 512), BF16) for i in range(2)]
                ot = [sb(st, f"ot{i}", (128, 512), BF16) for i in range(2)]
                s1 = ps(st, "s1", (128, 512))
                s2 = ps(st, "s2", (128, 512))
                po = [ps(st, f"po{i}", (128, 512)) for i in range(2)]

                def C1(T):
                    s = T % 2
                    for g_ in range(2):
                        P.dma('sp', ycl[s][:, g_ * 4:(g_ + 1) * 4, :],
                              ycT[g_ * 512:(g_ + 1) * 512, TS(T)].rearrange("(j p) t -> p j t", p=128),
                              w=[f'ycl{s}_{g_}'])
                        P.dma('sp', gl[s][:, g_ * 4:(g_ + 1) * 4, :],
                              gT[g_ * 512:(g_ + 1) * 512, TS(T)].rearrange("(j p) t -> p j t", p=128),
                              w=[f'gl{s}_{g_}'])
                    yk = [f'ycl{s}_0', f'ycl{s}_1']
                    P.act(lambda e: e.activation(out=sq[:], in_=ycl[s][:], func=AF.Square), r=yk, w=['sq'])
                    for j in range(8):
                        P.pe(lambda e, j=j: e.matmul(s1[:], lhsT=ones_f[:], rhs=ycl[s][:, j, :],
                                                     start=(j == 0), stop=(j == 7)), r=yk, w=['s1'])
                    for j in range(8):
                        P.pe(lambda e, j=j: e.matmul(s2[:], lhsT=ones_f[:], rhs=sq[:, j, :],
                                                     start=(j == 0), stop=(j == 7)), r=['sq'], w=['s2'])
                    P.dve(lambda e: e.tensor_scalar(out=mean[s][:], in0=s1[:], scalar1=1.0 / D, scalar2=None,
                                                    op0=ALU.mult), r=['s1'], w=[f'mean{s}'])
                    P.dve(lambda e: e.tensor_tensor(out=msq[:], in0=mean[s][:], in1=mean[s][:], op=ALU.mult),
                          r=[f'mean{s}'], w=['msq'])
                    P.dve(lambda e: e.scalar_tensor_tensor(out=var[:], in0=s2[:], scalar=1.0 / D, in1=msq[:],
                                                           op0=ALU.mult, op1=ALU.subtract),
                          r=['s2', 'msq'], w=['var'])
                    P.act(lambda e: e.activation(out=sd[:], in_=var[:], func=AF.Sqrt, bias=EPS, scale=1.0),
                          r=['var'], w=['sd'])
                    P.dve(lambda e: e.reciprocal(out=rstd[s][:], in_=sd[:]), r=['sd'], w=[f'rstd{s}'])

                def C2(T):
                    s = T % 2
                    yk = [f'ycl{s}_0', f'ycl{s}_1']
                    for j in range(8):
                        b = j % 2
                        P.dve(lambda e, j=j, b=b: e.tensor_tensor(
                            out=t1[b][:], in0=ycl[s][:, j, :], in1=mean[s][:], op=ALU.subtract),
                            r=yk + [f'mean{s}'], w=[f't1{b}'])
                        P.dve(lambda e, b=b: e.tensor_tensor(out=t1[b][:], in0=t1[b][:], in1=rstd[s][:],
                                                             op=ALU.mult), r=[f'rstd{s}'], w=[f't1{b}'])
                        P.act(lambda e, j=j, b=b: e.activation(out=ys[s][:, j, :], in_=t1[b][:], func=AF.Silu,
                                                               scale=pvc('lng', j, 1), bias=pvc('lnb', j, 1)),
                              r=[f't1{b}'], w=[f'ys{s}'])

                def C3(T):
                    s = T % 2
                    for dch in range(8):
                        b = dch % 2
                        for j in range(8):
                            P.pe(lambda e, dch=dch, j=j, b=b: e.matmul(
                                po[b][:], lhsT=pwt[:, j, dch * 128:(dch + 1) * 128], rhs=ys[s][:, j, :],
                                start=(j == 0), stop=(j == 7)), r=[f'ys{s}', 'pwt'], w=[f'po{b}'])
                        P.dve(lambda e, dch=dch, b=b: e.tensor_tensor(
                            out=ot[b][:], in0=po[b][:], in1=gl[s][:, dch, :], op=ALU.mult),
                            r=[f'po{b}', f'gl{s}_0', f'gl{s}_1'], w=[f'ot{b}'])
                        P.dma('pool', mconvT[dch * 128:(dch + 1) * 128, TS(T)], ot[b][:], r=[f'ot{b}'], w=['dram'])

                C1(0)
                for T in range(NT):
                    if T + 1 < NT:
                        C1(T + 1)
                    C2(T)
                    C3(T)
                P.barrier()
                P.emit()

        if upto >= 4:
            with ExitStack() as st0:
                mnT = sb(st0, "mnT", (128, 8, 256), BF16)
                mkT = sb(st0, "mkT", (128, 8, 256), BF16)
                mv = sb(st0, "mv", (128, 2, D), BF16)
                with ExitStack() as st:
                    norm_transpose_phase(st, mem, 2, 'gmem', mnT, "D")
                    P.barrier()
                    P.emit()
                with ExitStack() as st:
                    wk = [sb(st, f"wk{i}", (128, 8, 512), BF16) for i in range(2)]
                    pk = [ps(st, f"pk{i}", (128, 512)) for i in range(2)]
                    pmv = [ps(st, f"pmv{i}", (128, 512)) for i in range(2)]
                    for wi_ in range(2):
                        s = wi_ % 2
                        P.dma('pool', wk[s][:], wmkv[:, wi_ * 512:(wi_ + 1) * 512].rearrange(
                            "(k p) c -> p k c", p=128), w=[f'wk{s}'])
                        for cc in range(4):
                            c = wi_ * 4 + cc
                            b = c % 2
                            for k in range(8):
                                P.pe(lambda e, s=s, cc=cc, k=k, b=b: e.matmul(
                                    pk[b][:, 0:256], lhsT=wk[s][:, k, cc * 128:(cc + 1) * 128],
                                    rhs=mnT[:, k, :], start=(k == 0), stop=(k == 7)),
                                    r=[f'wk{s}'], w=[f'pk{b}'])
                            P.act(lambda e, c=c, b=b: e.activation(out=mkT[:, c, :], in_=pk[b][:, 0:256],
                                                                   func=AF.Copy), r=[f'pk{b}'], w=['mkT'])
                    for wi_ in range(2):
                        s = wi_ % 2
                        P.dma('pool', wk[s][:], wmkv[:, 1024 + wi_ * 512:1024 + (wi_ + 1) * 512].rearrange(
                            "(k p) c -> p k c", p=128), w=[f'wk{s}'])
                        for mc in range(2):
                            for k in range(8):
                                P.pe(lambda e, s=s, mc=mc, k=k: e.matmul(
                                    pmv[mc][:], lhsT=mnT[:, k, mc * 128:(mc + 1) * 128], rhs=wk[s][:, k, :],
                                    start=(k == 0), stop=(k == 7)), r=[f'wk{s}'], w=[f'pmv{mc}'])
                            P.act(lambda e, wi_=wi_, mc=mc: e.activation(
                                out=mv[:, mc, wi_ * 512:(wi_ + 1) * 512], in_=pmv[mc][:], func=AF.Copy),
                                r=[f'pmv{mc}'], w=['mv'])
                    P.barrier()
                    P.emit()
                with ExitStack() as st:
                    qml = [sb(st, f"qml{i}", (128, 2, 512), BF16) for i in range(2)]
                    gl = [sb(st, f"gl{i}", (128, 2, 512), BF16) for i in range(2)]
                    pT = [sb(st, f"pT{i}", (128, 2, 512), BF16) for i in range(2)]
                    rinv = sb(st, "rinv", (128, 512), F32)
                    o1 = [sb(st, f"o1{i}", (128, 512), F32) for i in range(2)]
                    ot = [sb(st, f"ot{i}", (128, 512), BF16) for i in range(2)]
                    pl = [ps(st, f"pl{i}", (128, 512)) for i in range(2)]
                    po = [ps(st, f"po{i}", (128, 512)) for i in range(2)]
                    pr = ps(st, "pr", (128, 512))
                    n = 0
                    for hm in range(4):
                        for T in range(NT):
                            s = n % 2
                            n += 1
                            P.dma('sp', qml[s][:], qmT[hm * 256:(hm + 1) * 256, TS(T)].rearrange(
                                "(c p) t -> p c t", p=128), w=[f'qml{s}'])
                            P.dma('sp', gl[s][:], gT[2048 + hm * 256:2048 + (hm + 1) * 256, TS(T)].rearrange(
                                "(c p) t -> p c t", p=128), w=[f'gl{s}'])
                            for mc in range(2):
                                for dmc in range(2):
                                    P.pe(lambda e, s=s, hm=hm, mc=mc, dmc=dmc: e.matmul(
                                        pl[mc][:], lhsT=mkT[:, hm * 2 + dmc, mc * 128:(mc + 1) * 128],
                                        rhs=qml[s][:, dmc, :], start=(dmc == 0), stop=(dmc == 1)),
                                        r=[f'qml{s}'], w=[f'pl{mc}'])
                                P.act(lambda e, s=s, mc=mc: e.activation(out=pT[s][:, mc, :], in_=pl[mc][:],
                                                                         func=AF.Exp),
                                      r=[f'pl{mc}'], w=[f'pT{s}'])
                            for dmc in range(2):
                                for mc in range(2):
                                    P.pe(lambda e, s=s, hm=hm, mc=mc, dmc=dmc: e.matmul(
                                        po[dmc][:], lhsT=mv[:, mc, hm * 256 + dmc * 128:hm * 256 + (dmc + 1) * 128],
                                        rhs=pT[s][:, mc, :], start=(mc == 0), stop=(mc == 1)),
                                        r=[f'pT{s}'], w=[f'po{dmc}'])
                            for mc in range(2):
                                P.pe(lambda e, s=s, mc=mc: e.matmul(pr[:], lhsT=ones_bf[:], rhs=pT[s][:, mc, :],
                                                                    start=(mc == 0), stop=(mc == 1)),
                                     r=[f'pT{s}'], w=['pr'])
                            P.act(lambda e: e.activation(out=o1[0][:], in_=pr[:], func=AF.Ln), r=['pr'], w=['o10'])
                            P.act(lambda e: e.activation(out=rinv[:], in_=o1[0][:], func=AF.Exp, scale=-1.0),
                                  r=['o10'], w=['rinv'])
                            for dmc in range(2):
                                P.dve(lambda e, dmc=dmc: e.tensor_tensor(out=o1[dmc][:], in0=po[dmc][:], in1=rinv[:],
                                                                         op=ALU.mult),
                                      r=[f'po{dmc}', 'rinv'], w=[f'o1{dmc}'])
                                P.dve(lambda e, s=s, dmc=dmc: e.tensor_tensor(
                                    out=ot[dmc][:], in0=o1[dmc][:], in1=gl[s][:, dmc, :], op=ALU.mult),
                                    r=[f'o1{dmc}', f'gl{s}'], w=[f'ot{dmc}'])
                                P.dma('pool', mmemT[hm * 256 + dmc * 128:hm * 256 + (dmc + 1) * 128, TS(T)],
                                      ot[dmc][:], r=[f'ot{dmc}'], w=['dram'])
                    P.barrier()
                    P.emit()

        if upto >= 5:
            with ExitStack() as st:
                kil = sb(st, "kil", (128, S), BF16)
                qil = [sb(st, f"qil{i}", (128, 4, 128), BF16) for i in range(2)]
                wil = [sb(st, f"wil{i}", (128, 8), F32) for i in range(2)]
                score = [sb(st, f"score{i}", (128, S), F32) for i in range(4)]
                rl = [sb(st, f"rl{i}", (128, 512), F32) for i in range(3)]
                junk = sb(st, "junkE", (128, S), BF16)
                junkD = sb(st, "junkDE", (128, S), BF16)
                mk = [sb(st, f"mk{i}", (128, S), BF16) for i in range(2)]
                mT = [sb(st, f"mT{i}", (128, 8, 128), BF16) for i in range(2)]
                ptab = sb(st, "ptab", (128, NIT + 1), F32)
                thrc = sb(st, "thrc", (128, 1), F32)

                def two(name, shape):
                    return [sb(st, f"{name}{c}", shape, F32) for c in range(2)]
                lo = two("lo", (128, 1))
                hi = two("hi", (128, 1))
                Wd = two("Wd", (128, 1))
                hneg = two("hneg", (128, NIT + 1))
                nm = two("nm", (128, 1))
                ssum = two("ssum", (128, 1))
                sgn = two("sgn", (128, 1))
                thr_t = two("thr_t", (128, 1))
                midp = two("midp", (128, 1))
                cntd = two("cntd", (128, 1))
                t2 = two("t2", (128, 1))
                pd = [ps(st, f"pd{i}", (128, 512)) for i in range(4)]
                ptm = [ps(st, f"ptm{i}", (128, 1024), BF16) for i in range(2)]
                for n in range(NIT + 1):
                    P.pool(lambda e, n=n: e.memset(ptab[:, n:n + 1], -(2.0 ** -(n + 1))), w=['ptab'])
                P.pool(lambda e: e.memset(thrc[:], -1.0e29), w=['thrc'])
                P.dma('sp', kil[:], kiT[:, :], w=['kil'])
                cnts = {'c': 0, 't': 0, 'q': 0}
                DVE_IT = ((1, 5, 9, 13), (3, 7, 11, 15))

                def score_units(i):
                    s = i % 4
                    L = (i + 1) * 128
                    units = []
                    qs = cnts['q'] % 2
                    cnts['q'] += 1

                    def loads():
                        P.dma('sp', qil[qs][:], qiT[:, i * 128:(i + 1) * 128].rearrange("(c p) t -> p c t", p=128),
                              w=[f'qil{qs}'])
                        P.dma('sp', wil[qs][:], witm[i * 128:(i + 1) * 128, :], w=[f'wil{qs}'])
                    units.append(loads)
                    nst = (L + 511) // 512
                    for stile in range(nst):
                        w_ = min(512, L - stile * 512)
                        c0 = stile * 512
                        for h in range(8):
                            def unit(stile=stile, w_=w_, c0=c0, h=h):
                                p2, half = divmod(h, 2)
                                b = cnts['c'] % 4
                                r3 = cnts['c'] % 3
                                cnts['c'] += 1
                                P.pe(lambda e: e.matmul(
                                    pd[b][:, 0:w_], lhsT=qil[qs][half * 64:(half + 1) * 64, p2, :],
                                    rhs=kil[half * 64:(half + 1) * 64, c0:c0 + w_], start=True, stop=True),
                                    r=[f'qil{qs}', 'kil'], w=[f'pd{b}'])
                                P.act(lambda e: e.activation(
                                    out=rl[r3][:, 0:w_], in_=pd[b][:, 0:w_], func=AF.Relu),
                                    r=[f'pd{b}'], w=[f'rl{r3}'])
                                if h == 0:
                                    P.dve(lambda e: e.tensor_scalar(
                                        out=score[s][:, c0:c0 + w_], in0=rl[r3][:, 0:w_], scalar1=wil[qs][:, 0:1],
                                        scalar2=None, op0=ALU.mult),
                                        r=[f'rl{r3}', f'wil{qs}'], w=[f'score{s}'])
                                else:
                                    P.dve(lambda e: e.scalar_tensor_tensor(
                                        out=score[s][:, c0:c0 + w_], in0=rl[r3][:, 0:w_], scalar=wil[qs][:, h:h + 1],
                                        in1=score[s][:, c0:c0 + w_], op0=ALU.mult, op1=ALU.add),
                                        r=[f'rl{r3}', f'wil{qs}'], w=[f'score{s}'])
                            units.append(unit)

                    def fin():
                        P.dve(lambda e: e.tensor_tensor(
                            out=score[s][:, i * 128:(i + 1) * 128], in0=score[s][:, i * 128:(i + 1) * 128],
                            in1=caus[:], op=ALU.add), w=[f'score{s}'])
                    units.append(fin)
                    return units

                def bisect_ops(i, c):
                    s = i % 4
                    L = (i + 1) * 128
                    sk = f'score{s}'
                    K = lambda nme: f'{nme}{c}'
                    units = []
                    if i >= 2:
                        units.append([
                            lambda: P.dve(lambda e: e.tensor_reduce(out=lo[c][:], in_=score[s][:, 0:i * 128],
                                                                    axis=AX.X, op=ALU.min), r=[sk], w=[K('lo')]),
                            lambda: P.dve(lambda e: e.tensor_reduce(out=hi[c][:], in_=score[s][:, 0:L],
                                                                    axis=AX.X, op=ALU.max), r=[sk], w=[K('hi')]),
                            lambda: P.dve(lambda e: e.tensor_tensor(out=Wd[c][:], in0=hi[c][:], in1=lo[c][:],
                                                                    op=ALU.subtract),
                                          r=[K('hi'), K('lo')], w=[K('Wd')]),
                            lambda: P.dve(lambda e: e.tensor_scalar(out=hneg[c][:], in0=ptab[:], scalar1=Wd[c][:, 0:1],
                                                                    scalar2=None, op0=ALU.mult),
                                          r=[K('Wd'), 'ptab'], w=[K('hneg')]),
                            lambda: P.dve(lambda e: e.tensor_scalar(out=nm[c][:], in0=lo[c][:], scalar1=-1.0,
                                                                    scalar2=hneg[c][:, 0:1], op0=ALU.mult,
                                                                    op1=ALU.add),
                                          r=[K('lo'), K('hneg')], w=[K('nm')]),
                        ])
                        for n in range(NIT):
                            if n in DVE_IT[c]:
                                units.append([
                                    lambda: P.dve(lambda e: e.tensor_scalar(
                                        out=midp[c][:], in0=nm[c][:], scalar1=-1.0, scalar2=None, op0=ALU.mult),
                                        r=[K('nm')], w=[K('midp')]),
                                    lambda: P.dve(lambda e: e.tensor_scalar(
                                        out=junkD[:, 0:L], in0=score[s][:, 0:L], scalar1=midp[c][:, 0:1],
                                        scalar2=None, op0=ALU.is_ge, op1=ALU.add, accum_out=cntd[c][:]),
                                        r=[sk, K('midp')], w=[K('cntd')]),
                                    lambda: P.dve(lambda e: e.tensor_scalar(
                                        out=t2[c][:], in0=cntd[c][:], scalar1=TOPK - 0.5, scalar2=2.0,
                                        op0=ALU.is_ge, op1=ALU.mult), r=[K('cntd')], w=[K('t2')]),
                                    lambda n=n: P.dve(lambda e: e.tensor_scalar(
                                        out=t2[c][:], in0=t2[c][:], scalar1=-1.0, scalar2=hneg[c][:, n + 1:n + 2],
                                        op0=ALU.add, op1=ALU.mult), r=[K('hneg')], w=[K('t2')]),
                                    lambda: P.dve(lambda e: e.tensor_tensor(
                                        out=nm[c][:], in0=nm[c][:], in1=t2[c][:], op=ALU.add),
                                        r=[K('t2')], w=[K('nm')]),
                                ])
                            else:
                                units.append([
                                    lambda: P.act(lambda e: e.activation(
                                        out=junk[:, 0:L], in_=score[s][:, 0:L], func=AF.Sign, bias=nm[c][:, 0:1],
                                        scale=1.0, accum_out=ssum[c][:]), r=[sk, K('nm')], w=[K('ssum')]),
                                    lambda: P.act(lambda e: e.activation(
                                        out=sgn[c][:], in_=ssum[c][:], func=AF.Sign,
                                        bias=float(L - 2 * TOPK) + 0.5, scale=1.0), r=[K('ssum')], w=[K('sgn')]),
                                    lambda n=n: P.act(lambda e: e.activation(
                                        out=nm[c][:], in_=sgn[c][:], func=AF.Identity,
                                        scale=hneg[c][:, n + 1:n + 2], bias=nm[c][:, 0:1]),
                                        r=[K('sgn'), K('hneg')], w=[K('nm')]),
                                ])

                    def epi():
                        if i >= 2:
                            P.dve(lambda e: e.tensor_scalar(out=thr_t[c][:], in0=nm[c][:], scalar1=-1.0,
                                                            scalar2=hneg[c][:, NIT:NIT + 1], op0=ALU.mult,
                                                            op1=ALU.add),
                                  r=[K('nm'), K('hneg')], w=[K('thr_t')])
                            thr, thrk = thr_t[c], K('thr_t')
                        else:
                            thr, thrk = thrc, 'thrc'
                        P.dve(lambda e: e.tensor_scalar(
                            out=mk[c][:, 0:L], in0=score[s][:, 0:L], scalar1=thr[:, 0:1], scalar2=None,
                            op0=ALU.is_ge), r=[sk, thrk], w=[K('mk')])
                        for g0 in range(0, i + 1, 4):
                            nb_ = min(4, i + 1 - g0)
                            b = cnts['t'] % 2
                            cnts['t'] += 1
                            for q_ in range(nb_):
                                P.pe(lambda e, b=b, q_=q_, g0=g0: e.transpose(
                                    out=ptm[b][:, q_ * 128:(q_ + 1) * 128],
                                    in_=mk[c][:, (g0 + q_) * 128:(g0 + q_ + 1) * 128], identity=ident[:]),
                                    r=[K('mk')], w=[f'ptm{b}'])
                            P.act(lambda e, b=b, nb_=nb_: e.activation(
                                out=mT[b][:, 0:nb_, :],
                                in_=ptm[b][:, 0:nb_ * 128].rearrange("p (q t) -> p q t", q=nb_), func=AF.Copy),
                                r=[f'ptm{b}'], w=[f'mT{b}'])
                            P.dma('pool', maskT[g0 * 128:(g0 + nb_) * 128, i * 128:(i + 1) * 128].rearrange(
                                "(q p) t -> p q t", p=128), mT[b][:, 0:nb_, :], r=[f'mT{b}'], w=['dram'])
                    units.append([epi])
                    return units

                def run_units(us):
                    for u in us:
                        u()

                run_units(score_units(0))
                run_units(score_units(1))
                for pidx in range(NB // 2):
                    ia, ib = 2 * pidx, 2 * pidx + 1
                    A_ = bisect_ops(ia, 0)
                    B_ = bisect_ops(ib, 1)
                    S_ = []
                    if ia + 2 < NB:
                        s1, s2 = score_units(ia + 2), score_units(ib + 2)
                        for k_ in range(max(len(s1), len(s2))):
                            if k_ < len(s1):
                                S_.append(s1[k_])
                            if k_ < len(s2):
                                S_.append(s2[k_])
                    nun = max(len(A_), len(B_))
                    done = 0
                    for idx in range(nun):
                        ua = A_[idx] if idx < len(A_) else []
                        ub = B_[idx] if idx < len(B_) else []
                        for k_ in range(max(len(ua), len(ub))):
                            if k_ < len(ua):
                                ua[k_]()
                            if k_ < len(ub):
                                ub[k_]()
                        tgt = (len(S_) * (idx + 1)) // nun
                        while done < tgt:
                            S_[done]()
                            done += 1
                P.barrier()
                P.emit()

        if upto >= 6:
            with ExitStack() as st:
                ql = [sb(st, f"ql{i}", (128, S), BF16) for i in range(2)]
                kl = [sb(st, f"kl{i}", (128, S), BF16) for i in range(2)]
                vl = [sb(st, f"vl{i}", (128, NB, 128), BF16) for i in range(2)]
                NML = 6
                ml = [sb(st, f"ml{i}", (128, 512), BF16) for i in range(NML)]
                ex = [sb(st, f"ex{i}", (128, 512), BF16) for i in range(4)]
                pm = [sb(st, f"pm{i}", (128, 512), BF16) for i in range(4)]
                gl = [sb(st, f"gl{i}", (128, 512), BF16) for i in range(2)]
                rinv = sb(st, "rinv", (128, 512), F32)
                o1 = sb(st, "o1", (128, 512), F32)
                ot = [sb(st, f"ot{i}", (128, 512), BF16) for i in range(2)]
                psc = [ps(st, f"psc{i}", (128, 512)) for i in range(4)]
                po = [ps(st, f"po{i}", (128, 512)) for i in range(2)]
                pr = [ps(st, f"pr{i}", (128, 512)) for i in range(2)]
                its = []
                for h in range(8):
                    for T in range(NT):
                        nsb = 4 * T + 4
                        for sbk in range(nsb):
                            its.append((h, T, sbk, nsb))
                DEPTH = 3

                def front(n):
                    h, T, sbk, nsb = its[n]
                    hs = h % 2
                    a = (h * NT + T) % 2
                    if T == 0 and sbk == 0:
                        P.dma('sp', ql[hs][:], qT[h * 128:(h + 1) * 128, :], w=[f'ql{hs}'])
                        P.dma('sp', kl[hs][:], kT[h * 128:(h + 1) * 128, :], w=[f'kl{hs}'])
                        for g_ in range(8):
                            P.dma('sp', vl[hs][:, g_ * 4:(g_ + 1) * 4, :],
                                  vtm[g_ * 512:(g_ + 1) * 512, h * 128:(h + 1) * 128].rearrange(
                                      "(b p) d -> p b d", p=128), w=[f'vl{hs}_{g_}'])
                    if sbk == 0:
                        P.dma('sp', gl[a][:], gT[1024 + h * 128:1024 + (h + 1) * 128, TS(T)], w=[f'gl{a}'])
                    m4 = n % NML
                    b3 = n % 4
                    r0 = max(0, sbk - 4 * T) * 128
                    t0 = T * 512 + r0
                    P.dma('sp', ml[m4][:, r0:512], maskT[sbk * 128:(sbk + 1) * 128, t0:(T + 1) * 512],
                          w=[f'ml{m4}'])
                    P.pe(lambda e: e.matmul(
                        psc[b3][:, r0:512], lhsT=kl[hs][:, sbk * 128:(sbk + 1) * 128],
                        rhs=ql[hs][:, t0:(T + 1) * 512], start=True, stop=True),
                        r=[f'kl{hs}', f'ql{hs}'], w=[f'psc{b3}'])
                    P.act(lambda e: e.activation(out=ex[b3][:, r0:512], in_=psc[b3][:, r0:512], func=AF.Exp),
                          r=[f'psc{b3}'], w=[f'ex{b3}'])
                    P.dve(lambda e: e.tensor_tensor(
                        out=pm[b3][:, r0:512], in0=ex[b3][:, r0:512], in1=ml[m4][:, r0:512], op=ALU.mult),
                        r=[f'ex{b3}', f'ml{m4}'], w=[f'pm{b3}'])

                def back(n):
                    h, T, sbk, nsb = its[n]
                    hs = h % 2
                    a = (h * NT + T) % 2
                    b3 = n % 4
                    r0 = max(0, sbk - 4 * T) * 128
                    P.pe(lambda e: e.matmul(
                        po[a][:, r0:512], lhsT=vl[hs][:, sbk, :], rhs=pm[b3][:, r0:512],
                        start=(sbk == 0), stop=(sbk == nsb - 1)),
                        r=[f'pm{b3}', f'vl{hs}_{sbk // 4}'], w=[f'po{a}'])
                    P.pe(lambda e: e.matmul(
                        pr[a][:, r0:512], lhsT=ones_bf[:], rhs=pm[b3][:, r0:512],
                        start=(sbk == 0), stop=(sbk == nsb - 1)),
                        r=[f'pm{b3}'], w=[f'pr{a}'])
                    if sbk == nsb - 1:
                        P.act(lambda e: e.activation(out=o1[:], in_=pr[a][:], func=AF.Ln), r=[f'pr{a}'], w=['o1'])
                        P.act(lambda e: e.activation(out=rinv[:], in_=o1[:], func=AF.Exp, scale=-1.0),
                              r=['o1'], w=['rinv'])
                        P.dve(lambda e: e.tensor_tensor(out=o1[:], in0=po[a][:], in1=rinv[:], op=ALU.mult),
                              r=[f'po{a}', 'rinv'], w=['o1'])
                        P.dve(lambda e: e.tensor_tensor(out=ot[a][:], in0=o1[:], in1=gl[a][:], op=ALU.mult),
                              r=['o1', f'gl{a}'], w=[f'ot{a}'])
                        P.dma('pool', mattT[h * 128:(h + 1) * 128, TS(T)], ot[a][:], r=[f'ot{a}'], w=['dram'])

                for n in range(len(its) + DEPTH):
                    if n < len(its):
                        front(n)
                    if n >= DEPTH:
                        back(n - DEPTH)
                P.barrier()
                P.emit()

        def out_norm_phase(st, nk, load_lhs, wres, gname, resid, dst, tag, post=None):
            gp = sb(st, tag + "gp", (128, D), F32)
            o, n_ = PCOL[gname]
            P.dma('sp', gp[:], params[:, o:o + n_], w=['gp'])
            xt = [sb(st, f"{tag}xt{i}", (128, D), F32) for i in range(2)]
            xo = [sb(st, f"{tag}xo{i}", (128, D), F32) for i in range(2)]
            tmp = sb(st, tag + "tmp", (128, D), F32)
            junkA = sb(st, tag + "junkA", (128, 512), BF16)
            ssh = [sb(st, f"{tag}ssh{i}", (128, 2), F32) for i in range(2)]
            ss = [sb(st, f"{tag}ss{i}", (128, 1), F32) for i in range(2)]
            rt = [sb(st, f"{tag}rt{i}", (128, 1), F32) for i in range(2)]
            rs = [sb(st, f"{tag}rs{i}", (128, 1), F32) for i in range(2)]
            po = [ps(st, f"{tag}po{i}", (128, 512)) for i in range(4)]
            cache = {}
            nblk = NT * 4

            def S1(i):
                T, tb = divmod(i, 4)
                s2 = i % 2
                if T not in cache:
                    cache[T] = load_lhs(T)
                if tb == 1 and T + 1 < NT and (T + 1) not in cache:
                    cache[T + 1] = load_lhs(T + 1)
                lhs, lkey = cache[T]
                P.dma('sp', xt[s2][:], resid[i * 128:(i + 1) * 128, :], w=[f'xt{s2}'])
                for hh in range(2):
                    b = s2 * 2 + hh
                    for j in range(nk):
                        P.pe(lambda e, lhs=lhs, j=j, tb=tb, hh=hh, b=b: e.matmul(
                            po[b][:], lhsT=lhs[:, j, tb * 128:(tb + 1) * 128],
                            rhs=wres[:, j, hh * 512:(hh + 1) * 512], start=(j == 0), stop=(j == nk - 1)),
                            r=[lkey(j) if callable(lkey) else lkey, 'wres'], w=[f'po{b}'])

            def S1b(i):
                s2 = i % 2
                for hh in range(2):
                    b = s2 * 2 + hh
                    P.act(lambda e, b=b, s2=s2, hh=hh: e.activation(
                        out=junkA[:], in_=po[b][:], func=AF.Square, accum_out=ssh[s2][:, hh:hh + 1]),
                        r=[f'po{b}'], w=[f'ssh{s2}'])

            def S2(i):
                s2 = i % 2
                P.dve(lambda e: e.tensor_tensor(out=ss[s2][:], in0=ssh[s2][:, 0:1], in1=ssh[s2][:, 1:2],
                                                op=ALU.add), r=[f'ssh{s2}'], w=[f'ss{s2}'])
                P.act(lambda e: e.activation(out=rt[s2][:], in_=ss[s2][:], func=AF.Sqrt, bias=EPS,
                                             scale=1.0 / D), r=[f'ss{s2}'], w=[f'rt{s2}'])
                P.dve(lambda e: e.reciprocal(out=rs[s2][:], in_=rt[s2][:]), r=[f'rt{s2}'], w=[f'rs{s2}'])
                for hh in range(2):
                    b = s2 * 2 + hh
                    P.dve(lambda e, b=b, hh=hh: e.scalar_tensor_tensor(
                        out=tmp[:, hh * 512:(hh + 1) * 512], in0=po[b][:], scalar=rs[s2][:, 0:1],
                        in1=gp[:, hh * 512:(hh + 1) * 512], op0=ALU.mult, op1=ALU.mult),
                        r=[f'po{b}', f'rs{s2}', 'gp'], w=['tmp'])
                P.dve(lambda e: e.tensor_tensor(out=xo[s2][:], in0=tmp[:], in1=xt[s2][:], op=ALU.add),
                      r=['tmp', f'xt{s2}'], w=[f'xo{s2}'])
                P.dma('pool', dst[i * 128:(i + 1) * 128, :], xo[s2][:], r=[f'xo{s2}'], w=['dram'])
                if post is not None:
                    post[0](i, s2, xo[s2], f'xo{s2}')

            def S3(i):
                s2 = i % 2
                if post is not None:
                    post[1](i, s2, xo[s2], f'xo{s2}')

            for step in range(nblk + 2):
                if step < nblk:
                    S1(step)
                if 1 <= step <= nblk:
                    S2(step - 1)
                if step < nblk:
                    S1b(step)
                if 2 <= step:
                    S3(step - 2)

        h2st = ExitStack()
        if upto >= 7:
            h2T = sb(h2st, "h2T", (128, 8, S), BF16)
            with ExitStack() as st:
                wo = sb(st, "wo", (128, 8, D), BF16)
                for hh in range(2):
                    P.dma('pool', wo[:, :, hh * 512:(hh + 1) * 512],
                          wout[:, hh * 512:(hh + 1) * 512].rearrange("(k p) c -> p k c", p=128), w=['wres'])
                g2 = sb(st, "g2bc", (128, D), F32)
                o, n_ = PCOL['g2']
                P.dma('sp', g2[:], params[:, o:o + n_], w=['g2'])
                ma = [sb(st, f"ma{i}", (128, 8, 512), BF16) for i in range(2)]
                mb = [sb(st, f"mb{i}", (128, 8, 512), BF16) for i in range(2)]
                mc_ = [sb(st, f"mc{i}", (128, 8, 512), BF16) for i in range(2)]
                mg = [sb(st, f"mg{i}", (128, 8, 512), BF16) for i in range(2)]
                junkD = sb(st, "junkD", (128, D), BF16)
                hb = [sb(st, f"Ghb{i}", (128, D), BF16) for i in range(2)]
                ss2 = [sb(st, f"Gss2{i}", (128, 1), F32) for i in range(2)]
                rt2 = [sb(st, f"Grt2{i}", (128, 1), F32) for i in range(2)]
                rs2 = [sb(st, f"Grs2{i}", (128, 1), F32) for i in range(2)]
                pt = [ps(st, f"Gpt{i}", (128, D), BF16) for i in range(2)]

                def load_merged(T):
                    s = T % 2
                    for (buf, src, nm) in ((ma, mconvT, 'ma'), (mb, mattT, 'mb'), (mc_, mmemT, 'mc')):
                        for g_ in range(2):
                            P.dma('sp', buf[s][:, g_ * 4:(g_ + 1) * 4, :],
                                  src[g_ * 512:(g_ + 1) * 512, TS(T)].rearrange("(j p) t -> p j t", p=128),
                                  w=[f'{nm}{s}_{g_}'])
                    P.dve(lambda e, s=s: e.tensor_tensor(out=mg[s][:], in0=ma[s][:], in1=mb[s][:], op=ALU.add),
                          r=[f'ma{s}_0', f'ma{s}_1', f'mb{s}_0', f'mb{s}_1'], w=[f'mg{s}'])
                    P.dve(lambda e, s=s: e.tensor_tensor(out=mg[s][:], in0=mg[s][:], in1=mc_[s][:], op=ALU.add),
                          r=[f'mc{s}_0', f'mc{s}_1'], w=[f'mg{s}'])
                    return mg[s], f'mg{s}'

                def post_h2a(i, s2, xo, xkey):
                    P.dve(lambda e, s2=s2: e.scalar_tensor_tensor(
                        out=junkD[:], in0=xo[:], scalar=1.0, in1=xo[:], op0=ALU.mult, op1=ALU.mult,
                        accum_out=ss2[s2][:]), r=[xkey], w=[f'ss2{s2}'])

                def post_h2b(i, s2, xo, xkey):
                    P.act(lambda e, s2=s2: e.activation(out=rt2[s2][:], in_=ss2[s2][:], func=AF.Sqrt, bias=EPS,
                                                        scale=1.0 / D), r=[f'ss2{s2}'], w=[f'rt2{s2}'])
                    P.dve(lambda e, s2=s2: e.reciprocal(out=rs2[s2][:], in_=rt2[s2][:]),
                          r=[f'rt2{s2}'], w=[f'rs2{s2}'])
                    P.dve(lambda e, s2=s2: e.scalar_tensor_tensor(
                        out=hb[s2][:], in0=xo[:], scalar=rs2[s2][:, 0:1], in1=g2[:], op0=ALU.mult, op1=ALU.mult),
                        r=[xkey, f'rs2{s2}', 'g2'], w=[f'hb{s2}'])
                    for j in range(8):
                        P.pe(lambda e, s2=s2, j=j: e.transpose(out=pt[s2][:, j * 128:(j + 1) * 128],
                                                               in_=hb[s2][:, j * 128:(j + 1) * 128],
                                                               identity=ident[:]),
                             r=[f'hb{s2}'], w=[f'pt{s2}'])
                    P.act(lambda e, s2=s2, i=i: e.activation(
                        out=h2T[:, :, i * 128:(i + 1) * 128],
                        in_=pt[s2][:].rearrange("p (j t) -> p j t", j=8), func=AF.Copy),
                        r=[f'pt{s2}'], w=[f'h2T{i}'])

                out_norm_phase(st, 8, load_merged, wo, 'gpost1', x, x1d, "G", post=(post_h2a, post_h2b))
                P.barrier()
                P.emit()

        if upto >= 8:
            with ExitStack() as st:
                wt = [sb(st, f"Hwt{i}", (128, 8, 512), BF16) for i in range(2)]
                ug = [sb(st, f"ug{i}", (128, 514), F32) for i in range(3)]
                uvv = [sb(st, f"uv{i}", (128, 514), F32) for i in range(3)]
                cg = [sb(st, f"cg{i}", (128, 512), F32) for i in range(3)]
                cv = [sb(st, f"cv{i}", (128, 512), F32) for i in range(3)]
                sgl = [sb(st, f"sgl{i}", (128, 512), F32) for i in range(3)]
                pend = []
                ast = [sb(st, f"ast{i}", (128, S), BF16) for i in range(2)]
                pg = [ps(st, f"Hpg{i}", (128, 512)) for i in range(2)]
                pvv = [ps(st, f"Hpv{i}", (128, 512)) for i in range(2)]

                def fw(c, tap):
                    return pvc('ffnw', c * 3 + tap, 1)

                n = 0
                for wi_ in range(11):
                    s = wi_ % 2
                    P.dma('pool', wt[s][:], wup[:, wi_ * 512:(wi_ + 1) * 512].rearrange(
                        "(k p) c -> p k c", p=128), w=[f'wt{s}'])
                    for jj in range(2):
                        j = wi_ * 2 + jj
                        a2 = j % 2
                        for T in range(NT):
                            b = n % 2
                            s3 = n % 3
                            p3 = (n - 1) % 3
                            b3 = n % 3
                            n += 1
                            for k in range(8):
                                P.pe(lambda e, s=s, jj=jj, T=T, k=k, b=b: e.matmul(
                                    pg[b][:], lhsT=wt[s][:, k, jj * 256:jj * 256 + 128],
                                    rhs=h2T[:, k, TS(T)], start=(k == 0), stop=(k == 7)),
                                    r=[f'wt{s}'], w=[f'pg{b}'])
                            for k in range(8):
                                P.pe(lambda e, s=s, jj=jj, T=T, k=k, b=b: e.matmul(
                                    pvv[b][:], lhsT=wt[s][:, k, jj * 256 + 128:jj * 256 + 256],
                                    rhs=h2T[:, k, TS(T)], start=(k == 0), stop=(k == 7)),
                                    r=[f'wt{s}'], w=[f'pvv{b}'])
                            for (u_, nm_) in ((ug, 'ug'), (uvv, 'uv')):
                                if T == 0:
                                    P.pool(lambda e, u_=u_, s3=s3: e.memset(u_[s3][:, 0:2], 0.0), w=[f'{nm_}{s3}'])
                                else:
                                    P.pool(lambda e, u_=u_, s3=s3, p3=p3: e.tensor_copy(
                                        out=u_[s3][:, 0:2], in_=u_[p3][:, 512:514]),
                                        r=[f'{nm_}{p3}'], w=[f'{nm_}{s3}'])
                            for (u_, nm_, pp_, pk_, co, cname, c) in (
                                    (ug, 'ug', pg, 'pg', cg, 'cg', 2 * j), (uvv, 'uv', pvv, 'pvv', cv, 'cv', 2 * j + 1)):
                                P.act(lambda e, u_=u_, pp_=pp_, b=b, s3=s3: e.activation(
                                    out=u_[s3][:, 2:514], in_=pp_[b][:], func=AF.Copy),
                                    r=[f'{pk_}{b}'], w=[f'{nm_}{s3}'])
                                P.act(lambda e, co=co, pp_=pp_, b=b, c=c, b3=b3: e.activation(
                                    out=co[b3][:], in_=pp_[b][:], func=AF.Identity, scale=fw(c, 2),
                                    bias=pvc('ffnb', c, 1)), r=[f'{pk_}{b}'], w=[f'{cname}{b3}'])
                                for tap in (1, 0):
                                    P.dve(lambda e, u_=u_, co=co, b3=b3, s3=s3, c=c, tap=tap: e.scalar_tensor_tensor(
                                        out=co[b3][:], in0=u_[s3][:, tap:tap + 512], scalar=fw(c, tap), in1=co[b3][:],
                                        op0=ALU.mult, op1=ALU.add), r=[f'{nm_}{s3}'], w=[f'{cname}{b3}'])
                            def tail(b3=b3, a2=a2, T=T, j=j):
                                P.act(lambda e: e.activation(out=sgl[b3][:], in_=cg[b3][:], func=AF.Silu),
                                      r=[f'cg{b3}'], w=[f'sgl{b3}'])
                                P.dve(lambda e: e.tensor_tensor(
                                    out=ast[a2][:, TS(T)], in0=sgl[b3][:], in1=cv[b3][:], op=ALU.mult),
                                    r=[f'sgl{b3}', f'cv{b3}'], w=[f'ast{a2}'])
                                if T == NT - 1:
                                    P.dma('sp', aT[j * 128:(j + 1) * 128, :], ast[a2][:], r=[f'ast{a2}'], w=['dram'])
                            while pend:
                                pend.pop(0)()
                            pend.append(tail)
                while pend:
                    pend.pop(0)()
                P.barrier()
                P.emit()
        h2st.close()

        if upto >= 9:
            with ExitStack() as st:
                wd = sb(st, "wd", (128, NFC, D), BF16)
                for hh in range(2):
                    P.dma('pool', wd[:, :, hh * 512:(hh + 1) * 512],
                          wdown[:, hh * 512:(hh + 1) * 512].rearrange("(k p) c -> p k c", p=128), w=['wres'])
                at = [sb(st, f"at{i}", (128, NFC, 512), BF16) for i in range(2)]

                def load_act(T):
                    s = T % 2
                    for g_ in range(0, NFC, 4):
                        g1 = min(NFC, g_ + 4)
                        P.dma('sp', at[s][:, g_:g1, :], aT[g_ * 128:g1 * 128, TS(T)].rearrange(
                            "(j p) t -> p j t", p=128), w=[f'at{s}_{g_ // 4}'])
                    return at[s], (lambda j, s=s: f'at{s}_{j // 4}')

                out_norm_phase(st, NFC, load_act, wd, 'gpost2', x1d, y, "I")
                P.barrier()
                P.emit()


        P.barrier()
        P.emit()
    return nc


_CACHE = {}


def kernel(**inputs):
    shared = pack_inputs(inputs)
    if 'nc' not in _CACHE:
        _CACHE['nc'] = build()
    nc = _CACHE['nc']
    x = np.asarray(inputs['x'], np.float32)
    mem = np.asarray(inputs['mem'], np.float32)
    in_maps = []
    for b in range(8):
        m = dict(shared)
        m['x'] = np.ascontiguousarray(x[b])
        m['mem'] = np.ascontiguousarray(mem[b])
        in_maps.append(m)
    res = run_bass_kernel_spmd(nc, in_maps, core_ids=list(range(8)))
    return np.stack([np.asarray(r['y'], np.float32) for r in res.results], axis=0)
```

```python
import numpy as np
from contextlib import ExitStack
import concourse.bass as bass
import concourse.mybir as mybir
from concourse.bass_utils import run_bass_kernel_spmd

F32 = mybir.dt.float32
BF16 = mybir.dt.bfloat16
I32 = mybir.dt.int32
AF = mybir.ActivationFunctionType
ALU = mybir.AluOpType
AX = mybir.AxisListType

S = 4096
D = 1024
NB = S // 128
NT = S // 512
EPS = 1e-6
FFN = 2816
NFC = FFN // 128
TOPK = 256
NIT = 16
DVE_ITERS = (3, 6, 9, 12, 14)
NEG = -1.0e30

ENGS = ['pe', 'act', 'dve', 'pool', 'sp']
BLK = {'pe': 'tensor', 'act': 'scalar', 'dve': 'vector', 'pool': 'gpsimd', 'sp': 'sync'}
EPOCH = 20000
NDS = 16


class Prog:
    def __init__(self, nc, stack):
        self.nc = nc
        self.stack = stack
        self.lists = {e: [] for e in ENGS}
        self.count = {e: 0 for e in ENGS}
        self.sems = {e: [] for e in ENGS}
        self.waited = {e: {} for e in ENGS}
        self.lastw = {}
        self.reads = {}
        self.dsem = {}
        self.dval = {}
        self.dnext = {}
        self.nsem = 0

    def _esem(self, e, idx):
        while len(self.sems[e]) <= idx:
            self.sems[e].append(self.stack.enter_context(
                self.nc.semaphore(f"s_{e}{len(self.sems[e])}")))
        return self.sems[e][idx]

    def _collect(self, e, r, w, extra):
        toks = list(extra)
        for k in r:
            t = self.lastw.get(k)
            if t is not None:
                toks.append(t)
        for k in w:
            t = self.lastw.get(k)
            if t is not None:
                toks.append(t)
            toks.extend(self.reads.get(k, ()))
        need = []
        for (sid, sem, v, te) in toks:
            if te == e and e == 'pe':
                continue
            if self.waited[e].get(sid, 0) >= v:
                continue
            self.waited[e][sid] = v
            need.append((sem, v))
        return need

    def _update(self, tok, r, w):
        for k in w:
            self.lastw[k] = tok
            self.reads[k] = []
        for k in r:
            self.reads.setdefault(k, []).append(tok)

    def op(self, e, fn, r=(), w=(), extra=()):
        need = self._collect(e, r, w, extra)
        n = self.count[e]
        ep, v = divmod(n, EPOCH)
        sem = self._esem(e, ep)
        self.count[e] = n + 1
        tok = ((e, ep), sem, v + 1, e)
        self.lists[e].append((need, fn, sem, 1))
        self._update(tok, r, w)
        return tok

    def pe(self, fn, r=(), w=(), extra=()):
        return self.op('pe', fn, r, w, extra)

    def act(self, fn, r=(), w=(), extra=()):
        return self.op('act', fn, r, w, extra)

    def dve(self, fn, r=(), w=(), extra=()):
        return self.op('dve', fn, r, w, extra)

    def pool(self, fn, r=(), w=(), extra=()):
        return self.op('pool', fn, r, w, extra)

    def dma(self, e, out, in_, r=(), w=(), extra=(), **kw):
        if e not in self.dsem:
            self.dsem[e] = [self.stack.enter_context(self.nc.semaphore(f"dma_{e}{j}")) for j in range(NDS)]
            self.dval[e] = [0] * NDS
            self.dnext[e] = 0
        j = self.dnext[e]
        self.dnext[e] = (j + 1) % NDS
        sem = self.dsem[e][j]
        extra = list(extra)
        if self.dval[e][j] > 0:
            extra.append((('d', e, j), sem, self.dval[e][j], 'dma'))
        need = self._collect(e, r, w, extra)
        self.dval[e][j] += 16
        tok = (('d', e, j), sem, self.dval[e][j], 'dma')
        self.lists[e].append((need, lambda eng: eng.dma_start(out=out, in_=in_, **kw), sem, 16))
        self._update(tok, r, w)
        return tok

    def barrier(self):
        toks = []
        for e in ENGS:
            n = self.count[e]
            if n == 0:
                continue
            ep, v = divmod(n - 1, EPOCH)
            toks.append(((e, ep), self.sems[e][ep], v + 1, e))
        for q in self.dsem:
            for j in range(NDS):
                if self.dval[q][j] > 0:
                    toks.append((('d', q, j), self.dsem[q][j], self.dval[q][j], 'dma'))
        for e in ENGS:
            need = []
            for (sid, sem, v, te) in toks:
                if te == e:
                    continue
                if self.waited[e].get(sid, 0) >= v:
                    continue
                self.waited[e][sid] = v
                need.append((sem, v))
            if need:
                self.lists[e].append((need, None, None, 0))
        self.lastw = {}
        self.reads = {}

    def emit(self):
        with self.nc.Block() as block:
            for e in ENGS:
                lst = self.lists[e]
                if not lst:
                    continue

                def body(eng, lst=lst):
                    for (need, fn, sem, inc) in lst:
                        for (s, v) in need:
                            eng.wait_ge(s, v)
                        if fn is not None:
                            fn(eng).then_inc(sem, inc)

                getattr(block, BLK[e])(body)
        self.lists = {e: [] for e in ENGS}


PCOL = {}
_off = 0
for _name, _n in [('g1', D), ('gmem', D), ('gpost1', D), ('g2', D), ('gpost2', D),
                  ('convw', 8 * 31), ('convb', 8), ('lng', 8), ('lnb', 8),
                  ('bgate', 24), ('ffnw', 44 * 3), ('ffnb', 44)]:
    PCOL[_name] = (_off, _n)
    _off += _n
NPCOL = _off

WCM_COLS = 45 * 128
WTM_COLS = 1032


def pack_inputs(inp):
    f = np.float32
    w_in = np.asarray(inp['w_in'][0], f)
    c0 = 0
    conv = w_in[:, 0:2048]
    q = w_in[:, 2048:3072]
    k = w_in[:, 3072:4096]
    v = w_in[:, 4096:5120]
    qi = w_in[:, 5120:5632]
    wi = w_in[:, 5632:5640]
    ki = w_in[:, 5640:5704]
    qm = w_in[:, 5704:6728]
    a, g = conv[:, :1024], conv[:, 1024:]
    inter = np.stack([a.reshape(D, 8, 128), g.reshape(D, 8, 128)], axis=2).reshape(D, 2048)
    wcm = np.ascontiguousarray(np.concatenate([inter, q, k, qm, qi, ki, ki], axis=1))
    wtm = np.ascontiguousarray(np.concatenate([v, wi], axis=1))
    w_up = np.asarray(inp['w_up'][0], f)
    ug, uv = w_up[:, :FFN], w_up[:, FFN:]
    wup = np.ascontiguousarray(
        np.stack([ug.reshape(D, NFC, 128), uv.reshape(D, NFC, 128)], axis=2).reshape(D, 2 * FFN))
    P = np.zeros((128, NPCOL), f)

    def put(name, arr):
        o, n = PCOL[name]
        P[:, o:o + n] = arr

    put('g1', np.broadcast_to(inp['norm1_pre_g'][0], (128, D)))
    put('gmem', np.broadcast_to(inp['mem_norm_g'][0], (128, D)))
    put('gpost1', np.broadcast_to(inp['norm1_post_g'][0], (128, D)))
    put('g2', np.broadcast_to(inp['norm2_pre_g'][0], (128, D)))
    put('gpost2', np.broadcast_to(inp['norm2_post_g'][0], (128, D)))
    cw = np.asarray(inp['conv_dw_w'][0], f)
    put('convw', cw.T.reshape(8, 128, 31).transpose(1, 0, 2).reshape(128, 8 * 31))
    put('convb', np.asarray(inp['conv_dw_b'][0], f).reshape(8, 128).T)
    put('lng', np.asarray(inp['conv_ln_g'][0], f).reshape(8, 128).T)
    put('lnb', np.asarray(inp['conv_ln_b'][0], f).reshape(8, 128).T)
    put('bgate', np.asarray(inp['b_gate'][0], f).reshape(24, 128).T)
    fw = np.asarray(inp['ffn_dw_w'][0], f)
    fwg, fwv = fw[:, :FFN], fw[:, FFN:]
    fwi = np.stack([fwg.reshape(3, NFC, 128), fwv.reshape(3, NFC, 128)], axis=2).reshape(3, 44, 128)
    put('ffnw', fwi.transpose(2, 1, 0).reshape(128, 44 * 3))
    fb = np.asarray(inp['ffn_dw_b'][0], f)
    fbi = np.stack([fb[:FFN].reshape(NFC, 128), fb[FFN:].reshape(NFC, 128)], axis=1).reshape(44, 128)
    put('ffnb', fbi.T)
    shared = {
        'wcm': wcm, 'wtm': wtm, 'wup': wup, 'params': P,
        'pw2': np.ascontiguousarray(inp['conv_pw2'][0], f),
        'wmkv': np.ascontiguousarray(inp['w_mem_kv'][0], f),
        'wgate': np.ascontiguousarray(inp['w_gate'][0], f),
        'wout': np.ascontiguousarray(inp['w_out'][0], f),
        'wdown': np.ascontiguousarray(inp['w_down'][0], f),
    }
    return shared


def build(debug=(), upto=99):
    nc = bass.Bass("TRN2", target_bir_lowering=False)
    dbg = set(debug)

    def din(name, shape):
        return nc.dram_tensor(name, list(shape), F32, kind="ExternalInput").ap()

    def scratch(name, shape, dt):
        kind = "ExternalOutput" if name in dbg else "Internal"
        return nc.dram_tensor(name, list(shape), dt, kind=kind).ap()

    x = din("x", (S, D))
    mem = din("mem", (256, D))
    wcm = din("wcm", (D, WCM_COLS))
    wtm = din("wtm", (D, WTM_COLS))
    wup = din("wup", (D, 2 * FFN))
    params = din("params", (128, NPCOL))
    pw2 = din("pw2", (D, D))
    wmkv = din("wmkv", (D, 2 * D))
    wgate = din("wgate", (D, 3 * D))
    wout = din("wout", (D, D))
    wdown = din("wdown", (FFN, D))
    y = nc.dram_tensor("y", [S, D], F32, kind="ExternalOutput").ap()

    ycT = scratch("ycT", (D, S), F32)
    qT = scratch("qT", (D, S), BF16)
    kT = scratch("kT", (D, S), BF16)
    qmT = scratch("qmT", (D, S), BF16)
    qiT = scratch("qiT", (512, S), BF16)
    kiT = scratch("kiT", (128, S), BF16)
    vtm = scratch("vtm", (S, D), BF16)
    witm = scratch("witm", (S, 8), F32)
    gT = scratch("gT", (3 * D, S), BF16)
    mconvT = scratch("mconvT", (D, S), BF16)
    mattT = scratch("mattT", (D, S), BF16)
    mmemT = scratch("mmemT", (D, S), BF16)
    maskT = scratch("maskT", (S, S), BF16)
    x1d = scratch("x1d", (S, D), F32)
    aT = scratch("aT", (FFN, S), BF16)

    def pcols(t, name):
        o, n = PCOL[name]
        return t[:, o:o + n]

    with ExitStack() as gs:
        P = Prog(nc, gs)

        uniq = [0]

        def sb(st, name, shape, dt):
            uniq[0] += 1
            return st.enter_context(nc.sbuf_tensor(f"{name}_{uniq[0]}", list(shape), dt))

        def ps(st, name, shape, dt=F32):
            uniq[0] += 1
            return st.enter_context(nc.psum_tensor(f"{name}_{uniq[0]}", list(shape), dt))

        ident = sb(gs, "ident", (128, 128), BF16)
        ones_bf = sb(gs, "ones_bf", (128, 128), BF16)
        ones_f = sb(gs, "ones_f", (128, 128), F32)
        caus = sb(gs, "caus", (128, 128), F32)
        pv = sb(gs, "pv", (128, 472), F32)
        pvo = PCOL['convw'][0]

        def pvc(name, j0=0, n=None):
            o, nn = PCOL[name]
            o -= pvo
            if n is None:
                n = nn
            return pv[:, o + j0:o + j0 + n]

        with ExitStack() as st:
            iot = sb(st, "iot", (128, 128), I32)
            P.pool(lambda e: e.iota(out=iot[:], pattern=[[1, 128]], base=0, channel_multiplier=-1),
                   w=['iot'])
            P.dve(lambda e: e.tensor_scalar(out=ident[:], in0=iot[:], scalar1=0.0, scalar2=None,
                                            op0=ALU.is_equal), r=['iot'], w=['ident'])
            P.dve(lambda e: e.tensor_scalar(out=caus[:], in0=iot[:], scalar1=0.5, scalar2=NEG,
                                            op0=ALU.is_gt, op1=ALU.mult), r=['iot'], w=['caus'])
            P.dve(lambda e: e.memset(ones_bf[:], 1.0), w=['ones_bf'])
            P.dve(lambda e: e.memset(ones_f[:], 1.0), w=['ones_f'])
            P.dma('sp', pv[:], params[:, pvo:pvo + 472], w=['pv'])
            P.barrier()
            P.emit()

        hst = ExitStack()
        hT = sb(hst, "hT", (128, 8, S), BF16)

        def norm_transpose_phase(st, src, nblk, gname, dstT, tag):
            gbc = sb(st, tag + "gbc", (128, D), F32)
            xt = [sb(st, f"{tag}xt{i}", (128, D), F32) for i in range(2)]
            junk = sb(st, tag + "junk", (128, D), BF16)
            hb = [sb(st, f"{tag}hb{i}", (128, D), BF16) for i in range(2)]
            ss = [sb(st, f"{tag}ss{i}", (128, 1), F32) for i in range(2)]
            rt = [sb(st, f"{tag}rt{i}", (128, 1), F32) for i in range(2)]
            rs = [sb(st, f"{tag}rs{i}", (128, 1), F32) for i in range(2)]
            pt = [ps(st, f"{tag}pt{i}", (128, D), BF16) for i in range(2)]
            o, n = PCOL[gname]
            P.dma('sp', gbc[:], params[:, o:o + n], w=['gbc'])
            def N1(i):
                s = i % 2
                P.dma('sp', xt[s][:], src[i * 128:(i + 1) * 128, :], w=[f'xt{s}'])
                P.dve(lambda e: e.scalar_tensor_tensor(
                    out=junk[:], in0=xt[s][:], scalar=1.0, in1=xt[s][:], op0=ALU.mult, op1=ALU.mult,
                    accum_out=ss[s][:]), r=[f'xt{s}'], w=[f'ss{s}'])
                P.act(lambda e: e.activation(out=rt[s][:], in_=ss[s][:], func=AF.Sqrt,
                                             bias=EPS, scale=1.0 / D), r=[f'ss{s}'], w=[f'rt{s}'])
                P.dve(lambda e: e.reciprocal(out=rs[s][:], in_=rt[s][:]), r=[f'rt{s}'], w=[f'rs{s}'])
                P.dve(lambda e: e.scalar_tensor_tensor(
                    out=hb[s][:], in0=xt[s][:], scalar=rs[s][:], in1=gbc[:], op0=ALU.mult, op1=ALU.mult),
                    r=[f'xt{s}', f'rs{s}', 'gbc'], w=[f'hb{s}'])

            def N2(i):
                s = i % 2
                for j in range(8):
                    P.pe(lambda e, j=j: e.transpose(out=pt[s][:, j * 128:(j + 1) * 128],
                                                    in_=hb[s][:, j * 128:(j + 1) * 128],
                                                    identity=ident[:]), r=[f'hb{s}'], w=[f'pt{s}'])
                P.act(lambda e: e.activation(
                    out=dstT[:, :, i * 128:(i + 1) * 128],
                    in_=pt[s][:].rearrange("p (j t) -> p j t", j=8), func=AF.Copy),
                    r=[f'pt{s}'], w=[f'dstT{i}'])

            N1(0)
            for i in range(nblk):
                if i + 1 < nblk:
                    N1(i + 1)
                N2(i)

        with ExitStack() as st:
            norm_transpose_phase(st, x, NB, 'g1', hT, "A")
            P.barrier()
            P.emit()

        def proj_cm(st, W, ncols, evac, tag, wtile=512):
            wt = [sb(st, f"{tag}wt{i}", (128, 8, wtile), BF16) for i in range(2)]
            pp = [ps(st, f"{tag}pp{i}", (128, 512), F32) for i in range(4)]
            ntile = (ncols + wtile - 1) // wtile
            cnt = 0
            for wi_ in range(ntile):
                s = wi_ % 2
                c0 = wi_ * wtile
                wc = min(wtile, ncols - c0)
                P.dma('pool', wt[s][:, :, 0:wc],
                      W[:, c0:c0 + wc].rearrange("(k p) c -> p k c", p=128), w=[f'wt{s}'])
                for cc in range(wc // 128):
                    c = (c0 // 128) + cc
                    for T in range(NT):
                        b = cnt % 4
                        cnt += 1
                        for k in range(8):
                            P.pe(lambda e, s=s, cc=cc, T=T, k=k, b=b: e.matmul(
                                pp[b][:], lhsT=wt[s][:, k, cc * 128:(cc + 1) * 128],
                                rhs=hT[:, k, T * 512:(T + 1) * 512], start=(k == 0), stop=(k == 7)),
                                r=[f'wt{s}'], w=[f'pp{b}'])
                        evac(c, T, pp[b], f'pp{b}')

        if upto >= 2:
            with ExitStack() as st:
                NDT = 9
                yg = [sb(st, f"yg{i}", (128, 30 + 512), F32) for i in range(3)]
                ygb = [sb(st, f"ygb{i}", (128, 30 + 512), BF16) for i in range(3)]
                sg = [sb(st, f"sg{i}", (128, 512), F32) for i in range(2)]
                acc = [sb(st, f"acc{i}", (128, 512), F32) for i in range(2)]
                yc = [sb(st, f"yc{i}", (128, 512), F32) for i in range(2)]
                dg = [sb(st, f"dg{i}", (128, 31 - NDT, 128), BF16) for i in range(2)]
                wt = [sb(st, f"Bwt{i}", (128, 8, 512), BF16) for i in range(2)]
                pa = [ps(st, f"Bpa{i}", (128, 512), F32) for i in range(2)]
                pg = [ps(st, f"Bpg{i}", (128, 512), F32) for i in range(2)]
                pc = [ps(st, f"Bpc{i}", (128, 512), F32) for i in range(2)]
                n = 0
                cpend = []
                for wi_ in range(4):
                    s = wi_ % 2
                    P.dma('pool', wt[s][:], wcm[:, wi_ * 512:(wi_ + 1) * 512].rearrange(
                        "(k p) c -> p k c", p=128), w=[f'wt{s}'])
                    for jj in range(2):
                        j = wi_ * 2 + jj
                        js = j % 2
                        while cpend:
                            cpend.pop(0)()
                        for jn in ([0, 1] if j == 0 else ([j + 1] if j + 1 < 8 else [])):
                            for tap in range(NDT, 31):
                                P.pool(lambda e, jn=jn, tap=tap: e.tensor_scalar(
                                    out=dg[jn % 2][:, tap - NDT, :], in0=ident[:],
                                    scalar1=pvc('convw', jn * 31 + tap, 1), scalar2=0.0,
                                    op0=ALU.mult, op1=ALU.add), w=[f'dg{jn % 2}'])
                        for T in range(NT):
                            b = n % 2
                            s3 = n % 3
                            p3 = (n - 1) % 3
                            for k in range(8):
                                P.pe(lambda e, s=s, jj=jj, T=T, k=k, b=b: e.matmul(
                                    pa[b][:], lhsT=wt[s][:, k, jj * 256:jj * 256 + 128],
                                    rhs=hT[:, k, T * 512:(T + 1) * 512], start=(k == 0), stop=(k == 7)),
                                    r=[f'wt{s}'], w=[f'pa{b}'])
                            for k in range(8):
                                P.pe(lambda e, s=s, jj=jj, T=T, k=k, b=b: e.matmul(
                                    pg[b][:], lhsT=wt[s][:, k, jj * 256 + 128:jj * 256 + 256],
                                    rhs=hT[:, k, T * 512:(T + 1) * 512], start=(k == 0), stop=(k == 7)),
                                    r=[f'wt{s}'], w=[f'pg{b}'])
                            P.act(lambda e, b=b: e.activation(out=sg[b][:], in_=pg[b][:], func=AF.Sigmoid),
                                  r=[f'pg{b}'], w=[f'sg{b}'])
                            if T == 0:
                                P.pool(lambda e, s3=s3: e.memset(yg[s3][:, 0:30], 0.0), w=[f'yg{s3}'])
                                P.pool(lambda e, s3=s3: e.memset(ygb[s3][:, 0:30], 0.0), w=[f'ygb{s3}'])
                            else:
                                P.pool(lambda e, s3=s3, p3=p3: e.tensor_copy(
                                    out=yg[s3][:, 0:30], in_=yg[p3][:, 512:542]),
                                    r=[f'yg{p3}'], w=[f'yg{s3}'])
                                P.pool(lambda e, s3=s3, p3=p3: e.tensor_copy(
                                    out=ygb[s3][:, 0:30], in_=ygb[p3][:, 512:542]),
                                    r=[f'ygb{p3}'], w=[f'ygb{s3}'])
                            P.dve(lambda e, b=b, s3=s3: e.tensor_tensor(
                                out=yg[s3][:, 30:542], in0=pa[b][:], in1=sg[b][:], op=ALU.mult),
                                r=[f'pa{b}', f'sg{b}'], w=[f'yg{s3}'])
                            P.act(lambda e, s3=s3: e.activation(out=ygb[s3][:, 30:542], in_=yg[s3][:, 30:542],
                                                                func=AF.Copy),
                                  r=[f'yg{s3}'], w=[f'ygb{s3}'])
                            def taps(b=b, s3=s3, js=js):
                                for tap in range(NDT, 31):
                                    P.pe(lambda e, tap=tap: e.matmul(
                                        pc[b][:], lhsT=dg[js][:, tap - NDT, :], rhs=ygb[s3][:, tap:tap + 512],
                                        start=(tap == NDT), stop=(tap == 30)),
                                        r=[f'dg{js}', f'ygb{s3}'], w=[f'pc{b}'])
                            P.dve(lambda e, b=b, s3=s3, j=j: e.tensor_scalar(
                                out=acc[b][:], in0=yg[s3][:, 0:512], scalar1=pvc('convw', j * 31, 1),
                                scalar2=pvc('convb', j, 1), op0=ALU.mult, op1=ALU.add),
                                r=[f'yg{s3}'], w=[f'acc{b}'])
                            for tap in range(1, NDT):
                                P.dve(lambda e, b=b, s3=s3, j=j, tap=tap: e.scalar_tensor_tensor(
                                    out=acc[b][:], in0=yg[s3][:, tap:tap + 512],
                                    scalar=pvc('convw', j * 31 + tap, 1), in1=acc[b][:],
                                    op0=ALU.mult, op1=ALU.add),
                                    r=[f'yg{s3}'], w=[f'acc{b}'])
                            def fin_(b=b, j=j, T=T, taps=taps):
                                taps()
                                P.dve(lambda e: e.tensor_tensor(out=yc[b][:], in0=pc[b][:], in1=acc[b][:],
                                                                op=ALU.add),
                                      r=[f'pc{b}', f'acc{b}'], w=[f'yc{b}'])
                                P.dma('sp', ycT[j * 128:(j + 1) * 128, T * 512:(T + 1) * 512], yc[b][:],
                                      r=[f'yc{b}'], w=['dram'])
                            while cpend:
                                cpend.pop(0)()
                            cpend.append(fin_)
                            n += 1
                while cpend:
                    cpend.pop(0)()
                P.barrier()
                P.emit()

            with ExitStack() as st:
                stg = [sb(st, f"stg{i}", (128, S), BF16) for i in range(2)]

                def evac_gen(c, T, pp, key):
                    c = c + 16
                    if c < 24:
                        dst, row, scale = qT, (c - 16) * 128, 128.0 ** -0.5
                    elif c < 32:
                        dst, row, scale = kT, (c - 24) * 128, 1.0
                    elif c < 40:
                        dst, row, scale = qmT, (c - 32) * 128, 256.0 ** -0.5
                    elif c < 44:
                        dst, row, scale = qiT, (c - 40) * 128, 1.0
                    else:
                        dst, row, scale = kiT, 0, 1.0
                    s2 = c % 2
                    P.act(lambda e: e.activation(out=stg[s2][:, T * 512:(T + 1) * 512], in_=pp[:],
                                                 func=AF.Copy, scale=scale),
                          r=[key], w=[f'stg{s2}'])
                    if T == NT - 1:
                        P.dma('sp', dst[row:row + 128, :], stg[s2][:], r=[f'stg{s2}'], w=['dram'])

                proj_cm(st, wcm[:, 2048:], WCM_COLS - 2048, evac_gen, "Bq")
                P.barrier()
                P.emit()

            with ExitStack() as st:
                stg = [sb(st, f"stg{i}", (128, S), BF16) for i in range(2)]

                def evac_gate(c, T, pp, key):
                    s2 = c % 2
                    P.act(lambda e: e.activation(out=stg[s2][:, T * 512:(T + 1) * 512], in_=pp[:],
                                                 func=AF.Sigmoid, bias=pvc('bgate', c, 1), scale=1.0),
                          r=[key], w=[f'stg{s2}'])
                    if T == NT - 1:
                        P.dma('sp', gT[c * 128:(c + 1) * 128, :], stg[s2][:], r=[f'stg{s2}'], w=['dram'])

                proj_cm(st, wgate, 3 * D, evac_gate, "Bg")
                P.barrier()
                P.emit()

            with ExitStack() as st:
                wv = sb(st, "wv", (128, 8, WTM_COLS), BF16)
                vst = [sb(st, f"vst{i}", (128, D), BF16) for i in range(2)]
                wst = [sb(st, f"wst{i}", (128, 8), F32) for i in range(2)]
                pvv = [ps(st, f"pvv{i}", (128, 512), F32) for i in range(4)]
                pw = [ps(st, f"pw{i}", (128, 8), F32) for i in range(2)]
                for hh in range(2):
                    P.dma('pool', wv[:, :, hh * 512:(hh + 1) * 512],
                          wtm[:, hh * 512:(hh + 1) * 512].rearrange("(k p) c -> p k c", p=128), w=['wv'])
                P.dma('pool', wv[:, :, 1024:1032],
                      wtm[:, 1024:1032].rearrange("(k p) c -> p k c", p=128), w=['wv'])
                for i in range(NB):
                    s = i % 2
                    for hh in range(2):
                        b = (i * 2 + hh) % 4
                        for k in range(8):
                            P.pe(lambda e, i=i, hh=hh, k=k, b=b: e.matmul(
                                pvv[b][:], lhsT=hT[:, k, i * 128:(i + 1) * 128],
                                rhs=wv[:, k, hh * 512:(hh + 1) * 512], start=(k == 0), stop=(k == 7)),
                                r=['wv'], w=[f'pvv{b}'])
                        P.act(lambda e, s=s, hh=hh, b=b: e.activation(
                            out=vst[s][:, hh * 512:(hh + 1) * 512], in_=pvv[b][:], func=AF.Copy),
                            r=[f'pvv{b}'], w=[f'vst{s}'])
                    for k in range(8):
                        P.pe(lambda e, i=i, k=k, s=s: e.matmul(
                            pw[s][:], lhsT=hT[:, k, i * 128:(i + 1) * 128],
                            rhs=wv[:, k, 1024:1032], start=(k == 0), stop=(k == 7)),
                            r=['wv'], w=[f'pw{s}'])
                    P.dve(lambda e, s=s: e.tensor_copy(out=wst[s][:], in_=pw[s][:]),
                          r=[f'pw{s}'], w=[f'wst{s}'])
                    P.dma('sp', vtm[i * 128:(i + 1) * 128, :], vst[s][:], r=[f'vst{s}'], w=['dram'])
                    P.dma('sp', witm[i * 128:(i + 1) * 128, :], wst[s][:], r=[f'wst{s}'], w=['dram'])
                P.barrier()
                P.emit()
        hst.close()

        def TS(T):
            return slice(T * 512, (T + 1) * 512)

        if upto >= 3:
            with ExitStack() as st:
                pwt = sb(st, "pwt", (128, 8, D), BF16)
                for hh in range(2):
                    P.dma('pool', pwt[:, :, hh * 512:(hh + 1) * 512],
                          pw2[:, hh * 512:(hh + 1) * 512].rearrange("(k p) c -> p k c", p=128), w=['pwt'])
                ycl = [sb(st, f"ycl{i}", (128, 8, 512), F32) for i in range(2)]
                sq = sb(st, "sq", (128, 8, 512), F32)
                gl = [sb(st, f"gl{i}", (128, 8, 512), BF16) for i in range(2)]
                mean = [sb(st, f"mean{i}", (128, 512), F32) for i in range(2)]
                msq = sb(st, "msq", (128, 512), F32)
                var = sb(st, "var", (128, 512), F32)
                sd = sb(st, "sd", (128, 512), F32)
                rstd = [sb(st, f"rstd{i}", (128, 512), F32) for i in range(2)]
                t1 = [sb(st, f"t1{i}", (128, 512), F32) for i in range(2)]
                ys = [sb(st, f"ys{i}", (128, 8, 512), BF16) for i in range(2)]
                ot = [sb(st, f"ot{i}", (128, 512), BF16) for i in range(2)]
                s1 = ps(st, "s1", (128, 512))
                s2 = ps(st, "s2", (128, 512))
                po = [ps(st, f"po{i}", (128, 512)) for i in range(2)]

                def C0(T):
                    s = T % 2
                    for g_ in range(2):
                        P.dma('sp', ycl[s][:, g_ * 4:(g_ + 1) * 4, :],
                              ycT[g_ * 512:(g_ + 1) * 512, TS(T)].rearrange("(j p) t -> p j t", p=128),
                              w=[f'ycl{s}_{g_}'])
                        P.dma('sp', gl[s][:, g_ * 4:(g_ + 1) * 4, :],
                              gT[g_ * 512:(g_ + 1) * 512, TS(T)].rearrange("(j p) t -> p j t", p=128),
                              w=[f'gl{s}_{g_}'])

                def C1(T):
                    s = T % 2
                    yk = [f'ycl{s}_0', f'ycl{s}_1']
                    P.act(lambda e: e.activation(out=sq[:], in_=ycl[s][:], func=AF.Square), r=yk, w=['sq'])
                    for j in range(8):
                        P.pe(lambda e, j=j: e.matmul(s1[:], lhsT=ones_f[:], rhs=ycl[s][:, j, :],
                                                     start=(j == 0), stop=(j == 7)), r=yk, w=['s1'])
                    for j in range(8):
                        P.pe(lambda e, j=j: e.matmul(s2[:], lhsT=ones_f[:], rhs=sq[:, j, :],
                                                     start=(j == 0), stop=(j == 7)), r=['sq'], w=['s2'])
                    P.dve(lambda e: e.tensor_scalar(out=mean[s][:], in0=s1[:], scalar1=1.0 / D, scalar2=None,
                                                    op0=ALU.mult), r=['s1'], w=[f'mean{s}'])
                    P.dve(lambda e: e.tensor_tensor(out=msq[:], in0=mean[s][:], in1=mean[s][:], op=ALU.mult),
                          r=[f'mean{s}'], w=['msq'])
                    P.dve(lambda e: e.scalar_tensor_tensor(out=var[:], in0=s2[:], scalar=1.0 / D, in1=msq[:],
                                                           op0=ALU.mult, op1=ALU.subtract),
                          r=['s2', 'msq'], w=['var'])
                    P.act(lambda e: e.activation(out=sd[:], in_=var[:], func=AF.Sqrt, bias=EPS, scale=1.0),
                          r=['var'], w=['sd'])
                    P.dve(lambda e: e.reciprocal(out=rstd[s][:], in_=sd[:]), r=['sd'], w=[f'rstd{s}'])

                def C2(T):
                    s = T % 2
                    yk = [f'ycl{s}_0', f'ycl{s}_1']
                    for j in range(8):
                        b = j % 2
                        P.dve(lambda e, j=j, b=b: e.tensor_tensor(
                            out=t1[b][:], in0=ycl[s][:, j, :], in1=mean[s][:], op=ALU.subtract),
                            r=yk + [f'mean{s}'], w=[f't1{b}'])
                        P.dve(lambda e, b=b: e.tensor_tensor(out=t1[b][:], in0=t1[b][:], in1=rstd[s][:],
                                                             op=ALU.mult), r=[f'rstd{s}'], w=[f't1{b}'])
                        P.act(lambda e, j=j, b=b: e.activation(out=ys[s][:, j, :], in_=t1[b][:], func=AF.Silu,
                                                               scale=pvc('lng', j, 1), bias=pvc('lnb', j, 1)),
                              r=[f't1{b}'], w=[f'ys{s}'])

                def C3(T):
                    s = T % 2
                    for dch in range(8):
                        b = dch % 2
                        for j in range(8):
                            P.pe(lambda e, dch=dch, j=j, b=b: e.matmul(
                                po[b][:], lhsT=pwt[:, j, dch * 128:(dch + 1) * 128], rhs=ys[s][:, j, :],
                                start=(j == 0), stop=(j == 7)), r=[f'ys{s}', 'pwt'], w=[f'po{b}'])
                        P.dve(lambda e, dch=dch, b=b: e.tensor_tensor(
                            out=ot[b][:], in0=po[b][:], in1=gl[s][:, dch, :], op=ALU.mult),
                            r=[f'po{b}', f'gl{s}_0', f'gl{s}_1'], w=[f'ot{b}'])
                        P.dma('pool', mconvT[dch * 128:(dch + 1) * 128, TS(T)], ot[b][:], r=[f'ot{b}'], w=['dram'])

                C0(0)
                C1(0)
                for T in range(NT):
                    if T + 1 < NT:
                        C0(T + 1)
                    C2(T)
                    if T + 1 < NT:
                        C1(T + 1)
                    C3(T)
                P.barrier()
                P.emit()

        if upto >= 4:
            with ExitStack() as st0:
                mnT = sb(st0, "mnT", (128, 8, 256), BF16)
                mkT = sb(st0, "mkT", (128, 8, 256), BF16)
                mv = sb(st0, "mv", (128, 2, D), BF16)
                with ExitStack() as st:
                    norm_transpose_phase(st, mem, 2, 'gmem', mnT, "D")
                    P.barrier()
                    P.emit()
                with ExitStack() as st:
                    wk = [sb(st, f"wk{i}", (128, 8, 512), BF16) for i in range(2)]
                    pk = [ps(st, f"pk{i}", (128, 512)) for i in range(2)]
                    pmv = [ps(st, f"pmv{i}", (128, 512)) for i in range(2)]
                    for wi_ in range(2):
                        s = wi_ % 2
                        P.dma('pool', wk[s][:], wmkv[:, wi_ * 512:(wi_ + 1) * 512].rearrange(
                            "(k p) c -> p k c", p=128), w=[f'wk{s}'])
                        for cc in range(4):
                            c = wi_ * 4 + cc
                            b = c % 2
                            for k in range(8):
                                P.pe(lambda e, s=s, cc=cc, k=k, b=b: e.matmul(
                                    pk[b][:, 0:256], lhsT=wk[s][:, k, cc * 128:(cc + 1) * 128],
                                    rhs=mnT[:, k, :], start=(k == 0), stop=(k == 7)),
                                    r=[f'wk{s}'], w=[f'pk{b}'])
                            P.act(lambda e, c=c, b=b: e.activation(out=mkT[:, c, :], in_=pk[b][:, 0:256],
                                                                   func=AF.Copy), r=[f'pk{b}'], w=['mkT'])
                    for wi_ in range(2):
                        s = wi_ % 2
                        P.dma('pool', wk[s][:], wmkv[:, 1024 + wi_ * 512:1024 + (wi_ + 1) * 512].rearrange(
                            "(k p) c -> p k c", p=128), w=[f'wk{s}'])
                        for mc in range(2):
                            for k in range(8):
                                P.pe(lambda e, s=s, mc=mc, k=k: e.matmul(
                                    pmv[mc][:], lhsT=mnT[:, k, mc * 128:(mc + 1) * 128], rhs=wk[s][:, k, :],
                                    start=(k == 0), stop=(k == 7)), r=[f'wk{s}'], w=[f'pmv{mc}'])
                            P.act(lambda e, wi_=wi_, mc=mc: e.activation(
                                out=mv[:, mc, wi_ * 512:(wi_ + 1) * 512], in_=pmv[mc][:], func=AF.Copy),
                                r=[f'pmv{mc}'], w=['mv'])
                    P.barrier()
                    P.emit()
                with ExitStack() as st:
                    qml = [sb(st, f"qml{i}", (128, 2, 512), BF16) for i in range(2)]
                    gl = [sb(st, f"gl{i}", (128, 2, 512), BF16) for i in range(2)]
                    pT = [sb(st, f"pT{i}", (128, 2, 512), BF16) for i in range(2)]
                    rinv = sb(st, "rinv", (128, 512), F32)
                    o1 = [sb(st, f"o1{i}", (128, 512), F32) for i in range(2)]
                    ot = [sb(st, f"ot{i}", (128, 512), BF16) for i in range(2)]
                    pl = [ps(st, f"pl{i}", (128, 512)) for i in range(2)]
                    po = [ps(st, f"po{i}", (128, 512)) for i in range(2)]
                    pr = ps(st, "pr", (128, 512))
                    n = 0
                    for hm in range(4):
                        for T in range(NT):
                            s = n % 2
                            n += 1
                            P.dma('sp', qml[s][:], qmT[hm * 256:(hm + 1) * 256, TS(T)].rearrange(
                                "(c p) t -> p c t", p=128), w=[f'qml{s}'])
                            P.dma('sp', gl[s][:], gT[2048 + hm * 256:2048 + (hm + 1) * 256, TS(T)].rearrange(
                                "(c p) t -> p c t", p=128), w=[f'gl{s}'])
                            for mc in range(2):
                                for dmc in range(2):
                                    P.pe(lambda e, s=s, hm=hm, mc=mc, dmc=dmc: e.matmul(
                                        pl[mc][:], lhsT=mkT[:, hm * 2 + dmc, mc * 128:(mc + 1) * 128],
                                        rhs=qml[s][:, dmc, :], start=(dmc == 0), stop=(dmc == 1)),
                                        r=[f'qml{s}'], w=[f'pl{mc}'])
                                P.act(lambda e, s=s, mc=mc: e.activation(out=pT[s][:, mc, :], in_=pl[mc][:],
                                                                         func=AF.Exp),
                                      r=[f'pl{mc}'], w=[f'pT{s}'])
                            for dmc in range(2):
                                for mc in range(2):
                                    P.pe(lambda e, s=s, hm=hm, mc=mc, dmc=dmc: e.matmul(
                                        po[dmc][:], lhsT=mv[:, mc, hm * 256 + dmc * 128:hm * 256 + (dmc + 1) * 128],
                                        rhs=pT[s][:, mc, :], start=(mc == 0), stop=(mc == 1)),
                                        r=[f'pT{s}'], w=[f'po{dmc}'])
                            for mc in range(2):
                                P.pe(lambda e, s=s, mc=mc: e.matmul(pr[:], lhsT=ones_bf[:], rhs=pT[s][:, mc, :],
                                                                    start=(mc == 0), stop=(mc == 1)),
                                     r=[f'pT{s}'], w=['pr'])
                            P.act(lambda e: e.activation(out=o1[0][:], in_=pr[:], func=AF.Ln), r=['pr'], w=['o10'])
                            P.act(lambda e: e.activation(out=rinv[:], in_=o1[0][:], func=AF.Exp, scale=-1.0),
                                  r=['o10'], w=['rinv'])
                            for dmc in range(2):
                                P.dve(lambda e, dmc=dmc: e.tensor_tensor(out=o1[dmc][:], in0=po[dmc][:], in1=rinv[:],
                                                                         op=ALU.mult),
                                      r=[f'po{dmc}', 'rinv'], w=[f'o1{dmc}'])
                                P.dve(lambda e, s=s, dmc=dmc: e.tensor_tensor(
                                    out=ot[dmc][:], in0=o1[dmc][:], in1=gl[s][:, dmc, :], op=ALU.mult),
                                    r=[f'o1{dmc}', f'gl{s}'], w=[f'ot{dmc}'])
                                P.dma('pool', mmemT[hm * 256 + dmc * 128:hm * 256 + (dmc + 1) * 128, TS(T)],
                                      ot[dmc][:], r=[f'ot{dmc}'], w=['dram'])
                    P.barrier()
                    P.emit()

        if upto >= 5:
            with ExitStack() as st:
                kil = sb(st, "kil", (128, S), BF16)
                qil = [sb(st, f"qil{i}", (128, 4, 128), BF16) for i in range(2)]
                wil = [sb(st, f"wil{i}", (128, 8), F32) for i in range(2)]
                score = [sb(st, f"score{i}", (128, S), F32) for i in range(4)]
                rl = [sb(st, f"rl{i}", (128, 512), F32) for i in range(3)]
                junk = sb(st, "junkE", (128, S), BF16)
                junkD = sb(st, "junkDE", (128, S), BF16)
                mk = [sb(st, f"mk{i}", (128, S), BF16) for i in range(2)]
                mT = [sb(st, f"mT{i}", (128, 8, 128), BF16) for i in range(2)]
                ptab = sb(st, "ptab", (128, NIT + 1), F32)
                thrc = sb(st, "thrc", (128, 1), F32)

                def two(name, shape):
                    return [sb(st, f"{name}{c}", shape, F32) for c in range(2)]
                lo = two("lo", (128, 1))
                hi = two("hi", (128, 1))
                Wd = two("Wd", (128, 1))
                hneg = two("hneg", (128, NIT + 1))
                nm = two("nm", (128, 1))
                ssum = two("ssum", (128, 1))
                sgn = two("sgn", (128, 1))
                thr_t = two("thr_t", (128, 1))
                midp = two("midp", (128, 1))
                cntd = two("cntd", (128, 1))
                t2 = two("t2", (128, 1))
                pd = [ps(st, f"pd{i}", (128, 512)) for i in range(4)]
                ptm = [ps(st, f"ptm{i}", (128, 1024), BF16) for i in range(2)]
                for n in range(NIT + 1):
                    P.pool(lambda e, n=n: e.memset(ptab[:, n:n + 1], -(2.0 ** -(n + 1))), w=['ptab'])
                P.pool(lambda e: e.memset(thrc[:], -1.0e29), w=['thrc'])
                P.dma('sp', kil[:], kiT[:, :], w=['kil'])
                cnts = {'c': 0, 't': 0, 'q': 0}
                DVE_IT = ((1, 5, 9, 13), (3, 7, 11, 15))

                def score_units(i):
                    s = i % 4
                    L = (i + 1) * 128
                    units = []
                    qs = cnts['q'] % 2
                    cnts['q'] += 1

                    def loads():
                        P.dma('sp', qil[qs][:], qiT[:, i * 128:(i + 1) * 128].rearrange("(c p) t -> p c t", p=128),
                              w=[f'qil{qs}'])
                        P.dma('sp', wil[qs][:], witm[i * 128:(i + 1) * 128, :], w=[f'wil{qs}'])
                    units.append(loads)
                    nst = (L + 511) // 512
                    for stile in range(nst):
                        w_ = min(512, L - stile * 512)
                        c0 = stile * 512
                        for h in range(8):
                            def unit(stile=stile, w_=w_, c0=c0, h=h):
                                p2, half = divmod(h, 2)
                                b = cnts['c'] % 4
                                r3 = cnts['c'] % 3
                                cnts['c'] += 1
                                P.pe(lambda e: e.matmul(
                                    pd[b][:, 0:w_], lhsT=qil[qs][half * 64:(half + 1) * 64, p2, :],
                                    rhs=kil[half * 64:(half + 1) * 64, c0:c0 + w_], start=True, stop=True),
                                    r=[f'qil{qs}', 'kil'], w=[f'pd{b}'])
                                P.act(lambda e: e.activation(
                                    out=rl[r3][:, 0:w_], in_=pd[b][:, 0:w_], func=AF.Relu),
                                    r=[f'pd{b}'], w=[f'rl{r3}'])
                                if h == 0:
                                    P.dve(lambda e: e.tensor_scalar(
                                        out=score[s][:, c0:c0 + w_], in0=rl[r3][:, 0:w_], scalar1=wil[qs][:, 0:1],
                                        scalar2=None, op0=ALU.mult),
                                        r=[f'rl{r3}', f'wil{qs}'], w=[f'score{s}'])
                                else:
                                    P.dve(lambda e: e.scalar_tensor_tensor(
                                        out=score[s][:, c0:c0 + w_], in0=rl[r3][:, 0:w_], scalar=wil[qs][:, h:h + 1],
                                        in1=score[s][:, c0:c0 + w_], op0=ALU.mult, op1=ALU.add),
                                        r=[f'rl{r3}', f'wil{qs}'], w=[f'score{s}'])
                            units.append(unit)

                    def fin():
                        P.dve(lambda e: e.tensor_tensor(
                            out=score[s][:, i * 128:(i + 1) * 128], in0=score[s][:, i * 128:(i + 1) * 128],
                            in1=caus[:], op=ALU.add), w=[f'score{s}'])
                    units.append(fin)
                    return units

                def bisect_ops(i, c):
                    s = i % 4
                    L = (i + 1) * 128
                    sk = f'score{s}'
                    K = lambda nme: f'{nme}{c}'
                    units = []
                    if i >= 2:
                        units.append([
                            lambda: P.dve(lambda e: e.tensor_reduce(out=lo[c][:], in_=score[s][:, 0:i * 128],
                                                                    axis=AX.X, op=ALU.min), r=[sk], w=[K('lo')]),
                            lambda: P.dve(lambda e: e.tensor_reduce(out=hi[c][:], in_=score[s][:, 0:L],
                                                                    axis=AX.X, op=ALU.max), r=[sk], w=[K('hi')]),
                            lambda: P.dve(lambda e: e.tensor_tensor(out=Wd[c][:], in0=hi[c][:], in1=lo[c][:],
                                                                    op=ALU.subtract),
                                          r=[K('hi'), K('lo')], w=[K('Wd')]),
                            lambda: P.dve(lambda e: e.tensor_scalar(out=hneg[c][:], in0=ptab[:], scalar1=Wd[c][:, 0:1],
                                                                    scalar2=None, op0=ALU.mult),
                                          r=[K('Wd'), 'ptab'], w=[K('hneg')]),
                            lambda: P.dve(lambda e: e.tensor_scalar(out=nm[c][:], in0=lo[c][:], scalar1=-1.0,
                                                                    scalar2=hneg[c][:, 0:1], op0=ALU.mult,
                                                                    op1=ALU.add),
                                          r=[K('lo'), K('hneg')], w=[K('nm')]),
                        ])
                        for n in range(NIT):
                            if n in DVE_IT[c]:
                                units.append([
                                    lambda: P.dve(lambda e: e.tensor_scalar(
                                        out=midp[c][:], in0=nm[c][:], scalar1=-1.0, scalar2=None, op0=ALU.mult),
                                        r=[K('nm')], w=[K('midp')]),
                                    lambda: P.dve(lambda e: e.tensor_scalar(
                                        out=junkD[:, 0:L], in0=score[s][:, 0:L], scalar1=midp[c][:, 0:1],
                                        scalar2=None, op0=ALU.is_ge, op1=ALU.add, accum_out=cntd[c][:]),
                                        r=[sk, K('midp')], w=[K('cntd')]),
                                    lambda: P.dve(lambda e: e.tensor_scalar(
                                        out=t2[c][:], in0=cntd[c][:], scalar1=TOPK - 0.5, scalar2=2.0,
                                        op0=ALU.is_ge, op1=ALU.mult), r=[K('cntd')], w=[K('t2')]),
                                    lambda n=n: P.dve(lambda e: e.tensor_scalar(
                                        out=t2[c][:], in0=t2[c][:], scalar1=-1.0, scalar2=hneg[c][:, n + 1:n + 2],
                                        op0=ALU.add, op1=ALU.mult), r=[K('hneg')], w=[K('t2')]),
                                    lambda: P.dve(lambda e: e.tensor_tensor(
                                        out=nm[c][:], in0=nm[c][:], in1=t2[c][:], op=ALU.add),
                                        r=[K('t2')], w=[K('nm')]),
                                ])
                            else:
                                units.append([
                                    lambda: P.act(lambda e: e.activation(
                                        out=junk[:, 0:L], in_=score[s][:, 0:L], func=AF.Sign, bias=nm[c][:, 0:1],
                                        scale=1.0, accum_out=ssum[c][:]), r=[sk, K('nm')], w=[K('ssum')]),
                                    lambda: P.act(lambda e: e.activation(
                                        out=sgn[c][:], in_=ssum[c][:], func=AF.Sign,
                                        bias=float(L - 2 * TOPK) + 0.5, scale=1.0), r=[K('ssum')], w=[K('sgn')]),
                                    lambda n=n: P.act(lambda e: e.activation(
                                        out=nm[c][:], in_=sgn[c][:], func=AF.Identity,
                                        scale=hneg[c][:, n + 1:n + 2], bias=nm[c][:, 0:1]),
                                        r=[K('sgn'), K('hneg')], w=[K('nm')]),
                                ])

                    def epi():
                        if i >= 2:
                            P.dve(lambda e: e.tensor_scalar(out=thr_t[c][:], in0=nm[c][:], scalar1=-1.0,
                                                            scalar2=hneg[c][:, NIT:NIT + 1], op0=ALU.mult,
                                                            op1=ALU.add),
                                  r=[K('nm'), K('hneg')], w=[K('thr_t')])
                            thr, thrk = thr_t[c], K('thr_t')
                        else:
                            thr, thrk = thrc, 'thrc'
                        P.dve(lambda e: e.tensor_scalar(
                            out=mk[c][:, 0:L], in0=score[s][:, 0:L], scalar1=thr[:, 0:1], scalar2=None,
                            op0=ALU.is_ge), r=[sk, thrk], w=[K('mk')])
                        for g0 in range(0, i + 1, 8):
                            nb_ = min(8, i + 1 - g0)
                            b = cnts['t'] % 2
                            cnts['t'] += 1
                            for q_ in range(nb_):
                                P.pe(lambda e, b=b, q_=q_, g0=g0: e.transpose(
                                    out=ptm[b][:, q_ * 128:(q_ + 1) * 128],
                                    in_=mk[c][:, (g0 + q_) * 128:(g0 + q_ + 1) * 128], identity=ident[:]),
                                    r=[K('mk')], w=[f'ptm{b}'])
                            P.act(lambda e, b=b, nb_=nb_: e.activation(
                                out=mT[b][:, 0:nb_, :],
                                in_=ptm[b][:, 0:nb_ * 128].rearrange("p (q t) -> p q t", q=nb_), func=AF.Copy),
                                r=[f'ptm{b}'], w=[f'mT{b}'])
                            for q0 in range(0, nb_, 4):
                                q1 = min(nb_, q0 + 4)
                                P.dma('pool', maskT[(g0 + q0) * 128:(g0 + q1) * 128, i * 128:(i + 1) * 128].rearrange(
                                    "(q p) t -> p q t", p=128), mT[b][:, q0:q1, :], r=[f'mT{b}'], w=['dram'])
                    units.append([epi])
                    return units

                def run_units(us):
                    for u in us:
                        u()

                run_units(score_units(0))
                run_units(score_units(1))
                for pidx in range(NB // 2):
                    ia, ib = 2 * pidx, 2 * pidx + 1
                    A_ = bisect_ops(ia, 0)
                    B_ = bisect_ops(ib, 1)
                    S_ = []
                    if ia + 2 < NB:
                        s1, s2 = score_units(ia + 2), score_units(ib + 2)
                        for k_ in range(max(len(s1), len(s2))):
                            if k_ < len(s1):
                                S_.append(s1[k_])
                            if k_ < len(s2):
                                S_.append(s2[k_])
                    nun = max(len(A_), len(B_))
                    done = 0
                    for idx in range(nun):
                        ua = A_[idx] if idx < len(A_) else []
                        ub = B_[idx] if idx < len(B_) else []
                        for k_ in range(max(len(ua), len(ub))):
                            if k_ < len(ua):
                                ua[k_]()
                            if k_ < len(ub):
                                ub[k_]()
                        tgt = (len(S_) * (idx + 1)) // nun
                        while done < tgt:
                            S_[done]()
                            done += 1
                P.barrier()
                P.emit()

        if upto >= 6:
            with ExitStack() as st:
                ql = [sb(st, f"ql{i}", (128, S), BF16) for i in range(2)]
                kl = [sb(st, f"kl{i}", (128, S), BF16) for i in range(2)]
                vl = [sb(st, f"vl{i}", (128, NB, 128), BF16) for i in range(2)]
                NML = 6
                ml = [sb(st, f"ml{i}", (128, 512), BF16) for i in range(NML)]
                ex = [sb(st, f"ex{i}", (128, 512), BF16) for i in range(4)]
                pm = [sb(st, f"pm{i}", (128, 512), BF16) for i in range(4)]
                gl = [sb(st, f"gl{i}", (128, 512), BF16) for i in range(2)]
                rinv = sb(st, "rinv", (128, 512), F32)
                o1 = sb(st, "o1", (128, 512), F32)
                ot = [sb(st, f"ot{i}", (128, 512), BF16) for i in range(2)]
                psc = [ps(st, f"psc{i}", (128, 512)) for i in range(4)]
                po = [ps(st, f"po{i}", (128, 512)) for i in range(2)]
                pr = [ps(st, f"pr{i}", (128, 512)) for i in range(2)]
                its = []
                for h in range(8):
                    for T in range(NT):
                        nsb = 4 * T + 4
                        for sbk in range(nsb):
                            its.append((h, T, sbk, nsb))
                DEPTH = 3

                def front(n):
                    h, T, sbk, nsb = its[n]
                    hs = h % 2
                    a = (h * NT + T) % 2
                    if T == 0 and sbk == 0:
                        P.dma('sp', ql[hs][:], qT[h * 128:(h + 1) * 128, :], w=[f'ql{hs}'])
                        P.dma('sp', kl[hs][:], kT[h * 128:(h + 1) * 128, :], w=[f'kl{hs}'])
                        for g_ in range(8):
                            P.dma('sp', vl[hs][:, g_ * 4:(g_ + 1) * 4, :],
                                  vtm[g_ * 512:(g_ + 1) * 512, h * 128:(h + 1) * 128].rearrange(
                                      "(b p) d -> p b d", p=128), w=[f'vl{hs}_{g_}'])
                    if sbk == 0:
                        P.dma('sp', gl[a][:], gT[1024 + h * 128:1024 + (h + 1) * 128, TS(T)], w=[f'gl{a}'])
                    m4 = n % NML
                    b3 = n % 4
                    r0 = max(0, sbk - 4 * T) * 128
                    t0 = T * 512 + r0
                    P.dma('sp', ml[m4][:, r0:512], maskT[sbk * 128:(sbk + 1) * 128, t0:(T + 1) * 512],
                          w=[f'ml{m4}'])
                    P.pe(lambda e: e.matmul(
                        psc[b3][:, r0:512], lhsT=kl[hs][:, sbk * 128:(sbk + 1) * 128],
                        rhs=ql[hs][:, t0:(T + 1) * 512], start=True, stop=True),
                        r=[f'kl{hs}', f'ql{hs}'], w=[f'psc{b3}'])
                    P.act(lambda e: e.activation(out=ex[b3][:, r0:512], in_=psc[b3][:, r0:512], func=AF.Exp),
                          r=[f'psc{b3}'], w=[f'ex{b3}'])
                    P.dve(lambda e: e.tensor_tensor(
                        out=pm[b3][:, r0:512], in0=ex[b3][:, r0:512], in1=ml[m4][:, r0:512], op=ALU.mult),
                        r=[f'ex{b3}', f'ml{m4}'], w=[f'pm{b3}'])

                def back(n):
                    h, T, sbk, nsb = its[n]
                    hs = h % 2
                    a = (h * NT + T) % 2
                    b3 = n % 4
                    r0 = max(0, sbk - 4 * T) * 128
                    P.pe(lambda e: e.matmul(
                        po[a][:, r0:512], lhsT=vl[hs][:, sbk, :], rhs=pm[b3][:, r0:512],
                        start=(sbk == 0), stop=(sbk == nsb - 1)),
                        r=[f'pm{b3}', f'vl{hs}_{sbk // 4}'], w=[f'po{a}'])
                    P.pe(lambda e: e.matmul(
                        pr[a][:, r0:512], lhsT=ones_bf[:], rhs=pm[b3][:, r0:512],
                        start=(sbk == 0), stop=(sbk == nsb - 1)),
                        r=[f'pm{b3}'], w=[f'pr{a}'])
                    if sbk == nsb - 1:
                        P.act(lambda e: e.activation(out=o1[:], in_=pr[a][:], func=AF.Ln), r=[f'pr{a}'], w=['o1'])
                        P.act(lambda e: e.activation(out=rinv[:], in_=o1[:], func=AF.Exp, scale=-1.0),
                              r=['o1'], w=['rinv'])
                        P.dve(lambda e: e.tensor_tensor(out=o1[:], in0=po[a][:], in1=rinv[:], op=ALU.mult),
                              r=[f'po{a}', 'rinv'], w=['o1'])
                        P.dve(lambda e: e.tensor_tensor(out=ot[a][:], in0=o1[:], in1=gl[a][:], op=ALU.mult),
                              r=['o1', f'gl{a}'], w=[f'ot{a}'])
                        P.dma('pool', mattT[h * 128:(h + 1) * 128, TS(T)], ot[a][:], r=[f'ot{a}'], w=['dram'])

                for n in range(len(its) + DEPTH):
                    if n < len(its):
                        front(n)
                    if n >= DEPTH:
                        back(n - DEPTH)
                P.barrier()
                P.emit()

        def out_norm_phase(st, nk, load_lhs, wres, gname, resid, dst, tag, post=None):
            gp = sb(st, tag + "gp", (128, D), F32)
            o, n_ = PCOL[gname]
            P.dma('sp', gp[:], params[:, o:o + n_], w=['gp'])
            xt = [sb(st, f"{tag}xt{i}", (128, D), F32) for i in range(2)]
            xo = [sb(st, f"{tag}xo{i}", (128, D), F32) for i in range(2)]
            tmp = sb(st, tag + "tmp", (128, D), F32)
            junkA = sb(st, tag + "junkA", (128, 512), BF16)
            ssh = [sb(st, f"{tag}ssh{i}", (128, 2), F32) for i in range(2)]
            ss = [sb(st, f"{tag}ss{i}", (128, 1), F32) for i in range(2)]
            rt = [sb(st, f"{tag}rt{i}", (128, 1), F32) for i in range(2)]
            rs = [sb(st, f"{tag}rs{i}", (128, 1), F32) for i in range(2)]
            po = [ps(st, f"{tag}po{i}", (128, 512)) for i in range(4)]
            cache = {}
            nblk = NT * 4

            def S1(i):
                T, tb = divmod(i, 4)
                s2 = i % 2
                if T not in cache:
                    cache[T] = load_lhs(T)
                if tb == 1 and T + 1 < NT and (T + 1) not in cache:
                    cache[T + 1] = load_lhs(T + 1)
                lhs, lkey = cache[T]
                P.dma('sp', xt[s2][:], resid[i * 128:(i + 1) * 128, :], w=[f'xt{s2}'])
                for hh in range(2):
                    b = s2 * 2 + hh
                    for j in range(nk):
                        P.pe(lambda e, lhs=lhs, j=j, tb=tb, hh=hh, b=b: e.matmul(
                            po[b][:], lhsT=lhs[:, j, tb * 128:(tb + 1) * 128],
                            rhs=wres[:, j, hh * 512:(hh + 1) * 512], start=(j == 0), stop=(j == nk - 1)),
                            r=[lkey(j) if callable(lkey) else lkey, 'wres'], w=[f'po{b}'])

            def S1b(i):
                s2 = i % 2
                for hh in range(2):
                    b = s2 * 2 + hh
                    P.act(lambda e, b=b, s2=s2, hh=hh: e.activation(
                        out=junkA[:], in_=po[b][:], func=AF.Square, accum_out=ssh[s2][:, hh:hh + 1]),
                        r=[f'po{b}'], w=[f'ssh{s2}'])

            def S2(i):
                s2 = i % 2
                P.dve(lambda e: e.tensor_tensor(out=ss[s2][:], in0=ssh[s2][:, 0:1], in1=ssh[s2][:, 1:2],
                                                op=ALU.add), r=[f'ssh{s2}'], w=[f'ss{s2}'])
                P.act(lambda e: e.activation(out=rt[s2][:], in_=ss[s2][:], func=AF.Sqrt, bias=EPS,
                                             scale=1.0 / D), r=[f'ss{s2}'], w=[f'rt{s2}'])
                P.dve(lambda e: e.reciprocal(out=rs[s2][:], in_=rt[s2][:]), r=[f'rt{s2}'], w=[f'rs{s2}'])
                for hh in range(2):
                    b = s2 * 2 + hh
                    P.dve(lambda e, b=b, hh=hh: e.scalar_tensor_tensor(
                        out=tmp[:, hh * 512:(hh + 1) * 512], in0=po[b][:], scalar=rs[s2][:, 0:1],
                        in1=gp[:, hh * 512:(hh + 1) * 512], op0=ALU.mult, op1=ALU.mult),
                        r=[f'po{b}', f'rs{s2}', 'gp'], w=['tmp'])
                P.dve(lambda e: e.tensor_tensor(out=xo[s2][:], in0=tmp[:], in1=xt[s2][:], op=ALU.add),
                      r=['tmp', f'xt{s2}'], w=[f'xo{s2}'])
                P.dma('pool', dst[i * 128:(i + 1) * 128, :], xo[s2][:], r=[f'xo{s2}'], w=['dram'])
                if post is not None:
                    post[0](i, s2, xo[s2], f'xo{s2}')

            def S3(i):
                s2 = i % 2
                if post is not None:
                    post[1](i, s2, xo[s2], f'xo{s2}')

            for step in range(nblk + 2):
                if step < nblk:
                    S1(step)
                if 1 <= step <= nblk:
                    S2(step - 1)
                if step < nblk:
                    S1b(step)
                if 2 <= step:
                    S3(step - 2)

        h2st = ExitStack()
        if upto >= 7:
            h2T = sb(h2st, "h2T", (128, 8, S), BF16)
            with ExitStack() as st:
                wo = sb(st, "wo", (128, 8, D), BF16)
                for hh in range(2):
                    P.dma('pool', wo[:, :, hh * 512:(hh + 1) * 512],
                          wout[:, hh * 512:(hh + 1) * 512].rearrange("(k p) c -> p k c", p=128), w=['wres'])
                g2 = sb(st, "g2bc", (128, D), F32)
                o, n_ = PCOL['g2']
                P.dma('sp', g2[:], params[:, o:o + n_], w=['g2'])
                ma = [sb(st, f"ma{i}", (128, 8, 512), BF16) for i in range(2)]
                mb = [sb(st, f"mb{i}", (128, 8, 512), BF16) for i in range(2)]
                mc_ = [sb(st, f"mc{i}", (128, 8, 512), BF16) for i in range(2)]
                mg = [sb(st, f"mg{i}", (128, 8, 512), BF16) for i in range(2)]
                junkD = sb(st, "junkD", (128, D), BF16)
                hb = [sb(st, f"Ghb{i}", (128, D), BF16) for i in range(2)]
                ss2 = [sb(st, f"Gss2{i}", (128, 1), F32) for i in range(2)]
                rt2 = [sb(st, f"Grt2{i}", (128, 1), F32) for i in range(2)]
                rs2 = [sb(st, f"Grs2{i}", (128, 1), F32) for i in range(2)]
                pt = [ps(st, f"Gpt{i}", (128, D), BF16) for i in range(2)]

                def load_merged(T):
                    s = T % 2
                    for (buf, src, nm) in ((ma, mconvT, 'ma'), (mb, mattT, 'mb'), (mc_, mmemT, 'mc')):
                        for g_ in range(2):
                            P.dma('sp', buf[s][:, g_ * 4:(g_ + 1) * 4, :],
                                  src[g_ * 512:(g_ + 1) * 512, TS(T)].rearrange("(j p) t -> p j t", p=128),
                                  w=[f'{nm}{s}_{g_}'])
                    P.dve(lambda e, s=s: e.tensor_tensor(out=mg[s][:], in0=ma[s][:], in1=mb[s][:], op=ALU.add),
                          r=[f'ma{s}_0', f'ma{s}_1', f'mb{s}_0', f'mb{s}_1'], w=[f'mg{s}'])
                    P.dve(lambda e, s=s: e.tensor_tensor(out=mg[s][:], in0=mg[s][:], in1=mc_[s][:], op=ALU.add),
                          r=[f'mc{s}_0', f'mc{s}_1'], w=[f'mg{s}'])
                    return mg[s], f'mg{s}'

                def post_h2a(i, s2, xo, xkey):
                    P.dve(lambda e, s2=s2: e.scalar_tensor_tensor(
                        out=junkD[:], in0=xo[:], scalar=1.0, in1=xo[:], op0=ALU.mult, op1=ALU.mult,
                        accum_out=ss2[s2][:]), r=[xkey], w=[f'ss2{s2}'])

                def post_h2b(i, s2, xo, xkey):
                    P.act(lambda e, s2=s2: e.activation(out=rt2[s2][:], in_=ss2[s2][:], func=AF.Sqrt, bias=EPS,
                                                        scale=1.0 / D), r=[f'ss2{s2}'], w=[f'rt2{s2}'])
                    P.dve(lambda e, s2=s2: e.reciprocal(out=rs2[s2][:], in_=rt2[s2][:]),
                          r=[f'rt2{s2}'], w=[f'rs2{s2}'])
                    P.dve(lambda e, s2=s2: e.scalar_tensor_tensor(
                        out=hb[s2][:], in0=xo[:], scalar=rs2[s2][:, 0:1], in1=g2[:], op0=ALU.mult, op1=ALU.mult),
                        r=[xkey, f'rs2{s2}', 'g2'], w=[f'hb{s2}'])
                    for j in range(8):
                        P.pe(lambda e, s2=s2, j=j: e.transpose(out=pt[s2][:, j * 128:(j + 1) * 128],
                                                               in_=hb[s2][:, j * 128:(j + 1) * 128],
                                                               identity=ident[:]),
                             r=[f'hb{s2}'], w=[f'pt{s2}'])
                    P.act(lambda e, s2=s2, i=i: e.activation(
                        out=h2T[:, :, i * 128:(i + 1) * 128],
                        in_=pt[s2][:].rearrange("p (j t) -> p j t", j=8), func=AF.Copy),
                        r=[f'pt{s2}'], w=[f'h2T{i}'])

                out_norm_phase(st, 8, load_merged, wo, 'gpost1', x, x1d, "G", post=(post_h2a, post_h2b))
                P.barrier()
                P.emit()

        if upto >= 8:
            with ExitStack() as st:
                wt = [sb(st, f"Hwt{i}", (128, 8, 512), BF16) for i in range(2)]
                ug = [sb(st, f"ug{i}", (128, 514), F32) for i in range(3)]
                uvv = [sb(st, f"uv{i}", (128, 514), F32) for i in range(3)]
                cg = [sb(st, f"cg{i}", (128, 512), F32) for i in range(3)]
                cv = [sb(st, f"cv{i}", (128, 512), F32) for i in range(3)]
                sgl = [sb(st, f"sgl{i}", (128, 512), F32) for i in range(3)]
                pend = []
                ast = [sb(st, f"ast{i}", (128, S), BF16) for i in range(2)]
                pg = [ps(st, f"Hpg{i}", (128, 512)) for i in range(2)]
                pvv = [ps(st, f"Hpv{i}", (128, 512)) for i in range(2)]

                def fw(c, tap):
                    return pvc('ffnw', c * 3 + tap, 1)

                n = 0
                for wi_ in range(11):
                    s = wi_ % 2
                    P.dma('pool', wt[s][:], wup[:, wi_ * 512:(wi_ + 1) * 512].rearrange(
                        "(k p) c -> p k c", p=128), w=[f'wt{s}'])
                    for jj in range(2):
                        j = wi_ * 2 + jj
                        a2 = j % 2
                        for T in range(NT):
                            b = n % 2
                            s3 = n % 3
                            p3 = (n - 1) % 3
                            b3 = n % 3
                            n += 1
                            for k in range(8):
                                P.pe(lambda e, s=s, jj=jj, T=T, k=k, b=b: e.matmul(
                                    pg[b][:], lhsT=wt[s][:, k, jj * 256:jj * 256 + 128],
                                    rhs=h2T[:, k, TS(T)], start=(k == 0), stop=(k == 7)),
                                    r=[f'wt{s}'], w=[f'pg{b}'])
                            for k in range(8):
                                P.pe(lambda e, s=s, jj=jj, T=T, k=k, b=b: e.matmul(
                                    pvv[b][:], lhsT=wt[s][:, k, jj * 256 + 128:jj * 256 + 256],
                                    rhs=h2T[:, k, TS(T)], start=(k == 0), stop=(k == 7)),
                                    r=[f'wt{s}'], w=[f'pvv{b}'])
                            for (u_, nm_) in ((ug, 'ug'), (uvv, 'uv')):
                                if T == 0:
                                    P.pool(lambda e, u_=u_, s3=s3: e.memset(u_[s3][:, 0:2], 0.0), w=[f'{nm_}{s3}'])
                                else:
                                    P.pool(lambda e, u_=u_, s3=s3, p3=p3: e.tensor_copy(
                                        out=u_[s3][:, 0:2], in_=u_[p3][:, 512:514]),
                                        r=[f'{nm_}{p3}'], w=[f'{nm_}{s3}'])
                            for (u_, nm_, pp_, pk_, co, cname, c) in (
                                    (ug, 'ug', pg, 'pg', cg, 'cg', 2 * j), (uvv, 'uv', pvv, 'pvv', cv, 'cv', 2 * j + 1)):
                                P.act(lambda e, u_=u_, pp_=pp_, b=b, s3=s3: e.activation(
                                    out=u_[s3][:, 2:514], in_=pp_[b][:], func=AF.Copy),
                                    r=[f'{pk_}{b}'], w=[f'{nm_}{s3}'])
                                P.act(lambda e, co=co, pp_=pp_, b=b, c=c, b3=b3: e.activation(
                                    out=co[b3][:], in_=pp_[b][:], func=AF.Identity, scale=fw(c, 2),
                                    bias=pvc('ffnb', c, 1)), r=[f'{pk_}{b}'], w=[f'{cname}{b3}'])
                                for tap in (1, 0):
                                    P.dve(lambda e, u_=u_, co=co, b3=b3, s3=s3, c=c, tap=tap: e.scalar_tensor_tensor(
                                        out=co[b3][:], in0=u_[s3][:, tap:tap + 512], scalar=fw(c, tap), in1=co[b3][:],
                                        op0=ALU.mult, op1=ALU.add), r=[f'{nm_}{s3}'], w=[f'{cname}{b3}'])
                            def tail(b3=b3, a2=a2, T=T, j=j):
                                P.act(lambda e: e.activation(out=sgl[b3][:], in_=cg[b3][:], func=AF.Silu),
                                      r=[f'cg{b3}'], w=[f'sgl{b3}'])
                                P.dve(lambda e: e.tensor_tensor(
                                    out=ast[a2][:, TS(T)], in0=sgl[b3][:], in1=cv[b3][:], op=ALU.mult),
                                    r=[f'sgl{b3}', f'cv{b3}'], w=[f'ast{a2}'])
                                if T == NT - 1:
                                    P.dma('sp', aT[j * 128:(j + 1) * 128, :], ast[a2][:], r=[f'ast{a2}'], w=['dram'])
                            while pend:
                                pend.pop(0)()
                            pend.append(tail)
                while pend:
                    pend.pop(0)()
                P.barrier()
                P.emit()
        h2st.close()

        if upto >= 9:
            with ExitStack() as st:
                wd = sb(st, "wd", (128, NFC, D), BF16)
                for hh in range(2):
                    P.dma('pool', wd[:, :, hh * 512:(hh + 1) * 512],
                          wdown[:, hh * 512:(hh + 1) * 512].rearrange("(k p) c -> p k c", p=128), w=['wres'])
                at = [sb(st, f"at{i}", (128, NFC, 512), BF16) for i in range(2)]

                def load_act(T):
                    s = T % 2
                    for g_ in range(0, NFC, 4):
                        g1 = min(NFC, g_ + 4)
                        P.dma('sp', at[s][:, g_:g1, :], aT[g_ * 128:g1 * 128, TS(T)].rearrange(
                            "(j p) t -> p j t", p=128), w=[f'at{s}_{g_ // 4}'])
                    return at[s], (lambda j, s=s: f'at{s}_{j // 4}')

                out_norm_phase(st, NFC, load_act, wd, 'gpost2', x1d, y, "I")
                P.barrier()
                P.emit()


        P.barrier()
        P.emit()
    return nc


_CACHE = {}


def kernel(**inputs):
    shared = pack_inputs(inputs)
    if 'nc' not in _CACHE:
        _CACHE['nc'] = build()
    nc = _CACHE['nc']
    x = np.asarray(inputs['x'], np.float32)
    mem = np.asarray(inputs['mem'], np.float32)
    in_maps = []
    for b in range(8):
        m = dict(shared)
        m['x'] = np.ascontiguousarray(x[b])
        m['mem'] = np.ascontiguousarray(mem[b])
        in_maps.append(m)
    res = run_bass_kernel_spmd(nc, in_maps, core_ids=list(range(8)))
    return np.stack([np.asarray(r['y'], np.float32) for r in res.results], axis=0)
```

```python
import numpy as np
from contextlib import ExitStack
import concourse.bass as bass
import concourse.mybir as mybir
from concourse.bass_utils import run_bass_kernel_spmd

F32 = mybir.dt.float32
BF16 = mybir.dt.bfloat16
I32 = mybir.dt.int32
AF = mybir.ActivationFunctionType
ALU = mybir.AluOpType
AX = mybir.AxisListType

S = 4096
D = 1024
NB = S // 128
NT = S // 512
EPS = 1e-6
FFN = 2816
NFC = FFN // 128
TOPK = 256
NIT = 16
DVE_ITERS = (3, 6, 9, 12, 14)
NEG = -1.0e30

ENGS = ['pe', 'act', 'dve', 'pool', 'sp']
BLK = {'pe': 'tensor', 'act': 'scalar', 'dve': 'vector', 'pool': 'gpsimd', 'sp': 'sync'}
EPOCH = 20000
NDS = 16


class Prog:
    def __init__(self, nc, stack):
        self.nc = nc
        self.stack = stack
        self.lists = {e: [] for e in ENGS}
        self.count = {e: 0 for e in ENGS}
        self.sems = {e: [] for e in ENGS}
        self.waited = {e: {} for e in ENGS}
        self.lastw = {}
        self.reads = {}
        self.dsem = {}
        self.dval = {}
        self.dnext = {}
        self.nsem = 0

    def _esem(self, e, idx):
        while len(self.sems[e]) <= idx:
            self.sems[e].append(self.stack.enter_context(
                self.nc.semaphore(f"s_{e}{len(self.sems[e])}")))
        return self.sems[e][idx]

    def _collect(self, e, r, w, extra):
        toks = list(extra)
        for k in r:
            t = self.lastw.get(k)
            if t is not None:
                toks.append(t)
        for k in w:
            t = self.lastw.get(k)
            if t is not None:
                toks.append(t)
            toks.extend(self.reads.get(k, ()))
        need = []
        for (sid, sem, v, te) in toks:
            if te == e and e == 'pe':
                continue
            if self.waited[e].get(sid, 0) >= v:
                continue
            self.waited[e][sid] = v
            need.append((sem, v))
        return need

    def _update(self, tok, r, w):
        for k in w:
            self.lastw[k] = tok
            self.reads[k] = []
        for k in r:
            self.reads.setdefault(k, []).append(tok)

    def op(self, e, fn, r=(), w=(), extra=()):
        need = self._collect(e, r, w, extra)
        n = self.count[e]
        ep, v = divmod(n, EPOCH)
        sem = self._esem(e, ep)
        self.count[e] = n + 1
        tok = ((e, ep), sem, v + 1, e)
        self.lists[e].append((need, fn, sem, 1))
        self._update(tok, r, w)
        return tok

    def pe(self, fn, r=(), w=(), extra=()):
        return self.op('pe', fn, r, w, extra)

    def act(self, fn, r=(), w=(), extra=()):
        return self.op('act', fn, r, w, extra)

    def dve(self, fn, r=(), w=(), extra=()):
        return self.op('dve', fn, r, w, extra)

    def pool(self, fn, r=(), w=(), extra=()):
        return self.op('pool', fn, r, w, extra)

    def dma(self, e, out, in_, r=(), w=(), extra=(), **kw):
        if e not in self.dsem:
            self.dsem[e] = [self.stack.enter_context(self.nc.semaphore(f"dma_{e}{j}")) for j in range(NDS)]
            self.dval[e] = [0] * NDS
            self.dnext[e] = 0
        j = self.dnext[e]
        self.dnext[e] = (j + 1) % NDS
        sem = self.dsem[e][j]
        extra = list(extra)
        if self.dval[e][j] > 0:
            extra.append((('d', e, j), sem, self.dval[e][j], 'dma'))
        need = self._collect(e, r, w, extra)
        self.dval[e][j] += 16
        tok = (('d', e, j), sem, self.dval[e][j], 'dma')
        self.lists[e].append((need, lambda eng: eng.dma_start(out=out, in_=in_, **kw), sem, 16))
        self._update(tok, r, w)
        return tok

    def barrier(self):
        toks = []
        for e in ENGS:
            n = self.count[e]
            if n == 0:
                continue
            ep, v = divmod(n - 1, EPOCH)
            toks.append(((e, ep), self.sems[e][ep], v + 1, e))
        for q in self.dsem:
            for j in range(NDS):
                if self.dval[q][j] > 0:
                    toks.append((('d', q, j), self.dsem[q][j], self.dval[q][j], 'dma'))
        for e in ENGS:
            need = []
            for (sid, sem, v, te) in toks:
                if te == e:
                    continue
                if self.waited[e].get(sid, 0) >= v:
                    continue
                self.waited[e][sid] = v
                need.append((sem, v))
            if need:
                self.lists[e].append((need, None, None, 0))
        self.lastw = {}
        self.reads = {}

    def emit(self):
        with self.nc.Block() as block:
            for e in ENGS:
                lst = self.lists[e]
                if not lst:
                    continue

                def body(eng, lst=lst):
                    for (need, fn, sem, inc) in lst:
                        for (s, v) in need:
                            eng.wait_ge(s, v)
                        if fn is not None:
                            fn(eng).then_inc(sem, inc)

                getattr(block, BLK[e])(body)
        self.lists = {e: [] for e in ENGS}


PCOL = {}
_off = 0
for _name, _n in [('g1', D), ('gmem', D), ('gpost1', D), ('g2', D), ('gpost2', D),
                  ('convw', 8 * 31), ('convb', 8), ('lng', 8), ('lnb', 8),
                  ('bgate', 24), ('ffnw', 44 * 3), ('ffnb', 44)]:
    PCOL[_name] = (_off, _n)
    _off += _n
NPCOL = _off

WCM_COLS = 45 * 128
WTM_COLS = 1032


def pack_inputs(inp):
    f = np.float32
    w_in = np.asarray(inp['w_in'][0], f)
    c0 = 0
    conv = w_in[:, 0:2048]
    q = w_in[:, 2048:3072]
    k = w_in[:, 3072:4096]
    v = w_in[:, 4096:5120]
    qi = w_in[:, 5120:5632]
    wi = w_in[:, 5632:5640]
    ki = w_in[:, 5640:5704]
    qm = w_in[:, 5704:6728]
    a, g = conv[:, :1024], conv[:, 1024:]
    inter = np.stack([a.reshape(D, 8, 128), g.reshape(D, 8, 128)], axis=2).reshape(D, 2048)
    wcm = np.ascontiguousarray(np.concatenate([inter, q, k, qm, qi, ki, ki], axis=1))
    wtm = np.ascontiguousarray(np.concatenate([v, wi], axis=1))
    w_up = np.asarray(inp['w_up'][0], f)
    ug, uv = w_up[:, :FFN], w_up[:, FFN:]
    wup = np.ascontiguousarray(
        np.stack([ug.reshape(D, NFC, 128), uv.reshape(D, NFC, 128)], axis=2).reshape(D, 2 * FFN))
    P = np.zeros((128, NPCOL), f)

    def put(name, arr):
        o, n = PCOL[name]
        P[:, o:o + n] = arr

    put('g1', np.broadcast_to(inp['norm1_pre_g'][0], (128, D)))
    put('gmem', np.broadcast_to(inp['mem_norm_g'][0], (128, D)))
    put('gpost1', np.broadcast_to(inp['norm1_post_g'][0], (128, D)))
    put('g2', np.broadcast_to(inp['norm2_pre_g'][0], (128, D)))
    put('gpost2', np.broadcast_to(inp['norm2_post_g'][0], (128, D)))
    cw = np.asarray(inp['conv_dw_w'][0], f)
    put('convw', cw.T.reshape(8, 128, 31).transpose(1, 0, 2).reshape(128, 8 * 31))
    put('convb', np.asarray(inp['conv_dw_b'][0], f).reshape(8, 128).T)
    put('lng', np.asarray(inp['conv_ln_g'][0], f).reshape(8, 128).T)
    put('lnb', np.asarray(inp['conv_ln_b'][0], f).reshape(8, 128).T)
    put('bgate', np.asarray(inp['b_gate'][0], f).reshape(24, 128).T)
    fw = np.asarray(inp['ffn_dw_w'][0], f)
    fwg, fwv = fw[:, :FFN], fw[:, FFN:]
    fwi = np.stack([fwg.reshape(3, NFC, 128), fwv.reshape(3, NFC, 128)], axis=2).reshape(3, 44, 128)
    put('ffnw', fwi.transpose(2, 1, 0).reshape(128, 44 * 3))
    fb = np.asarray(inp['ffn_dw_b'][0], f)
    fbi = np.stack([fb[:FFN].reshape(NFC, 128), fb[FFN:].reshape(NFC, 128)], axis=1).reshape(44, 128)
    put('ffnb', fbi.T)
    shared = {
        'wcm': wcm, 'wtm': wtm, 'wup': wup, 'params': P,
        'pw2': np.ascontiguousarray(inp['conv_pw2'][0], f),
        'wmkv': np.ascontiguousarray(inp['w_mem_kv'][0], f),
        'wgate': np.ascontiguousarray(inp['w_gate'][0], f),
        'wout': np.ascontiguousarray(inp['w_out'][0], f),
        'wdown': np.ascontiguousarray(inp['w_down'][0], f),
    }
    return shared


def build(debug=(), upto=99):
    nc = bass.Bass("TRN2", target_bir_lowering=False)
    dbg = set(debug)

    def din(name, shape):
        return nc.dram_tensor(name, list(shape), F32, kind="ExternalInput").ap()

    def scratch(name, shape, dt):
        kind = "ExternalOutput" if name in dbg else "Internal"
        return nc.dram_tensor(name, list(shape), dt, kind=kind).ap()

    x = din("x", (S, D))
    mem = din("mem", (256, D))
    wcm = din("wcm", (D, WCM_COLS))
    wtm = din("wtm", (D, WTM_COLS))
    wup = din("wup", (D, 2 * FFN))
    params = din("params", (128, NPCOL))
    pw2 = din("pw2", (D, D))
    wmkv = din("wmkv", (D, 2 * D))
    wgate = din("wgate", (D, 3 * D))
    wout = din("wout", (D, D))
    wdown = din("wdown", (FFN, D))
    y = nc.dram_tensor("y", [S, D], F32, kind="ExternalOutput").ap()

    ycT = scratch("ycT", (D, S), F32)
    qT = scratch("qT", (D, S), BF16)
    kT = scratch("kT", (D, S), BF16)
    qmT = scratch("qmT", (D, S), BF16)
    qiT = scratch("qiT", (512, S), BF16)
    kiT = scratch("kiT", (128, S), BF16)
    vtm = scratch("vtm", (S, D), BF16)
    witm = scratch("witm", (S, 8), F32)
    gT = scratch("gT", (3 * D, S), BF16)
    mconvT = scratch("mconvT", (D, S), BF16)
    mattT = scratch("mattT", (D, S), BF16)
    mmemT = scratch("mmemT", (D, S), BF16)
    maskT = scratch("maskT", (S, S), BF16)
    x1d = scratch("x1d", (S, D), F32)
    aT = scratch("aT", (FFN, S), BF16)

    def pcols(t, name):
        o, n = PCOL[name]
        return t[:, o:o + n]

    with ExitStack() as gs:
        P = Prog(nc, gs)

        uniq = [0]

        def sb(st, name, shape, dt):
            uniq[0] += 1
            return st.enter_context(nc.sbuf_tensor(f"{name}_{uniq[0]}", list(shape), dt))

        def ps(st, name, shape, dt=F32):
            uniq[0] += 1
            return st.enter_context(nc.psum_tensor(f"{name}_{uniq[0]}", list(shape), dt))

        ident = sb(gs, "ident", (128, 128), BF16)
        ones_bf = sb(gs, "ones_bf", (128, 128), BF16)
        ones_f = sb(gs, "ones_f", (128, 128), F32)
        caus = sb(gs, "caus", (128, 128), F32)
        pv = sb(gs, "pv", (128, 472), F32)
        pvo = PCOL['convw'][0]

        def pvc(name, j0=0, n=None):
            o, nn = PCOL[name]
            o -= pvo
            if n is None:
                n = nn
            return pv[:, o + j0:o + j0 + n]

        with ExitStack() as st:
            iot = sb(st, "iot", (128, 128), I32)
            P.pool(lambda e: e.iota(out=iot[:], pattern=[[1, 128]], base=0, channel_multiplier=-1),
                   w=['iot'])
            P.dve(lambda e: e.tensor_scalar(out=ident[:], in0=iot[:], scalar1=0.0, scalar2=None,
                                            op0=ALU.is_equal), r=['iot'], w=['ident'])
            P.dve(lambda e: e.tensor_scalar(out=caus[:], in0=iot[:], scalar1=0.5, scalar2=NEG,
                                            op0=ALU.is_gt, op1=ALU.mult), r=['iot'], w=['caus'])
            P.dve(lambda e: e.memset(ones_bf[:], 1.0), w=['ones_bf'])
            P.dve(lambda e: e.memset(ones_f[:], 1.0), w=['ones_f'])
            P.dma('sp', pv[:], params[:, pvo:pvo + 472], w=['pv'])
            P.barrier()
            P.emit()

        hst = ExitStack()
        hT = sb(hst, "hT", (128, 8, S), BF16)

        def norm_transpose_phase(st, src, nblk, gname, dstT, tag):
            gbc = sb(st, tag + "gbc", (128, D), F32)
            xt = [sb(st, f"{tag}xt{i}", (128, D), F32) for i in range(2)]
            junk = sb(st, tag + "junk", (128, D), BF16)
            hb = [sb(st, f"{tag}hb{i}", (128, D), BF16) for i in range(2)]
            ss = [sb(st, f"{tag}ss{i}", (128, 1), F32) for i in range(2)]
            rt = [sb(st, f"{tag}rt{i}", (128, 1), F32) for i in range(2)]
            rs = [sb(st, f"{tag}rs{i}", (128, 1), F32) for i in range(2)]
            pt = [ps(st, f"{tag}pt{i}", (128, D), BF16) for i in range(2)]
            o, n = PCOL[gname]
            P.dma('sp', gbc[:], params[:, o:o + n], w=['gbc'])
            def N1(i):
                s = i % 2
                P.dma('sp', xt[s][:], src[i * 128:(i + 1) * 128, :], w=[f'xt{s}'])
                P.dve(lambda e: e.scalar_tensor_tensor(
                    out=junk[:], in0=xt[s][:], scalar=1.0, in1=xt[s][:], op0=ALU.mult, op1=ALU.mult,
                    accum_out=ss[s][:]), r=[f'xt{s}'], w=[f'ss{s}'])
                P.act(lambda e: e.activation(out=rt[s][:], in_=ss[s][:], func=AF.Sqrt,
                                             bias=EPS, scale=1.0 / D), r=[f'ss{s}'], w=[f'rt{s}'])
                P.dve(lambda e: e.reciprocal(out=rs[s][:], in_=rt[s][:]), r=[f'rt{s}'], w=[f'rs{s}'])
                P.dve(lambda e: e.scalar_tensor_tensor(
                    out=hb[s][:], in0=xt[s][:], scalar=rs[s][:], in1=gbc[:], op0=ALU.mult, op1=ALU.mult),
                    r=[f'xt{s}', f'rs{s}', 'gbc'], w=[f'hb{s}'])

            def N2(i):
                s = i % 2
                for j in range(8):
                    P.pe(lambda e, j=j: e.transpose(out=pt[s][:, j * 128:(j + 1) * 128],
                                                    in_=hb[s][:, j * 128:(j + 1) * 128],
                                                    identity=ident[:]), r=[f'hb{s}'], w=[f'pt{s}'])
                P.act(lambda e: e.activation(
                    out=dstT[:, :, i * 128:(i + 1) * 128],
                    in_=pt[s][:].rearrange("p (j t) -> p j t", j=8), func=AF.Copy),
                    r=[f'pt{s}'], w=[f'dstT{i}'])

            N1(0)
            for i in range(nblk):
                if i + 1 < nblk:
                    N1(i + 1)
                N2(i)

        with ExitStack() as st:
            norm_transpose_phase(st, x, NB, 'g1', hT, "A")
            P.barrier()
            P.emit()

        def proj_cm(st, W, ncols, evac, tag, wtile=512):
            wt = [sb(st, f"{tag}wt{i}", (128, 8, wtile), BF16) for i in range(2)]
            pp = [ps(st, f"{tag}pp{i}", (128, 512), F32) for i in range(4)]
            ntile = (ncols + wtile - 1) // wtile
            cnt = 0
            for wi_ in range(ntile):
                s = wi_ % 2
                c0 = wi_ * wtile
                wc = min(wtile, ncols - c0)
                P.dma('pool', wt[s][:, :, 0:wc],
                      W[:, c0:c0 + wc].rearrange("(k p) c -> p k c", p=128), w=[f'wt{s}'])
                for cc in range(wc // 128):
                    c = (c0 // 128) + cc
                    for T in range(NT):
                        b = cnt % 4
                        cnt += 1
                        for k in range(8):
                            P.pe(lambda e, s=s, cc=cc, T=T, k=k, b=b: e.matmul(
                                pp[b][:], lhsT=wt[s][:, k, cc * 128:(cc + 1) * 128],
                                rhs=hT[:, k, T * 512:(T + 1) * 512], start=(k == 0), stop=(k == 7)),
                                r=[f'wt{s}'], w=[f'pp{b}'])
                        evac(c, T, pp[b], f'pp{b}')

        if upto >= 2:
            with ExitStack() as st:
                NDT = 9
                yg = [sb(st, f"yg{i}", (128, 30 + 512), F32) for i in range(3)]
                ygb = [sb(st, f"ygb{i}", (128, 30 + 512), BF16) for i in range(3)]
                sg = [sb(st, f"sg{i}", (128, 512), F32) for i in range(2)]
                acc = [sb(st, f"acc{i}", (128, 512), F32) for i in range(2)]
                yc = [sb(st, f"yc{i}", (128, 512), F32) for i in range(2)]
                dg = [sb(st, f"dg{i}", (128, 31 - NDT, 128), BF16) for i in range(2)]
                wt = [sb(st, f"Bwt{i}", (128, 8, 512), BF16) for i in range(2)]
                pa = [ps(st, f"Bpa{i}", (128, 512), F32) for i in range(2)]
                pg = [ps(st, f"Bpg{i}", (128, 512), F32) for i in range(2)]
                pc = [ps(st, f"Bpc{i}", (128, 512), F32) for i in range(2)]
                n = 0
                cpend = []
                for wi_ in range(4):
                    s = wi_ % 2
                    P.dma('pool', wt[s][:], wcm[:, wi_ * 512:(wi_ + 1) * 512].rearrange(
                        "(k p) c -> p k c", p=128), w=[f'wt{s}'])
                    for jj in range(2):
                        j = wi_ * 2 + jj
                        js = j % 2
                        while cpend:
                            cpend.pop(0)()
                        for jn in ([0, 1] if j == 0 else ([j + 1] if j + 1 < 8 else [])):
                            for tap in range(NDT, 31):
                                P.pool(lambda e, jn=jn, tap=tap: e.tensor_scalar(
                                    out=dg[jn % 2][:, tap - NDT, :], in0=ident[:],
                                    scalar1=pvc('convw', jn * 31 + tap, 1), scalar2=0.0,
                                    op0=ALU.mult, op1=ALU.add), w=[f'dg{jn % 2}'])
                        for T in range(NT):
                            b = n % 2
                            s3 = n % 3
                            p3 = (n - 1) % 3
                            for k in range(8):
                                P.pe(lambda e, s=s, jj=jj, T=T, k=k, b=b: e.matmul(
                                    pa[b][:], lhsT=wt[s][:, k, jj * 256:jj * 256 + 128],
                                    rhs=hT[:, k, T * 512:(T + 1) * 512], start=(k == 0), stop=(k == 7)),
                                    r=[f'wt{s}'], w=[f'pa{b}'])
                            for k in range(8):
                                P.pe(lambda e, s=s, jj=jj, T=T, k=k, b=b: e.matmul(
                                    pg[b][:], lhsT=wt[s][:, k, jj * 256 + 128:jj * 256 + 256],
                                    rhs=hT[:, k, T * 512:(T + 1) * 512], start=(k == 0), stop=(k == 7)),
                                    r=[f'wt{s}'], w=[f'pg{b}'])
                            P.act(lambda e, b=b: e.activation(out=sg[b][:], in_=pg[b][:], func=AF.Sigmoid),
                                  r=[f'pg{b}'], w=[f'sg{b}'])
                            if T == 0:
                                P.pool(lambda e, s3=s3: e.memset(yg[s3][:, 0:30], 0.0), w=[f'yg{s3}'])
                                P.pool(lambda e, s3=s3: e.memset(ygb[s3][:, 0:30], 0.0), w=[f'ygb{s3}'])
                            else:
                                P.pool(lambda e, s3=s3, p3=p3: e.tensor_copy(
                                    out=yg[s3][:, 0:30], in_=yg[p3][:, 512:542]),
                                    r=[f'yg{p3}'], w=[f'yg{s3}'])
                                P.pool(lambda e, s3=s3, p3=p3: e.tensor_copy(
                                    out=ygb[s3][:, 0:30], in_=ygb[p3][:, 512:542]),
                                    r=[f'ygb{p3}'], w=[f'ygb{s3}'])
                            P.dve(lambda e, b=b, s3=s3: e.tensor_tensor(
                                out=yg[s3][:, 30:542], in0=pa[b][:], in1=sg[b][:], op=ALU.mult),
                                r=[f'pa{b}', f'sg{b}'], w=[f'yg{s3}'])
                            P.act(lambda e, s3=s3: e.activation(out=ygb[s3][:, 30:542], in_=yg[s3][:, 30:542],
                                                                func=AF.Copy),
                                  r=[f'yg{s3}'], w=[f'ygb{s3}'])
                            def taps(b=b, s3=s3, js=js):
                                for tap in range(NDT, 31):
                                    P.pe(lambda e, tap=tap: e.matmul(
                                        pc[b][:], lhsT=dg[js][:, tap - NDT, :], rhs=ygb[s3][:, tap:tap + 512],
                                        start=(tap == NDT), stop=(tap == 30)),
                                        r=[f'dg{js}', f'ygb{s3}'], w=[f'pc{b}'])
                            P.dve(lambda e, b=b, s3=s3, j=j: e.tensor_scalar(
                                out=acc[b][:], in0=yg[s3][:, 0:512], scalar1=pvc('convw', j * 31, 1),
                                scalar2=pvc('convb', j, 1), op0=ALU.mult, op1=ALU.add),
                                r=[f'yg{s3}'], w=[f'acc{b}'])
                            for tap in range(1, NDT):
                                P.dve(lambda e, b=b, s3=s3, j=j, tap=tap: e.scalar_tensor_tensor(
                                    out=acc[b][:], in0=yg[s3][:, tap:tap + 512],
                                    scalar=pvc('convw', j * 31 + tap, 1), in1=acc[b][:],
                                    op0=ALU.mult, op1=ALU.add),
                                    r=[f'yg{s3}'], w=[f'acc{b}'])
                            def fin_(b=b, j=j, T=T, taps=taps):
                                taps()
                                P.dve(lambda e: e.tensor_tensor(out=yc[b][:], in0=pc[b][:], in1=acc[b][:],
                                                                op=ALU.add),
                                      r=[f'pc{b}', f'acc{b}'], w=[f'yc{b}'])
                                P.dma('sp', ycT[j * 128:(j + 1) * 128, T * 512:(T + 1) * 512], yc[b][:],
                                      r=[f'yc{b}'], w=['dram'])
                            while cpend:
                                cpend.pop(0)()
                            cpend.append(fin_)
                            n += 1
                while cpend:
                    cpend.pop(0)()
                P.barrier()
                P.emit()

            with ExitStack() as st:
                stg = [sb(st, f"stg{i}", (128, S), BF16) for i in range(2)]

                def evac_gen(c, T, pp, key):
                    c = c + 16
                    if c < 24:
                        dst, row, scale = qT, (c - 16) * 128, 128.0 ** -0.5
                    elif c < 32:
                        dst, row, scale = kT, (c - 24) * 128, 1.0
                    elif c < 40:
                        dst, row, scale = qmT, (c - 32) * 128, 256.0 ** -0.5
                    elif c < 44:
                        dst, row, scale = qiT, (c - 40) * 128, 1.0
                    else:
                        dst, row, scale = kiT, 0, 1.0
                    s2 = c % 2
                    P.act(lambda e: e.activation(out=stg[s2][:, T * 512:(T + 1) * 512], in_=pp[:],
                                                 func=AF.Copy, scale=scale),
                          r=[key], w=[f'stg{s2}'])
                    if T == NT - 1:
                        P.dma('sp', dst[row:row + 128, :], stg[s2][:], r=[f'stg{s2}'], w=['dram'])

                proj_cm(st, wcm[:, 2048:], WCM_COLS - 2048, evac_gen, "Bq")
                P.barrier()
                P.emit()

            with ExitStack() as st:
                stg = [sb(st, f"stg{i}", (128, S), BF16) for i in range(2)]

                def evac_gate(c, T, pp, key):
                    s2 = c % 2
                    P.act(lambda e: e.activation(out=stg[s2][:, T * 512:(T + 1) * 512], in_=pp[:],
                                                 func=AF.Sigmoid, bias=pvc('bgate', c, 1), scale=1.0),
                          r=[key], w=[f'stg{s2}'])
                    if T == NT - 1:
                        P.dma('sp', gT[c * 128:(c + 1) * 128, :], stg[s2][:], r=[f'stg{s2}'], w=['dram'])

                proj_cm(st, wgate, 3 * D, evac_gate, "Bg")
                P.barrier()
                P.emit()

            with ExitStack() as st:
                wv = sb(st, "wv", (128, 8, WTM_COLS), BF16)
                vst = [sb(st, f"vst{i}", (128, D), BF16) for i in range(2)]
                wst = [sb(st, f"wst{i}", (128, 8), F32) for i in range(2)]
                pvv = [ps(st, f"pvv{i}", (128, 512), F32) for i in range(4)]
                pw = [ps(st, f"pw{i}", (128, 8), F32) for i in range(2)]
                for hh in range(2):
                    P.dma('pool', wv[:, :, hh * 512:(hh + 1) * 512],
                          wtm[:, hh * 512:(hh + 1) * 512].rearrange("(k p) c -> p k c", p=128), w=['wv'])
                P.dma('pool', wv[:, :, 1024:1032],
                      wtm[:, 1024:1032].rearrange("(k p) c -> p k c", p=128), w=['wv'])
                for i in range(NB):
                    s = i % 2
                    for hh in range(2):
                        b = (i * 2 + hh) % 4
                        for k in range(8):
                            P.pe(lambda e, i=i, hh=hh, k=k, b=b: e.matmul(
                                pvv[b][:], lhsT=hT[:, k, i * 128:(i + 1) * 128],
                                rhs=wv[:, k, hh * 512:(hh + 1) * 512], start=(k == 0), stop=(k == 7)),
                                r=['wv'], w=[f'pvv{b}'])
                        P.act(lambda e, s=s, hh=hh, b=b: e.activation(
                            out=vst[s][:, hh * 512:(hh + 1) * 512], in_=pvv[b][:], func=AF.Copy),
                            r=[f'pvv{b}'], w=[f'vst{s}'])
                    for k in range(8):
                        P.pe(lambda e, i=i, k=k, s=s: e.matmul(
                            pw[s][:], lhsT=hT[:, k, i * 128:(i + 1) * 128],
                            rhs=wv[:, k, 1024:1032], start=(k == 0), stop=(k == 7)),
                            r=['wv'], w=[f'pw{s}'])
                    P.dve(lambda e, s=s: e.tensor_copy(out=wst[s][:], in_=pw[s][:]),
                          r=[f'pw{s}'], w=[f'wst{s}'])
                    P.dma('sp', vtm[i * 128:(i + 1) * 128, :], vst[s][:], r=[f'vst{s}'], w=['dram'])
                    P.dma('sp', witm[i * 128:(i + 1) * 128, :], wst[s][:], r=[f'wst{s}'], w=['dram'])
                P.barrier()
                P.emit()
        hst.close()

        def TS(T):
            return slice(T * 512, (T + 1) * 512)

        if upto >= 3:
            with ExitStack() as st:
                pwt = sb(st, "pwt", (128, 8, D), BF16)
                for hh in range(2):
                    P.dma('pool', pwt[:, :, hh * 512:(hh + 1) * 512],
                          pw2[:, hh * 512:(hh + 1) * 512].rearrange("(k p) c -> p k c", p=128), w=['pwt'])
                ycl = [sb(st, f"ycl{i}", (128, 8, 512), F32) for i in range(2)]
                sq = sb(st, "sq", (128, 8, 512), F32)
                gl = [sb(st, f"gl{i}", (128, 8, 512), BF16) for i in range(2)]
                mean = [sb(st, f"mean{i}", (128, 512), F32) for i in range(2)]
                msq = sb(st, "msq", (128, 512), F32)
                var = sb(st, "var", (128, 512), F32)
                sd = sb(st, "sd", (128, 512), F32)
                rstd = [sb(st, f"rstd{i}", (128, 512), F32) for i in range(2)]
                t1 = [sb(st, f"t1{i}", (128, 512), F32) for i in range(2)]
                ys = [sb(st, f"ys{i}", (128, 8, 512), BF16) for i in range(2)]
                ot = [sb(st, f"ot{i}", (128, 512), BF16) for i in range(2)]
                s1 = ps(st, "s1", (128, 512))
                s2 = ps(st, "s2", (128, 512))
                po = [ps(st, f"po{i}", (128, 512)) for i in range(2)]

                def C0(T):
                    s = T % 2
                    for g_ in range(2):
                        P.dma('sp', ycl[s][:, g_ * 4:(g_ + 1) * 4, :],
                              ycT[g_ * 512:(g_ + 1) * 512, TS(T)].rearrange("(j p) t -> p j t", p=128),
                              w=[f'ycl{s}_{g_}'])
                        P.dma('sp', gl[s][:, g_ * 4:(g_ + 1) * 4, :],
                              gT[g_ * 512:(g_ + 1) * 512, TS(T)].rearrange("(j p) t -> p j t", p=128),
                              w=[f'gl{s}_{g_}'])

                def C1(T):
                    s = T % 2
                    yk = [f'ycl{s}_0', f'ycl{s}_1']
                    P.act(lambda e: e.activation(out=sq[:], in_=ycl[s][:], func=AF.Square), r=yk, w=['sq'])
                    for j in range(8):
                        P.pe(lambda e, j=j: e.matmul(s1[:], lhsT=ones_f[:], rhs=ycl[s][:, j, :],
                                                     start=(j == 0), stop=(j == 7)), r=yk, w=['s1'])
                    for j in range(8):
                        P.pe(lambda e, j=j: e.matmul(s2[:], lhsT=ones_f[:], rhs=sq[:, j, :],
                                                     start=(j == 0), stop=(j == 7)), r=['sq'], w=['s2'])
                    P.dve(lambda e: e.tensor_scalar(out=mean[s][:], in0=s1[:], scalar1=1.0 / D, scalar2=None,
                                                    op0=ALU.mult), r=['s1'], w=[f'mean{s}'])
                    P.dve(lambda e: e.tensor_tensor(out=msq[:], in0=mean[s][:], in1=mean[s][:], op=ALU.mult),
                          r=[f'mean{s}'], w=['msq'])
                    P.dve(lambda e: e.scalar_tensor_tensor(out=var[:], in0=s2[:], scalar=1.0 / D, in1=msq[:],
                                                           op0=ALU.mult, op1=ALU.subtract),
                          r=['s2', 'msq'], w=['var'])
                    P.act(lambda e: e.activation(out=sd[:], in_=var[:], func=AF.Sqrt, bias=EPS, scale=1.0),
                          r=['var'], w=['sd'])
                    P.dve(lambda e: e.reciprocal(out=rstd[s][:], in_=sd[:]), r=['sd'], w=[f'rstd{s}'])

                def C2(T):
                    s = T % 2
                    yk = [f'ycl{s}_0', f'ycl{s}_1']
                    for j in range(8):
                        b = j % 2
                        P.dve(lambda e, j=j, b=b: e.tensor_tensor(
                            out=t1[b][:], in0=ycl[s][:, j, :], in1=mean[s][:], op=ALU.subtract),
                            r=yk + [f'mean{s}'], w=[f't1{b}'])
                        P.dve(lambda e, b=b: e.tensor_tensor(out=t1[b][:], in0=t1[b][:], in1=rstd[s][:],
                                                             op=ALU.mult), r=[f'rstd{s}'], w=[f't1{b}'])
                        P.act(lambda e, j=j, b=b: e.activation(out=ys[s][:, j, :], in_=t1[b][:], func=AF.Silu,
                                                               scale=pvc('lng', j, 1), bias=pvc('lnb', j, 1)),
                              r=[f't1{b}'], w=[f'ys{s}'])

                def C3(T):
                    s = T % 2
                    for dch in range(8):
                        b = dch % 2
                        for j in range(8):
                            P.pe(lambda e, dch=dch, j=j, b=b: e.matmul(
                                po[b][:], lhsT=pwt[:, j, dch * 128:(dch + 1) * 128], rhs=ys[s][:, j, :],
                                start=(j == 0), stop=(j == 7)), r=[f'ys{s}', 'pwt'], w=[f'po{b}'])
                        P.dve(lambda e, dch=dch, b=b: e.tensor_tensor(
                            out=ot[b][:], in0=po[b][:], in1=gl[s][:, dch, :], op=ALU.mult),
                            r=[f'po{b}', f'gl{s}_0', f'gl{s}_1'], w=[f'ot{b}'])
                        P.dma('pool', mconvT[dch * 128:(dch + 1) * 128, TS(T)], ot[b][:], r=[f'ot{b}'], w=['dram'])

                C0(0)
                C1(0)
                for T in range(NT):
                    if T + 1 < NT:
                        C0(T + 1)
                    C2(T)
                    if T + 1 < NT:
                        C1(T + 1)
                    C3(T)
                P.barrier()
                P.emit()

        if upto >= 4:
            with ExitStack() as st0:
                mnT = sb(st0, "mnT", (128, 8, 256), BF16)
                mkT = sb(st0, "mkT", (128, 8, 256), BF16)
                mv = sb(st0, "mv", (128, 2, D), BF16)
                with ExitStack() as st:
                    norm_transpose_phase(st, mem, 2, 'gmem', mnT, "D")
                    P.barrier()
                    P.emit()
                with ExitStack() as st:
                    wk = [sb(st, f"wk{i}", (128, 8, 512), BF16) for i in range(2)]
                    pk = [ps(st, f"pk{i}", (128, 512)) for i in range(2)]
                    pmv = [ps(st, f"pmv{i}", (128, 512)) for i in range(2)]
                    for wi_ in range(2):
                        s = wi_ % 2
                        P.dma('pool', wk[s][:], wmkv[:, wi_ * 512:(wi_ + 1) * 512].rearrange(
                            "(k p) c -> p k c", p=128), w=[f'wk{s}'])
                        for cc in range(4):
                            c = wi_ * 4 + cc
                            b = c % 2
                            for k in range(8):
                                P.pe(lambda e, s=s, cc=cc, k=k, b=b: e.matmul(
                                    pk[b][:, 0:256], lhsT=wk[s][:, k, cc * 128:(cc + 1) * 128],
                                    rhs=mnT[:, k, :], start=(k == 0), stop=(k == 7)),
                                    r=[f'wk{s}'], w=[f'pk{b}'])
                            P.act(lambda e, c=c, b=b: e.activation(out=mkT[:, c, :], in_=pk[b][:, 0:256],
                                                                   func=AF.Copy), r=[f'pk{b}'], w=['mkT'])
                    for wi_ in range(2):
                        s = wi_ % 2
                        P.dma('pool', wk[s][:], wmkv[:, 1024 + wi_ * 512:1024 + (wi_ + 1) * 512].rearrange(
                            "(k p) c -> p k c", p=128), w=[f'wk{s}'])
                        for mc in range(2):
                            for k in range(8):
                                P.pe(lambda e, s=s, mc=mc, k=k: e.matmul(
                                    pmv[mc][:], lhsT=mnT[:, k, mc * 128:(mc + 1) * 128], rhs=wk[s][:, k, :],
                                    start=(k == 0), stop=(k == 7)), r=[f'wk{s}'], w=[f'pmv{mc}'])
                            P.act(lambda e, wi_=wi_, mc=mc: e.activation(
                                out=mv[:, mc, wi_ * 512:(wi_ + 1) * 512], in_=pmv[mc][:], func=AF.Copy),
                                r=[f'pmv{mc}'], w=['mv'])
                    P.barrier()
                    P.emit()
                with ExitStack() as st:
                    qml = [sb(st, f"qml{i}", (128, 2, 512), BF16) for i in range(2)]
                    gl = [sb(st, f"gl{i}", (128, 2, 512), BF16) for i in range(2)]
                    pT = [sb(st, f"pT{i}", (128, 2, 512), BF16) for i in range(2)]
                    rinv = sb(st, "rinv", (128, 512), F32)
                    o1 = [sb(st, f"o1{i}", (128, 512), F32) for i in range(2)]
                    ot = [sb(st, f"ot{i}", (128, 512), BF16) for i in range(2)]
                    pl = [ps(st, f"pl{i}", (128, 512)) for i in range(2)]
                    po = [ps(st, f"po{i}", (128, 512)) for i in range(2)]
                    pr = ps(st, "pr", (128, 512))
                    n = 0
                    for hm in range(4):
                        for T in range(NT):
                            s = n % 2
                            n += 1
                            P.dma('sp', qml[s][:], qmT[hm * 256:(hm + 1) * 256, TS(T)].rearrange(
                                "(c p) t -> p c t", p=128), w=[f'qml{s}'])
                            P.dma('sp', gl[s][:], gT[2048 + hm * 256:2048 + (hm + 1) * 256, TS(T)].rearrange(
                                "(c p) t -> p c t", p=128), w=[f'gl{s}'])
                            for mc in range(2):
                                for dmc in range(2):
                                    P.pe(lambda e, s=s, hm=hm, mc=mc, dmc=dmc: e.matmul(
                                        pl[mc][:], lhsT=mkT[:, hm * 2 + dmc, mc * 128:(mc + 1) * 128],
                                        rhs=qml[s][:, dmc, :], start=(dmc == 0), stop=(dmc == 1)),
                                        r=[f'qml{s}'], w=[f'pl{mc}'])
                                P.act(lambda e, s=s, mc=mc: e.activation(out=pT[s][:, mc, :], in_=pl[mc][:],
                                                                         func=AF.Exp),
                                      r=[f'pl{mc}'], w=[f'pT{s}'])
                            for dmc in range(2):
                                for mc in range(2):
                                    P.pe(lambda e, s=s, hm=hm, mc=mc, dmc=dmc: e.matmul(
                                        po[dmc][:], lhsT=mv[:, mc, hm * 256 + dmc * 128:hm * 256 + (dmc + 1) * 128],
                                        rhs=pT[s][:, mc, :], start=(mc == 0), stop=(mc == 1)),
                                        r=[f'pT{s}'], w=[f'po{dmc}'])
                            for mc in range(2):
                                P.pe(lambda e, s=s, mc=mc: e.matmul(pr[:], lhsT=ones_bf[:], rhs=pT[s][:, mc, :],
                                                                    start=(mc == 0), stop=(mc == 1)),
                                     r=[f'pT{s}'], w=['pr'])
                            P.act(lambda e: e.activation(out=o1[0][:], in_=pr[:], func=AF.Ln), r=['pr'], w=['o10'])
                            P.act(lambda e: e.activation(out=rinv[:], in_=o1[0][:], func=AF.Exp, scale=-1.0),
                                  r=['o10'], w=['rinv'])
                            for dmc in range(2):
                                P.dve(lambda e, dmc=dmc: e.tensor_tensor(out=o1[dmc][:], in0=po[dmc][:], in1=rinv[:],
                                                                         op=ALU.mult),
                                      r=[f'po{dmc}', 'rinv'], w=[f'o1{dmc}'])
                                P.dve(lambda e, s=s, dmc=dmc: e.tensor_tensor(
                                    out=ot[dmc][:], in0=o1[dmc][:], in1=gl[s][:, dmc, :], op=ALU.mult),
                                    r=[f'o1{dmc}', f'gl{s}'], w=[f'ot{dmc}'])
                                P.dma('pool', mmemT[hm * 256 + dmc * 128:hm * 256 + (dmc + 1) * 128, TS(T)],
                                      ot[dmc][:], r=[f'ot{dmc}'], w=['dram'])
                    P.barrier()
                    P.emit()

        if upto >= 5:
            with ExitStack() as st:
                kil = sb(st, "kil", (128, S), BF16)
                qil = [sb(st, f"qil{i}", (128, 4, 128), BF16) for i in range(2)]
                wil = [sb(st, f"wil{i}", (128, 8), F32) for i in range(2)]
                score = [sb(st, f"score{i}", (128, S), F32) for i in range(4)]
                rl = [sb(st, f"rl{i}", (128, 512), F32) for i in range(3)]
                junk = sb(st, "junkE", (128, S), BF16)
                junkD = sb(st, "junkDE", (128, S), BF16)
                mk = [sb(st, f"mk{i}", (128, S), BF16) for i in range(2)]
                mT = [sb(st, f"mT{i}", (128, 8, 128), BF16) for i in range(2)]
                ptab = sb(st, "ptab", (128, NIT + 1), F32)
                thrc = sb(st, "thrc", (128, 1), F32)

                def two(name, shape):
                    return [sb(st, f"{name}{c}", shape, F32) for c in range(2)]
                lo = two("lo", (128, 1))
                hi = two("hi", (128, 1))
                Wd = two("Wd", (128, 1))
                hneg = two("hneg", (128, NIT + 1))
                nm = two("nm", (128, 1))
                ssum = two("ssum", (128, 1))
                sgn = two("sgn", (128, 1))
                thr_t = two("thr_t", (128, 1))
                midp = two("midp", (128, 1))
                cntd = two("cntd", (128, 1))
                t2 = two("t2", (128, 1))
                pd = [ps(st, f"pd{i}", (128, 512)) for i in range(4)]
                ptm = [ps(st, f"ptm{i}", (128, 1024), BF16) for i in range(2)]
                for n in range(NIT + 1):
                    P.pool(lambda e, n=n: e.memset(ptab[:, n:n + 1], -(2.0 ** -(n + 1))), w=['ptab'])
                P.pool(lambda e: e.memset(thrc[:], -1.0e29), w=['thrc'])
                P.dma('sp', kil[:], kiT[:, :], w=['kil'])
                cnts = {'c': 0, 't': 0, 'q': 0}
                DVE_IT = ((1, 5, 9, 13), (3, 7, 11, 15))

                def score_units(i):
                    s = i % 4
                    L = (i + 1) * 128
                    units = []
                    qs = cnts['q'] % 2
                    cnts['q'] += 1

                    def loads():
                        P.dma('sp', qil[qs][:], qiT[:, i * 128:(i + 1) * 128].rearrange("(c p) t -> p c t", p=128),
                              w=[f'qil{qs}'])
                        P.dma('sp', wil[qs][:], witm[i * 128:(i + 1) * 128, :], w=[f'wil{qs}'])
                    units.append(loads)
                    nst = (L + 511) // 512
                    for stile in range(nst):
                        w_ = min(512, L - stile * 512)
                        c0 = stile * 512
                        for h in range(8):
                            def unit(stile=stile, w_=w_, c0=c0, h=h):
                                p2, half = divmod(h, 2)
                                b = cnts['c'] % 4
                                r3 = cnts['c'] % 3
                                cnts['c'] += 1
                                P.pe(lambda e: e.matmul(
                                    pd[b][:, 0:w_], lhsT=qil[qs][half * 64:(half + 1) * 64, p2, :],
                                    rhs=kil[half * 64:(half + 1) * 64, c0:c0 + w_], start=True, stop=True),
                                    r=[f'qil{qs}', 'kil'], w=[f'pd{b}'])
                                P.act(lambda e: e.activation(
                                    out=rl[r3][:, 0:w_], in_=pd[b][:, 0:w_], func=AF.Relu),
                                    r=[f'pd{b}'], w=[f'rl{r3}'])
                                if h == 0:
                                    P.dve(lambda e: e.tensor_scalar(
                                        out=score[s][:, c0:c0 + w_], in0=rl[r3][:, 0:w_], scalar1=wil[qs][:, 0:1],
                                        scalar2=None, op0=ALU.mult),
                                        r=[f'rl{r3}', f'wil{qs}'], w=[f'score{s}'])
                                else:
                                    P.dve(lambda e: e.scalar_tensor_tensor(
                                        out=score[s][:, c0:c0 + w_], in0=rl[r3][:, 0:w_], scalar=wil[qs][:, h:h + 1],
                                        in1=score[s][:, c0:c0 + w_], op0=ALU.mult, op1=ALU.add),
                                        r=[f'rl{r3}', f'wil{qs}'], w=[f'score{s}'])
                            units.append(unit)

                    def fin():
                        P.dve(lambda e: e.tensor_tensor(
                            out=score[s][:, i * 128:(i + 1) * 128], in0=score[s][:, i * 128:(i + 1) * 128],
                            in1=caus[:], op=ALU.add), w=[f'score{s}'])
                    units.append(fin)
                    return units

                def bisect_ops(i, c):
                    s = i % 4
                    L = (i + 1) * 128
                    sk = f'score{s}'
                    K = lambda nme: f'{nme}{c}'
                    units = []
                    if i >= 2:
                        units.append([
                            lambda: P.dve(lambda e: e.tensor_reduce(out=lo[c][:], in_=score[s][:, 0:i * 128],
                                                                    axis=AX.X, op=ALU.min), r=[sk], w=[K('lo')]),
                            lambda: P.dve(lambda e: e.tensor_reduce(out=hi[c][:], in_=score[s][:, 0:L],
                                                                    axis=AX.X, op=ALU.max), r=[sk], w=[K('hi')]),
                            lambda: P.dve(lambda e: e.tensor_tensor(out=Wd[c][:], in0=hi[c][:], in1=lo[c][:],
                                                                    op=ALU.subtract),
                                          r=[K('hi'), K('lo')], w=[K('Wd')]),
                            lambda: P.dve(lambda e: e.tensor_scalar(out=hneg[c][:], in0=ptab[:], scalar1=Wd[c][:, 0:1],
                                                                    scalar2=None, op0=ALU.mult),
                                          r=[K('Wd'), 'ptab'], w=[K('hneg')]),
                            lambda: P.dve(lambda e: e.tensor_scalar(out=nm[c][:], in0=lo[c][:], scalar1=-1.0,
                                                                    scalar2=hneg[c][:, 0:1], op0=ALU.mult,
                                                                    op1=ALU.add),
                                          r=[K('lo'), K('hneg')], w=[K('nm')]),
                        ])
                        for n in range(NIT):
                            if n in DVE_IT[c]:
                                units.append([
                                    lambda: P.dve(lambda e: e.tensor_scalar(
                                        out=midp[c][:], in0=nm[c][:], scalar1=-1.0, scalar2=None, op0=ALU.mult),
                                        r=[K('nm')], w=[K('midp')]),
                                    lambda: P.dve(lambda e: e.tensor_scalar(
                                        out=junkD[:, 0:L], in0=score[s][:, 0:L], scalar1=midp[c][:, 0:1],
                                        scalar2=None, op0=ALU.is_ge, op1=ALU.add, accum_out=cntd[c][:]),
                                        r=[sk, K('midp')], w=[K('cntd')]),
                                    lambda: P.dve(lambda e: e.tensor_scalar(
                                        out=t2[c][:], in0=cntd[c][:], scalar1=TOPK - 0.5, scalar2=2.0,
                                        op0=ALU.is_ge, op1=ALU.mult), r=[K('cntd')], w=[K('t2')]),
                                    lambda n=n: P.dve(lambda e: e.tensor_scalar(
                                        out=t2[c][:], in0=t2[c][:], scalar1=-1.0, scalar2=hneg[c][:, n + 1:n + 2],
                                        op0=ALU.add, op1=ALU.mult), r=[K('hneg')], w=[K('t2')]),
                                    lambda: P.dve(lambda e: e.tensor_tensor(
                                        out=nm[c][:], in0=nm[c][:], in1=t2[c][:], op=ALU.add),
                                        r=[K('t2')], w=[K('nm')]),
                                ])
                            else:
                                units.append([
                                    lambda: P.act(lambda e: e.activation(
                                        out=junk[:, 0:L], in_=score[s][:, 0:L], func=AF.Sign, bias=nm[c][:, 0:1],
                                        scale=1.0, accum_out=ssum[c][:]), r=[sk, K('nm')], w=[K('ssum')]),
                                    lambda: P.act(lambda e: e.activation(
                                        out=sgn[c][:], in_=ssum[c][:], func=AF.Sign,
                                        bias=float(L - 2 * TOPK) + 0.5, scale=1.0), r=[K('ssum')], w=[K('sgn')]),
                                    lambda n=n: P.act(lambda e: e.activation(
                                        out=nm[c][:], in_=sgn[c][:], func=AF.Identity,
                                        scale=hneg[c][:, n + 1:n + 2], bias=nm[c][:, 0:1]),
                                        r=[K('sgn'), K('hneg')], w=[K('nm')]),
                                ])

                    def epi():
                        if i >= 2:
                            P.dve(lambda e: e.tensor_scalar(out=thr_t[c][:], in0=nm[c][:], scalar1=-1.0,
                                                            scalar2=hneg[c][:, NIT:NIT + 1], op0=ALU.mult,
                                                            op1=ALU.add),
                                  r=[K('nm'), K('hneg')], w=[K('thr_t')])
                            thr, thrk = thr_t[c], K('thr_t')
                        else:
                            thr, thrk = thrc, 'thrc'
                        P.dve(lambda e: e.tensor_scalar(
                            out=mk[c][:, 0:L], in0=score[s][:, 0:L], scalar1=thr[:, 0:1], scalar2=None,
                            op0=ALU.is_ge), r=[sk, thrk], w=[K('mk')])
                        for g0 in range(0, i + 1, 8):
                            nb_ = min(8, i + 1 - g0)
                            b = cnts['t'] % 2
                            cnts['t'] += 1
                            for q_ in range(nb_):
                                P.pe(lambda e, b=b, q_=q_, g0=g0: e.transpose(
                                    out=ptm[b][:, q_ * 128:(q_ + 1) * 128],
                                    in_=mk[c][:, (g0 + q_) * 128:(g0 + q_ + 1) * 128], identity=ident[:]),
                                    r=[K('mk')], w=[f'ptm{b}'])
                            P.act(lambda e, b=b, nb_=nb_: e.activation(
                                out=mT[b][:, 0:nb_, :],
                                in_=ptm[b][:, 0:nb_ * 128].rearrange("p (q t) -> p q t", q=nb_), func=AF.Copy),
                                r=[f'ptm{b}'], w=[f'mT{b}'])
                            for q0 in range(0, nb_, 4):
                                q1 = min(nb_, q0 + 4)
                                P.dma('pool', maskT[(g0 + q0) * 128:(g0 + q1) * 128, i * 128:(i + 1) * 128].rearrange(
                                    "(q p) t -> p q t", p=128), mT[b][:, q0:q1, :], r=[f'mT{b}'], w=['dram'])
                    units.append([epi])
                    return units

                def run_units(us):
                    for u in us:
                        u()

                run_units(score_units(0))
                run_units(score_units(1))
                for pidx in range(NB // 2):
                    ia, ib = 2 * pidx, 2 * pidx + 1
                    A_ = bisect_ops(ia, 0)
                    B_ = bisect_ops(ib, 1)
                    S_ = []
                    if ia + 2 < NB:
                        s1, s2 = score_units(ia + 2), score_units(ib + 2)
                        for k_ in range(max(len(s1), len(s2))):
                            if k_ < len(s1):
                                S_.append(s1[k_])
                            if k_ < len(s2):
                                S_.append(s2[k_])
                    nun = max(len(A_), len(B_))
                    done = 0
                    for idx in range(nun):
                        ua = A_[idx] if idx < len(A_) else []
                        ub = B_[idx] if idx < len(B_) else []
                        for k_ in range(max(len(ua), len(ub))):
                            if k_ < len(ua):
                                ua[k_]()
                            if k_ < len(ub):
                                ub[k_]()
                        tgt = (len(S_) * (idx + 1)) // nun
                        while done < tgt:
                            S_[done]()
                            done += 1
                P.barrier()
                P.emit()

        if upto >= 6:
            with ExitStack() as st:
                ql = [sb(st, f"ql{i}", (128, S), BF16) for i in range(2)]
                kl = [sb(st, f"kl{i}", (128, S), BF16) for i in range(2)]
                vl = [sb(st, f"vl{i}", (128, NB, 128), BF16) for i in range(2)]
                NML = 6
                ml = [sb(st, f"ml{i}", (128, 512), BF16) for i in range(NML)]
                ex = [sb(st, f"ex{i}", (128, 512), BF16) for i in range(4)]
                pm = [sb(st, f"pm{i}", (128, 512), BF16) for i in range(4)]
                gl = [sb(st, f"gl{i}", (128, 512), BF16) for i in range(2)]
                rinv = sb(st, "rinv", (128, 512), F32)
                o1 = sb(st, "o1", (128, 512), F32)
                ot = [sb(st, f"ot{i}", (128, 512), BF16) for i in range(2)]
                psc = [ps(st, f"psc{i}", (128, 512)) for i in range(4)]
                po = [ps(st, f"po{i}", (128, 512)) for i in range(2)]
                pr = [ps(st, f"pr{i}", (128, 512)) for i in range(2)]
                its = []
                for h in range(8):
                    for T in range(NT):
                        nsb = 4 * T + 4
                        for sbk in range(nsb):
                            its.append((h, T, sbk, nsb))
                DEPTH = 3

                def front(n):
                    h, T, sbk, nsb = its[n]
                    hs = h % 2
                    a = (h * NT + T) % 2
                    if T == 0 and sbk == 0:
                        P.dma('sp', ql[hs][:], qT[h * 128:(h + 1) * 128, :], w=[f'ql{hs}'])
                        P.dma('sp', kl[hs][:], kT[h * 128:(h + 1) * 128, :], w=[f'kl{hs}'])
                        for g_ in range(8):
                            P.dma('sp', vl[hs][:, g_ * 4:(g_ + 1) * 4, :],
                                  vtm[g_ * 512:(g_ + 1) * 512, h * 128:(h + 1) * 128].rearrange(
                                      "(b p) d -> p b d", p=128), w=[f'vl{hs}_{g_}'])
                    if sbk == 0:
                        P.dma('sp', gl[a][:], gT[1024 + h * 128:1024 + (h + 1) * 128, TS(T)], w=[f'gl{a}'])
                    m4 = n % NML
                    b3 = n % 4
                    r0 = max(0, sbk - 4 * T) * 128
                    t0 = T * 512 + r0
                    P.dma('sp', ml[m4][:, r0:512], maskT[sbk * 128:(sbk + 1) * 128, t0:(T + 1) * 512],
                          w=[f'ml{m4}'])
                    P.pe(lambda e: e.matmul(
                        psc[b3][:, r0:512], lhsT=kl[hs][:, sbk * 128:(sbk + 1) * 128],
                        rhs=ql[hs][:, t0:(T + 1) * 512], start=True, stop=True),
                        r=[f'kl{hs}', f'ql{hs}'], w=[f'psc{b3}'])
                    P.act(lambda e: e.activation(out=ex[b3][:, r0:512], in_=psc[b3][:, r0:512], func=AF.Exp),
                          r=[f'psc{b3}'], w=[f'ex{b3}'])
                    P.dve(lambda e: e.tensor_tensor(
                        out=pm[b3][:, r0:512], in0=ex[b3][:, r0:512], in1=ml[m4][:, r0:512], op=ALU.mult),
                        r=[f'ex{b3}', f'ml{m4}'], w=[f'pm{b3}'])

                def back(n):
                    h, T, sbk, nsb = its[n]
                    hs = h % 2
                    a = (h * NT + T) % 2
                    b3 = n % 4
                    r0 = max(0, sbk - 4 * T) * 128
                    P.pe(lambda e: e.matmul(
                        po[a][:, r0:512], lhsT=vl[hs][:, sbk, :], rhs=pm[b3][:, r0:512],
                        start=(sbk == 0), stop=(sbk == nsb - 1)),
                        r=[f'pm{b3}', f'vl{hs}_{sbk // 4}'], w=[f'po{a}'])
                    P.pe(lambda e: e.matmul(
                        pr[a][:, r0:512], lhsT=ones_bf[:], rhs=pm[b3][:, r0:512],
                        start=(sbk == 0), stop=(sbk == nsb - 1)),
                        r=[f'pm{b3}'], w=[f'pr{a}'])
                    if sbk == nsb - 1:
                        P.act(lambda e: e.activation(out=o1[:], in_=pr[a][:], func=AF.Ln), r=[f'pr{a}'], w=['o1'])
                        P.act(lambda e: e.activation(out=rinv[:], in_=o1[:], func=AF.Exp, scale=-1.0),
                              r=['o1'], w=['rinv'])
                        P.dve(lambda e: e.tensor_tensor(out=o1[:], in0=po[a][:], in1=rinv[:], op=ALU.mult),
                              r=[f'po{a}', 'rinv'], w=['o1'])
                        P.dve(lambda e: e.tensor_tensor(out=ot[a][:], in0=o1[:], in1=gl[a][:], op=ALU.mult),
                              r=['o1', f'gl{a}'], w=[f'ot{a}'])
                        P.dma('pool', mattT[h * 128:(h + 1) * 128, TS(T)], ot[a][:], r=[f'ot{a}'], w=['dram'])

                for n in range(len(its) + DEPTH):
                    if n < len(its):
                        front(n)
                    if n >= DEPTH:
                        back(n - DEPTH)
                P.barrier()
                P.emit()

        def out_norm_phase(st, nk, load_lhs, wres, gname, resid, dst, tag, post=None):
            gp = sb(st, tag + "gp", (128, D), F32)
            o, n_ = PCOL[gname]
            P.dma('sp', gp[:], params[:, o:o + n_], w=['gp'])
            xt = [sb(st, f"{tag}xt{i}", (128, D), F32) for i in range(2)]
            xo = [sb(st, f"{tag}xo{i}", (128, D), F32) for i in range(2)]
            tmp = sb(st, tag + "tmp", (128, D), F32)
            junkA = sb(st, tag + "junkA", (128, 512), BF16)
            ssh = [sb(st, f"{tag}ssh{i}", (128, 2), F32) for i in range(2)]
            ss = [sb(st, f"{tag}ss{i}", (128, 1), F32) for i in range(2)]
            rt = [sb(st, f"{tag}rt{i}", (128, 1), F32) for i in range(2)]
            rs = [sb(st, f"{tag}rs{i}", (128, 1), F32) for i in range(2)]
            po = [ps(st, f"{tag}po{i}", (128, 512)) for i in range(4)]
            cache = {}
            nblk = NT * 4

            def S1(i):
                T, tb = divmod(i, 4)
                s2 = i % 2
                if T not in cache:
                    cache[T] = load_lhs(T)
                if tb == 1 and T + 1 < NT and (T + 1) not in cache:
                    cache[T + 1] = load_lhs(T + 1)
                lhs, lkey = cache[T]
                P.dma('sp', xt[s2][:], resid[i * 128:(i + 1) * 128, :], w=[f'xt{s2}'])
                for hh in range(2):
                    b = s2 * 2 + hh
                    for j in range(nk):
                        P.pe(lambda e, lhs=lhs, j=j, tb=tb, hh=hh, b=b: e.matmul(
                            po[b][:], lhsT=lhs[:, j, tb * 128:(tb + 1) * 128],
                            rhs=wres[:, j, hh * 512:(hh + 1) * 512], start=(j == 0), stop=(j == nk - 1)),
                            r=[lkey(j) if callable(lkey) else lkey, 'wres'], w=[f'po{b}'])

            def S1b(i):
                s2 = i % 2
                for hh in range(2):
                    b = s2 * 2 + hh
                    P.act(lambda e, b=b, s2=s2, hh=hh: e.activation(
                        out=junkA[:], in_=po[b][:], func=AF.Square, accum_out=ssh[s2][:, hh:hh + 1]),
                        r=[f'po{b}'], w=[f'ssh{s2}'])

            def S2(i):
                s2 = i % 2
                P.dve(lambda e: e.tensor_tensor(out=ss[s2][:], in0=ssh[s2][:, 0:1], in1=ssh[s2][:, 1:2],
                                                op=ALU.add), r=[f'ssh{s2}'], w=[f'ss{s2}'])
                P.act(lambda e: e.activation(out=rt[s2][:], in_=ss[s2][:], func=AF.Sqrt, bias=EPS,
                                             scale=1.0 / D), r=[f'ss{s2}'], w=[f'rt{s2}'])
                P.dve(lambda e: e.reciprocal(out=rs[s2][:], in_=rt[s2][:]), r=[f'rt{s2}'], w=[f'rs{s2}'])
                for hh in range(2):
                    b = s2 * 2 + hh
                    P.dve(lambda e, b=b, hh=hh: e.scalar_tensor_tensor(
                        out=tmp[:, hh * 512:(hh + 1) * 512], in0=po[b][:], scalar=rs[s2][:, 0:1],
                        in1=gp[:, hh * 512:(hh + 1) * 512], op0=ALU.mult, op1=ALU.mult),
                        r=[f'po{b}', f'rs{s2}', 'gp'], w=['tmp'])
                P.dve(lambda e: e.tensor_tensor(out=xo[s2][:], in0=tmp[:], in1=xt[s2][:], op=ALU.add),
                      r=['tmp', f'xt{s2}'], w=[f'xo{s2}'])
                P.dma('pool', dst[i * 128:(i + 1) * 128, :], xo[s2][:], r=[f'xo{s2}'], w=['dram'])
                if post is not None:
                    post[0](i, s2, xo[s2], f'xo{s2}')

            def S3(i):
                s2 = i % 2
                if post is not None:
                    post[1](i, s2, xo[s2], f'xo{s2}')

            for step in range(nblk + 2):
                if step < nblk:
                    S1(step)
                if 1 <= step <= nblk:
                    S2(step - 1)
                if step < nblk:
                    S1b(step)
                if 2 <= step:
                    S3(step - 2)

        h2st = ExitStack()
        if upto >= 7:
            h2T = sb(h2st, "h2T", (128, 8, S), BF16)
            with ExitStack() as st:
                wo = sb(st, "wo", (128, 8, D), BF16)
                for hh in range(2):
                    P.dma('pool', wo[:, :, hh * 512:(hh + 1) * 512],
                          wout[:, hh * 512:(hh + 1) * 512].rearrange("(k p) c -> p k c", p=128), w=['wres'])
                g2 = sb(st, "g2bc", (128, D), F32)
                o, n_ = PCOL['g2']
                P.dma('sp', g2[:], params[:, o:o + n_], w=['g2'])
                ma = [sb(st, f"ma{i}", (128, 8, 512), BF16) for i in range(2)]
                mb = [sb(st, f"mb{i}", (128, 8, 512), BF16) for i in range(2)]
                mc_ = [sb(st, f"mc{i}", (128, 8, 512), BF16) for i in range(2)]
                mg = [sb(st, f"mg{i}", (128, 8, 512), BF16) for i in range(2)]
                junkD = sb(st, "junkD", (128, D), BF16)
                hb = [sb(st, f"Ghb{i}", (128, D), BF16) for i in range(2)]
                ss2 = [sb(st, f"Gss2{i}", (128, 1), F32) for i in range(2)]
                rt2 = [sb(st, f"Grt2{i}", (128, 1), F32) for i in range(2)]
                rs2 = [sb(st, f"Grs2{i}", (128, 1), F32) for i in range(2)]
                pt = [ps(st, f"Gpt{i}", (128, D), BF16) for i in range(2)]

                def load_merged(T):
                    s = T % 2
                    for (buf, src, nm) in ((ma, mconvT, 'ma'), (mb, mattT, 'mb'), (mc_, mmemT, 'mc')):
                        for g_ in range(2):
                            P.dma('sp', buf[s][:, g_ * 4:(g_ + 1) * 4, :],
                                  src[g_ * 512:(g_ + 1) * 512, TS(T)].rearrange("(j p) t -> p j t", p=128),
                                  w=[f'{nm}{s}_{g_}'])
                    P.dve(lambda e, s=s: e.tensor_tensor(out=mg[s][:], in0=ma[s][:], in1=mb[s][:], op=ALU.add),
                          r=[f'ma{s}_0', f'ma{s}_1', f'mb{s}_0', f'mb{s}_1'], w=[f'mg{s}'])
                    P.dve(lambda e, s=s: e.tensor_tensor(out=mg[s][:], in0=mg[s][:], in1=mc_[s][:], op=ALU.add),
                          r=[f'mc{s}_0', f'mc{s}_1'], w=[f'mg{s}'])
                    return mg[s], f'mg{s}'

                def post_h2a(i, s2, xo, xkey):
                    P.dve(lambda e, s2=s2: e.scalar_tensor_tensor(
                        out=junkD[:], in0=xo[:], scalar=1.0, in1=xo[:], op0=ALU.mult, op1=ALU.mult,
                        accum_out=ss2[s2][:]), r=[xkey], w=[f'ss2{s2}'])

                def post_h2b(i, s2, xo, xkey):
                    P.act(lambda e, s2=s2: e.activation(out=rt2[s2][:], in_=ss2[s2][:], func=AF.Sqrt, bias=EPS,
                                                        scale=1.0 / D), r=[f'ss2{s2}'], w=[f'rt2{s2}'])
                    P.dve(lambda e, s2=s2: e.reciprocal(out=rs2[s2][:], in_=rt2[s2][:]),
                          r=[f'rt2{s2}'], w=[f'rs2{s2}'])
                    P.dve(lambda e, s2=s2: e.scalar_tensor_tensor(
                        out=hb[s2][:], in0=xo[:], scalar=rs2[s2][:, 0:1], in1=g2[:], op0=ALU.mult, op1=ALU.mult),
                        r=[xkey, f'rs2{s2}', 'g2'], w=[f'hb{s2}'])
                    for j in range(8):
                        P.pe(lambda e, s2=s2, j=j: e.transpose(out=pt[s2][:, j * 128:(j + 1) * 128],
                                                               in_=hb[s2][:, j * 128:(j + 1) * 128],
                                                               identity=ident[:]),
                             r=[f'hb{s2}'], w=[f'pt{s2}'])
                    P.act(lambda e, s2=s2, i=i: e.activation(
                        out=h2T[:, :, i * 128:(i + 1) * 128],
                        in_=pt[s2][:].rearrange("p (j t) -> p j t", j=8), func=AF.Copy),
                        r=[f'pt{s2}'], w=[f'h2T{i}'])

                out_norm_phase(st, 8, load_merged, wo, 'gpost1', x, x1d, "G", post=(post_h2a, post_h2b))
                P.barrier()
                P.emit()

        if upto >= 8:
            with ExitStack() as st:
                wt = [sb(st, f"Hwt{i}", (128, 8, 512), BF16) for i in range(3)]
                ug = [sb(st, f"ug{i}", (128, 514), F32) for i in range(3)]
                uvv = [sb(st, f"uv{i}", (128, 514), F32) for i in range(3)]
                cg = [sb(st, f"cg{i}", (128, 512), F32) for i in range(3)]
                cv = [sb(st, f"cv{i}", (128, 512), F32) for i in range(3)]
                sgl = [sb(st, f"sgl{i}", (128, 512), F32) for i in range(3)]
                pend = []
                ast = [sb(st, f"ast{i}", (128, S), BF16) for i in range(2)]
                pg = [ps(st, f"Hpg{i}", (128, 512)) for i in range(3)]
                pvv = [ps(st, f"Hpv{i}", (128, 512)) for i in range(3)]

                def fw(c, tap):
                    return pvc('ffnw', c * 3 + tap, 1)

                n = 0
                def wload(w_):
                    P.dma('pool', wt[w_ % 3][:], wup[:, w_ * 512:(w_ + 1) * 512].rearrange(
                        "(k p) c -> p k c", p=128), w=[f'wt{w_ % 3}'])
                wload(0)
                wload(1)
                for wi_ in range(11):
                    s = wi_ % 3
                    if wi_ + 2 < 11:
                        wload(wi_ + 2)
                    for jj in range(2):
                        j = wi_ * 2 + jj
                        a2 = j % 2
                        for T in range(NT):
                            b = n % 3
                            s3 = n % 3
                            p3 = (n - 1) % 3
                            b3 = n % 3
                            n += 1
                            for k in range(8):
                                P.pe(lambda e, s=s, jj=jj, T=T, k=k, b=b: e.matmul(
                                    pg[b][:], lhsT=wt[s][:, k, jj * 256:jj * 256 + 128],
                                    rhs=h2T[:, k, TS(T)], start=(k == 0), stop=(k == 7)),
                                    r=[f'wt{s}'], w=[f'pg{b}'])
                            for k in range(8):
                                P.pe(lambda e, s=s, jj=jj, T=T, k=k, b=b: e.matmul(
                                    pvv[b][:], lhsT=wt[s][:, k, jj * 256 + 128:jj * 256 + 256],
                                    rhs=h2T[:, k, TS(T)], start=(k == 0), stop=(k == 7)),
                                    r=[f'wt{s}'], w=[f'pvv{b}'])
                            for (u_, nm_) in ((ug, 'ug'), (uvv, 'uv')):
                                if T == 0:
                                    P.pool(lambda e, u_=u_, s3=s3: e.memset(u_[s3][:, 0:2], 0.0), w=[f'{nm_}{s3}'])
                                else:
                                    P.pool(lambda e, u_=u_, s3=s3, p3=p3: e.tensor_copy(
                                        out=u_[s3][:, 0:2], in_=u_[p3][:, 512:514]),
                                        r=[f'{nm_}{p3}'], w=[f'{nm_}{s3}'])
                            for (u_, nm_, pp_, pk_, co, cname, c) in (
                                    (ug, 'ug', pg, 'pg', cg, 'cg', 2 * j), (uvv, 'uv', pvv, 'pvv', cv, 'cv', 2 * j + 1)):
                                P.act(lambda e, u_=u_, pp_=pp_, b=b, s3=s3: e.activation(
                                    out=u_[s3][:, 2:514], in_=pp_[b][:], func=AF.Copy),
                                    r=[f'{pk_}{b}'], w=[f'{nm_}{s3}'])
                                P.act(lambda e, co=co, pp_=pp_, b=b, c=c, b3=b3: e.activation(
                                    out=co[b3][:], in_=pp_[b][:], func=AF.Identity, scale=fw(c, 2),
                                    bias=pvc('ffnb', c, 1)), r=[f'{pk_}{b}'], w=[f'{cname}{b3}'])
                                for tap in (1, 0):
                                    P.dve(lambda e, u_=u_, co=co, b3=b3, s3=s3, c=c, tap=tap: e.scalar_tensor_tensor(
                                        out=co[b3][:], in0=u_[s3][:, tap:tap + 512], scalar=fw(c, tap), in1=co[b3][:],
                                        op0=ALU.mult, op1=ALU.add), r=[f'{nm_}{s3}'], w=[f'{cname}{b3}'])
                            def tail(b3=b3, a2=a2, T=T, j=j):
                                P.act(lambda e: e.activation(out=sgl[b3][:], in_=cg[b3][:], func=AF.Silu),
                                      r=[f'cg{b3}'], w=[f'sgl{b3}'])
                                P.dve(lambda e: e.tensor_tensor(
                                    out=ast[a2][:, TS(T)], in0=sgl[b3][:], in1=cv[b3][:], op=ALU.mult),
                                    r=[f'sgl{b3}', f'cv{b3}'], w=[f'ast{a2}'])
                                if T == NT - 1:
                                    P.dma('sp', aT[j * 128:(j + 1) * 128, :], ast[a2][:], r=[f'ast{a2}'], w=['dram'])
                            while pend:
                                pend.pop(0)()
                            pend.append(tail)
                while pend:
                    pend.pop(0)()
                P.barrier()
                P.emit()
        h2st.close()

        if upto >= 9:
            with ExitStack() as st:
                wd = sb(st, "wd", (128, NFC, D), BF16)
                for hh in range(2):
                    P.dma('pool', wd[:, :, hh * 512:(hh + 1) * 512],
                          wdown[:, hh * 512:(hh + 1) * 512].rearrange("(k p) c -> p k c", p=128), w=['wres'])
                at = [sb(st, f"at{i}", (128, NFC, 512), BF16) for i in range(2)]

                def load_act(T):
                    s = T % 2
                    for g_ in range(0, NFC, 4):
                        g1 = min(NFC, g_ + 4)
                        P.dma('sp', at[s][:, g_:g1, :], aT[g_ * 128:g1 * 128, TS(T)].rearrange(
                            "(j p) t -> p j t", p=128), w=[f'at{s}_{g_ // 4}'])
                    return at[s], (lambda j, s=s: f'at{s}_{j // 4}')

                out_norm_phase(st, NFC, load_act, wd, 'gpost2', x1d, y, "I")
                P.barrier()
                P.emit()


        P.barrier()
        P.emit()
    return nc


_CACHE = {}


def kernel(**inputs):
    shared = pack_inputs(inputs)
    if 'nc' not in _CACHE:
        _CACHE['nc'] = build()
    nc = _CACHE['nc']
    x = np.asarray(inputs['x'], np.float32)
    mem = np.asarray(inputs['mem'], np.float32)
    in_maps = []
    for b in range(8):
        m = dict(shared)
        m['x'] = np.ascontiguousarray(x[b])
        m['mem'] = np.ascontiguousarray(mem[b])
        in_maps.append(m)
    res = run_bass_kernel_spmd(nc, in_maps, core_ids=list(range(8)))
    return np.stack([np.asarray(r['y'], np.float32) for r in res.results], axis=0)
```
